# Optimizing a Trainium2 kernel written in Bass

```python
import math
import jax, jax.numpy as jnp
from jax import lax
import numpy as np

D_MODEL = 1024
BATCH = 8
SEQ = 4096
DEPTH = 1

MLA_HEADS = 16
MLA_Q_RANK = 256
MLA_KV_RANK = 256
MLA_NOPE_DIM = 64
MLA_ROPE_DIM = 32
MLA_QK_DIM = MLA_NOPE_DIM + MLA_ROPE_DIM
MLA_V_DIM = D_MODEL // MLA_HEADS
ROPE_THETA = 10000.0

DIFF_HEADS = 8
DIFF_QK_DIM = 64
DIFF_V_DIM = D_MODEL // DIFF_HEADS

N_EXPERTS = 16
CAPACITY_FACTOR = 2
EXPERT_FF = 2 * D_MODEL

Q_BLOCK = 128
EPS = 1e-6

IN_COLS = (
    MLA_Q_RANK,
    MLA_KV_RANK,
    MLA_ROPE_DIM,
    DIFF_HEADS * 2 * DIFF_QK_DIM,
    DIFF_HEADS * 2 * DIFF_QK_DIM,
    DIFF_HEADS * DIFF_V_DIM,
    D_MODEL,
    D_MODEL,
)
IN_WIDTH = sum(IN_COLS)

kernel_name = "hybrid_mla_diffattn_ec_moe_block"


def _rms_norm(x, w):
    xf = x.astype(jnp.float32)
    y = xf * lax.rsqrt(jnp.mean(xf * xf, axis=-1, keepdims=True) + EPS)
    return (y * w.astype(jnp.float32)).astype(x.dtype)


def _split_columns(z):
    parts, start = [], 0
    for width in IN_COLS:
        parts.append(z[..., start:start + width])
        start += width
    return parts


def _rope_tables(positions):
    inv_freq = 1.0 / (ROPE_THETA ** (jnp.arange(0, MLA_ROPE_DIM, 2, dtype=jnp.float32) / MLA_ROPE_DIM))
    ang = positions.astype(jnp.float32)[..., None] * inv_freq
    return jnp.cos(ang)[:, :, None, :], jnp.sin(ang)[:, :, None, :]


def _apply_rope(x, cos, sin):
    half = x.shape[-1] // 2
    x1, x2 = x[..., :half], x[..., half:]
    c, s = cos.astype(x.dtype), sin.astype(x.dtype)
    return jnp.concatenate([x1 * c - x2 * s, x1 * s + x2 * c], axis=-1)


def _alibi_slopes(n_heads):
    return jnp.asarray([2.0 ** (-8.0 * (i + 1) / n_heads) for i in range(n_heads)], dtype=jnp.float32)


def _to_blocks(a, axis):
    t = a.shape[axis]
    new_shape = a.shape[:axis] + (t // Q_BLOCK, Q_BLOCK) + a.shape[axis + 1:]
    return jnp.moveaxis(a.reshape(new_shape), axis, 0)


def _from_blocks(o, axis):
    o = jnp.moveaxis(o, 0, axis)
    return o.reshape(o.shape[:axis] + (o.shape[axis] * o.shape[axis + 1],) + o.shape[axis + 2:])


def _mla_attention(q, k, v):
    scale = MLA_QK_DIM ** -0.5

    def block(qb):
        s = jnp.einsum('bhqd,bhkd->bhqk', qb, k).astype(jnp.float32) * scale
        p = jax.nn.softmax(s, axis=-1)
        return jnp.einsum('bhqk,bhkd->bhqd', p.astype(v.dtype), v)

    out = lax.map(block, _to_blocks(q, 2))
    return _from_blocks(out, 2)


def _diff_attention(q1, q2, k1, k2, v, lam, slopes, positions):
    scale = DIFF_QK_DIM ** -0.5

    def block(args):
        q1b, q2b, pb = args
        dist = jnp.abs(pb[:, :, None] - positions[:, None, :]).astype(jnp.float32)
        bias = -slopes[None, :, None, None] * dist[:, None, :, :]
        a1 = jax.nn.softmax(jnp.einsum('bhqd,bhkd->bhqk', q1b, k1).astype(jnp.float32) * scale + bias, axis=-1)
        a2 = jax.nn.softmax(jnp.einsum('bhqd,bhkd->bhqk', q2b, k2).astype(jnp.float32) * scale + bias, axis=-1)
        a = a1 - lam * a2
        return jnp.einsum('bhqk,bhkd->bhqd', a.astype(v.dtype), v)

    out = lax.map(block, (_to_blocks(q1, 2), _to_blocks(q2, 2), _to_blocks(positions, 1)))
    return _from_blocks(out, 2)


def _expert_choice_moe(h, router_w, w_gate, w_up, w_down):
    b, t, d = h.shape
    capacity = CAPACITY_FACTOR * t // N_EXPERTS
    logits = jnp.einsum('btd,de->bte', h, router_w).astype(jnp.float32)
    affinity = jax.nn.softmax(logits, axis=-1)
    gate, idx = lax.top_k(jnp.swapaxes(affinity, 1, 2), capacity)
    xe = jax.vmap(lambda hb, ib: hb[ib])(h, idx)
    hidden = jax.nn.silu(jnp.einsum('becd,edf->becf', xe, w_gate)) * jnp.einsum('becd,edf->becf', xe, w_up)
    ye = jnp.einsum('becf,efd->becd', hidden, w_down) * gate[..., None].astype(h.dtype)
    return jax.vmap(
        lambda ib, yb: jnp.zeros((t, d), yb.dtype).at[ib.reshape(-1)].add(yb.reshape(-1, d))
    )(idx, ye)


def _hybrid_layer(x, positions, layer_idx, attn_norm_w, w_in, b_gate,
                  mla_q_norm_w, mla_w_uq, mla_kv_norm_w, mla_w_ukv,
                  mla_q_hnorm_w, mla_k_hnorm_w,
                  diff_q_hnorm_w, diff_k_hnorm_w, diff_lambda, diff_subln_w,
                  w_out, ffn_norm_w, router_w, expert_w_gate, expert_w_up, expert_w_down):
    b, t, _ = x.shape

    h = _rms_norm(x, attn_norm_w)
    z = jnp.einsum('btd,dc->btc', h, w_in)
    c_q, c_kv, k_rope, dq, dk, dv, g_mla, g_diff = _split_columns(z)

    cos, sin = _rope_tables(positions)
    q = jnp.einsum('btr,rc->btc', _rms_norm(c_q, mla_q_norm_w), mla_w_uq).reshape(b, t, MLA_HEADS, MLA_QK_DIM)
    kv = jnp.einsum('btr,rc->btc', _rms_norm(c_kv, mla_kv_norm_w), mla_w_ukv).reshape(
        b, t, MLA_HEADS, MLA_NOPE_DIM + MLA_V_DIM)
    k_nope, v_mla = kv[..., :MLA_NOPE_DIM], kv[..., MLA_NOPE_DIM:]
    k_r = jnp.broadcast_to(k_rope[:, :, None, :], (b, t, MLA_HEADS, MLA_ROPE_DIM))
    k = jnp.concatenate([k_nope, k_r], axis=-1)
    q = _rms_norm(q, mla_q_hnorm_w)
    k = _rms_norm(k, mla_k_hnorm_w)
    q = jnp.concatenate([q[..., :MLA_NOPE_DIM], _apply_rope(q[..., MLA_NOPE_DIM:], cos, sin)], axis=-1)
    k = jnp.concatenate([k[..., :MLA_NOPE_DIM], _apply_rope(k[..., MLA_NOPE_DIM:], cos, sin)], axis=-1)
    o_mla = _mla_attention(q.transpose(0, 2, 1, 3), k.transpose(0, 2, 1, 3), v_mla.transpose(0, 2, 1, 3))
    o_mla = o_mla.transpose(0, 2, 1, 3).reshape(b, t, D_MODEL)

    qd = _rms_norm(dq.reshape(b, t, DIFF_HEADS, 2, DIFF_QK_DIM), diff_q_hnorm_w)
    kd = _rms_norm(dk.reshape(b, t, DIFF_HEADS, 2, DIFF_QK_DIM), diff_k_hnorm_w)
    qd = qd.transpose(0, 2, 3, 1, 4)
    kd = kd.transpose(0, 2, 3, 1, 4)
    vd = dv.reshape(b, t, DIFF_HEADS, DIFF_V_DIM).transpose(0, 2, 1, 3)
    lam_init = 0.8 - 0.6 * math.exp(-0.3 * layer_idx)
    lf = diff_lambda.astype(jnp.float32)
    lam = jnp.exp(jnp.sum(lf[0] * lf[1])) - jnp.exp(jnp.sum(lf[2] * lf[3])) + lam_init
    o_diff = _diff_attention(qd[:, :, 0], qd[:, :, 1], kd[:, :, 0], kd[:, :, 1], vd, lam,
                             _alibi_slopes(DIFF_HEADS), positions)
    o_diff = _rms_norm(o_diff, diff_subln_w) * (1.0 - lam_init)
    o_diff = o_diff.transpose(0, 2, 1, 3).reshape(b, t, D_MODEL)

    gate_a = jax.nn.sigmoid(g_mla + b_gate[:D_MODEL])
    gate_b = jax.nn.sigmoid(g_diff + b_gate[D_MODEL:])
    merged = gate_a * o_mla + gate_b * o_diff
    x = x + jnp.einsum('btc,cd->btd', merged, w_out)

    h2 = _rms_norm(x, ffn_norm_w)
    return x + _expert_choice_moe(h2, router_w, expert_w_gate, expert_w_up, expert_w_down)


def setup_inputs(seed: int = 0) -> dict:
    key = jax.random.key(seed)
    ks = jax.random.split(key, 24)
    f32 = jnp.float32

    def normal(k, shape, fan_in):
        return jax.random.normal(k, shape, f32) * (fan_in ** -0.5)

    def gain(k, shape):
        return 1.0 + 0.02 * jax.random.normal(k, shape, f32)

    x = jax.random.normal(ks[0], (BATCH, SEQ, D_MODEL), f32)
    offsets = jax.random.randint(ks[1], (BATCH, 1), 0, 1024, dtype=jnp.int32)
    positions = offsets + jnp.arange(SEQ, dtype=jnp.int32)[None, :]
    return {
        "x": x,
        "positions": positions,
        "attn_norm_w": gain(ks[2], (DEPTH, D_MODEL)),
        "w_in": normal(ks[3], (DEPTH, D_MODEL, IN_WIDTH), D_MODEL),
        "b_gate": 0.02 * jax.random.normal(ks[4], (DEPTH, 2 * D_MODEL), f32),
        "mla_q_norm_w": gain(ks[5], (DEPTH, MLA_Q_RANK)),
        "mla_w_uq": normal(ks[6], (DEPTH, MLA_Q_RANK, MLA_HEADS * MLA_QK_DIM), MLA_Q_RANK),
        "mla_kv_norm_w": gain(ks[7], (DEPTH, MLA_KV_RANK)),
        "mla_w_ukv": normal(ks[8], (DEPTH, MLA_KV_RANK, MLA_HEADS * (MLA_NOPE_DIM + MLA_V_DIM)), MLA_KV_RANK),
        "mla_q_hnorm_w": gain(ks[9], (DEPTH, MLA_QK_DIM)),
        "mla_k_hnorm_w": gain(ks[10], (DEPTH, MLA_QK_DIM)),
        "diff_q_hnorm_w": gain(ks[11], (DEPTH, DIFF_QK_DIM)),
        "diff_k_hnorm_w": gain(ks[12], (DEPTH, DIFF_QK_DIM)),
        "diff_lambda": 0.1 * jax.random.normal(ks[13], (DEPTH, 4, DIFF_QK_DIM), f32),
        "diff_subln_w": gain(ks[14], (DEPTH, DIFF_V_DIM)),
        "w_out": normal(ks[15], (DEPTH, D_MODEL, D_MODEL), D_MODEL),
        "ffn_norm_w": gain(ks[16], (DEPTH, D_MODEL)),
        "router_w": normal(ks[17], (DEPTH, D_MODEL, N_EXPERTS), D_MODEL),
        "expert_w_gate": normal(ks[18], (DEPTH, N_EXPERTS, D_MODEL, EXPERT_FF), D_MODEL),
        "expert_w_up": normal(ks[19], (DEPTH, N_EXPERTS, D_MODEL, EXPERT_FF), D_MODEL),
        "expert_w_down": normal(ks[20], (DEPTH, N_EXPERTS, EXPERT_FF, D_MODEL), EXPERT_FF),
    }


def reference(x, positions, attn_norm_w, w_in, b_gate, mla_q_norm_w, mla_w_uq, mla_kv_norm_w,
              mla_w_ukv, mla_q_hnorm_w, mla_k_hnorm_w, diff_q_hnorm_w, diff_k_hnorm_w,
              diff_lambda, diff_subln_w, w_out, ffn_norm_w, router_w,
              expert_w_gate, expert_w_up, expert_w_down):
    for l in range(DEPTH):
        x = _hybrid_layer(
            x, positions, l, attn_norm_w[l], w_in[l], b_gate[l],
            mla_q_norm_w[l], mla_w_uq[l], mla_kv_norm_w[l], mla_w_ukv[l],
            mla_q_hnorm_w[l], mla_k_hnorm_w[l],
            diff_q_hnorm_w[l], diff_k_hnorm_w[l], diff_lambda[l], diff_subln_w[l],
            w_out[l], ffn_norm_w[l], router_w[l],
            expert_w_gate[l], expert_w_up[l], expert_w_down[l])
    return x
```

```python
import math
from functools import partial as I
from contextlib import ExitStack
import numpy as np
import concourse.bass as bass
import concourse.mybir as mybir
from concourse.bass_utils import run_bass_kernel_spmd

F32 = mybir.dt.float32
BF16 = mybir.dt.bfloat16
I32 = mybir.dt.int32
AF = mybir.ActivationFunctionType
ALU = mybir.AluOpType
AX = mybir.AxisListType

T = 4096
D = 1024
NT = T // 128
EPS = 1e-6
LAM_INIT = 0.8 - 0.6 * math.exp(-0.3 * 0)
NE = 16
CAP = 512
FF = 2048
TWO_PI = 2.0 * math.pi


class R:
    __slots__ = ("name", "w", "r", "pr", "dsem", "dcnt")

    def __init__(self, name):
        self.name = name
        self.w = {}
        self.r = {}
        self.pr = {}
        self.dsem = None
        self.dcnt = 0


class Prog:
    ENG = ("pe", "act", "dve", "pool", "sp")

    def __init__(self, nc):
        self.nc = nc
        self.streams = {e: [] for e in self.ENG}
        self.nops = {e: 0 for e in self.ENG}
        self.known = {e: {} for e in self.ENG}
        self.signal = {e: set() for e in self.ENG}
        self.sigcount = {e: 0 for e in self.ENG}
        self.cnt = {e: {} for e in self.ENG}
        self.sems = {}
        self._semctx = []
        self.dstreams = []
        self.tag = ""
        for e in self.ENG:
            self.sems[("E", e)] = self._new_sem("sem_" + e)

    def _new_sem(self, name):
        ctx = self.nc.semaphore(name)
        s = ctx.__enter__()
        self._semctx.append(ctx)
        return s

    def close(self):
        for ctx in reversed(self._semctx):
            ctx.__exit__(None, None, None)

    def _wait(self, eng, key, val):
        k = self.known[eng]
        if k.get(key, -1) >= val:
            return
        k[key] = val
        if key[0] == "E":
            self.signal[key[1]].add(val)
        self.streams[eng].append(("wait", key, val))

    def _deps(self, eng, reads, writes, pwrites):
        me = ("E", eng)
        for res in reads:
            for key, val in res.w.items():
                if key == me and eng == "pe":
                    continue
                self._wait(eng, key, val)
        for res in writes:
            for key, val in list(res.w.items()) + list(res.r.items()):
                if key == me and eng == "pe":
                    continue
                self._wait(eng, key, val)
        for res in pwrites:
            for key, val in list(res.r.items()) + list(res.pr.items()):
                if key == me and eng == "pe":
                    continue
                self._wait(eng, key, val)

    def _mark(self, key, val, reads, writes, pwrites):
        for res in reads:
            if res.r.get(key, -1) < val:
                res.r[key] = val
        for res in writes:
            pr = dict(res.w)
            for k_, v_ in res.r.items():
                if pr.get(k_, -1) < v_:
                    pr[k_] = v_
            res.pr = pr
            res.w = {key: val}
            res.r = {}
        for res in pwrites:
            if res.w.get(key, -1) < val:
                res.w[key] = val

    def op(self, eng, fn, reads=(), writes=(), pwrites=()):
        self._deps(eng, reads, writes, pwrites)
        idx = self.nops[eng]
        self.nops[eng] += 1
        self.streams[eng].append(("op", fn, idx, self.tag))
        self._mark(("E", eng), idx, reads, writes, pwrites)

    def dma(self, eng, fn, stream, reads=(), writes=(), pwrites=()):
        self._deps(eng, reads, writes, pwrites)
        if stream.dsem is None:
            stream.dsem = {}
            stream.dcnt = {}
        key = ("D", id(stream), eng)
        if eng not in stream.dsem:
            stream.dsem[eng] = self._new_sem("d_%s_%s" % (stream.name, eng))
            stream.dcnt[eng] = 0
            self.sems[key] = stream.dsem[eng]
            self.dstreams.append((stream, eng))
        stream.dcnt[eng] += 1
        val = 16 * stream.dcnt[eng]
        self.streams[eng].append(("dma", fn, stream.dsem[eng]))
        self._mark(key, val, reads, writes, pwrites)

    def barrier(self):
        for e in self.ENG:
            for f in self.ENG:
                if f != e and f != "sp" and self.nops[f] > 0:
                    self._wait(e, ("E", f), self.nops[f] - 1)
            for s, q in self.dstreams:
                self._wait(e, ("D", id(s), q), 16 * s.dcnt[q])

    def emit(self):
        nc = self.nc
        for e in self.ENG:
            for idx in sorted(self.signal[e]):
                if idx not in self.cnt[e]:
                    self.sigcount[e] += 1
                    self.cnt[e][idx] = self.sigcount[e]
            self.signal[e] = set()
        streams = self.streams
        self.streams = {e: [] for e in self.ENG}

        def make(e):
            stream = streams[e]

            def body(engine):
                for ent in stream:
                    if ent[0] == "wait":
                        _, key, val = ent
                        v = self.cnt[key[1]][val] if key[0] == "E" else val
                        engine.wait_ge(self.sems[key], v)
                    elif ent[0] == "op":
                        _, fn, idx, tag = ent
                        ins = fn()
                        if tag:
                            ins.annotate(tag)
                        if idx in self.cnt[e]:
                            ins.then_inc(self.sems[("E", e)], 1)
                    else:
                        _, fn, sem = ent
                        try:
                            ins = fn()
                        except Exception:
                            print("DMA build failed:", fn.func.__name__, {k: str(v)[:200] for k, v in fn.keywords.items()})
                            raise
                        ins.then_inc(sem, 16)
            return body

        with nc.Block() as block:
            block.tensor(make("pe"))
            block.scalar(make("act"))
            block.vector(make("dve"))
            block.gpsimd(make("pool"))
            block.sync(make("sp"))


SKIP_T = 48.0


def build(dbg=False, upto="E", CLS=None, DMIN=None, skip_t=None):
    global SKIP_T
    if skip_t is not None:
        SKIP_T = float(skip_t)
    if CLS is None:
        CLS = np.zeros((16, NT), np.int64)
        DMIN = np.zeros((16, NT), np.float64)
    nc = bass.Bass("TRN2", target_bir_lowering=False)

    def din(name, shape, dt=F32):
        return nc.dram_tensor(name, list(shape), dt, kind="ExternalInput").ap()

    def dscr(name, shape, dt=BF16):
        return nc.dram_tensor(name, list(shape), dt, kind="ExternalOutput" if dbg else "Internal").ap()

    x_d = din("x", [T, D])
    pos_d = din("pos", [T], I32)
    post_d = din("pos_t", [128, NT], I32)
    win_d = din("w_in", [D, 5664])
    bg_d = din("b_gate", [2048])
    wuq_d = din("w_uq", [256, 1536])
    wukv_d = din("w_ukv", [256, 2048])
    wout_d = din("w_out", [D, D])
    rw_d = din("router_w", [D, NE])
    wg_d = din("w_gate", [NE, D, FF])
    wu_d = din("w_up", [NE, D, FF])
    wd_d = din("w_down", [NE, FF, D])
    vec_d = {n: din(n, [k]) for n, k in [("attn_norm_w", 1024), ("q_norm_w", 256), ("kv_norm_w", 256),
                                          ("q_hn", 96), ("k_hn", 96), ("dq_hn", 64), ("dk_hn", 64),
                                          ("lam", 256), ("subln", 128), ("ffn_norm_w", 1024)]}
    ident_d = din("ident", [128, 128])
    invf_d = din("invf", [16])
    iota_d = din("iota512", [512])
    slopes_d = din("slopes", [8])
    tokpi_d = din("tokpi", [128, NT, 2])
    out_d = nc.dram_tensor("out", [T, D], F32, kind="ExternalOutput").ap()

    QmT = dscr("QmT", [16, 96, T])
    KmT = dscr("KmT", [16, 96, T])
    Vm = dscr("Vm", [T, 1024])
    QdT = dscr("QdT", [8, 128, T])
    KdT = dscr("KdT", [8, 128, T])
    Vd = dscr("Vd", [T, 1024])
    G = dscr("G", [T, 2048])
    H2 = dscr("H2", [T, D])
    DBG = dbg
    dbg_idx = nc.dram_tensor("dbg_idx", [4, 128, 1], I32, kind="ExternalOutput").ap() if dbg else None
    dbg_gate = nc.dram_tensor("dbg_gate", [128, 4], F32, kind="ExternalOutput").ap() if dbg else None
    dbg_xe = nc.dram_tensor("dbg_xe", [128, 1024], BF16, kind="ExternalOutput").ap() if dbg else None
    dbg_ye = nc.dram_tensor("dbg_ye", [128, 1024], F32, kind="ExternalOutput").ap() if dbg else None
    dbg_posm = nc.dram_tensor("dbg_posm", [128, NT * NE], F32, kind="ExternalOutput").ap() if dbg else None
    dbg_aff = nc.dram_tensor("dbg_aff", [128, NT * NE], F32, kind="ExternalOutput").ap() if dbg else None
    dbg_hT = nc.dram_tensor("dbg_hT", [128, 16 * CAP], BF16, kind="ExternalOutput").ap() if dbg else None
    PQHL = dscr("PQHL", [8, 2, T])
    MG = dscr("MG", [T, D])
    RQmT, RKmT, RVm, RQdT, RKdT, RVd, RG, RH2, RMG, Rout = [R(n) for n in
                                                          "QmT KmT Vm QdT KdT Vd G H2 MG out".split()]

    P = Prog(nc)
    sb = nc.sbuf_tensor
    with ExitStack() as es1:
        identb = es1.enter_context(sb("identb", [128, 128], BF16))
        identf = es1.enter_context(sb("identf", [128, 128], F32))
        mhalf = es1.enter_context(sb("mhalf", [128, 64], F32))
        cosT = es1.enter_context(sb("cosT", [128, NT, 16], F32))
        sinT = es1.enter_context(sb("sinT", [128, NT, 16], F32))
        posf = es1.enter_context(sb("posf", [128, NT], F32))
        nlam = es1.enter_context(sb("nlam", [128, 2], F32))
        vq_hn = es1.enter_context(sb("vq_hn", [128, 96], F32))
        vk_hn = es1.enter_context(sb("vk_hn", [128, 96], F32))
        vdq_hn = es1.enter_context(sb("vdq_hn", [128, 64], F32))
        vdk_hn = es1.enter_context(sb("vdk_hn", [128, 64], F32))
        vsubln = es1.enter_context(sb("vsubln", [128, 128], F32))
        vqn = es1.enter_context(sb("vqn", [128, 256], F32))
        vkvn = es1.enter_context(sb("vkvn", [128, 256], F32))
        Rid, Rmh, Rcs, Rposf, Rnlam, Rvec = [R(n) for n in "id mh cs posf nlam vec".split()]

        with ExitStack() as es2:
            posi = es2.enter_context(sb("p0_posi", [128, NT], I32))
            invf = es2.enter_context(sb("p0_invf", [128, 16], F32))
            ang = es2.enter_context(sb("p0_ang", [128, NT, 16], F32))
            kf = es2.enter_context(sb("p0_kf", [128, NT, 16], F32))
            ki = es2.enter_context(sb("p0_ki", [128, NT, 16], I32))
            mm_ = es2.enter_context(sb("p0_m", [128, NT, 16], F32))
            lamt = es2.enter_context(sb("p0_lam", [128, 256], F32))
            lp = es2.enter_context(sb("p0_lp", [128, 128], F32))
            ls = es2.enter_context(sb("p0_ls", [128, 4], F32))
            Rt = R("p0tmp")
            P.dma("sp", I(nc.sync.dma_start, out=identf[:], in_=ident_d[:, :]), Rid, pwrites=[Rid])
            P.dma("pool", I(nc.gpsimd.dma_start, out=identb[:], in_=ident_d[:, :]), Rid, pwrites=[Rid])
            P.op("pool", I(nc.gpsimd.memset, mhalf[:], -0.5), writes=[Rmh])
            for tl, nm in [(vq_hn, "q_hn"), (vk_hn, "k_hn"), (vdq_hn, "dq_hn"), (vdk_hn, "dk_hn"),
                           (vsubln, "subln"), (vqn, "q_norm_w"), (vkvn, "kv_norm_w"), (lamt, "lam")]:
                P.dma("sp", I(nc.sync.dma_start, out=tl[:], in_=vec_d[nm].partition_broadcast(128)), Rvec,
                      pwrites=[Rvec])
            P.dma("sp", I(nc.sync.dma_start, out=invf[:], in_=invf_d.partition_broadcast(128)), Rvec, pwrites=[Rvec])
            P.dma("sp", I(nc.sync.dma_start, out=posi[:], in_=post_d[:, :]), Rvec,
                  pwrites=[Rvec])
            P.op("dve", I(nc.vector.tensor_scalar, out=vq_hn[:], in0=vq_hn[:], scalar1=96.0 ** -0.5, scalar2=None,
                          op0=ALU.mult), reads=[Rvec], pwrites=[Rvec])
            P.op("dve", I(nc.vector.tensor_scalar, out=vdq_hn[:], in0=vdq_hn[:], scalar1=64.0 ** -0.5, scalar2=None,
                          op0=ALU.mult), reads=[Rvec], pwrites=[Rvec])
            P.op("dve", I(nc.vector.tensor_scalar, out=vsubln[:], in0=vsubln[:], scalar1=1.0 - LAM_INIT, scalar2=None,
                          op0=ALU.mult), reads=[Rvec], pwrites=[Rvec])
            P.op("dve", I(nc.vector.tensor_tensor, out=lp[:].rearrange("p (a b) -> p a b", a=2),
                          in0=lamt[:].rearrange("p (a two b) -> p a two b", a=2, two=2)[:, :, 0, :],
                          in1=lamt[:].rearrange("p (a two b) -> p a two b", a=2, two=2)[:, :, 1, :], op=ALU.mult),
                 reads=[Rvec], writes=[Rt])
            P.op("dve", I(nc.vector.tensor_reduce, out=ls[:, 0:2], in_=lp[:].rearrange("p (a b) -> p a b", a=2),
                          axis=AX.X, op=ALU.add), reads=[Rt], writes=[Rnlam])
            P.op("act", I(nc.scalar.activation, out=ls[:, 2:4], in_=ls[:, 0:2], func=AF.Exp), reads=[Rnlam],
                 writes=[Rt])
            P.op("dve", I(nc.vector.tensor_tensor, out=nlam[:, 0:1], in0=ls[:, 3:4], in1=ls[:, 2:3], op=ALU.subtract),
                 reads=[Rt], writes=[Rnlam])
            P.op("dve", I(nc.vector.tensor_scalar, out=nlam[:, 0:1], in0=nlam[:, 0:1], scalar1=-LAM_INIT, scalar2=None,
                          op0=ALU.add), reads=[Rnlam], writes=[Rnlam])
            P.op("dve", I(nc.vector.tensor_copy, out=posf[:], in_=posi[:]), reads=[Rvec], writes=[Rposf])
            P.op("dve", I(nc.vector.tensor_tensor, out=ang[:], in0=posf[:].unsqueeze(2).to_broadcast([128, NT, 16]),
                          in1=invf[:].unsqueeze(1).to_broadcast([128, NT, 16]), op=ALU.mult),
                 reads=[Rposf, Rvec], writes=[Rt])

            Rk = R("rk")

            def reduce_sin(dst, shift):
                P.op("dve", I(nc.vector.tensor_scalar, out=kf[:], in0=ang[:], scalar1=1.0 / TWO_PI,
                              scalar2=0.5 + shift / TWO_PI, op0=ALU.mult, op1=ALU.add), reads=[Rt], writes=[Rk])
                P.op("dve", I(nc.vector.tensor_copy, out=ki[:], in_=kf[:]), reads=[Rk], writes=[Rk])
                P.op("dve", I(nc.vector.tensor_copy, out=kf[:], in_=ki[:]), reads=[Rk], writes=[Rk])
                c1 = 6.28125
                c2 = TWO_PI - c1
                P.op("dve", I(nc.vector.scalar_tensor_tensor, out=mm_[:], in0=kf[:], scalar=-c1, in1=ang[:],
                              op0=ALU.mult, op1=ALU.add), reads=[Rk, Rt], writes=[Rk])
                P.op("dve", I(nc.vector.scalar_tensor_tensor, out=mm_[:], in0=kf[:], scalar=-c2, in1=mm_[:],
                              op0=ALU.mult, op1=ALU.add), reads=[Rk], writes=[Rk])
                if shift:
                    P.op("dve", I(nc.vector.tensor_scalar, out=mm_[:], in0=mm_[:], scalar1=shift, scalar2=None,
                                  op0=ALU.add), reads=[Rk], writes=[Rk])
                for thr, op_, adj in [(math.pi, ALU.is_gt, -TWO_PI), (-math.pi, ALU.is_lt, TWO_PI)]:
                    P.op("dve", I(nc.vector.tensor_scalar, out=kf[:], in0=mm_[:], scalar1=thr, scalar2=adj,
                                  op0=op_, op1=ALU.mult), reads=[Rk], writes=[Rk])
                    P.op("dve", I(nc.vector.tensor_tensor, out=mm_[:], in0=mm_[:], in1=kf[:], op=ALU.add),
                         reads=[Rk], writes=[Rk])
                P.op("dve", I(nc.vector.tensor_scalar, out=mm_[:], in0=mm_[:], scalar1=math.pi, scalar2=-math.pi,
                              op0=ALU.min, op1=ALU.max), reads=[Rk], writes=[Rk])
                P.op("act", I(nc.scalar.activation, out=dst[:], in_=mm_[:], func=AF.Sin), reads=[Rk], pwrites=[Rcs])

            reduce_sin(sinT, 0.0)
            reduce_sin(cosT, math.pi / 2)
            P.barrier()
            P.emit()

        env = locals()
        if upto >= "A":
            phaseA(nc, P, env)
        if upto >= "B":
            phaseBC(nc, P, env, upto)
        if upto >= "D":
            phaseDE(nc, P, env, upto)
        P.barrier()
        P.emit()
    P.close()
    return nc


def phaseA(nc, P, env):
    g = env
    sb = nc.sbuf_tensor
    x_d, win_d, bg_d, wuq_d, wukv_d, vec_d = g["x_d"], g["win_d"], g["bg_d"], g["wuq_d"], g["wukv_d"], g["vec_d"]
    identb, mhalf, cosT, sinT = g["identb"], g["mhalf"], g["cosT"], g["sinT"]
    vq_hn, vk_hn, vdq_hn, vdk_hn, vqn, vkvn = g["vq_hn"], g["vk_hn"], g["vdq_hn"], g["vdk_hn"], g["vqn"], g["vkvn"]
    Rid, Rmh, Rcs, Rvec = g["Rid"], g["Rmh"], g["Rcs"], g["Rvec"]
    QmT, KmT, Vm, QdT, KdT, Vd, G = g["QmT"], g["KmT"], g["Vm"], g["QdT"], g["KdT"], g["Vd"], g["G"]
    RQmT, RKmT, RVm, RQdT, RKdT, RVd, RG = g["RQmT"], g["RKmT"], g["RVm"], g["RQdT"], g["RKdT"], g["RVd"], g["RG"]
    with ExitStack() as es3:
        win = es3.enter_context(sb("a_win", [128, 8, 5664], BF16))
        wuq = es3.enter_context(sb("a_wuq", [128, 2, 1536], BF16))
        wukv = es3.enter_context(sb("a_wukv", [128, 2, 2048], BF16))
        wn = es3.enter_context(sb("a_wn", [128, 1024], F32))
        biasr = es3.enter_context(sb("a_bias", [1, 2048], BF16))
        onesr = es3.enter_context(sb("a_ones", [1, 128], BF16))
        xt0 = es3.enter_context(sb("a_xt0", [128, 1024], F32))
        xt1 = es3.enter_context(sb("a_xt1", [128, 1024], F32))
        hb = es3.enter_context(sb("a_hb", [128, 1024], BF16))
        hT = es3.enter_context(sb("a_hT", [128, 1024], BF16))
        sq = es3.enter_context(sb("a_sq", [128, 2048], F32))
        st = es3.enter_context(sb("a_st", [128, 64], F32))
        rstd = es3.enter_context(sb("a_rstd", [128, 64], F32))
        lat = es3.enter_context(sb("a_lat", [128, 512], BF16))
        latT = es3.enter_context(sb("a_latT", [128, 4, 128], BF16))
        qf = es3.enter_context(sb("a_qf", [128, 16, 96], F32))
        qb = es3.enter_context(sb("a_qb", [128, 16, 96], BF16))
        kf = es3.enter_context(sb("a_kf", [128, 16, 32], F32))
        kb = es3.enter_context(sb("a_kb", [128, 16, 96], BF16))
        rt = es3.enter_context(sb("a_rt", [128, 4, 16, 16], F32))
        vb = es3.enter_context(sb("a_vb", [128, 1024], BF16))
        qTs = es3.enter_context(sb("a_qTs", [128, 16, 128], BF16))
        kTs = es3.enter_context(sb("a_kTs", [128, 16, 128], BF16))
        df = es3.enter_context(sb("a_df", [128, 512], F32))
        dqb = es3.enter_context(sb("a_dqb", [128, 1024], BF16))
        dkb = es3.enter_context(sb("a_dkb", [128, 1024], BF16))
        dvb = es3.enter_context(sb("a_dvb", [128, 1024], BF16))
        dqTs = es3.enter_context(sb("a_dqTs", [128, 8, 128], BF16))
        dkTs = es3.enter_context(sb("a_dkTs", [128, 8, 128], BF16))
        gt = es3.enter_context(sb("a_gt", [128, 512], BF16))
        gb = es3.enter_context(sb("a_gb", [128, 2048], BF16))
        kr = es3.enter_context(sb("a_kr", [128, 32], F32))
        krw = es3.enter_context(sb("a_krw", [128, 32], F32))
        psF = es3.enter_context(nc.psum_tensor("a_psF", [128, 6 * 512], F32))
        psB = es3.enter_context(nc.psum_tensor("a_psB", [128, 2 * 1024], BF16))
        Rw = R("aw")
        P.dma("pool", I(nc.gpsimd.dma_start, out=win[:], in_=win_d.rearrange("(kc p) c -> p kc c", p=128)), Rw,
              pwrites=[Rw])
        P.dma("pool", I(nc.gpsimd.dma_start, out=wuq[:], in_=wuq_d.rearrange("(kc p) c -> p kc c", p=128)), Rw,
              pwrites=[Rw])
        P.dma("pool", I(nc.gpsimd.dma_start, out=wukv[:], in_=wukv_d.rearrange("(kc p) c -> p kc c", p=128)), Rw,
              pwrites=[Rw])
        P.dma("pool", I(nc.gpsimd.dma_start, out=biasr[:], in_=bg_d.rearrange("(o c) -> o c", o=1)), Rw, pwrites=[Rw])
        P.dma("sp", I(nc.sync.dma_start, out=wn[:], in_=vec_d["attn_norm_w"].partition_broadcast(128)), Rw,
              pwrites=[Rw])
        P.op("pool", I(nc.gpsimd.memset, onesr[:], 1.0), pwrites=[Rw])

        xts = [xt0, xt1]
        Rxt = [R("xt0"), R("xt1")]
        Rz = [R("z0"), R("z1")]
        zb = [psF[:, 0:512], psF[:, 512:1024]]
        ps4 = psF[:, 1024:3072]
        Rps4 = R("ps4")
        Tb = [psB[:, 0:1024], psB[:, 1024:2048]]
        RT = [R("T0"), R("T1")]
        names = "hb hT sq stx stl stq stk stkr std lat latT qf qb kf kb rt vb qTs kTs df dqb dkb dvb dqTs dkTs gt gb kr krw"
        RR = {n: R(n) for n in names.split()}

        def rs(src, dst, n, Rs, w):
            P.op("dve", I(nc.vector.tensor_scalar, out=dst, in0=src, scalar1=1.0 / n, scalar2=EPS, op0=ALU.mult,
                          op1=ALU.add), reads=[Rs], writes=[Rs])
            P.op("pool", I(nc.gpsimd.tensor_tensor, out=dst, in0=dst, in1=mhalf[:, 0:w], op=ALU.pow),
                 reads=[Rs, Rmh], writes=[Rs])

        def zblock(i, blk):
            P.tag = "z%d" % blk
            bank = zb[blk % 2]
            Rb = Rz[blk % 2]
            ncols = 512 if blk < 11 else 32
            c0 = blk * 512
            gate = 7 <= blk <= 10
            for kc in range(8):
                P.op("pe", I(nc.tensor.matmul, bank[:, 0:ncols], lhsT=hT[:, kc * 128:(kc + 1) * 128],
                             rhs=win[:, kc, c0:c0 + ncols], start=(kc == 0), stop=(kc == 7 and not gate)),
                     reads=[RR["hT"], Rw], writes=[Rb] if kc == 0 else (), pwrites=[Rb] if kc else ())
            if gate:
                gc = (blk - 7) * 512
                P.op("pe", I(nc.tensor.matmul, bank[:, 0:512], lhsT=onesr[0:1, :], rhs=biasr[0:1, gc:gc + 512],
                             start=False, stop=True), reads=[Rw], pwrites=[Rb])
            return bank, Rb

        def rope(i, src, dst, Rsrc, Rdst):
            c = cosT[:, i, :].unsqueeze(1).to_broadcast([128, 16, 16])
            s = sinT[:, i, :].unsqueeze(1).to_broadcast([128, 16, 16])
            x1 = src[:, :, 0:16]
            x2 = src[:, :, 16:32]
            Rrt = RR["rt"]
            P.op("dve", I(nc.vector.tensor_tensor, out=rt[:, 0], in0=x1, in1=c, op=ALU.mult), reads=[Rsrc, Rcs],
                 pwrites=[Rrt])
            P.op("dve", I(nc.vector.tensor_tensor, out=rt[:, 1], in0=x2, in1=s, op=ALU.mult), reads=[Rsrc, Rcs],
                 pwrites=[Rrt])
            P.op("dve", I(nc.vector.tensor_tensor, out=rt[:, 2], in0=x1, in1=s, op=ALU.mult), reads=[Rsrc, Rcs],
                 pwrites=[Rrt])
            P.op("dve", I(nc.vector.tensor_tensor, out=rt[:, 3], in0=x2, in1=c, op=ALU.mult), reads=[Rsrc, Rcs],
                 pwrites=[Rrt])
            P.op("dve", I(nc.vector.tensor_tensor, out=dst[:, :, 0:16], in0=rt[:, 0], in1=rt[:, 1], op=ALU.subtract),
                 reads=[Rrt], pwrites=[Rdst])
            P.op("dve", I(nc.vector.tensor_tensor, out=dst[:, :, 16:32], in0=rt[:, 2], in1=rt[:, 3], op=ALU.add),
                 reads=[Rrt], pwrites=[Rdst])

        latT2 = [latT, es3.enter_context(sb("a_latT1", [128, 4, 128], BF16))]
        kr2 = [kr, es3.enter_context(sb("a_kr1", [128, 32], F32))]
        krw2 = [krw, es3.enter_context(sb("a_krw1", [128, 32], F32))]
        qb2 = [qb, es3.enter_context(sb("a_qb1", [128, 16, 96], BF16))]
        kb2 = [kb, es3.enter_context(sb("a_kb1", [128, 16, 96], BF16))]
        sq2 = sq[:, 512:2048]
        RlatT = [R("latT0"), R("latT1")]
        Rkr = [R("kr0"), R("kr1")]
        Rkrw = [R("krw0"), R("krw1")]
        Rstkr = [R("stkr0"), R("stkr1")]
        Rqb = [R("qb0"), R("qb1")]
        Rkb = [R("kb0"), R("kb1")]
        Rsq2 = R("sq2")

        dfr = [df, es3.enter_context(sb("a_df1", [128, 512], F32)), es3.enter_context(sb("a_df2", [128, 512], F32))]
        Rdfr = [R("df0"), R("df1"), R("df2")]
        Rstd = [R("std0"), R("std1")]
        dfc = [0]

        def dqk(i, blks, dstb, hn, Rdst, dTs, RdTs, dram, Rdram):
            t0 = i * 128
            for j, blk in enumerate(blks):
                bank, Rb = zblock(i, blk)
                k3 = dfc[0] % 3
                dfc[0] += 1
                dfk, Rdfk = dfr[k3], Rdfr[k3]
                c0 = 40 + 8 * (dfc[0] % 2)
                P.op("act", I(nc.scalar.copy, out=dfk[:], in_=bank[:, 0:512]), reads=[Rb], writes=[Rdfk])
                P.op("act", I(nc.scalar.activation, out=sq[:, 0:512], in_=dfk[:], func=AF.Square),
                     reads=[Rdfk], writes=[RR["sq"]])
                P.op("dve", I(nc.vector.tensor_reduce, out=st[:, c0:c0 + 8],
                              in_=sq[:, 0:512].rearrange("p (a b) -> p a b", a=8), axis=AX.X, op=ALU.add),
                     reads=[RR["sq"]], writes=[Rstd[dfc[0] % 2]])
                rs(st[:, c0:c0 + 8], rstd[:, c0:c0 + 8], 64.0, Rstd[dfc[0] % 2], 8)
                P.op("dve", I(nc.vector.tensor_tensor, out=dfk[:].rearrange("p (a b) -> p a b", a=8),
                              in0=dfk[:].rearrange("p (a b) -> p a b", a=8),
                              in1=rstd[:, c0:c0 + 8].unsqueeze(2).to_broadcast([128, 8, 64]),
                              op=ALU.mult), reads=[Rdfk, Rstd[dfc[0] % 2]], writes=[Rdfk])
                P.op("dve", I(nc.vector.tensor_tensor,
                              out=dstb[:, j * 512:(j + 1) * 512].rearrange("p (a b) -> p a b", a=8),
                              in0=dfk[:].rearrange("p (a b) -> p a b", a=8),
                              in1=hn[:].unsqueeze(1).to_broadcast([128, 8, 64]), op=ALU.mult),
                     reads=[Rdfk, Rvec], writes=[Rdst] if j == 0 else (), pwrites=[Rdst] if j else ())

        def dqkT(i, dstb, Rdst, dTs, RdTs, dram, Rdram):
            P.tag = "dqkT"
            t0 = i * 128
            for h in range(8):
                P.op("pe", I(nc.tensor.transpose, out=Tb[0][:, h * 128:(h + 1) * 128],
                             in_=dstb[:, h * 128:(h + 1) * 128], identity=identb[:]),
                     reads=[Rdst, Rid], writes=[RT[0]] if h == 0 else (), pwrites=[RT[0]] if h else ())
            P.op("act", I(nc.scalar.copy, out=dTs[:].rearrange("p a b -> p (a b)"), in_=Tb[0][:, 0:1024]),
                 reads=[RT[0]], writes=[RdTs])
            P.dma("sp", I(nc.sync.dma_start, out=dram[:, :, t0:t0 + 128].rearrange("h d t -> d h t"), in_=dTs[:]),
                  RdTs, reads=[RdTs], pwrites=[Rdram])

        xts3 = [xt0, xt1, es3.enter_context(sb("a_xt2", [128, 1024], F32))]
        Rxt3 = [R("xt0"), R("xt1"), R("xt2")]
        hb2 = [hb, es3.enter_context(sb("a_hb1", [128, 1024], BF16))]
        Rhb = [R("hb0"), R("hb1")]
        Rstx = [R("stx0"), R("stx1")]

        def S1a(i):
            P.tag = "S1a"
            s_ = i % 2
            xt = xts3[i % 3]
            Rx = Rxt3[i % 3]
            t0 = i * 128
            c = 56 + 2 * s_
            P.dma("sp", I(nc.sync.dma_start, out=xt[:], in_=x_d[t0:t0 + 128, :]), Rx, writes=[Rx])
            P.op("act", I(nc.scalar.activation, out=sq[:, 0:1024], in_=xt[:], func=AF.Square,
                          accum_out=st[:, c:c + 1]), reads=[Rx], writes=[RR["sq"], Rstx[s_]])
            rs(st[:, c:c + 1], rstd[:, c:c + 1], 1024.0, Rstx[s_], 1)
            P.op("dve", I(nc.vector.scalar_tensor_tensor, out=hb2[s_][:], in0=xt[:], scalar=rstd[:, c:c + 1], in1=wn[:],
                          op0=ALU.mult, op1=ALU.mult), reads=[Rx, Rstx[s_], Rw], writes=[Rhb[s_]])

        def S1(i):
            P.tag = "S1head"
            s_ = i % 2
            t0 = i * 128
            for kc in range(8):
                P.op("pe", I(nc.tensor.transpose, out=Tb[0][:, kc * 128:(kc + 1) * 128],
                             in_=hb2[s_][:, kc * 128:(kc + 1) * 128], identity=identb[:]),
                     reads=[Rhb[s_], Rid], writes=[RT[0]] if kc == 0 else (), pwrites=[RT[0]] if kc else ())
            P.op("act", I(nc.scalar.copy, out=hT[:], in_=Tb[0][:, 0:1024]), reads=[RT[0]], writes=[RR["hT"]])
            bank, Rb = zblock(i, 0)
            k3 = dfc[0] % 3
            dfc[0] += 1
            latf, Rlatf = dfr[k3], Rdfr[k3]
            P.op("act", I(nc.scalar.copy, out=latf[:], in_=bank[:, 0:512]), reads=[Rb], writes=[Rlatf])
            P.op("act", I(nc.scalar.activation, out=sq[:, 0:512], in_=latf[:], func=AF.Square), reads=[Rlatf],
                 writes=[RR["sq"]])
            P.op("dve", I(nc.vector.tensor_reduce, out=st[:, 1:3], in_=sq[:, 0:512].rearrange("p (a b) -> p a b", a=2),
                          axis=AX.X, op=ALU.add), reads=[RR["sq"]], writes=[RR["stl"]])
            rs(st[:, 1:3], rstd[:, 1:3], 256.0, RR["stl"], 2)
            P.op("dve", I(nc.vector.scalar_tensor_tensor, out=lat[:, 0:256], in0=latf[:, 0:256], scalar=rstd[:, 1:2],
                          in1=vqn[:], op0=ALU.mult, op1=ALU.mult), reads=[Rlatf, RR["stl"], Rvec], writes=[RR["lat"]])
            P.op("dve", I(nc.vector.scalar_tensor_tensor, out=lat[:, 256:512], in0=latf[:, 256:512],
                          scalar=rstd[:, 2:3], in1=vkvn[:], op0=ALU.mult, op1=ALU.mult),
                 reads=[Rlatf, RR["stl"], Rvec], pwrites=[RR["lat"]])
            dqk(i, [1, 2], dqb, vdq_hn, RR["dqb"], dqTs, RR["dqTs"], QdT, RQdT)
            P.tag = "latT"
            for j in range(4):
                P.op("pe", I(nc.tensor.transpose, out=Tb[1][:, j * 128:(j + 1) * 128],
                             in_=lat[:, j * 128:(j + 1) * 128], identity=identb[:]),
                     reads=[RR["lat"], Rid], writes=[RT[1]] if j == 0 else (), pwrites=[RT[1]] if j else ())
            P.op("dve", I(nc.vector.tensor_copy, out=latT2[s_][:].rearrange("p a b -> p (a b)"), in_=Tb[1][:, 0:512]),
                 reads=[RT[1]], writes=[RlatT[s_]])
            dqk(i, [3, 4], dkb, vdk_hn, RR["dkb"], dkTs, RR["dkTs"], KdT, RKdT)
            for j, blk in enumerate([5, 6]):
                bank, Rb = zblock(i, blk)
                P.op("act", I(nc.scalar.copy, out=dvb[:, j * 512:(j + 1) * 512], in_=bank[:, 0:512]), reads=[Rb],
                     writes=[RR["dvb"]] if j == 0 else (), pwrites=[RR["dvb"]] if j else ())
            P.dma("sp", I(nc.sync.dma_start, out=Vd[t0:t0 + 128, :], in_=dvb[:]), RR["dvb"], reads=[RR["dvb"]],
                  pwrites=[RVd])
            dqkT(i, dqb, RR["dqb"], dqTs, RR["dqTs"], QdT, RQdT)
            for j, blk in enumerate([7, 8, 9, 10]):
                if j == 2:
                    dqkT(i, dkb, RR["dkb"], dkTs, RR["dkTs"], KdT, RKdT)
                bank, Rb = zblock(i, blk)
                P.op("act", I(nc.scalar.activation, out=gt[:], in_=bank[:, 0:512], func=AF.Tanh, scale=0.5),
                     reads=[Rb], writes=[RR["gt"]])
                P.op("dve", I(nc.vector.tensor_scalar, out=gb[:, j * 512:(j + 1) * 512], in0=gt[:], scalar1=0.5,
                              scalar2=0.5, op0=ALU.mult, op1=ALU.add), reads=[RR["gt"]],
                     writes=[RR["gb"]] if j == 0 else (), pwrites=[RR["gb"]] if j else ())
            P.dma("sp", I(nc.sync.dma_start, out=G[t0:t0 + 128, :], in_=gb[:]), RR["gb"], reads=[RR["gb"]],
                  pwrites=[RG])
            bank, Rb = zblock(i, 11)
            P.op("act", I(nc.scalar.copy, out=kr2[s_][:], in_=bank[:, 0:32]), reads=[Rb], writes=[Rkr[s_]])
            P.op("act", I(nc.scalar.activation, out=sq[:, 0:32], in_=kr2[s_][:], func=AF.Square,
                          accum_out=st[:, 36 + s_:37 + s_]), reads=[Rkr[s_]], writes=[RR["sq"], Rstkr[s_]])
            P.op("dve", I(nc.vector.tensor_tensor, out=krw2[s_][:], in0=kr2[s_][:], in1=vk_hn[:, 64:96], op=ALU.mult),
                 reads=[Rkr[s_], Rvec], writes=[Rkrw[s_]])

        def S2q(i):
            P.tag = "S2q"
            s_ = i % 2
            for b_ in range(4):
                for kc in range(2):
                    P.op("pe", I(nc.tensor.matmul, ps4[:, b_ * 512:b_ * 512 + 384], lhsT=latT2[s_][:, kc, :],
                                 rhs=wuq[:, kc, b_ * 384:(b_ + 1) * 384], start=(kc == 0), stop=(kc == 1)),
                         reads=[RlatT[s_], Rw], writes=[Rps4] if (b_ == 0 and kc == 0) else (),
                         pwrites=() if (b_ == 0 and kc == 0) else [Rps4])
            qv = ps4.rearrange("p (b x) -> p b x", b=4)[:, :, 0:384].rearrange("p b (h d) -> p b h d", h=4)
            sqv = sq2.rearrange("p (b h d) -> p b h d", b=4, h=4)
            qfv = qf[:].rearrange("p (b h) d -> p b h d", b=4)
            P.op("act", I(nc.scalar.copy, out=qfv, in_=qv), reads=[Rps4], writes=[RR["qf"]])
            P.op("act", I(nc.scalar.activation, out=sqv, in_=qfv, func=AF.Square), reads=[RR["qf"]], writes=[Rsq2])
            P.op("dve", I(nc.vector.tensor_reduce, out=st[:, 4:20].rearrange("p (b h) -> p b h", b=4), in_=sqv,
                          axis=AX.X, op=ALU.add), reads=[Rsq2], writes=[RR["stq"]])
            rs(st[:, 4:20], rstd[:, 4:20], 96.0, RR["stq"], 16)
            P.op("dve", I(nc.vector.tensor_tensor, out=qfv, in0=qfv,
                          in1=rstd[:, 4:20].rearrange("p (b h) -> p b h", b=4).unsqueeze(3).to_broadcast(
                              [128, 4, 4, 96]), op=ALU.mult), reads=[RR["qf"], RR["stq"]], writes=[RR["qf"]])
            P.op("dve", I(nc.vector.tensor_tensor, out=qf[:], in0=qf[:],
                          in1=vq_hn[:].unsqueeze(1).to_broadcast([128, 16, 96]), op=ALU.mult),
                 reads=[RR["qf"], Rvec], writes=[RR["qf"]])
            P.op("dve", I(nc.vector.tensor_copy, out=qb2[s_][:, :, 0:64], in_=qf[:, :, 0:64]), reads=[RR["qf"]],
                 writes=[Rqb[s_]])
            rope(i, qf[:, :, 64:96], qb2[s_][:, :, 64:96], RR["qf"], Rqb[s_])

        def S2k(i):
            P.tag = "S2k"
            s_ = i % 2
            t0 = i * 128
            for b_ in range(4):
                for kc in range(2):
                    P.op("pe", I(nc.tensor.matmul, ps4[:, b_ * 512:(b_ + 1) * 512], lhsT=latT2[s_][:, 2 + kc, :],
                                 rhs=wukv[:, kc, b_ * 512:(b_ + 1) * 512], start=(kc == 0), stop=(kc == 1)),
                         reads=[RlatT[s_], Rw], writes=[Rps4] if (b_ == 0 and kc == 0) else (),
                         pwrites=() if (b_ == 0 and kc == 0) else [Rps4])
            kvv = ps4.rearrange("p (h d) -> p h d", h=16)
            sqk = sq2[:, 0:1024].rearrange("p (h d) -> p h d", h=16)
            P.op("act", I(nc.scalar.activation, out=sqk, in_=kvv[:, :, 0:64], func=AF.Square), reads=[Rps4],
                 writes=[Rsq2])
            P.op("dve", I(nc.vector.tensor_reduce, out=st[:, 20:36], in_=sqk, axis=AX.X, op=ALU.add),
                 reads=[Rsq2], writes=[RR["stk"]])
            P.op("dve", I(nc.vector.tensor_scalar, out=st[:, 20:36], in0=st[:, 20:36], scalar1=st[:, 36 + s_:37 + s_],
                          scalar2=None, op0=ALU.add), reads=[RR["stk"], Rstkr[s_]], writes=[RR["stk"]])
            rs(st[:, 20:36], rstd[:, 20:36], 96.0, RR["stk"], 16)
            P.op("dve", I(nc.vector.tensor_tensor, out=qf[:, :, 0:64], in0=kvv[:, :, 0:64],
                          in1=rstd[:, 20:36].unsqueeze(2).to_broadcast([128, 16, 64]), op=ALU.mult),
                 reads=[Rps4, RR["stk"]], writes=[RR["qf"]])
            P.op("dve", I(nc.vector.tensor_tensor, out=kb2[s_][:, :, 0:64], in0=qf[:, :, 0:64],
                          in1=vk_hn[:, 0:64].unsqueeze(1).to_broadcast([128, 16, 64]), op=ALU.mult),
                 reads=[RR["qf"], Rvec], writes=[Rkb[s_]])
            P.op("dve", I(nc.vector.tensor_tensor, out=kf[:], in0=krw2[s_][:].unsqueeze(1).to_broadcast([128, 16, 32]),
                          in1=rstd[:, 20:36].unsqueeze(2).to_broadcast([128, 16, 32]), op=ALU.mult),
                 reads=[Rkrw[s_], RR["stk"]], writes=[RR["kf"]])
            rope(i, kf[:], kb2[s_][:, :, 64:96], RR["kf"], Rkb[s_])
            P.op("act", I(nc.scalar.copy, out=vb[:].rearrange("p (h d) -> p h d", h=16), in_=kvv[:, :, 64:128]),
                 reads=[Rps4], writes=[RR["vb"]])
            P.dma("sp", I(nc.sync.dma_start, out=Vm[t0:t0 + 128, :], in_=vb[:]), RR["vb"], reads=[RR["vb"]],
                  pwrites=[RVm])

        def S3(i):
            P.tag = "S3"
            s_ = i % 2
            t0 = i * 128
            for src, Rsrc, dst, Rdst, dram, Rdram in [(qb2[s_], Rqb[s_], qTs, RR["qTs"], QmT, RQmT),
                                                       (kb2[s_], Rkb[s_], kTs, RR["kTs"], KmT, RKmT)]:
                for half in range(2):
                    for hh in range(8):
                        h = half * 8 + hh
                        P.op("pe", I(nc.tensor.transpose, out=Tb[half][0:96, hh * 128:(hh + 1) * 128],
                                     in_=src[:, h, :], identity=identb[:]), reads=[Rsrc, Rid],
                             writes=[RT[half]] if hh == 0 else (), pwrites=[RT[half]] if hh else ())
                    eng = "act" if half == 0 else "dve"
                    fn = nc.scalar.copy if half == 0 else nc.vector.tensor_copy
                    P.op(eng, I(fn, out=dst[0:96, half * 8:(half + 1) * 8, :].rearrange("p a b -> p (a b)"),
                                in_=Tb[half][0:96, 0:1024]), reads=[RT[half]],
                         writes=[Rdst] if half == 0 else (), pwrites=[Rdst] if half else ())
                P.dma("sp", I(nc.sync.dma_start, out=dram[:, :, t0:t0 + 128].rearrange("h d t -> d h t"),
                              in_=dst[0:96, :, :]), Rdst, reads=[Rdst], pwrites=[Rdram])

        S1a(0)
        S1a(1)
        S1(0)
        for i in range(NT):
            if i + 1 < NT:
                S1(i + 1)
            if i >= 1:
                S3(i - 1)
            if i + 2 < NT:
                S1a(i + 2)
            S2q(i)
            S2k(i)
        S3(NT - 1)
        P.barrier()
        P.emit()


def skip_tile(h, q0, q1, k0, k1):
    return False


def phaseBC(nc, P, env, upto):
    g = env
    sb = nc.sbuf_tensor
    QmT, KmT, Vm, QdT, KdT, Vd, G, MG = [g[k] for k in "QmT KmT Vm QdT KdT Vd G MG".split()]
    RMG = g["RMG"]
    mhalf, nlam, vsubln, posf, pos_d = g["mhalf"], g["nlam"], g["vsubln"], g["posf"], g["pos_d"]
    Rmh, Rnlam, Rvec, Rposf = g["Rmh"], g["Rnlam"], g["Rvec"], g["Rposf"]
    with ExitStack() as es:
        omla = es.enter_context(sb("om", [128, NT, 1024], BF16))
        Rom = R("omla")
        with ExitStack() as es2:
            qT = [es2.enter_context(sb("b_qT%d" % k, [96, T], BF16)) for k in range(2)]
            kT = [es2.enter_context(sb("b_kT%d" % k, [96, T], BF16)) for k in range(2)]
            vv = [es2.enter_context(sb("b_v%d" % k, [128, NT, 65], BF16)) for k in range(2)]
            ga = [es2.enter_context(sb("b_ga%d" % k, [128, NT, 64], BF16)) for k in range(2)]
            pT = [es2.enter_context(sb("b_pT%d" % k, [128, 1024], BF16)) for k in range(3)]
            rc = es2.enter_context(sb("b_rc", [128, 8], F32))
            ps = es2.enter_context(nc.psum_tensor("b_ps", [128, 8 * 512], F32))
            Rq = [R("bq0"), R("bq1")]
            RpT = [R("pT0"), R("pT1"), R("pT2")]
            Rs = [R("bs0"), R("bs1"), R("bs2")]
            Ro = [R("bo0"), R("bo1")]
            Rrc = R("brc")
            sbank = [ps[:, 1024 * k:1024 * (k + 1)] for k in range(3)]

            def obank(os_, qs):
                b0 = 3072 + 512 * os_ + 66 * qs
                return ps[:, b0:b0 + 65]
            for k in range(2):
                P.op("pool", I(nc.gpsimd.memset, vv[k][:, :, 64:65], 1.0), pwrites=[Rq[k]])
            step = 0
            for h in range(16):
                s_ = h % 2
                P.dma("sp", I(nc.sync.dma_start, out=qT[s_][:], in_=QmT[h, :, :]), Rq[s_], pwrites=[Rq[s_]])
                P.dma("sp", I(nc.sync.dma_start, out=kT[s_][:], in_=KmT[h, :, :]), Rq[s_], pwrites=[Rq[s_]])
                P.dma("sp", I(nc.sync.dma_start, out=vv[s_][:, :, 0:64],
                              in_=Vm[:, h * 64:(h + 1) * 64].rearrange("(i p) d -> p i d", p=128)), Rq[s_],
                      pwrites=[Rq[s_]])
                P.dma("sp", I(nc.sync.dma_start, out=ga[s_][:],
                              in_=G[:, h * 64:(h + 1) * 64].rearrange("(i p) d -> p i d", p=128)), Rq[s_],
                      pwrites=[Rq[s_]])
                steps = [(qb, kp) for qb in range(8) for kp in range(16)]

                def b_front(qb, kp, sb_, pb_, s_=s_):
                    P.tag = "Bf"
                    for kk in range(2):
                        kt = kp * 2 + kk
                        P.op("pe", I(nc.tensor.matmul, sbank[sb_][:, kk * 512:(kk + 1) * 512],
                                     lhsT=kT[s_][:, kt * 128:(kt + 1) * 128],
                                     rhs=qT[s_][:, qb * 512:(qb + 1) * 512], start=True, stop=True),
                             reads=[Rq[s_]], writes=[Rs[sb_]] if kk == 0 else (), pwrites=[Rs[sb_]] if kk else ())
                    P.op("act", I(nc.scalar.activation, out=pT[pb_][:], in_=sbank[sb_], func=AF.Exp),
                         reads=[Rs[sb_]], writes=[RpT[pb_]])

                def b_back(qb, kp, pb_, s_=s_, h=h):
                    P.tag = "Bb"
                    os_ = (h * 8 + qb) % 2
                    for kk in range(2):
                        kt = kp * 2 + kk
                        for qs in range(4):
                            w_ = kt == 0 and qs == 0
                            P.op("pe", I(nc.tensor.matmul, obank(os_, qs),
                                         lhsT=pT[pb_][:, kk * 512 + qs * 128:kk * 512 + (qs + 1) * 128],
                                         rhs=vv[s_][:, kt, :], start=w_, stop=(kt == NT - 1), skip_group_check=True),
                                 reads=[RpT[pb_], Rq[s_]], writes=[Ro[os_]] if w_ else (),
                                 pwrites=() if w_ else [Ro[os_]])
                    if kp == 15:
                        P.tag = "Bep"
                        ov = ps[:, 3072 + 512 * os_:3072 + 512 * os_ + 264].rearrange("p (q c) -> p q c", q=4)
                        P.op("dve", I(nc.vector.reciprocal, out=rc[:, 0:4], in_=ov[:, :, 64]), reads=[Ro[os_]],
                             writes=[Rrc])
                        for qs in range(4):
                            ti = qb * 4 + qs
                            P.op("dve", I(nc.vector.scalar_tensor_tensor, out=omla[:, ti, h * 64:(h + 1) * 64],
                                          in0=obank(os_, qs)[:, 0:64], scalar=rc[:, qs:qs + 1], in1=ga[s_][:, ti, :],
                                          op0=ALU.mult, op1=ALU.mult), reads=[Ro[os_], Rrc, Rq[s_]], pwrites=[Rom])

                ring = []
                for n in range(len(steps) + 1):
                    if n < len(steps):
                        sb_, pb_ = step % 3, step % 3
                        step += 1
                        b_front(steps[n][0], steps[n][1], sb_, pb_)
                        ring.append(pb_)
                    if n >= 1:
                        b_back(steps[n - 1][0], steps[n - 1][1], ring[n - 1])
            if upto < "C":
                for i in range(NT):
                    P.dma("sp", I(nc.sync.dma_start, out=MG[i * 128:(i + 1) * 128, :], in_=omla[:, i, :]), Rom,
                          reads=[Rom], pwrites=[RMG])
            P.barrier()
            P.emit()
        if upto < "C":
            return

        CLS, DMIN = g["CLS"], g["DMIN"]
        slp_d, PQHL = g["slopes_d"], g["PQHL"]
        RPQHL = R("PQHL")
        with ExitStack() as es2:
            pq = es2.enter_context(sb("c_pq", [128, T], F32))
            Rpq = R("cpq")
            with ExitStack() as es3:
                pqi = es3.enter_context(sb("c_pqi", [128, T], I32))
                ahi = es3.enter_context(sb("c_ahi", [8, T], BF16))
                alo = es3.enter_context(sb("c_alo", [8, T], BF16))
                shi = es3.enter_context(sb("c_shi", [8, T], BF16))
                slo = es3.enter_context(sb("c_slo", [8, T], BF16))
                msl = es3.enter_context(sb("c_msl", [8, 1], F32))
                Rt = R("ctmp")
                P.dma("sp", I(nc.sync.dma_start, out=pqi[:], in_=pos_d.partition_broadcast(128)), Rpq, writes=[Rpq])
                P.dma("sp", I(nc.sync.dma_start, out=msl[:], in_=slp_d.rearrange("(h o) -> h o", o=1)), Rt,
                      writes=[Rt])
                P.op("dve", I(nc.vector.tensor_copy, out=pq[:], in_=pqi[:]), reads=[Rpq], writes=[Rpq])
                P.op("dve", I(nc.vector.tensor_copy, out=ahi[:], in_=pq[0:8, :]), reads=[Rpq], pwrites=[Rt])
                P.op("dve", I(nc.vector.tensor_tensor, out=alo[:], in0=pq[0:8, :], in1=ahi[:], op=ALU.subtract),
                     reads=[Rpq, Rt], pwrites=[Rt])
                P.op("dve", I(nc.vector.tensor_scalar, out=shi[:], in0=ahi[:], scalar1=msl[:, 0:1], scalar2=None,
                              op0=ALU.mult), reads=[Rt], pwrites=[Rt])
                P.op("dve", I(nc.vector.tensor_scalar, out=slo[:], in0=alo[:], scalar1=msl[:, 0:1], scalar2=None,
                              op0=ALU.mult), reads=[Rt], pwrites=[Rt])
                P.dma("sp", I(nc.sync.dma_start, out=PQHL[:, 0, :], in_=shi[:]), Rt, reads=[Rt], pwrites=[RPQHL])
                P.dma("sp", I(nc.sync.dma_start, out=PQHL[:, 1, :], in_=slo[:]), Rt, reads=[Rt], pwrites=[RPQHL])
                P.barrier()
                P.emit()
            nposf = es2.enter_context(sb("c_nposf", [128, NT], F32))
            bia = es2.enter_context(sb("c_bia", [128, 2, 2, NT], F32))
            P.op("dve", I(nc.vector.tensor_scalar, out=nposf[:], in0=posf[:], scalar1=-1.0, scalar2=None,
                          op0=ALU.mult), reads=[Rposf], writes=[Rpq])
            qT = [[es2.enter_context(sb("c_qT%d%d" % (k, m), [68, T], BF16)) for m in range(2)] for k in range(2)]
            kT = [[es2.enter_context(sb("c_kT%d%d" % (k, m), [68, T], BF16)) for m in range(2)] for k in range(2)]
            vv = [es2.enter_context(sb("c_v%d" % k, [128, NT, 129], BF16)) for k in range(2)]
            gbt = [es2.enter_context(sb("c_gb%d" % k, [128, NT, 128], BF16)) for k in range(2)]
            pT = [es2.enter_context(sb("c_pT%d" % k, [128, 512], BF16)) for k in range(4)]
            sp_ = [es2.enter_context(sb("c_sp%d" % k, [128, 512], F32)) for k in range(2)]
            dt_ = [es2.enter_context(sb("c_dt%d" % k, [128, 256], F32)) for k in range(2)]
            rc = es2.enter_context(sb("c_rc", [128, 16], F32))
            of = es2.enter_context(sb("c_of", [128, 4, 128], F32))
            jk = es2.enter_context(sb("c_jk", [128, 2, 128], F32))
            ps = es2.enter_context(nc.psum_tensor("c_ps", [128, 8 * 512], F32))
            Rq = [R("cq0"), R("cq1")]
            Rgs = [R("cgs0"), R("cgs1")]
            Rbia = [R("cbia0"), R("cbia1")]
            RpT = [R("cpT%d" % k) for k in range(4)]
            Rsp = [R("csp%d" % k) for k in range(2)]
            Rdt = [R("cdt%d" % k) for k in range(2)]
            Rs = [R("cs%d" % k) for k in range(4)]
            Ro = [R("co0"), R("co1")]
            Rrc, Rof, Rjk = R("crc"), R("cof"), R("cjk")
            sbank = [ps[:, 512 * k:512 * (k + 1)] for k in range(4)]
            def oacc(s, mp, qs):
                b0 = 2048 + (2 * s + mp) * 512 + qs * 132
                return ps[:, b0:b0 + 129]

            def oset(s):
                return ps[:, 2048 + 2 * s * 512:2048 + (2 * s + 2) * 512]
            for k in range(2):
                P.op("pool", I(nc.gpsimd.memset, vv[k][:, :, 128:129], 1.0), pwrites=[Rq[k]])
                for m in range(2):
                    P.op("dve", I(nc.vector.memset, kT[k][m][64:68, :], 2.0), pwrites=[Rq[k]])
                    P.op("dve", I(nc.vector.memset, kT[k][m][64:66, :], -1.0), pwrites=[Rq[k]])
            step = 0
            dstep = 0
            oset_i = 0
            for h in range(8):
                s_ = h % 2
                slope = 2.0 ** (-(h + 1))
                for m in range(2):
                    P.dma("sp", I(nc.sync.dma_start, out=qT[s_][m][0:64, :], in_=QdT[h, m * 64:(m + 1) * 64, :]),
                          Rq[s_], pwrites=[Rq[s_]])
                    for a_ in range(2):
                        P.dma("sp", I(nc.sync.dma_start, out=qT[s_][m][64 + 2 * a_:66 + 2 * a_, :], in_=PQHL[h, :, :]),
                              Rq[s_], reads=[RPQHL], pwrites=[Rq[s_]])
                    P.dma("sp", I(nc.sync.dma_start, out=kT[s_][m][0:64, :], in_=KdT[h, m * 64:(m + 1) * 64, :]),
                          Rq[s_], pwrites=[Rq[s_]])
                P.dma("sp", I(nc.sync.dma_start, out=vv[s_][:, :, 0:128],
                              in_=Vd[:, h * 128:(h + 1) * 128].rearrange("(i p) d -> p i d", p=128)), Rq[s_],
                      pwrites=[Rq[s_]])
                P.dma("sp", I(nc.sync.dma_start, out=gbt[s_][:],
                              in_=G[:, 1024 + h * 128:1024 + (h + 1) * 128].rearrange("(i p) d -> p i d", p=128)),
                      Rq[s_], pwrites=[Rq[s_]])
                P.op("dve", I(nc.vector.tensor_tensor, out=gbt[s_][:], in0=gbt[s_][:],
                              in1=vsubln[:].unsqueeze(1).to_broadcast([128, NT, 128]), op=ALU.mult),
                     reads=[Rq[s_], Rvec], writes=[Rgs[s_]])
                P.op("dve", I(nc.vector.tensor_scalar, out=bia[:, s_, 0, :], in0=posf[:], scalar1=slope, scalar2=None,
                              op0=ALU.mult), reads=[Rposf], writes=[Rbia[s_]])
                P.op("dve", I(nc.vector.tensor_scalar, out=bia[:, s_, 1, :], in0=posf[:], scalar1=-slope, scalar2=None,
                              op0=ALU.mult), reads=[Rposf], pwrites=[Rbia[s_]])
                steps = []
                for qb in range(16):
                    kts = [kt for kt in range(NT) if slope * DMIN[qb][kt] < SKIP_T]
                    for n_, kt in enumerate(kts):
                        steps.append((qb, kt, int(CLS[qb][kt]), n_ == 0, n_ == len(kts) - 1))

                def st1(n, r4, d2, s_=s_):
                    P.tag = "C1"
                    qb, kt, cl, first, last = steps[n]
                    q0 = qb * 256
                    K_ = (64, 66, 68)[cl]
                    for mp in range(2):
                        P.op("pe", I(nc.tensor.matmul, sbank[r4][:, mp * 256:(mp + 1) * 256],
                                     lhsT=kT[s_][mp][0:K_, kt * 128:(kt + 1) * 128],
                                     rhs=qT[s_][mp][0:K_, q0:q0 + 256], start=True, stop=True),
                             reads=[Rq[s_]], writes=[Rs[r4]] if mp == 0 else (),
                             pwrites=[Rs[r4]] if mp else ())
                    if cl == 0:
                        P.op("act", I(nc.scalar.activation, out=dt_[d2][:], in_=pq[:, q0:q0 + 256], func=AF.Abs,
                                      bias=nposf[:, kt:kt + 1]), reads=[Rpq], writes=[Rdt[d2]])

                def st23(n, r4, d2, s_=s_, slope=slope):
                    P.tag = "C2"
                    qb, kt, cl, first, last = steps[n]
                    if cl == 0:
                        P.op("dve", I(nc.vector.scalar_tensor_tensor,
                                      out=sp_[d2][:].rearrange("p (m q) -> p m q", m=2),
                                      in0=dt_[d2][:].unsqueeze(1).to_broadcast([128, 2, 256]), scalar=-slope,
                                      in1=sbank[r4].rearrange("p (m q) -> p m q", m=2), op0=ALU.mult, op1=ALU.add),
                             reads=[Rdt[d2], Rs[r4]], writes=[Rsp[d2]])
                        P.op("act", I(nc.scalar.activation, out=pT[r4][:], in_=sp_[d2][:], func=AF.Exp),
                             reads=[Rsp[d2]], writes=[RpT[r4]])
                    else:
                        P.op("act", I(nc.scalar.activation, out=pT[r4][:], in_=sbank[r4], func=AF.Exp,
                                      bias=bia[:, s_, cl - 1, kt:kt + 1]), reads=[Rs[r4], Rbia[s_]],
                             writes=[RpT[r4]])

                def st4(n, r4, os_, s_=s_, h=h):
                    P.tag = "C4"
                    qb, kt, cl, first, last = steps[n]
                    for mp in range(2):
                        for qs in range(2):
                            w_ = first and mp == 0 and qs == 0
                            P.op("pe", I(nc.tensor.matmul, oacc(os_, mp, qs),
                                         lhsT=pT[r4][:, mp * 256 + qs * 128:mp * 256 + (qs + 1) * 128],
                                         rhs=vv[s_][:, kt, :], start=(first and qs == 0), stop=last,
                                         skip_group_check=True),
                                 reads=[RpT[r4], Rq[s_]], writes=[Ro[os_]] if w_ else (),
                                 pwrites=() if w_ else [Ro[os_]])
                    if last:
                        pend.append((qb, os_))

                def ep(qb, os_, s_=s_, h=h):
                    P.tag = "Cep"
                    ti = qb * 2
                    v = oset(os_).rearrange("p (m c) -> p m c", m=2)[:, :, 0:264].rearrange("p m (q c) -> p m q c", q=2)
                    o1, o2 = v[:, 0, :, 0:128], v[:, 1, :, 0:128]
                    bc = lambda ap: ap.unsqueeze(2).to_broadcast([128, 2, 128])
                    P.op("dve", I(nc.vector.reciprocal, out=rc[:, 0:4].rearrange("p (m q) -> p m q", m=2),
                                  in_=v[:, :, :, 128]), reads=[Ro[os_]], writes=[Rrc])
                    P.op("dve", I(nc.vector.tensor_scalar, out=rc[:, 4:6], in0=rc[:, 2:4], scalar1=nlam[:, 0:1],
                                  scalar2=None, op0=ALU.mult), reads=[Rrc, Rnlam], pwrites=[Rrc])
                    P.op("dve", I(nc.vector.tensor_tensor, out=of[:, 0:2, :], in0=o2, in1=bc(rc[:, 4:6]), op=ALU.mult),
                         reads=[Ro[os_], Rrc], writes=[Rof])
                    P.op("dve", I(nc.vector.tensor_tensor, out=of[:, 2:4, :], in0=o1, in1=bc(rc[:, 0:2]), op=ALU.mult),
                         reads=[Ro[os_], Rrc], pwrites=[Rof])
                    P.op("dve", I(nc.vector.tensor_tensor, out=of[:, 0:2, :], in0=of[:, 0:2, :], in1=of[:, 2:4, :],
                                  op=ALU.add), reads=[Rof], pwrites=[Rof])
                    P.op("act", I(nc.scalar.activation, out=jk[:], in_=of[:, 0:2, :], func=AF.Square), reads=[Rof],
                         writes=[Rjk])
                    P.op("dve", I(nc.vector.tensor_reduce, out=rc[:, 6:8], in_=jk[:], axis=AX.X, op=ALU.add),
                         reads=[Rjk], pwrites=[Rrc])
                    P.op("dve", I(nc.vector.tensor_scalar, out=rc[:, 8:10], in0=rc[:, 6:8], scalar1=1.0 / 128,
                                  scalar2=EPS, op0=ALU.mult, op1=ALU.add), reads=[Rrc], pwrites=[Rrc])
                    P.op("pool", I(nc.gpsimd.tensor_tensor, out=rc[:, 10:12], in0=rc[:, 8:10], in1=mhalf[:, 0:2],
                                   op=ALU.pow), reads=[Rrc, Rmh], pwrites=[Rrc])
                    P.op("dve", I(nc.vector.tensor_tensor, out=of[:, 2:4, :], in0=of[:, 0:2, :], in1=bc(rc[:, 10:12]),
                                  op=ALU.mult), reads=[Rof, Rrc], pwrites=[Rof])
                    P.op("dve", I(nc.vector.tensor_tensor, out=of[:, 0:2, :], in0=of[:, 2:4, :],
                                  in1=gbt[s_][:, ti:ti + 2, :], op=ALU.mult), reads=[Rof, Rgs[s_], Rq[s_]],
                         pwrites=[Rof])
                    P.op("dve", I(nc.vector.tensor_tensor, out=omla[:, ti:ti + 2, h * 128:(h + 1) * 128],
                                  in0=of[:, 0:2, :], in1=omla[:, ti:ti + 2, h * 128:(h + 1) * 128], op=ALU.add),
                         reads=[Rof, Rom], pwrites=[Rom])

                ep_after = {}
                qbs = sorted(set(st_[0] for st_ in steps))
                for a_, qb_ in enumerate(qbs[:-1]):
                    nq = qbs[a_ + 1]
                    idxs = [k for k, st_ in enumerate(steps) if st_[0] == nq]
                    dg = [k for k in idxs if steps[k][2] == 0]
                    at = dg[-1] if dg else min(idxs[0] + 1, idxs[-1])
                    at = min(at, idxs[-1] - 1) if len(idxs) > 1 else idxs[0]
                    ep_after.setdefault(at, []).append(qb_)
                pend = []
                due = set()
                r4s, d2s, oss = [], [], []
                for n in range(len(steps) + 2):
                    if n < len(steps):
                        r4 = step % 4
                        step += 1
                        d2 = dstep % 2
                        if steps[n][2] == 0:
                            dstep += 1
                        if steps[n][3]:
                            oset_i += 1
                        r4s.append(r4)
                        d2s.append(d2)
                        oss.append(oset_i % 2)
                        st1(n, r4, d2)
                    if 1 <= n <= len(steps):
                        st23(n - 1, r4s[n - 1], d2s[n - 1])
                        due.update(ep_after.get(n - 1, []))
                        for pq_ in [p_ for p_ in pend if p_[0] in due]:
                            pend.remove(pq_)
                            ep(*pq_)
                    if n >= 2:
                        st4(n - 2, r4s[n - 2], oss[n - 2])
                        for pq_ in [p_ for p_ in pend if p_[0] in due]:
                            pend.remove(pq_)
                            ep(*pq_)
                for pq_ in pend:
                    ep(*pq_)
                pend = []
            for i in range(NT):
                P.dma("sp", I(nc.sync.dma_start, out=MG[i * 128:(i + 1) * 128, :], in_=omla[:, i, :]), Rom,
                      reads=[Rom], pwrites=[RMG])
            P.barrier()
            P.emit()


def phaseDE(nc, P, env, upto):
    g = env
    sb = nc.sbuf_tensor
    x_d, out_d, MG, H2, wout_d, rw_d, vec_d = [g[k] for k in "x_d out_d MG H2 wout_d rw_d vec_d".split()]
    wg_d, wu_d, wd_d, iota_d, tokpi_d = [g[k] for k in "wg_d wu_d wd_d iota_d tokpi_d".split()]
    identb, identf, mhalf = g["identb"], g["identf"], g["mhalf"]
    Rid, Rmh = g["Rid"], g["Rmh"]
    with ExitStack() as es:
        aff = es.enter_context(sb("aff", [128, NT, NE], F32))
        posm = es.enter_context(sb("posm", [128, NT, NE], F32))
        Raff, Rposm = R("aff"), R("posm")
        wsl = [es.enter_context(sb("e_w%d" % k, [128, 16384], BF16)) for k in range(3)]
        Rws = [R("ew%d" % k) for k in range(4)]

        def wload(e, which):
            for k, src in enumerate([wg_d, wu_d]):
                if k not in which:
                    continue
                sl = (3 * e + k) % 4
                P.dma("pool", I(nc.gpsimd.dma_start, out=wsl[sl][:].rearrange("p (kc f) -> p kc f", kc=8),
                                in_=src[e].rearrange("(kc p) f -> p kc f", p=128)), Rws[sl], writes=[Rws[sl]])
            if 2 in which:
                sl = (3 * e + 2) % 4
                P.dma("pool", I(nc.gpsimd.dma_start, out=wsl[sl][:].rearrange("p (j c) -> p j c", j=16),
                                in_=wd_d[e].rearrange("(j p) c -> p j c", p=128)), Rws[sl], writes=[Rws[sl]])
        with ExitStack() as es2:
            wout = es2.enter_context(sb("d_wout", [128, 8, 1024], BF16))
            rw = es2.enter_context(sb("d_rw", [128, 8, NE], F32))
            wn2 = es2.enter_context(sb("d_wn2", [128, 1024], F32))
            mg = [es2.enter_context(sb("d_mg%d" % k, [128, 1024], BF16)) for k in range(2)]
            xt = [es2.enter_context(sb("d_xt%d" % k, [128, 1024], F32)) for k in range(2)]
            x1 = [es2.enter_context(sb("d_x1%d" % k, [128, 1024], F32)) for k in range(2)]
            mT = es2.enter_context(sb("d_mT", [128, 1024], BF16))
            sq = es2.enter_context(sb("d_sq", [128, 1024], F32))
            h2f = es2.enter_context(sb("d_h2f", [128, 1024], F32))
            h2b = [es2.enter_context(sb("d_h2b%d" % k, [128, 1024], BF16)) for k in range(2)]
            h2T = es2.enter_context(sb("d_h2T", [128, 1024], F32))
            sm = es2.enter_context(sb("d_sm", [128, 64], F32))
            psB = es2.enter_context(nc.psum_tensor("d_psB", [128, 1024], BF16))
            psF = es2.enter_context(nc.psum_tensor("d_psF", [128, 5 * 512], F32))
            Rw = R("dw")
            Rmg, Rxt, Rx1, Rh2b = [[R(n + str(k)) for k in range(2)] for n in ("dmg", "dxt", "dx1", "dh2b")]
            RmT, Rsq, Rh2f, Rh2T, Rsm, RTb, Racc, RTf, Rlg = [R(n) for n in
                                                              "dmT dsq dh2f dh2T dsm dTb dacc dTf dlg".split()]
            RH2, Rout = g["RH2"], g["Rout"]
            P.dma("pool", I(nc.gpsimd.dma_start, out=wout[:], in_=wout_d.rearrange("(kc p) c -> p kc c", p=128)), Rw,
                  pwrites=[Rw])
            P.dma("sp", I(nc.sync.dma_start, out=rw[:], in_=rw_d.rearrange("(kc p) c -> p kc c", p=128)), Rw,
                  pwrites=[Rw])
            P.dma("sp", I(nc.sync.dma_start, out=wn2[:], in_=vec_d["ffn_norm_w"].partition_broadcast(128)), Rw,
                  pwrites=[Rw])
            wload(0, (0, 1, 2))
            acc = psF[:, 0:1024]
            Tf = psF[:, 1024:2048]
            lg = psF[:, 2048:2048 + NE]
            for i in range(NT):
                s_ = i % 2
                t0 = i * 128
                P.dma("sp", I(nc.sync.dma_start, out=mg[s_][:], in_=MG[t0:t0 + 128, :]), Rmg[s_], writes=[Rmg[s_]])
                P.dma("sp", I(nc.sync.dma_start, out=xt[s_][:], in_=x_d[t0:t0 + 128, :]), Rxt[s_], writes=[Rxt[s_]])
                for kc in range(8):
                    P.op("pe", I(nc.tensor.transpose, out=psB[:, kc * 128:(kc + 1) * 128],
                                 in_=mg[s_][:, kc * 128:(kc + 1) * 128], identity=identb[:]),
                         reads=[Rmg[s_], Rid], writes=[RTb] if kc == 0 else (), pwrites=[RTb] if kc else ())
                P.op("act", I(nc.scalar.copy, out=mT[:], in_=psB[:, 0:1024]), reads=[RTb], writes=[RmT])
                for half in range(2):
                    for kc in range(8):
                        first = half == 0 and kc == 0
                        P.op("pe", I(nc.tensor.matmul, acc[:, half * 512:(half + 1) * 512],
                                     lhsT=mT[:, kc * 128:(kc + 1) * 128], rhs=wout[:, kc, half * 512:(half + 1) * 512],
                                     start=(kc == 0), stop=(kc == 7)), reads=[RmT, Rw],
                             writes=[Racc] if first else (), pwrites=() if first else [Racc])
                P.op("dve", I(nc.vector.tensor_tensor, out=x1[s_][:], in0=acc, in1=xt[s_][:], op=ALU.add),
                     reads=[Racc, Rxt[s_]], writes=[Rx1[s_]])
                P.dma("sp", I(nc.sync.dma_start, out=out_d[t0:t0 + 128, :], in_=x1[s_][:]), Rx1[s_], reads=[Rx1[s_]],
                      pwrites=[Rout])
                P.op("act", I(nc.scalar.activation, out=sq[:], in_=x1[s_][:], func=AF.Square, accum_out=sm[:, 0:1]),
                     reads=[Rx1[s_]], writes=[Rsq, Rsm])
                P.op("dve", I(nc.vector.tensor_scalar, out=sm[:, 1:2], in0=sm[:, 0:1], scalar1=1.0 / D, scalar2=EPS,
                              op0=ALU.mult, op1=ALU.add), reads=[Rsm], pwrites=[Rsm])
                P.op("pool", I(nc.gpsimd.tensor_tensor, out=sm[:, 2:3], in0=sm[:, 1:2], in1=mhalf[:, 0:1], op=ALU.pow),
                     reads=[Rsm, Rmh], pwrites=[Rsm])
                P.op("dve", I(nc.vector.scalar_tensor_tensor, out=h2f[:], in0=x1[s_][:], scalar=sm[:, 2:3], in1=wn2[:],
                              op0=ALU.mult, op1=ALU.mult), reads=[Rx1[s_], Rsm, Rw], writes=[Rh2f])
                P.op("act", I(nc.scalar.copy, out=h2b[s_][:], in_=h2f[:]), reads=[Rh2f], writes=[Rh2b[s_]])
                P.dma("sp", I(nc.sync.dma_start, out=H2[t0:t0 + 128, :], in_=h2b[s_][:]), Rh2b[s_], reads=[Rh2b[s_]],
                      pwrites=[RH2])
                for kc in range(8):
                    P.op("pe", I(nc.tensor.transpose, out=Tf[:, kc * 128:(kc + 1) * 128],
                                 in_=h2f[:, kc * 128:(kc + 1) * 128], identity=identf[:]),
                         reads=[Rh2f, Rid], writes=[RTf] if kc == 0 else (), pwrites=[RTf] if kc else ())
                P.op("dve", I(nc.vector.tensor_copy, out=h2T[:], in_=Tf), reads=[RTf], writes=[Rh2T])
                for kc in range(8):
                    P.op("pe", I(nc.tensor.matmul, lg, lhsT=h2T[:, kc * 128:(kc + 1) * 128], rhs=rw[:, kc, :],
                                 start=(kc == 0), stop=(kc == 7)), reads=[Rh2T, Rw],
                         writes=[Rlg] if kc == 0 else (), pwrites=[Rlg] if kc else ())
                P.op("dve", I(nc.vector.tensor_reduce, out=sm[:, 8:9], in_=lg, axis=AX.X, op=ALU.max), reads=[Rlg],
                     pwrites=[Rsm])
                P.op("dve", I(nc.vector.tensor_scalar, out=sm[:, 9:10], in0=sm[:, 8:9], scalar1=-1.0, scalar2=None,
                              op0=ALU.mult), reads=[Rsm], pwrites=[Rsm])
                P.op("act", I(nc.scalar.activation, out=sm[:, 16:32], in_=lg, func=AF.Exp, bias=sm[:, 9:10],
                              accum_out=sm[:, 10:11]), reads=[Rlg, Rsm], pwrites=[Rsm])
                P.op("dve", I(nc.vector.reciprocal, out=sm[:, 11:12], in_=sm[:, 10:11]), reads=[Rsm], pwrites=[Rsm])
                P.op("dve", I(nc.vector.tensor_scalar, out=aff[:, i, :], in0=sm[:, 16:32], scalar1=sm[:, 11:12],
                              scalar2=None, op0=ALU.mult), reads=[Rsm], pwrites=[Raff])
            P.barrier()
            P.emit()
        if upto < "E":
            return
        with ExitStack() as es2:
            affT = es2.enter_context(sb("e_affT", [NE, T], F32))
            mk = es2.enter_context(sb("e_mk", [NE, T], F32))
            cs = es2.enter_context(sb("e_cs", [NE, T], F32))
            on = es2.enter_context(sb("e_on", [NE, T], F32))
            bs = es2.enter_context(sb("e_bs", [NE, 8], F32))
            ps = es2.enter_context(nc.psum_tensor("e_ps", [128, 4 * 512], F32))
            RaT, Rmk, Rcs, Ron, Rbs, Rps = [R(n) for n in "eaT emk ecs eon ebs eps".split()]
            P.op("pool", I(nc.gpsimd.memset, on[:], 1.0), writes=[Ron])
            P.op("pool", I(nc.gpsimd.memset, bs[:], 0.0), writes=[Rbs])
            for half in range(2):
                for j in range(16):
                    i = half * 16 + j
                    P.op("pe", I(nc.tensor.transpose, out=ps[0:NE, j * 128:(j + 1) * 128], in_=aff[:, i, :],
                                 identity=identf[:]), reads=[Raff, Rid], writes=[Rps] if j == 0 else (),
                         pwrites=[Rps] if j else ())
                P.op("act", I(nc.scalar.copy, out=affT[:, half * 2048:(half + 1) * 2048], in_=ps[0:NE, 0:2048]),
                     reads=[Rps], pwrites=[RaT])
            lo, mid, cntc, gw = bs[:, 0:1], bs[:, 1:2], bs[:, 2:3], bs[:, 3:4]
            for it in range(28):
                w = 2.0 ** (-(it + 1))
                P.op("dve", I(nc.vector.tensor_scalar, out=mid, in0=lo, scalar1=w, scalar2=None, op0=ALU.add),
                     reads=[Rbs], pwrites=[Rbs])
                P.op("dve", I(nc.vector.tensor_scalar, out=mk[:], in0=affT[:], scalar1=mid, scalar2=0.0, op0=ALU.is_gt,
                              op1=ALU.add, accum_out=cntc), reads=[RaT, Rbs], writes=[Rmk], pwrites=[Rbs])
                P.op("dve", I(nc.vector.tensor_scalar, out=gw, in0=cntc, scalar1=CAP - 0.5, scalar2=w, op0=ALU.is_gt,
                              op1=ALU.mult), reads=[Rbs], pwrites=[Rbs])
                P.op("dve", I(nc.vector.tensor_tensor, out=lo, in0=lo, in1=gw, op=ALU.add), reads=[Rbs],
                     pwrites=[Rbs])
            P.op("dve", I(nc.vector.tensor_scalar, out=mk[:], in0=affT[:], scalar1=lo, scalar2=None, op0=ALU.is_gt),
                 reads=[RaT, Rbs], writes=[Rmk])
            P.op("dve", I(nc.vector.tensor_tensor_scan, out=cs[:], data0=on[:], data1=mk[:], initial=0.0, op0=ALU.mult,
                          op1=ALU.add), reads=[Ron, Rmk], writes=[Rcs])
            P.op("dve", I(nc.vector.tensor_tensor, out=cs[:], in0=cs[:], in1=mk[:], op=ALU.mult), reads=[Rcs, Rmk],
                 writes=[Rcs])
            for i in range(NT):
                P.op("pe", I(nc.tensor.transpose, out=ps[:, i * NE:(i + 1) * NE], in_=cs[:, i * 128:(i + 1) * 128],
                             identity=identf[0:NE, 0:NE]), reads=[Rcs, Rid], writes=[Rps] if i == 0 else (),
                     pwrites=[Rps] if i else ())
            P.op("act", I(nc.scalar.copy, out=posm[:].rearrange("p a b -> p (a b)"), in_=ps[:, 0:NT * NE]),
                 reads=[Rps], writes=[Rposm])
            P.barrier()
            P.emit()
        with ExitStack() as es2:
            wsl.append(es2.enter_context(sb("e_w3", [128, 16384], BF16)))
            iota = es2.enter_context(sb("e_iota", [128, CAP], F32))
            tokpi = es2.enter_context(sb("e_tokpi", [128, NT, 4], BF16))
            sel = [es2.enter_context(sb("e_sel%d" % k, [128, CAP], BF16)) for k in range(3)]
            Rsel = [R("esel%d" % k) for k in range(3)]
            idxrow = es2.enter_context(sb("e_idxrow", [4, CAP], F32))
            ic = es2.enter_context(sb("e_ic", [128, 4, 4], F32))
            idf = es2.enter_context(sb("e_idf", [128, 4], F32))
            idx = [[es2.enter_context(sb("e_idx%d%d" % (k, c), [128, 1], I32)) for c in range(4)] for k in range(2)]
            gate = [es2.enter_context(sb("e_gate%d" % k, [128, 4], F32)) for k in range(2)]
            xe = [es2.enter_context(sb("e_xe%d" % k, [128, 1024], BF16)) for k in range(4)]
            Rxe = [R("exe%d" % k) for k in range(4)]
            xeT = es2.enter_context(sb("e_xeT", [128, 8, CAP], BF16))
            hT = es2.enter_context(sb("e_hT", [128, 16, CAP], BF16))
            sg = [es2.enter_context(sb("e_sg%d" % k, [128, CAP], F32)) for k in range(2)]
            Rsg = [R("esg0"), R("esg1")]
            ye = [es2.enter_context(sb("e_ye%d" % k, [128, 1024], F32)) for k in range(2)]
            Rye = [R("eye0"), R("eye1")]
            psF = es2.enter_context(nc.psum_tensor("e_psF", [128, 7 * 512], F32))
            psB = es2.enter_context(nc.psum_tensor("e_psB", [128, 1024], BF16))
            gub = [(psF[:, 0:512], psF[:, 512:1024]), (psF[:, 1024:1536], psF[:, 1536:2048])]
            Rgu = [R("egu0"), R("egu1")]
            dbk = [psF[:, 2048:2560], psF[:, 2560:3072]]
            Rdb = [R("edb0"), R("edb1")]
            ipb = psF[:, 3072:3584]
            Ripb, RTb = R("eipb"), R("eTb")
            Rtok, Ridr, Ric, Ridx, RxeT, RhT, Rsc, Rc = [R(n) for n in "etok eidr eic eidx exeT ehT esc ec".split()]
            Ridxs = [R("eidx0"), R("eidx1")]
            P.dma("sp", I(nc.sync.dma_start, out=iota[:], in_=iota_d.partition_broadcast(128)), Rc, pwrites=[Rc])
            P.dma("pool", I(nc.gpsimd.dma_start, out=tokpi[:, :, 0:2], in_=tokpi_d[:, :, :]), Rc, pwrites=[Rtok])

            def route_tok(e):
                P.op("dve", I(nc.vector.tensor_copy, out=tokpi[:, :, 2], in_=aff[:, :, e]), reads=[Raff],
                     writes=[Rtok])
                P.op("dve", I(nc.vector.tensor_tensor, out=tokpi[:, :, 3], in0=aff[:, :, e], in1=tokpi[:, :, 2],
                              op=ALU.subtract), reads=[Raff, Rtok], pwrites=[Rtok])

            def route_sel(e, i):
                r3 = (e * NT + i) % 3
                P.op("dve", I(nc.vector.tensor_scalar, out=sel[r3][:], in0=iota[:], scalar1=posm[:, i, e:e + 1],
                              scalar2=None, op0=ALU.is_equal), reads=[Rc, Rposm], writes=[Rsel[r3]])
                P.op("pe", I(nc.tensor.matmul, ipb[0:4, :], lhsT=tokpi[:, i, :], rhs=sel[r3][:], start=(i == 0),
                             stop=(i == NT - 1)), reads=[Rtok, Rsel[r3]], writes=[Ripb] if i == 0 else (),
                     pwrites=[Ripb] if i else ())

            def route_idx(e):
                es_ = e % 2
                P.op("act", I(nc.scalar.copy, out=idxrow[:], in_=ipb[0:4, :]), reads=[Ripb], writes=[Ridr])
                for cc in range(4):
                    P.op("pe", I(nc.tensor.transpose, out=ipb[:, cc * 4:(cc + 1) * 4],
                                 in_=idxrow[0:4, cc * 128:(cc + 1) * 128], identity=identf[0:4, 0:4]),
                         reads=[Ridr, Rid], writes=[Ripb] if cc == 0 else (), pwrites=[Ripb] if cc else ())
                P.op("dve", I(nc.vector.tensor_copy, out=ic[:].rearrange("p a b -> p (a b)"), in_=ipb[:, 0:16]),
                     reads=[Ripb], writes=[Ric])
                P.op("dve", I(nc.vector.scalar_tensor_tensor, out=idf[:], in0=ic[:, :, 1], scalar=128.0,
                              in1=ic[:, :, 0], op0=ALU.mult, op1=ALU.add), reads=[Ric], writes=[Ridx])
                P.op("dve", I(nc.vector.tensor_tensor, out=gate[es_][:], in0=ic[:, :, 2], in1=ic[:, :, 3], op=ALU.add),
                     reads=[Ric], writes=[Ridxs[es_]])
                for cc in range(4):
                    P.op("dve", I(nc.vector.tensor_copy, out=idx[es_][cc][:], in_=idf[:, cc:cc + 1]), reads=[Ridx],
                         pwrites=[Ridxs[es_]])
                for cc in range(4):
                    P.dma("pool", I(nc.gpsimd.indirect_dma_start, out=xe[cc][:], out_offset=None, in_=H2[:, :],
                                    in_offset=bass.IndirectOffsetOnAxis(ap=idx[es_][cc][:, :], axis=0)), Rxe[cc],
                          reads=[Ridxs[es_]], writes=[Rxe[cc]])

            def route_T(e):
                for kc in range(8):
                    for cc in range(4):
                        P.op("pe", I(nc.tensor.transpose, out=psB[:, cc * 128:(cc + 1) * 128],
                                     in_=xe[cc][:, kc * 128:(kc + 1) * 128], identity=identb[:]),
                             reads=[Rxe[cc], Rid], writes=[RTb] if cc == 0 else (), pwrites=[RTb] if cc else ())
                    if kc % 2 == 0:
                        P.op("act", I(nc.scalar.copy, out=xeT[:, kc, :], in_=psB[:, 0:512]), reads=[RTb],
                             writes=[RxeT] if kc == 0 else (), pwrites=[RxeT] if kc else ())
                    else:
                        P.op("dve", I(nc.vector.tensor_copy, out=xeT[:, kc, :], in_=psB[:, 0:512]), reads=[RTb],
                             pwrites=[RxeT])

            route_tok(0)
            for i in range(NT):
                route_sel(0, i)
            route_idx(0)
            route_T(0)
            gstep = 0
            dstep = 0
            for e in range(NE):
                es_ = e % 2
                nxt = e + 1 < NE
                wgt = wsl[(3 * e) % 4][:].rearrange("p (kc f) -> p kc f", kc=8)
                wut = wsl[(3 * e + 1) % 4][:].rearrange("p (kc f) -> p kc f", kc=8)
                wdt = wsl[(3 * e + 2) % 4][:].rearrange("p (j c) -> p j c", j=16)
                Rwg, Rwu, Rwd = Rws[(3 * e) % 4], Rws[(3 * e + 1) % 4], Rws[(3 * e + 2) % 4]
                if nxt:
                    wload(e + 1, (0,))
                    route_tok(e + 1)
                for j in range(16):
                    gs = gstep % 2
                    gstep += 1
                    gb_, ub_ = gub[gs]
                    for kc in range(8):
                        P.op("pe", I(nc.tensor.matmul, gb_, lhsT=wgt[:, kc, j * 128:(j + 1) * 128], rhs=xeT[:, kc, :],
                                     start=(kc == 0), stop=(kc == 7)), reads=[Rwg, RxeT],
                             writes=[Rgu[gs]] if kc == 0 else (), pwrites=[Rgu[gs]] if kc else ())
                    for kc in range(8):
                        P.op("pe", I(nc.tensor.matmul, ub_, lhsT=wut[:, kc, j * 128:(j + 1) * 128], rhs=xeT[:, kc, :],
                                     start=(kc == 0), stop=(kc == 7)), reads=[Rwu, RxeT], pwrites=[Rgu[gs]])
                    if nxt:
                        route_sel(e + 1, 2 * j)
                        route_sel(e + 1, 2 * j + 1)
                    P.op("act", I(nc.scalar.activation, out=sg[gs][:], in_=gb_, func=AF.Tanh, scale=0.5),
                         reads=[Rgu[gs]], writes=[Rsg[gs]])
                    P.op("dve", I(nc.vector.scalar_tensor_tensor, out=sg[gs][:], in0=sg[gs][:], scalar=1.0, in1=gb_,
                                  op0=ALU.add, op1=ALU.mult), reads=[Rsg[gs], Rgu[gs]], writes=[Rsg[gs]])
                    P.op("dve", I(nc.vector.scalar_tensor_tensor, out=hT[:, j, :], in0=sg[gs][:], scalar=0.5, in1=ub_,
                                  op0=ALU.mult, op1=ALU.mult), reads=[Rsg[gs], Rgu[gs]],
                         writes=[RhT] if j == 0 else (), pwrites=[RhT] if j else ())
                if nxt:
                    route_idx(e + 1)
                    wload(e + 1, (1,))
                for cc in range(4):
                    ys = cc % 2
                    for half in range(2):
                        ds = dstep % 2
                        dstep += 1
                        for j in range(16):
                            P.op("pe", I(nc.tensor.matmul, dbk[ds], lhsT=hT[:, j, cc * 128:(cc + 1) * 128],
                                         rhs=wdt[:, j, half * 512:(half + 1) * 512], start=(j == 0), stop=(j == 15)),
                                 reads=[RhT, Rwd], writes=[Rdb[ds]] if j == 0 else (), pwrites=[Rdb[ds]] if j else ())
                        if half == 0:
                            P.op("dve", I(nc.vector.tensor_scalar, out=ye[ys][:, 0:512], in0=dbk[ds],
                                          scalar1=gate[es_][:, cc:cc + 1], scalar2=None, op0=ALU.mult),
                                 reads=[Rdb[ds], Ridxs[es_]], writes=[Rye[ys]])
                        else:
                            P.op("dve", I(nc.vector.tensor_scalar, out=ye[ys][:, 512:1024], in0=dbk[ds],
                                          scalar1=gate[es_][:, cc:cc + 1], scalar2=None, op0=ALU.mult),
                                 reads=[Rdb[ds], Ridxs[es_]], pwrites=[Rye[ys]])
                    P.dma("pool", I(nc.gpsimd.indirect_dma_start, out=out_d[:, :],
                                    out_offset=bass.IndirectOffsetOnAxis(ap=idx[es_][cc][:, :], axis=0),
                                    in_=ye[ys][:], in_offset=None, compute_op=ALU.add), Rye[ys],
                          reads=[Rye[ys], Ridxs[es_], Rsc], pwrites=[Rsc])
                if nxt:
                    wload(e + 1, (2,))
                    route_T(e + 1)
            P.barrier()
            P.emit()


IN_COLS = (256, 256, 32, 1024, 1024, 1024, 1024, 1024)


def _win_perm():
    off = np.cumsum((0,) + IN_COLS)
    seg = [np.arange(off[k], off[k + 1]) for k in range(8)]
    return np.concatenate([seg[0], seg[1], seg[3], seg[4], seg[5], seg[6], seg[7], seg[2]])


def make_in_maps(inputs, cores):
    f = lambda a: np.ascontiguousarray(np.asarray(a))
    perm = _win_perm()
    bg = f(inputs["b_gate"])[0]
    shared = {
        "w_in": f(f(inputs["w_in"])[0][:, perm]),
        "b_gate": bg,
        "w_uq": f(inputs["mla_w_uq"])[0],
        "w_ukv": f(inputs["mla_w_ukv"])[0],
        "w_out": f(inputs["w_out"])[0],
        "router_w": f(inputs["router_w"])[0],
        "w_gate": f(inputs["expert_w_gate"])[0],
        "w_up": f(inputs["expert_w_up"])[0],
        "w_down": f(inputs["expert_w_down"])[0],
        "attn_norm_w": f(inputs["attn_norm_w"])[0],
        "q_norm_w": f(inputs["mla_q_norm_w"])[0],
        "kv_norm_w": f(inputs["mla_kv_norm_w"])[0],
        "q_hn": f(inputs["mla_q_hnorm_w"])[0],
        "k_hn": f(inputs["mla_k_hnorm_w"])[0],
        "dq_hn": f(inputs["diff_q_hnorm_w"])[0],
        "dk_hn": f(inputs["diff_k_hnorm_w"])[0],
        "lam": f(inputs["diff_lambda"])[0].reshape(-1),
        "subln": f(inputs["diff_subln_w"])[0],
        "ffn_norm_w": f(inputs["ffn_norm_w"])[0],
        "ident": np.eye(128, dtype=np.float32),
        "invf": (1.0 / (10000.0 ** (np.arange(0, 32, 2, dtype=np.float32) / 32.0))).astype(np.float32),
        "iota512": np.arange(1, 513, dtype=np.float32),
        "tokpi": np.ascontiguousarray(np.stack([np.broadcast_to(np.arange(128, dtype=np.float32)[:, None], (128, NT)),
                                                np.broadcast_to(np.arange(NT, dtype=np.float32)[None, :], (128, NT))],
                                               axis=-1)),
        "slopes": np.array([2.0 ** (-(i + 1)) for i in range(8)], dtype=np.float32),
    }
    x = f(inputs["x"])
    pos = f(inputs["positions"]).astype(np.int32)
    return [dict(shared, x=x[c], pos=pos[c], pos_t=np.ascontiguousarray(pos[c].reshape(NT, 128).T)) for c in cores]


_NC = {}


def classify(pos):
    pos = np.asarray(pos).astype(np.int64)
    n = pos.shape[0]
    q = pos.reshape(n, 16, 256)
    k = pos.reshape(n, NT, 128)
    qmin, qmax = q.min(-1)[:, :, None], q.max(-1)[:, :, None]
    kmin, kmax = k.min(-1)[:, None, :], k.max(-1)[:, None, :]
    below = (kmax <= qmin).all(0)
    above = (kmin >= qmax).all(0)
    cls = np.where(below, 1, np.where(above, 2, 0))
    dmin = np.maximum(np.maximum(qmin - kmax, kmin - qmax), 0).min(0).astype(np.float64)
    return cls, dmin


def kernel(**inputs):
    cls, dmin = classify(np.asarray(inputs["positions"]))
    gq = float(np.abs(np.asarray(inputs["diff_q_hnorm_w"])).max())
    gk = float(np.abs(np.asarray(inputs["diff_k_hnorm_w"])).max())
    skip_t = max(48.0, 2.0 * 8.0 * gq * gk + 25.0)
    key = (cls.tobytes(), dmin.tobytes(), skip_t)
    if key not in _NC:
        _NC[key] = build(CLS=cls, DMIN=dmin, skip_t=skip_t)
    nc = _NC[key]
    in_maps = make_in_maps(inputs, list(range(8)))
    res = run_bass_kernel_spmd(nc, in_maps, core_ids=list(range(8)))
    return np.stack([r["out"] for r in res.results], axis=0).astype(np.float32)
```

```python
import math
from functools import partial as I
from contextlib import ExitStack
import numpy as np
import concourse.bass as bass
import concourse.mybir as mybir
from concourse.bass_utils import run_bass_kernel_spmd

F32 = mybir.dt.float32
BF16 = mybir.dt.bfloat16
I32 = mybir.dt.int32
AF = mybir.ActivationFunctionType
ALU = mybir.AluOpType
AX = mybir.AxisListType

T = 4096
D = 1024
NT = T // 128
EPS = 1e-6
LAM_INIT = 0.8 - 0.6 * math.exp(-0.3 * 0)
NE = 16
CAP = 512
FF = 2048
TWO_PI = 2.0 * math.pi


class R:
    __slots__ = ("name", "w", "r", "pr", "dsem", "dcnt")

    def __init__(self, name):
        self.name = name
        self.w = {}
        self.r = {}
        self.pr = {}
        self.dsem = None
        self.dcnt = 0


class Prog:
    ENG = ("pe", "act", "dve", "pool", "sp")

    def __init__(self, nc):
        self.nc = nc
        self.streams = {e: [] for e in self.ENG}
        self.nops = {e: 0 for e in self.ENG}
        self.known = {e: {} for e in self.ENG}
        self.signal = {e: set() for e in self.ENG}
        self.sigcount = {e: 0 for e in self.ENG}
        self.cnt = {e: {} for e in self.ENG}
        self.sems = {}
        self._semctx = []
        self.dstreams = []
        self.tag = ""
        for e in self.ENG:
            self.sems[("E", e)] = self._new_sem("sem_" + e)

    def _new_sem(self, name):
        ctx = self.nc.semaphore(name)
        s = ctx.__enter__()
        self._semctx.append(ctx)
        return s

    def close(self):
        for ctx in reversed(self._semctx):
            ctx.__exit__(None, None, None)

    def _wait(self, eng, key, val):
        k = self.known[eng]
        if k.get(key, -1) >= val:
            return
        k[key] = val
        if key[0] == "E":
            self.signal[key[1]].add(val)
        self.streams[eng].append(("wait", key, val))

    def _deps(self, eng, reads, writes, pwrites):
        me = ("E", eng)
        for res in reads:
            for key, val in res.w.items():
                if key == me and eng == "pe":
                    continue
                self._wait(eng, key, val)
        for res in writes:
            for key, val in list(res.w.items()) + list(res.r.items()):
                if key == me and eng == "pe":
                    continue
                self._wait(eng, key, val)
        for res in pwrites:
            for key, val in list(res.r.items()) + list(res.pr.items()):
                if key == me and eng == "pe":
                    continue
                self._wait(eng, key, val)

    def _mark(self, key, val, reads, writes, pwrites):
        for res in reads:
            if res.r.get(key, -1) < val:
                res.r[key] = val
        for res in writes:
            pr = dict(res.w)
            for k_, v_ in res.r.items():
                if pr.get(k_, -1) < v_:
                    pr[k_] = v_
            res.pr = pr
            res.w = {key: val}
            res.r = {}
        for res in pwrites:
            if res.w.get(key, -1) < val:
                res.w[key] = val

    def op(self, eng, fn, reads=(), writes=(), pwrites=()):
        self._deps(eng, reads, writes, pwrites)
        idx = self.nops[eng]
        self.nops[eng] += 1
        self.streams[eng].append(("op", fn, idx, self.tag))
        self._mark(("E", eng), idx, reads, writes, pwrites)

    def dma(self, eng, fn, stream, reads=(), writes=(), pwrites=()):
        self._deps(eng, reads, writes, pwrites)
        if stream.dsem is None:
            stream.dsem = {}
            stream.dcnt = {}
        key = ("D", id(stream), eng)
        if eng not in stream.dsem:
            stream.dsem[eng] = self._new_sem("d_%s_%s" % (stream.name, eng))
            stream.dcnt[eng] = 0
            self.sems[key] = stream.dsem[eng]
            self.dstreams.append((stream, eng))
        stream.dcnt[eng] += 1
        val = 16 * stream.dcnt[eng]
        self.streams[eng].append(("dma", fn, stream.dsem[eng]))
        self._mark(key, val, reads, writes, pwrites)

    def barrier(self):
        for e in self.ENG:
            for f in self.ENG:
                if f != e and f != "sp" and self.nops[f] > 0:
                    self._wait(e, ("E", f), self.nops[f] - 1)
            for s, q in self.dstreams:
                self._wait(e, ("D", id(s), q), 16 * s.dcnt[q])

    def emit(self):
        nc = self.nc
        for e in self.ENG:
            for idx in sorted(self.signal[e]):
                if idx not in self.cnt[e]:
                    self.sigcount[e] += 1
                    self.cnt[e][idx] = self.sigcount[e]
            self.signal[e] = set()
        streams = self.streams
        self.streams = {e: [] for e in self.ENG}

        def make(e):
            stream = streams[e]

            def body(engine):
                for ent in stream:
                    if ent[0] == "wait":
                        _, key, val = ent
                        v = self.cnt[key[1]][val] if key[0] == "E" else val
                        engine.wait_ge(self.sems[key], v)
                    elif ent[0] == "op":
                        _, fn, idx, tag = ent
                        ins = fn()
                        if tag:
                            ins.annotate(tag)
                        if idx in self.cnt[e]:
                            ins.then_inc(self.sems[("E", e)], 1)
                    else:
                        _, fn, sem = ent
                        try:
                            ins = fn()
                        except Exception:
                            print("DMA build failed:", fn.func.__name__, {k: str(v)[:200] for k, v in fn.keywords.items()})
                            raise
                        ins.then_inc(sem, 16)
            return body

        with nc.Block() as block:
            block.tensor(make("pe"))
            block.scalar(make("act"))
            block.vector(make("dve"))
            block.gpsimd(make("pool"))
            block.sync(make("sp"))


SKIP_T = 48.0


def build(dbg=False, upto="E", CLS=None, DMIN=None, skip_t=None):
    global SKIP_T
    if skip_t is not None:
        SKIP_T = float(skip_t)
    if CLS is None:
        CLS = np.zeros((16, NT), np.int64)
        DMIN = np.zeros((16, NT), np.float64)
    nc = bass.Bass("TRN2", target_bir_lowering=False)

    def din(name, shape, dt=F32):
        return nc.dram_tensor(name, list(shape), dt, kind="ExternalInput").ap()

    def dscr(name, shape, dt=BF16):
        return nc.dram_tensor(name, list(shape), dt, kind="ExternalOutput" if dbg else "Internal").ap()

    x_d = din("x", [T, D])
    pos_d = din("pos", [T], I32)
    post_d = din("pos_t", [128, NT], I32)
    win_d = din("w_in", [D, 5664])
    bg_d = din("b_gate", [2048])
    wuq_d = din("w_uq", [256, 1536])
    wukv_d = din("w_ukv", [256, 2048])
    wout_d = din("w_out", [D, D])
    rw_d = din("router_w", [D, NE])
    wg_d = din("w_gate", [NE, D, FF])
    wu_d = din("w_up", [NE, D, FF])
    wd_d = din("w_down", [NE, FF, D])
    vec_d = {n: din(n, [k]) for n, k in [("attn_norm_w", 1024), ("q_norm_w", 256), ("kv_norm_w", 256),
                                          ("q_hn", 96), ("k_hn", 96), ("dq_hn", 64), ("dk_hn", 64),
                                          ("lam", 256), ("subln", 128), ("ffn_norm_w", 1024)]}
    ident_d = din("ident", [128, 128])
    invf_d = din("invf", [16])
    iota_d = din("iota512", [512])
    slopes_d = din("slopes", [8])
    tokpi_d = din("tokpi", [128, NT, 2])
    out_d = nc.dram_tensor("out", [T, D], F32, kind="ExternalOutput").ap()

    QmT = dscr("QmT", [16, 96, T])
    KmT = dscr("KmT", [16, 96, T])
    Vm = dscr("Vm", [T, 1024])
    QdT = dscr("QdT", [8, 128, T])
    KdT = dscr("KdT", [8, 128, T])
    Vd = dscr("Vd", [T, 1024])
    G = dscr("G", [T, 2048])
    H2 = dscr("H2", [T, D])
    DBG = dbg
    dbg_idx = nc.dram_tensor("dbg_idx", [4, 128, 1], I32, kind="ExternalOutput").ap() if dbg else None
    dbg_gate = nc.dram_tensor("dbg_gate", [128, 4], F32, kind="ExternalOutput").ap() if dbg else None
    dbg_xe = nc.dram_tensor("dbg_xe", [128, 1024], BF16, kind="ExternalOutput").ap() if dbg else None
    dbg_ye = nc.dram_tensor("dbg_ye", [128, 1024], F32, kind="ExternalOutput").ap() if dbg else None
    dbg_posm = nc.dram_tensor("dbg_posm", [128, NT * NE], F32, kind="ExternalOutput").ap() if dbg else None
    dbg_aff = nc.dram_tensor("dbg_aff", [128, NT * NE], F32, kind="ExternalOutput").ap() if dbg else None
    dbg_hT = nc.dram_tensor("dbg_hT", [128, 16 * CAP], BF16, kind="ExternalOutput").ap() if dbg else None
    PQHL = dscr("PQHL", [8, 2, T])
    MG = dscr("MG", [T, D])
    RQmT, RKmT, RVm, RQdT, RKdT, RVd, RG, RH2, RMG, Rout = [R(n) for n in
                                                          "QmT KmT Vm QdT KdT Vd G H2 MG out".split()]

    P = Prog(nc)
    sb = nc.sbuf_tensor
    with ExitStack() as es1:
        identb = es1.enter_context(sb("identb", [128, 128], BF16))
        identf = es1.enter_context(sb("identf", [128, 128], F32))
        mhalf = es1.enter_context(sb("mhalf", [128, 64], F32))
        cosT = es1.enter_context(sb("cosT", [128, NT, 16], F32))
        sinT = es1.enter_context(sb("sinT", [128, NT, 16], F32))
        posf = es1.enter_context(sb("posf", [128, NT], F32))
        nlam = es1.enter_context(sb("nlam", [128, 2], F32))
        vq_hn = es1.enter_context(sb("vq_hn", [128, 96], F32))
        vk_hn = es1.enter_context(sb("vk_hn", [128, 96], F32))
        vdq_hn = es1.enter_context(sb("vdq_hn", [128, 64], F32))
        vdk_hn = es1.enter_context(sb("vdk_hn", [128, 64], F32))
        vsubln = es1.enter_context(sb("vsubln", [128, 128], F32))
        vqn = es1.enter_context(sb("vqn", [128, 256], F32))
        vkvn = es1.enter_context(sb("vkvn", [128, 256], F32))
        Rid, Rmh, Rcs, Rposf, Rnlam, Rvec = [R(n) for n in "id mh cs posf nlam vec".split()]

        with ExitStack() as es2:
            posi = es2.enter_context(sb("p0_posi", [128, NT], I32))
            invf = es2.enter_context(sb("p0_invf", [128, 16], F32))
            ang = es2.enter_context(sb("p0_ang", [128, NT, 16], F32))
            kf = es2.enter_context(sb("p0_kf", [128, NT, 16], F32))
            ki = es2.enter_context(sb("p0_ki", [128, NT, 16], I32))
            mm_ = es2.enter_context(sb("p0_m", [128, NT, 16], F32))
            lamt = es2.enter_context(sb("p0_lam", [128, 256], F32))
            lp = es2.enter_context(sb("p0_lp", [128, 128], F32))
            ls = es2.enter_context(sb("p0_ls", [128, 4], F32))
            Rt = R("p0tmp")
            P.dma("sp", I(nc.sync.dma_start, out=identf[:], in_=ident_d[:, :]), Rid, pwrites=[Rid])
            P.dma("pool", I(nc.gpsimd.dma_start, out=identb[:], in_=ident_d[:, :]), Rid, pwrites=[Rid])
            P.op("pool", I(nc.gpsimd.memset, mhalf[:], -0.5), writes=[Rmh])
            for tl, nm in [(vq_hn, "q_hn"), (vk_hn, "k_hn"), (vdq_hn, "dq_hn"), (vdk_hn, "dk_hn"),
                           (vsubln, "subln"), (vqn, "q_norm_w"), (vkvn, "kv_norm_w"), (lamt, "lam")]:
                P.dma("sp", I(nc.sync.dma_start, out=tl[:], in_=vec_d[nm].partition_broadcast(128)), Rvec,
                      pwrites=[Rvec])
            P.dma("sp", I(nc.sync.dma_start, out=invf[:], in_=invf_d.partition_broadcast(128)), Rvec, pwrites=[Rvec])
            P.dma("sp", I(nc.sync.dma_start, out=posi[:], in_=post_d[:, :]), Rvec,
                  pwrites=[Rvec])
            P.op("dve", I(nc.vector.tensor_scalar, out=vq_hn[:], in0=vq_hn[:], scalar1=96.0 ** -0.5, scalar2=None,
                          op0=ALU.mult), reads=[Rvec], pwrites=[Rvec])
            P.op("dve", I(nc.vector.tensor_scalar, out=vdq_hn[:], in0=vdq_hn[:], scalar1=64.0 ** -0.5, scalar2=None,
                          op0=ALU.mult), reads=[Rvec], pwrites=[Rvec])
            P.op("dve", I(nc.vector.tensor_scalar, out=vsubln[:], in0=vsubln[:], scalar1=1.0 - LAM_INIT, scalar2=None,
                          op0=ALU.mult), reads=[Rvec], pwrites=[Rvec])
            P.op("dve", I(nc.vector.tensor_tensor, out=lp[:].rearrange("p (a b) -> p a b", a=2),
                          in0=lamt[:].rearrange("p (a two b) -> p a two b", a=2, two=2)[:, :, 0, :],
                          in1=lamt[:].rearrange("p (a two b) -> p a two b", a=2, two=2)[:, :, 1, :], op=ALU.mult),
                 reads=[Rvec], writes=[Rt])
            P.op("dve", I(nc.vector.tensor_reduce, out=ls[:, 0:2], in_=lp[:].rearrange("p (a b) -> p a b", a=2),
                          axis=AX.X, op=ALU.add), reads=[Rt], writes=[Rnlam])
            P.op("act", I(nc.scalar.activation, out=ls[:, 2:4], in_=ls[:, 0:2], func=AF.Exp), reads=[Rnlam],
                 writes=[Rt])
            P.op("dve", I(nc.vector.tensor_tensor, out=nlam[:, 0:1], in0=ls[:, 3:4], in1=ls[:, 2:3], op=ALU.subtract),
                 reads=[Rt], writes=[Rnlam])
            P.op("dve", I(nc.vector.tensor_scalar, out=nlam[:, 0:1], in0=nlam[:, 0:1], scalar1=-LAM_INIT, scalar2=None,
                          op0=ALU.add), reads=[Rnlam], writes=[Rnlam])
            P.op("dve", I(nc.vector.tensor_copy, out=posf[:], in_=posi[:]), reads=[Rvec], writes=[Rposf])
            P.op("dve", I(nc.vector.tensor_tensor, out=ang[:], in0=posf[:].unsqueeze(2).to_broadcast([128, NT, 16]),
                          in1=invf[:].unsqueeze(1).to_broadcast([128, NT, 16]), op=ALU.mult),
                 reads=[Rposf, Rvec], writes=[Rt])

            Rk = R("rk")

            def reduce_sin(dst, shift):
                P.op("dve", I(nc.vector.tensor_scalar, out=kf[:], in0=ang[:], scalar1=1.0 / TWO_PI,
                              scalar2=0.5 + shift / TWO_PI, op0=ALU.mult, op1=ALU.add), reads=[Rt], writes=[Rk])
                P.op("dve", I(nc.vector.tensor_copy, out=ki[:], in_=kf[:]), reads=[Rk], writes=[Rk])
                P.op("dve", I(nc.vector.tensor_copy, out=kf[:], in_=ki[:]), reads=[Rk], writes=[Rk])
                c1 = 6.28125
                c2 = TWO_PI - c1
                P.op("dve", I(nc.vector.scalar_tensor_tensor, out=mm_[:], in0=kf[:], scalar=-c1, in1=ang[:],
                              op0=ALU.mult, op1=ALU.add), reads=[Rk, Rt], writes=[Rk])
                P.op("dve", I(nc.vector.scalar_tensor_tensor, out=mm_[:], in0=kf[:], scalar=-c2, in1=mm_[:],
                              op0=ALU.mult, op1=ALU.add), reads=[Rk], writes=[Rk])
                if shift:
                    P.op("dve", I(nc.vector.tensor_scalar, out=mm_[:], in0=mm_[:], scalar1=shift, scalar2=None,
                                  op0=ALU.add), reads=[Rk], writes=[Rk])
                for thr, op_, adj in [(math.pi, ALU.is_gt, -TWO_PI), (-math.pi, ALU.is_lt, TWO_PI)]:
                    P.op("dve", I(nc.vector.tensor_scalar, out=kf[:], in0=mm_[:], scalar1=thr, scalar2=adj,
                                  op0=op_, op1=ALU.mult), reads=[Rk], writes=[Rk])
                    P.op("dve", I(nc.vector.tensor_tensor, out=mm_[:], in0=mm_[:], in1=kf[:], op=ALU.add),
                         reads=[Rk], writes=[Rk])
                P.op("dve", I(nc.vector.tensor_scalar, out=mm_[:], in0=mm_[:], scalar1=math.pi, scalar2=-math.pi,
                              op0=ALU.min, op1=ALU.max), reads=[Rk], writes=[Rk])
                P.op("act", I(nc.scalar.activation, out=dst[:], in_=mm_[:], func=AF.Sin), reads=[Rk], pwrites=[Rcs])

            reduce_sin(sinT, 0.0)
            reduce_sin(cosT, math.pi / 2)
            P.barrier()
            P.emit()

        env = locals()
        if upto >= "A":
            phaseA(nc, P, env)
        if upto >= "B":
            phaseBC(nc, P, env, upto)
        if upto >= "D":
            phaseDE(nc, P, env, upto)
        P.barrier()
        P.emit()
    P.close()
    return nc


def phaseA(nc, P, env):
    g = env
    sb = nc.sbuf_tensor
    x_d, win_d, bg_d, wuq_d, wukv_d, vec_d = g["x_d"], g["win_d"], g["bg_d"], g["wuq_d"], g["wukv_d"], g["vec_d"]
    identb, mhalf, cosT, sinT = g["identb"], g["mhalf"], g["cosT"], g["sinT"]
    vq_hn, vk_hn, vdq_hn, vdk_hn, vqn, vkvn = g["vq_hn"], g["vk_hn"], g["vdq_hn"], g["vdk_hn"], g["vqn"], g["vkvn"]
    Rid, Rmh, Rcs, Rvec = g["Rid"], g["Rmh"], g["Rcs"], g["Rvec"]
    QmT, KmT, Vm, QdT, KdT, Vd, G = g["QmT"], g["KmT"], g["Vm"], g["QdT"], g["KdT"], g["Vd"], g["G"]
    RQmT, RKmT, RVm, RQdT, RKdT, RVd, RG = g["RQmT"], g["RKmT"], g["RVm"], g["RQdT"], g["RKdT"], g["RVd"], g["RG"]
    with ExitStack() as es3:
        win = es3.enter_context(sb("a_win", [128, 8, 5664], BF16))
        wuq = es3.enter_context(sb("a_wuq", [128, 2, 1536], BF16))
        wukv = es3.enter_context(sb("a_wukv", [128, 2, 2048], BF16))
        wn = es3.enter_context(sb("a_wn", [128, 1024], F32))
        biasr = es3.enter_context(sb("a_bias", [1, 2048], BF16))
        onesr = es3.enter_context(sb("a_ones", [1, 128], BF16))
        xt0 = es3.enter_context(sb("a_xt0", [128, 1024], F32))
        xt1 = es3.enter_context(sb("a_xt1", [128, 1024], F32))
        hb = es3.enter_context(sb("a_hb", [128, 1024], BF16))
        hT = es3.enter_context(sb("a_hT", [128, 1024], BF16))
        sq = es3.enter_context(sb("a_sq", [128, 2048], F32))
        st = es3.enter_context(sb("a_st", [128, 64], F32))
        rstd = es3.enter_context(sb("a_rstd", [128, 64], F32))
        lat = es3.enter_context(sb("a_lat", [128, 512], BF16))
        latT = es3.enter_context(sb("a_latT", [128, 4, 128], BF16))
        qf = es3.enter_context(sb("a_qf", [128, 16, 96], F32))
        qb = es3.enter_context(sb("a_qb", [128, 16, 96], BF16))
        kf = es3.enter_context(sb("a_kf", [128, 16, 32], F32))
        kb = es3.enter_context(sb("a_kb", [128, 16, 96], BF16))
        rt = es3.enter_context(sb("a_rt", [128, 4, 16, 16], F32))
        vb = es3.enter_context(sb("a_vb", [128, 1024], BF16))
        qTs = es3.enter_context(sb("a_qTs", [128, 16, 128], BF16))
        kTs = es3.enter_context(sb("a_kTs", [128, 16, 128], BF16))
        df = es3.enter_context(sb("a_df", [128, 512], F32))
        dqb = es3.enter_context(sb("a_dqb", [128, 1024], BF16))
        dkb = es3.enter_context(sb("a_dkb", [128, 1024], BF16))
        dvb = es3.enter_context(sb("a_dvb", [128, 1024], BF16))
        dqTs = es3.enter_context(sb("a_dqTs", [128, 8, 128], BF16))
        dkTs = es3.enter_context(sb("a_dkTs", [128, 8, 128], BF16))
        gt = es3.enter_context(sb("a_gt", [128, 512], BF16))
        gb = es3.enter_context(sb("a_gb", [128, 2048], BF16))
        kr = es3.enter_context(sb("a_kr", [128, 32], F32))
        krw = es3.enter_context(sb("a_krw", [128, 32], F32))
        psF = es3.enter_context(nc.psum_tensor("a_psF", [128, 6 * 512], F32))
        psB = es3.enter_context(nc.psum_tensor("a_psB", [128, 2 * 1024], BF16))
        Rw = R("aw")
        P.dma("pool", I(nc.gpsimd.dma_start, out=win[:], in_=win_d.rearrange("(kc p) c -> p kc c", p=128)), Rw,
              pwrites=[Rw])
        P.dma("pool", I(nc.gpsimd.dma_start, out=wuq[:], in_=wuq_d.rearrange("(kc p) c -> p kc c", p=128)), Rw,
              pwrites=[Rw])
        P.dma("pool", I(nc.gpsimd.dma_start, out=wukv[:], in_=wukv_d.rearrange("(kc p) c -> p kc c", p=128)), Rw,
              pwrites=[Rw])
        P.dma("pool", I(nc.gpsimd.dma_start, out=biasr[:], in_=bg_d.rearrange("(o c) -> o c", o=1)), Rw, pwrites=[Rw])
        P.dma("sp", I(nc.sync.dma_start, out=wn[:], in_=vec_d["attn_norm_w"].partition_broadcast(128)), Rw,
              pwrites=[Rw])
        P.op("pool", I(nc.gpsimd.memset, onesr[:], 1.0), pwrites=[Rw])

        xts = [xt0, xt1]
        Rxt = [R("xt0"), R("xt1")]
        Rz = [R("z0"), R("z1")]
        zb = [psF[:, 0:512], psF[:, 512:1024]]
        ps4 = psF[:, 1024:3072]
        Rps4 = R("ps4")
        Tb = [psB[:, 0:1024], psB[:, 1024:2048]]
        RT = [R("T0"), R("T1")]
        names = "hb hT sq stx stl stq stk stkr std lat latT qf qb kf kb rt vb qTs kTs df dqb dkb dvb dqTs dkTs gt gb kr krw"
        RR = {n: R(n) for n in names.split()}

        def rs(src, dst, n, Rs, w):
            P.op("dve", I(nc.vector.tensor_scalar, out=dst, in0=src, scalar1=1.0 / n, scalar2=EPS, op0=ALU.mult,
                          op1=ALU.add), reads=[Rs], writes=[Rs])
            P.op("pool", I(nc.gpsimd.tensor_tensor, out=dst, in0=dst, in1=mhalf[:, 0:w], op=ALU.pow),
                 reads=[Rs, Rmh], writes=[Rs])

        def zblock(i, blk):
            P.tag = "z%d" % blk
            bank = zb[blk % 2]
            Rb = Rz[blk % 2]
            ncols = 512 if blk < 11 else 32
            c0 = blk * 512
            gate = 7 <= blk <= 10
            for kc in range(8):
                P.op("pe", I(nc.tensor.matmul, bank[:, 0:ncols], lhsT=hT[:, kc * 128:(kc + 1) * 128],
                             rhs=win[:, kc, c0:c0 + ncols], start=(kc == 0), stop=(kc == 7 and not gate)),
                     reads=[RR["hT"], Rw], writes=[Rb] if kc == 0 else (), pwrites=[Rb] if kc else ())
            if gate:
                gc = (blk - 7) * 512
                P.op("pe", I(nc.tensor.matmul, bank[:, 0:512], lhsT=onesr[0:1, :], rhs=biasr[0:1, gc:gc + 512],
                             start=False, stop=True), reads=[Rw], pwrites=[Rb])
            return bank, Rb

        def rope(i, src, dst, Rsrc, Rdst):
            c = cosT[:, i, :].unsqueeze(1).to_broadcast([128, 16, 16])
            s = sinT[:, i, :].unsqueeze(1).to_broadcast([128, 16, 16])
            x1 = src[:, :, 0:16]
            x2 = src[:, :, 16:32]
            Rrt = RR["rt"]
            P.op("dve", I(nc.vector.tensor_tensor, out=rt[:, 0], in0=x1, in1=c, op=ALU.mult), reads=[Rsrc, Rcs],
                 pwrites=[Rrt])
            P.op("dve", I(nc.vector.tensor_tensor, out=rt[:, 1], in0=x2, in1=s, op=ALU.mult), reads=[Rsrc, Rcs],
                 pwrites=[Rrt])
            P.op("dve", I(nc.vector.tensor_tensor, out=rt[:, 2], in0=x1, in1=s, op=ALU.mult), reads=[Rsrc, Rcs],
                 pwrites=[Rrt])
            P.op("dve", I(nc.vector.tensor_tensor, out=rt[:, 3], in0=x2, in1=c, op=ALU.mult), reads=[Rsrc, Rcs],
                 pwrites=[Rrt])
            P.op("dve", I(nc.vector.tensor_tensor, out=dst[:, :, 0:16], in0=rt[:, 0], in1=rt[:, 1], op=ALU.subtract),
                 reads=[Rrt], pwrites=[Rdst])
            P.op("dve", I(nc.vector.tensor_tensor, out=dst[:, :, 16:32], in0=rt[:, 2], in1=rt[:, 3], op=ALU.add),
                 reads=[Rrt], pwrites=[Rdst])

        latT2 = [latT, es3.enter_context(sb("a_latT1", [128, 4, 128], BF16))]
        kr2 = [kr, es3.enter_context(sb("a_kr1", [128, 32], F32))]
        krw2 = [krw, es3.enter_context(sb("a_krw1", [128, 32], F32))]
        qb2 = [qb, es3.enter_context(sb("a_qb1", [128, 16, 96], BF16))]
        kb2 = [kb, es3.enter_context(sb("a_kb1", [128, 16, 96], BF16))]
        sq2 = sq[:, 512:2048]
        RlatT = [R("latT0"), R("latT1")]
        Rkr = [R("kr0"), R("kr1")]
        Rkrw = [R("krw0"), R("krw1")]
        Rstkr = [R("stkr0"), R("stkr1")]
        Rqb = [R("qb0"), R("qb1")]
        Rkb = [R("kb0"), R("kb1")]
        Rsq2 = R("sq2")

        dfr = [df, es3.enter_context(sb("a_df1", [128, 512], F32)), es3.enter_context(sb("a_df2", [128, 512], F32))]
        Rdfr = [R("df0"), R("df1"), R("df2")]
        Rstd = [R("std0"), R("std1")]
        dfc = [0]

        def dqk(i, blks, dstb, hn, Rdst, dTs, RdTs, dram, Rdram):
            t0 = i * 128
            for j, blk in enumerate(blks):
                bank, Rb = zblock(i, blk)
                k3 = dfc[0] % 3
                dfc[0] += 1
                dfk, Rdfk = dfr[k3], Rdfr[k3]
                c0 = 40 + 8 * (dfc[0] % 2)
                P.op("act", I(nc.scalar.copy, out=dfk[:], in_=bank[:, 0:512]), reads=[Rb], writes=[Rdfk])
                P.op("act", I(nc.scalar.activation, out=sq[:, 0:512], in_=dfk[:], func=AF.Square),
                     reads=[Rdfk], writes=[RR["sq"]])
                P.op("dve", I(nc.vector.tensor_reduce, out=st[:, c0:c0 + 8],
                              in_=sq[:, 0:512].rearrange("p (a b) -> p a b", a=8), axis=AX.X, op=ALU.add),
                     reads=[RR["sq"]], writes=[Rstd[dfc[0] % 2]])
                rs(st[:, c0:c0 + 8], rstd[:, c0:c0 + 8], 64.0, Rstd[dfc[0] % 2], 8)
                P.op("dve", I(nc.vector.tensor_tensor, out=dfk[:].rearrange("p (a b) -> p a b", a=8),
                              in0=dfk[:].rearrange("p (a b) -> p a b", a=8),
                              in1=rstd[:, c0:c0 + 8].unsqueeze(2).to_broadcast([128, 8, 64]),
                              op=ALU.mult), reads=[Rdfk, Rstd[dfc[0] % 2]], writes=[Rdfk])
                P.op("dve", I(nc.vector.tensor_tensor,
                              out=dstb[:, j * 512:(j + 1) * 512].rearrange("p (a b) -> p a b", a=8),
                              in0=dfk[:].rearrange("p (a b) -> p a b", a=8),
                              in1=hn[:].unsqueeze(1).to_broadcast([128, 8, 64]), op=ALU.mult),
                     reads=[Rdfk, Rvec], writes=[Rdst] if j == 0 else (), pwrites=[Rdst] if j else ())

        def dqkT(i, dstb, Rdst, dTs, RdTs, dram, Rdram):
            P.tag = "dqkT"
            t0 = i * 128
            for h in range(8):
                P.op("pe", I(nc.tensor.transpose, out=Tb[0][:, h * 128:(h + 1) * 128],
                             in_=dstb[:, h * 128:(h + 1) * 128], identity=identb[:]),
                     reads=[Rdst, Rid], writes=[RT[0]] if h == 0 else (), pwrites=[RT[0]] if h else ())
            P.op("act", I(nc.scalar.copy, out=dTs[:].rearrange("p a b -> p (a b)"), in_=Tb[0][:, 0:1024]),
                 reads=[RT[0]], writes=[RdTs])
            P.dma("sp", I(nc.sync.dma_start, out=dram[:, :, t0:t0 + 128].rearrange("h d t -> d h t"), in_=dTs[:]),
                  RdTs, reads=[RdTs], pwrites=[Rdram])

        xts3 = [xt0, xt1, es3.enter_context(sb("a_xt2", [128, 1024], F32))]
        Rxt3 = [R("xt0"), R("xt1"), R("xt2")]
        hb2 = [hb, es3.enter_context(sb("a_hb1", [128, 1024], BF16))]
        Rhb = [R("hb0"), R("hb1")]
        Rstx = [R("stx0"), R("stx1")]

        def S1a(i):
            P.tag = "S1a"
            s_ = i % 2
            xt = xts3[i % 3]
            Rx = Rxt3[i % 3]
            t0 = i * 128
            c = 56 + 2 * s_
            P.dma("sp", I(nc.sync.dma_start, out=xt[:], in_=x_d[t0:t0 + 128, :]), Rx, writes=[Rx])
            P.op("act", I(nc.scalar.activation, out=sq[:, 0:1024], in_=xt[:], func=AF.Square,
                          accum_out=st[:, c:c + 1]), reads=[Rx], writes=[RR["sq"], Rstx[s_]])
            rs(st[:, c:c + 1], rstd[:, c:c + 1], 1024.0, Rstx[s_], 1)
            P.op("dve", I(nc.vector.scalar_tensor_tensor, out=hb2[s_][:], in0=xt[:], scalar=rstd[:, c:c + 1], in1=wn[:],
                          op0=ALU.mult, op1=ALU.mult), reads=[Rx, Rstx[s_], Rw], writes=[Rhb[s_]])

        def S1(i):
            P.tag = "S1head"
            s_ = i % 2
            t0 = i * 128
            for kc in range(8):
                P.op("pe", I(nc.tensor.transpose, out=Tb[0][:, kc * 128:(kc + 1) * 128],
                             in_=hb2[s_][:, kc * 128:(kc + 1) * 128], identity=identb[:]),
                     reads=[Rhb[s_], Rid], writes=[RT[0]] if kc == 0 else (), pwrites=[RT[0]] if kc else ())
            P.op("act", I(nc.scalar.copy, out=hT[:], in_=Tb[0][:, 0:1024]), reads=[RT[0]], writes=[RR["hT"]])
            bank, Rb = zblock(i, 0)
            k3 = dfc[0] % 3
            dfc[0] += 1
            latf, Rlatf = dfr[k3], Rdfr[k3]
            P.op("act", I(nc.scalar.copy, out=latf[:], in_=bank[:, 0:512]), reads=[Rb], writes=[Rlatf])
            P.op("act", I(nc.scalar.activation, out=sq[:, 0:512], in_=latf[:], func=AF.Square), reads=[Rlatf],
                 writes=[RR["sq"]])
            P.op("dve", I(nc.vector.tensor_reduce, out=st[:, 1:3], in_=sq[:, 0:512].rearrange("p (a b) -> p a b", a=2),
                          axis=AX.X, op=ALU.add), reads=[RR["sq"]], writes=[RR["stl"]])
            rs(st[:, 1:3], rstd[:, 1:3], 256.0, RR["stl"], 2)
            P.op("dve", I(nc.vector.scalar_tensor_tensor, out=lat[:, 0:256], in0=latf[:, 0:256], scalar=rstd[:, 1:2],
                          in1=vqn[:], op0=ALU.mult, op1=ALU.mult), reads=[Rlatf, RR["stl"], Rvec], writes=[RR["lat"]])
            P.op("dve", I(nc.vector.scalar_tensor_tensor, out=lat[:, 256:512], in0=latf[:, 256:512],
                          scalar=rstd[:, 2:3], in1=vkvn[:], op0=ALU.mult, op1=ALU.mult),
                 reads=[Rlatf, RR["stl"], Rvec], pwrites=[RR["lat"]])
            dqk(i, [1, 2], dqb, vdq_hn, RR["dqb"], dqTs, RR["dqTs"], QdT, RQdT)
            P.tag = "latT"
            for j in range(4):
                P.op("pe", I(nc.tensor.transpose, out=Tb[1][:, j * 128:(j + 1) * 128],
                             in_=lat[:, j * 128:(j + 1) * 128], identity=identb[:]),
                     reads=[RR["lat"], Rid], writes=[RT[1]] if j == 0 else (), pwrites=[RT[1]] if j else ())
            P.op("dve", I(nc.vector.tensor_copy, out=latT2[s_][:].rearrange("p a b -> p (a b)"), in_=Tb[1][:, 0:512]),
                 reads=[RT[1]], writes=[RlatT[s_]])
            dqk(i, [3, 4], dkb, vdk_hn, RR["dkb"], dkTs, RR["dkTs"], KdT, RKdT)
            for j, blk in enumerate([5, 6]):
                bank, Rb = zblock(i, blk)
                P.op("act", I(nc.scalar.copy, out=dvb[:, j * 512:(j + 1) * 512], in_=bank[:, 0:512]), reads=[Rb],
                     writes=[RR["dvb"]] if j == 0 else (), pwrites=[RR["dvb"]] if j else ())
            P.dma("sp", I(nc.sync.dma_start, out=Vd[t0:t0 + 128, :], in_=dvb[:]), RR["dvb"], reads=[RR["dvb"]],
                  pwrites=[RVd])
            dqkT(i, dqb, RR["dqb"], dqTs, RR["dqTs"], QdT, RQdT)
            for j, blk in enumerate([7, 8, 9, 10]):
                if j == 2:
                    dqkT(i, dkb, RR["dkb"], dkTs, RR["dkTs"], KdT, RKdT)
                bank, Rb = zblock(i, blk)
                P.op("act", I(nc.scalar.activation, out=gt[:], in_=bank[:, 0:512], func=AF.Tanh, scale=0.5),
                     reads=[Rb], writes=[RR["gt"]])
                P.op("dve", I(nc.vector.tensor_scalar, out=gb[:, j * 512:(j + 1) * 512], in0=gt[:], scalar1=0.5,
                              scalar2=0.5, op0=ALU.mult, op1=ALU.add), reads=[RR["gt"]],
                     writes=[RR["gb"]] if j == 0 else (), pwrites=[RR["gb"]] if j else ())
            P.dma("sp", I(nc.sync.dma_start, out=G[t0:t0 + 128, :], in_=gb[:]), RR["gb"], reads=[RR["gb"]],
                  pwrites=[RG])
            bank, Rb = zblock(i, 11)
            P.op("act", I(nc.scalar.copy, out=kr2[s_][:], in_=bank[:, 0:32]), reads=[Rb], writes=[Rkr[s_]])
            P.op("act", I(nc.scalar.activation, out=sq[:, 0:32], in_=kr2[s_][:], func=AF.Square,
                          accum_out=st[:, 36 + s_:37 + s_]), reads=[Rkr[s_]], writes=[RR["sq"], Rstkr[s_]])
            P.op("dve", I(nc.vector.tensor_tensor, out=krw2[s_][:], in0=kr2[s_][:], in1=vk_hn[:, 64:96], op=ALU.mult),
                 reads=[Rkr[s_], Rvec], writes=[Rkrw[s_]])

        def S2q(i):
            P.tag = "S2q"
            s_ = i % 2
            for b_ in range(4):
                for kc in range(2):
                    P.op("pe", I(nc.tensor.matmul, ps4[:, b_ * 512:b_ * 512 + 384], lhsT=latT2[s_][:, kc, :],
                                 rhs=wuq[:, kc, b_ * 384:(b_ + 1) * 384], start=(kc == 0), stop=(kc == 1)),
                         reads=[RlatT[s_], Rw], writes=[Rps4] if (b_ == 0 and kc == 0) else (),
                         pwrites=() if (b_ == 0 and kc == 0) else [Rps4])
            qv = ps4.rearrange("p (b x) -> p b x", b=4)[:, :, 0:384].rearrange("p b (h d) -> p b h d", h=4)
            sqv = sq2.rearrange("p (b h d) -> p b h d", b=4, h=4)
            qfv = qf[:].rearrange("p (b h) d -> p b h d", b=4)
            P.op("act", I(nc.scalar.copy, out=qfv, in_=qv), reads=[Rps4], writes=[RR["qf"]])
            P.op("act", I(nc.scalar.activation, out=sqv, in_=qfv, func=AF.Square), reads=[RR["qf"]], writes=[Rsq2])
            P.op("dve", I(nc.vector.tensor_reduce, out=st[:, 4:20].rearrange("p (b h) -> p b h", b=4), in_=sqv,
                          axis=AX.X, op=ALU.add), reads=[Rsq2], writes=[RR["stq"]])
            rs(st[:, 4:20], rstd[:, 4:20], 96.0, RR["stq"], 16)
            P.op("dve", I(nc.vector.tensor_tensor, out=qfv, in0=qfv,
                          in1=rstd[:, 4:20].rearrange("p (b h) -> p b h", b=4).unsqueeze(3).to_broadcast(
                              [128, 4, 4, 96]), op=ALU.mult), reads=[RR["qf"], RR["stq"]], writes=[RR["qf"]])
            P.op("dve", I(nc.vector.tensor_tensor, out=qf[:], in0=qf[:],
                          in1=vq_hn[:].unsqueeze(1).to_broadcast([128, 16, 96]), op=ALU.mult),
                 reads=[RR["qf"], Rvec], writes=[RR["qf"]])
            P.op("dve", I(nc.vector.tensor_copy, out=qb2[s_][:, :, 0:64], in_=qf[:, :, 0:64]), reads=[RR["qf"]],
                 writes=[Rqb[s_]])
            rope(i, qf[:, :, 64:96], qb2[s_][:, :, 64:96], RR["qf"], Rqb[s_])

        def S2k(i):
            P.tag = "S2k"
            s_ = i % 2
            t0 = i * 128
            for b_ in range(4):
                for kc in range(2):
                    P.op("pe", I(nc.tensor.matmul, ps4[:, b_ * 512:(b_ + 1) * 512], lhsT=latT2[s_][:, 2 + kc, :],
                                 rhs=wukv[:, kc, b_ * 512:(b_ + 1) * 512], start=(kc == 0), stop=(kc == 1)),
                         reads=[RlatT[s_], Rw], writes=[Rps4] if (b_ == 0 and kc == 0) else (),
                         pwrites=() if (b_ == 0 and kc == 0) else [Rps4])
            kvv = ps4.rearrange("p (h d) -> p h d", h=16)
            sqk = sq2[:, 0:1024].rearrange("p (h d) -> p h d", h=16)
            P.op("act", I(nc.scalar.activation, out=sqk, in_=kvv[:, :, 0:64], func=AF.Square), reads=[Rps4],
                 writes=[Rsq2])
            P.op("dve", I(nc.vector.tensor_reduce, out=st[:, 20:36], in_=sqk, axis=AX.X, op=ALU.add),
                 reads=[Rsq2], writes=[RR["stk"]])
            P.op("dve", I(nc.vector.tensor_scalar, out=st[:, 20:36], in0=st[:, 20:36], scalar1=st[:, 36 + s_:37 + s_],
                          scalar2=None, op0=ALU.add), reads=[RR["stk"], Rstkr[s_]], writes=[RR["stk"]])
            rs(st[:, 20:36], rstd[:, 20:36], 96.0, RR["stk"], 16)
            P.op("dve", I(nc.vector.tensor_tensor, out=qf[:, :, 0:64], in0=kvv[:, :, 0:64],
                          in1=rstd[:, 20:36].unsqueeze(2).to_broadcast([128, 16, 64]), op=ALU.mult),
                 reads=[Rps4, RR["stk"]], writes=[RR["qf"]])
            P.op("dve", I(nc.vector.tensor_tensor, out=kb2[s_][:, :, 0:64], in0=qf[:, :, 0:64],
                          in1=vk_hn[:, 0:64].unsqueeze(1).to_broadcast([128, 16, 64]), op=ALU.mult),
                 reads=[RR["qf"], Rvec], writes=[Rkb[s_]])
            P.op("dve", I(nc.vector.tensor_tensor, out=kf[:], in0=krw2[s_][:].unsqueeze(1).to_broadcast([128, 16, 32]),
                          in1=rstd[:, 20:36].unsqueeze(2).to_broadcast([128, 16, 32]), op=ALU.mult),
                 reads=[Rkrw[s_], RR["stk"]], writes=[RR["kf"]])
            rope(i, kf[:], kb2[s_][:, :, 64:96], RR["kf"], Rkb[s_])
            P.op("act", I(nc.scalar.copy, out=vb[:].rearrange("p (h d) -> p h d", h=16), in_=kvv[:, :, 64:128]),
                 reads=[Rps4], writes=[RR["vb"]])
            P.dma("sp", I(nc.sync.dma_start, out=Vm[t0:t0 + 128, :], in_=vb[:]), RR["vb"], reads=[RR["vb"]],
                  pwrites=[RVm])

        def S3(i):
            P.tag = "S3"
            s_ = i % 2
            t0 = i * 128
            for src, Rsrc, dst, Rdst, dram, Rdram in [(qb2[s_], Rqb[s_], qTs, RR["qTs"], QmT, RQmT),
                                                       (kb2[s_], Rkb[s_], kTs, RR["kTs"], KmT, RKmT)]:
                for half in range(2):
                    for hh in range(8):
                        h = half * 8 + hh
                        P.op("pe", I(nc.tensor.transpose, out=Tb[half][0:96, hh * 128:(hh + 1) * 128],
                                     in_=src[:, h, :], identity=identb[:]), reads=[Rsrc, Rid],
                             writes=[RT[half]] if hh == 0 else (), pwrites=[RT[half]] if hh else ())
                    eng = "act" if half == 0 else "dve"
                    fn = nc.scalar.copy if half == 0 else nc.vector.tensor_copy
                    P.op(eng, I(fn, out=dst[0:96, half * 8:(half + 1) * 8, :].rearrange("p a b -> p (a b)"),
                                in_=Tb[half][0:96, 0:1024]), reads=[RT[half]],
                         writes=[Rdst] if half == 0 else (), pwrites=[Rdst] if half else ())
                P.dma("sp", I(nc.sync.dma_start, out=dram[:, :, t0:t0 + 128].rearrange("h d t -> d h t"),
                              in_=dst[0:96, :, :]), Rdst, reads=[Rdst], pwrites=[Rdram])

        S1a(0)
        S1a(1)
        S1(0)
        for i in range(NT):
            if i + 1 < NT:
                S1(i + 1)
            if i >= 1:
                S3(i - 1)
            if i + 2 < NT:
                S1a(i + 2)
            S2q(i)
            S2k(i)
        S3(NT - 1)
        P.barrier()
        P.emit()


def skip_tile(h, q0, q1, k0, k1):
    return False


def phaseBC(nc, P, env, upto):
    g = env
    sb = nc.sbuf_tensor
    QmT, KmT, Vm, QdT, KdT, Vd, G, MG = [g[k] for k in "QmT KmT Vm QdT KdT Vd G MG".split()]
    RMG = g["RMG"]
    mhalf, nlam, vsubln, posf, pos_d = g["mhalf"], g["nlam"], g["vsubln"], g["posf"], g["pos_d"]
    Rmh, Rnlam, Rvec, Rposf = g["Rmh"], g["Rnlam"], g["Rvec"], g["Rposf"]
    with ExitStack() as es:
        omla = es.enter_context(sb("om", [128, NT, 1024], BF16))
        Rom = R("omla")
        with ExitStack() as es2:
            qT = [es2.enter_context(sb("b_qT%d" % k, [96, T], BF16)) for k in range(2)]
            kT = [es2.enter_context(sb("b_kT%d" % k, [96, T], BF16)) for k in range(2)]
            vv = [es2.enter_context(sb("b_v%d" % k, [128, NT, 65], BF16)) for k in range(2)]
            ga = [es2.enter_context(sb("b_ga%d" % k, [128, NT, 64], BF16)) for k in range(2)]
            pT = [es2.enter_context(sb("b_pT%d" % k, [128, 1024], BF16)) for k in range(3)]
            rc = es2.enter_context(sb("b_rc", [128, 8], F32))
            ps = es2.enter_context(nc.psum_tensor("b_ps", [128, 8 * 512], F32))
            Rq = [R("bq0"), R("bq1")]
            RpT = [R("pT0"), R("pT1"), R("pT2")]
            Rs = [R("bs0"), R("bs1"), R("bs2")]
            Ro = [R("bo0"), R("bo1")]
            Rrc = R("brc")
            sbank = [ps[:, 1024 * k:1024 * (k + 1)] for k in range(3)]

            def obank(os_, qs):
                b0 = 3072 + 512 * os_ + 66 * qs
                return ps[:, b0:b0 + 65]
            for k in range(2):
                P.op("pool", I(nc.gpsimd.memset, vv[k][:, :, 64:65], 1.0), pwrites=[Rq[k]])
            step = 0
            for h in range(16):
                s_ = h % 2
                P.dma("sp", I(nc.sync.dma_start, out=qT[s_][:], in_=QmT[h, :, :]), Rq[s_], pwrites=[Rq[s_]])
                P.dma("sp", I(nc.sync.dma_start, out=kT[s_][:], in_=KmT[h, :, :]), Rq[s_], pwrites=[Rq[s_]])
                P.dma("sp", I(nc.sync.dma_start, out=vv[s_][:, :, 0:64],
                              in_=Vm[:, h * 64:(h + 1) * 64].rearrange("(i p) d -> p i d", p=128)), Rq[s_],
                      pwrites=[Rq[s_]])
                P.dma("sp", I(nc.sync.dma_start, out=ga[s_][:],
                              in_=G[:, h * 64:(h + 1) * 64].rearrange("(i p) d -> p i d", p=128)), Rq[s_],
                      pwrites=[Rq[s_]])
                steps = [(qb, kp) for qb in range(8) for kp in range(16)]

                def b_front(qb, kp, sb_, pb_, s_=s_):
                    P.tag = "Bf"
                    for kk in range(2):
                        kt = kp * 2 + kk
                        P.op("pe", I(nc.tensor.matmul, sbank[sb_][:, kk * 512:(kk + 1) * 512],
                                     lhsT=kT[s_][:, kt * 128:(kt + 1) * 128],
                                     rhs=qT[s_][:, qb * 512:(qb + 1) * 512], start=True, stop=True),
                             reads=[Rq[s_]], writes=[Rs[sb_]] if kk == 0 else (), pwrites=[Rs[sb_]] if kk else ())
                    P.op("act", I(nc.scalar.activation, out=pT[pb_][:], in_=sbank[sb_], func=AF.Exp),
                         reads=[Rs[sb_]], writes=[RpT[pb_]])

                def b_back(qb, kp, pb_, s_=s_, h=h):
                    P.tag = "Bb"
                    os_ = (h * 8 + qb) % 2
                    for kk in range(2):
                        kt = kp * 2 + kk
                        for qs in range(4):
                            w_ = kt == 0 and qs == 0
                            P.op("pe", I(nc.tensor.matmul, obank(os_, qs),
                                         lhsT=pT[pb_][:, kk * 512 + qs * 128:kk * 512 + (qs + 1) * 128],
                                         rhs=vv[s_][:, kt, :], start=w_, stop=(kt == NT - 1), skip_group_check=True),
                                 reads=[RpT[pb_], Rq[s_]], writes=[Ro[os_]] if w_ else (),
                                 pwrites=() if w_ else [Ro[os_]])
                    if kp == 15:
                        P.tag = "Bep"
                        ov = ps[:, 3072 + 512 * os_:3072 + 512 * os_ + 264].rearrange("p (q c) -> p q c", q=4)
                        P.op("dve", I(nc.vector.reciprocal, out=rc[:, 0:4], in_=ov[:, :, 64]), reads=[Ro[os_]],
                             writes=[Rrc])
                        for qs in range(4):
                            ti = qb * 4 + qs
                            P.op("dve", I(nc.vector.scalar_tensor_tensor, out=omla[:, ti, h * 64:(h + 1) * 64],
                                          in0=obank(os_, qs)[:, 0:64], scalar=rc[:, qs:qs + 1], in1=ga[s_][:, ti, :],
                                          op0=ALU.mult, op1=ALU.mult), reads=[Ro[os_], Rrc, Rq[s_]], pwrites=[Rom])

                ring = []
                for n in range(len(steps) + 1):
                    if n < len(steps):
                        sb_, pb_ = step % 3, step % 3
                        step += 1
                        b_front(steps[n][0], steps[n][1], sb_, pb_)
                        ring.append(pb_)
                    if n >= 1:
                        b_back(steps[n - 1][0], steps[n - 1][1], ring[n - 1])
            if upto < "C":
                for i in range(NT):
                    P.dma("sp", I(nc.sync.dma_start, out=MG[i * 128:(i + 1) * 128, :], in_=omla[:, i, :]), Rom,
                          reads=[Rom], pwrites=[RMG])
            P.barrier()
            P.emit()
        if upto < "C":
            return

        CLS, DMIN = g["CLS"], g["DMIN"]
        slp_d, PQHL = g["slopes_d"], g["PQHL"]
        RPQHL = R("PQHL")
        with ExitStack() as es2:
            pq = es2.enter_context(sb("c_pq", [128, T], F32))
            Rpq = R("cpq")
            with ExitStack() as es3:
                pqi = es3.enter_context(sb("c_pqi", [128, T], I32))
                ahi = es3.enter_context(sb("c_ahi", [8, T], BF16))
                alo = es3.enter_context(sb("c_alo", [8, T], BF16))
                shi = es3.enter_context(sb("c_shi", [8, T], BF16))
                slo = es3.enter_context(sb("c_slo", [8, T], BF16))
                msl = es3.enter_context(sb("c_msl", [8, 1], F32))
                Rt = R("ctmp")
                P.dma("sp", I(nc.sync.dma_start, out=pqi[:], in_=pos_d.partition_broadcast(128)), Rpq, writes=[Rpq])
                P.dma("sp", I(nc.sync.dma_start, out=msl[:], in_=slp_d.rearrange("(h o) -> h o", o=1)), Rt,
                      writes=[Rt])
                P.op("dve", I(nc.vector.tensor_copy, out=pq[:], in_=pqi[:]), reads=[Rpq], writes=[Rpq])
                P.op("dve", I(nc.vector.tensor_copy, out=ahi[:], in_=pq[0:8, :]), reads=[Rpq], pwrites=[Rt])
                P.op("dve", I(nc.vector.tensor_tensor, out=alo[:], in0=pq[0:8, :], in1=ahi[:], op=ALU.subtract),
                     reads=[Rpq, Rt], pwrites=[Rt])
                P.op("dve", I(nc.vector.tensor_scalar, out=shi[:], in0=ahi[:], scalar1=msl[:, 0:1], scalar2=None,
                              op0=ALU.mult), reads=[Rt], pwrites=[Rt])
                P.op("dve", I(nc.vector.tensor_scalar, out=slo[:], in0=alo[:], scalar1=msl[:, 0:1], scalar2=None,
                              op0=ALU.mult), reads=[Rt], pwrites=[Rt])
                P.dma("sp", I(nc.sync.dma_start, out=PQHL[:, 0, :], in_=shi[:]), Rt, reads=[Rt], pwrites=[RPQHL])
                P.dma("sp", I(nc.sync.dma_start, out=PQHL[:, 1, :], in_=slo[:]), Rt, reads=[Rt], pwrites=[RPQHL])
                P.barrier()
                P.emit()
            nposf = es2.enter_context(sb("c_nposf", [128, NT], F32))
            bia = es2.enter_context(sb("c_bia", [128, 2, 2, NT], F32))
            P.op("dve", I(nc.vector.tensor_scalar, out=nposf[:], in0=posf[:], scalar1=-1.0, scalar2=None,
                          op0=ALU.mult), reads=[Rposf], writes=[Rpq])
            qT = [[es2.enter_context(sb("c_qT%d%d" % (k, m), [68, T], BF16)) for m in range(2)] for k in range(2)]
            kT = [[es2.enter_context(sb("c_kT%d%d" % (k, m), [68, T], BF16)) for m in range(2)] for k in range(2)]
            vv = [es2.enter_context(sb("c_v%d" % k, [128, NT, 129], BF16)) for k in range(2)]
            gbt = [es2.enter_context(sb("c_gb%d" % k, [128, NT, 128], BF16)) for k in range(2)]
            pT = [es2.enter_context(sb("c_pT%d" % k, [128, 512], BF16)) for k in range(4)]
            sp_ = [es2.enter_context(sb("c_sp%d" % k, [128, 512], F32)) for k in range(2)]
            dt_ = [es2.enter_context(sb("c_dt%d" % k, [128, 256], F32)) for k in range(2)]
            rc = es2.enter_context(sb("c_rc", [128, 16], F32))
            of = es2.enter_context(sb("c_of", [128, 4, 128], F32))
            jk = es2.enter_context(sb("c_jk", [128, 2, 128], F32))
            ps = es2.enter_context(nc.psum_tensor("c_ps", [128, 8 * 512], F32))
            Rq = [R("cq0"), R("cq1")]
            Rgs = [R("cgs0"), R("cgs1")]
            Rbia = [R("cbia0"), R("cbia1")]
            RpT = [R("cpT%d" % k) for k in range(4)]
            Rsp = [R("csp%d" % k) for k in range(2)]
            Rdt = [R("cdt%d" % k) for k in range(2)]
            Rs = [R("cs%d" % k) for k in range(4)]
            Ro = [R("co0"), R("co1")]
            Rrc, Rof, Rjk = R("crc"), R("cof"), R("cjk")
            sbank = [ps[:, 512 * k:512 * (k + 1)] for k in range(4)]
            def oacc(s, mp, qs):
                b0 = 2048 + (2 * s + mp) * 512 + qs * 132
                return ps[:, b0:b0 + 129]

            def oset(s):
                return ps[:, 2048 + 2 * s * 512:2048 + (2 * s + 2) * 512]
            for k in range(2):
                P.op("pool", I(nc.gpsimd.memset, vv[k][:, :, 128:129], 1.0), pwrites=[Rq[k]])
                for m in range(2):
                    P.op("dve", I(nc.vector.memset, kT[k][m][64:68, :], 2.0), pwrites=[Rq[k]])
                    P.op("dve", I(nc.vector.memset, kT[k][m][64:66, :], -1.0), pwrites=[Rq[k]])
            step = 0
            dstep = 0
            oset_i = 0
            for h in range(8):
                s_ = h % 2
                slope = 2.0 ** (-(h + 1))
                for m in range(2):
                    P.dma("sp", I(nc.sync.dma_start, out=qT[s_][m][0:64, :], in_=QdT[h, m * 64:(m + 1) * 64, :]),
                          Rq[s_], pwrites=[Rq[s_]])
                    for a_ in range(2):
                        P.dma("sp", I(nc.sync.dma_start, out=qT[s_][m][64 + 2 * a_:66 + 2 * a_, :], in_=PQHL[h, :, :]),
                              Rq[s_], reads=[RPQHL], pwrites=[Rq[s_]])
                    P.dma("sp", I(nc.sync.dma_start, out=kT[s_][m][0:64, :], in_=KdT[h, m * 64:(m + 1) * 64, :]),
                          Rq[s_], pwrites=[Rq[s_]])
                P.dma("sp", I(nc.sync.dma_start, out=vv[s_][:, :, 0:128],
                              in_=Vd[:, h * 128:(h + 1) * 128].rearrange("(i p) d -> p i d", p=128)), Rq[s_],
                      pwrites=[Rq[s_]])
                P.dma("sp", I(nc.sync.dma_start, out=gbt[s_][:],
                              in_=G[:, 1024 + h * 128:1024 + (h + 1) * 128].rearrange("(i p) d -> p i d", p=128)),
                      Rq[s_], pwrites=[Rq[s_]])
                P.op("dve", I(nc.vector.tensor_tensor, out=gbt[s_][:], in0=gbt[s_][:],
                              in1=vsubln[:].unsqueeze(1).to_broadcast([128, NT, 128]), op=ALU.mult),
                     reads=[Rq[s_], Rvec], writes=[Rgs[s_]])
                P.op("dve", I(nc.vector.tensor_scalar, out=bia[:, s_, 0, :], in0=posf[:], scalar1=slope, scalar2=None,
                              op0=ALU.mult), reads=[Rposf], writes=[Rbia[s_]])
                P.op("dve", I(nc.vector.tensor_scalar, out=bia[:, s_, 1, :], in0=posf[:], scalar1=-slope, scalar2=None,
                              op0=ALU.mult), reads=[Rposf], pwrites=[Rbia[s_]])
                steps = []
                for qb in range(16):
                    kts = [kt for kt in range(NT) if slope * DMIN[qb][kt] < SKIP_T]
                    for n_, kt in enumerate(kts):
                        steps.append((qb, kt, int(CLS[qb][kt]), n_ == 0, n_ == len(kts) - 1))

                def st1(n, r4, d2, s_=s_):
                    P.tag = "C1"
                    qb, kt, cl, first, last = steps[n]
                    q0 = qb * 256
                    K_ = (64, 66, 68)[cl]
                    for mp in range(2):
                        P.op("pe", I(nc.tensor.matmul, sbank[r4][:, mp * 256:(mp + 1) * 256],
                                     lhsT=kT[s_][mp][0:K_, kt * 128:(kt + 1) * 128],
                                     rhs=qT[s_][mp][0:K_, q0:q0 + 256], start=True, stop=True),
                             reads=[Rq[s_]], writes=[Rs[r4]] if mp == 0 else (),
                             pwrites=[Rs[r4]] if mp else ())
                    if cl == 0:
                        P.op("act", I(nc.scalar.activation, out=dt_[d2][:], in_=pq[:, q0:q0 + 256], func=AF.Abs,
                                      bias=nposf[:, kt:kt + 1]), reads=[Rpq], writes=[Rdt[d2]])

                def st23(n, r4, d2, s_=s_, slope=slope):
                    P.tag = "C2"
                    qb, kt, cl, first, last = steps[n]
                    if cl == 0:
                        P.op("dve", I(nc.vector.scalar_tensor_tensor,
                                      out=sp_[d2][:].rearrange("p (m q) -> p m q", m=2),
                                      in0=dt_[d2][:].unsqueeze(1).to_broadcast([128, 2, 256]), scalar=-slope,
                                      in1=sbank[r4].rearrange("p (m q) -> p m q", m=2), op0=ALU.mult, op1=ALU.add),
                             reads=[Rdt[d2], Rs[r4]], writes=[Rsp[d2]])
                        P.op("act", I(nc.scalar.activation, out=pT[r4][:], in_=sp_[d2][:], func=AF.Exp),
                             reads=[Rsp[d2]], writes=[RpT[r4]])
                    else:
                        P.op("act", I(nc.scalar.activation, out=pT[r4][:], in_=sbank[r4], func=AF.Exp,
                                      bias=bia[:, s_, cl - 1, kt:kt + 1]), reads=[Rs[r4], Rbia[s_]],
                             writes=[RpT[r4]])

                def st4(n, r4, os_, s_=s_, h=h):
                    P.tag = "C4"
                    qb, kt, cl, first, last = steps[n]
                    for mp in range(2):
                        for qs in range(2):
                            w_ = first and mp == 0 and qs == 0
                            P.op("pe", I(nc.tensor.matmul, oacc(os_, mp, qs),
                                         lhsT=pT[r4][:, mp * 256 + qs * 128:mp * 256 + (qs + 1) * 128],
                                         rhs=vv[s_][:, kt, :], start=(first and qs == 0), stop=last,
                                         skip_group_check=True),
                                 reads=[RpT[r4], Rq[s_]], writes=[Ro[os_]] if w_ else (),
                                 pwrites=() if w_ else [Ro[os_]])
                    if last:
                        pend.append((qb, os_))

                def ep(qb, os_, s_=s_, h=h):
                    P.tag = "Cep"
                    ti = qb * 2
                    v = oset(os_).rearrange("p (m c) -> p m c", m=2)[:, :, 0:264].rearrange("p m (q c) -> p m q c", q=2)
                    o1, o2 = v[:, 0, :, 0:128], v[:, 1, :, 0:128]
                    bc = lambda ap: ap.unsqueeze(2).to_broadcast([128, 2, 128])
                    P.op("dve", I(nc.vector.reciprocal, out=rc[:, 0:4].rearrange("p (m q) -> p m q", m=2),
                                  in_=v[:, :, :, 128]), reads=[Ro[os_]], writes=[Rrc])
                    P.op("dve", I(nc.vector.tensor_scalar, out=rc[:, 4:6], in0=rc[:, 2:4], scalar1=nlam[:, 0:1],
                                  scalar2=None, op0=ALU.mult), reads=[Rrc, Rnlam], pwrites=[Rrc])
                    P.op("dve", I(nc.vector.tensor_tensor, out=of[:, 0:2, :], in0=o2, in1=bc(rc[:, 4:6]), op=ALU.mult),
                         reads=[Ro[os_], Rrc], writes=[Rof])
                    P.op("dve", I(nc.vector.tensor_tensor, out=of[:, 2:4, :], in0=o1, in1=bc(rc[:, 0:2]), op=ALU.mult),
                         reads=[Ro[os_], Rrc], pwrites=[Rof])
                    P.op("dve", I(nc.vector.tensor_tensor, out=of[:, 0:2, :], in0=of[:, 0:2, :], in1=of[:, 2:4, :],
                                  op=ALU.add), reads=[Rof], pwrites=[Rof])
                    P.op("act", I(nc.scalar.activation, out=jk[:], in_=of[:, 0:2, :], func=AF.Square), reads=[Rof],
                         writes=[Rjk])
                    P.op("dve", I(nc.vector.tensor_reduce, out=rc[:, 6:8], in_=jk[:], axis=AX.X, op=ALU.add),
                         reads=[Rjk], pwrites=[Rrc])
                    P.op("dve", I(nc.vector.tensor_scalar, out=rc[:, 8:10], in0=rc[:, 6:8], scalar1=1.0 / 128,
                                  scalar2=EPS, op0=ALU.mult, op1=ALU.add), reads=[Rrc], pwrites=[Rrc])
                    P.op("pool", I(nc.gpsimd.tensor_tensor, out=rc[:, 10:12], in0=rc[:, 8:10], in1=mhalf[:, 0:2],
                                   op=ALU.pow), reads=[Rrc, Rmh], pwrites=[Rrc])
                    P.op("dve", I(nc.vector.tensor_tensor, out=of[:, 2:4, :], in0=of[:, 0:2, :], in1=bc(rc[:, 10:12]),
                                  op=ALU.mult), reads=[Rof, Rrc], pwrites=[Rof])
                    P.op("dve", I(nc.vector.tensor_tensor, out=of[:, 0:2, :], in0=of[:, 2:4, :],
                                  in1=gbt[s_][:, ti:ti + 2, :], op=ALU.mult), reads=[Rof, Rgs[s_], Rq[s_]],
                         pwrites=[Rof])
                    P.op("dve", I(nc.vector.tensor_tensor, out=omla[:, ti:ti + 2, h * 128:(h + 1) * 128],
                                  in0=of[:, 0:2, :], in1=omla[:, ti:ti + 2, h * 128:(h + 1) * 128], op=ALU.add),
                         reads=[Rof, Rom], pwrites=[Rom])

                ep_after = {}
                qbs = sorted(set(st_[0] for st_ in steps))
                for a_, qb_ in enumerate(qbs[:-1]):
                    nq = qbs[a_ + 1]
                    idxs = [k for k, st_ in enumerate(steps) if st_[0] == nq]
                    dg = [k for k in idxs if steps[k][2] == 0]
                    at = dg[-1] if dg else min(idxs[0] + 1, idxs[-1])
                    at = min(at, idxs[-1] - 1) if len(idxs) > 1 else idxs[0]
                    ep_after.setdefault(at, []).append(qb_)
                pend = []
                due = set()
                r4s, d2s, oss = [], [], []
                for n in range(len(steps) + 2):
                    if n < len(steps):
                        r4 = step % 4
                        step += 1
                        d2 = dstep % 2
                        if steps[n][2] == 0:
                            dstep += 1
                        if steps[n][3]:
                            oset_i += 1
                        r4s.append(r4)
                        d2s.append(d2)
                        oss.append(oset_i % 2)
                        st1(n, r4, d2)
                    if 1 <= n <= len(steps):
                        st23(n - 1, r4s[n - 1], d2s[n - 1])
                        due.update(ep_after.get(n - 1, []))
                        for pq_ in [p_ for p_ in pend if p_[0] in due]:
                            pend.remove(pq_)
                            ep(*pq_)
                    if n >= 2:
                        st4(n - 2, r4s[n - 2], oss[n - 2])
                        for pq_ in [p_ for p_ in pend if p_[0] in due]:
                            pend.remove(pq_)
                            ep(*pq_)
                for pq_ in pend:
                    ep(*pq_)
                pend = []
            for i in range(NT):
                P.dma("sp", I(nc.sync.dma_start, out=MG[i * 128:(i + 1) * 128, :], in_=omla[:, i, :]), Rom,
                      reads=[Rom], pwrites=[RMG])
            P.barrier()
            P.emit()


def phaseDE(nc, P, env, upto):
    g = env
    sb = nc.sbuf_tensor
    x_d, out_d, MG, H2, wout_d, rw_d, vec_d = [g[k] for k in "x_d out_d MG H2 wout_d rw_d vec_d".split()]
    wg_d, wu_d, wd_d, iota_d, tokpi_d = [g[k] for k in "wg_d wu_d wd_d iota_d tokpi_d".split()]
    identb, identf, mhalf = g["identb"], g["identf"], g["mhalf"]
    Rid, Rmh = g["Rid"], g["Rmh"]
    with ExitStack() as es:
        aff = es.enter_context(sb("aff", [128, NT, NE], F32))
        posm = es.enter_context(sb("posm", [128, NT, NE], F32))
        Raff, Rposm = R("aff"), R("posm")
        wsl = [es.enter_context(sb("e_w%d" % k, [128, 16384], BF16)) for k in range(3)]
        Rws = [R("ew%d" % k) for k in range(4)]

        def wload(e, which):
            for k, src in enumerate([wg_d, wu_d]):
                if k not in which:
                    continue
                sl = (3 * e + k) % 4
                P.dma("pool", I(nc.gpsimd.dma_start, out=wsl[sl][:].rearrange("p (kc f) -> p kc f", kc=8),
                                in_=src[e].rearrange("(kc p) f -> p kc f", p=128)), Rws[sl], writes=[Rws[sl]])
            if 2 in which:
                sl = (3 * e + 2) % 4
                P.dma("pool", I(nc.gpsimd.dma_start, out=wsl[sl][:].rearrange("p (j c) -> p j c", j=16),
                                in_=wd_d[e].rearrange("(j p) c -> p j c", p=128)), Rws[sl], writes=[Rws[sl]])
        with ExitStack() as es2:
            wout = es2.enter_context(sb("d_wout", [128, 8, 1024], BF16))
            rw = es2.enter_context(sb("d_rw", [128, 8, NE], F32))
            wn2 = es2.enter_context(sb("d_wn2", [128, 1024], F32))
            mg = [es2.enter_context(sb("d_mg%d" % k, [128, 1024], BF16)) for k in range(2)]
            xt = [es2.enter_context(sb("d_xt%d" % k, [128, 1024], F32)) for k in range(2)]
            x1 = [es2.enter_context(sb("d_x1%d" % k, [128, 1024], F32)) for k in range(2)]
            mT = es2.enter_context(sb("d_mT", [128, 1024], BF16))
            sq = es2.enter_context(sb("d_sq", [128, 1024], F32))
            h2f = es2.enter_context(sb("d_h2f", [128, 1024], F32))
            h2b = [es2.enter_context(sb("d_h2b%d" % k, [128, 1024], BF16)) for k in range(2)]
            h2T = es2.enter_context(sb("d_h2T", [128, 1024], F32))
            sm = es2.enter_context(sb("d_sm", [128, 64], F32))
            psB = es2.enter_context(nc.psum_tensor("d_psB", [128, 1024], BF16))
            psF = es2.enter_context(nc.psum_tensor("d_psF", [128, 5 * 512], F32))
            Rw = R("dw")
            Rmg, Rxt, Rx1, Rh2b = [[R(n + str(k)) for k in range(2)] for n in ("dmg", "dxt", "dx1", "dh2b")]
            RmT, Rsq, Rh2f, Rh2T, Rsm, RTb, Racc, RTf, Rlg = [R(n) for n in
                                                              "dmT dsq dh2f dh2T dsm dTb dacc dTf dlg".split()]
            RH2, Rout = g["RH2"], g["Rout"]
            P.dma("pool", I(nc.gpsimd.dma_start, out=wout[:], in_=wout_d.rearrange("(kc p) c -> p kc c", p=128)), Rw,
                  pwrites=[Rw])
            P.dma("sp", I(nc.sync.dma_start, out=rw[:], in_=rw_d.rearrange("(kc p) c -> p kc c", p=128)), Rw,
                  pwrites=[Rw])
            P.dma("sp", I(nc.sync.dma_start, out=wn2[:], in_=vec_d["ffn_norm_w"].partition_broadcast(128)), Rw,
                  pwrites=[Rw])
            wload(0, (0, 1, 2))
            acc = psF[:, 0:1024]
            Tf = psF[:, 1024:2048]
            lg = psF[:, 2048:2048 + NE]
            h2f2 = [h2f, es2.enter_context(sb("d_h2f1", [128, 1024], F32))]
            Rh2f2 = [R("dh2f0"), R("dh2f1")]
            RsmA = [R("dsmA0"), R("dsmA1")]
            RsmB = R("dsmB")
            def Da(i):
                P.tag = "Da"
                s_ = i % 2
                t0 = i * 128
                ca = 48 + 4 * s_
                h2f = h2f2[s_]
                Rh2f = Rh2f2[s_]
                Rsm = RsmA[s_]
                P.dma("sp", I(nc.sync.dma_start, out=mg[s_][:], in_=MG[t0:t0 + 128, :]), Rmg[s_], writes=[Rmg[s_]])
                P.dma("sp", I(nc.sync.dma_start, out=xt[s_][:], in_=x_d[t0:t0 + 128, :]), Rxt[s_], writes=[Rxt[s_]])
                for kc in range(8):
                    P.op("pe", I(nc.tensor.transpose, out=psB[:, kc * 128:(kc + 1) * 128],
                                 in_=mg[s_][:, kc * 128:(kc + 1) * 128], identity=identb[:]),
                         reads=[Rmg[s_], Rid], writes=[RTb] if kc == 0 else (), pwrites=[RTb] if kc else ())
                P.op("act", I(nc.scalar.copy, out=mT[:], in_=psB[:, 0:1024]), reads=[RTb], writes=[RmT])
                for half in range(2):
                    for kc in range(8):
                        first = half == 0 and kc == 0
                        P.op("pe", I(nc.tensor.matmul, acc[:, half * 512:(half + 1) * 512],
                                     lhsT=mT[:, kc * 128:(kc + 1) * 128], rhs=wout[:, kc, half * 512:(half + 1) * 512],
                                     start=(kc == 0), stop=(kc == 7)), reads=[RmT, Rw],
                             writes=[Racc] if first else (), pwrites=() if first else [Racc])
                P.op("dve", I(nc.vector.tensor_tensor, out=x1[s_][:], in0=acc, in1=xt[s_][:], op=ALU.add),
                     reads=[Racc, Rxt[s_]], writes=[Rx1[s_]])
                P.dma("sp", I(nc.sync.dma_start, out=out_d[t0:t0 + 128, :], in_=x1[s_][:]), Rx1[s_], reads=[Rx1[s_]],
                      pwrites=[Rout])
                P.op("act", I(nc.scalar.activation, out=sq[:], in_=x1[s_][:], func=AF.Square, accum_out=sm[:, ca:ca + 1]),
                     reads=[Rx1[s_]], writes=[Rsq, Rsm])
                P.op("dve", I(nc.vector.tensor_scalar, out=sm[:, ca + 1:ca + 2], in0=sm[:, ca:ca + 1], scalar1=1.0 / D, scalar2=EPS,
                              op0=ALU.mult, op1=ALU.add), reads=[Rsm], pwrites=[Rsm])
                P.op("pool", I(nc.gpsimd.tensor_tensor, out=sm[:, ca + 2:ca + 3], in0=sm[:, ca + 1:ca + 2], in1=mhalf[:, 0:1], op=ALU.pow),
                     reads=[Rsm, Rmh], pwrites=[Rsm])
                P.op("dve", I(nc.vector.scalar_tensor_tensor, out=h2f[:], in0=x1[s_][:], scalar=sm[:, ca + 2:ca + 3], in1=wn2[:],
                              op0=ALU.mult, op1=ALU.mult), reads=[Rx1[s_], Rsm, Rw], writes=[Rh2f])
                P.op("act", I(nc.scalar.copy, out=h2b[s_][:], in_=h2f[:]), reads=[Rh2f], writes=[Rh2b[s_]])
                P.dma("sp", I(nc.sync.dma_start, out=H2[t0:t0 + 128, :], in_=h2b[s_][:]), Rh2b[s_], reads=[Rh2b[s_]],
                      pwrites=[RH2])
            def Db(i):
                P.tag = "Db"
                s_ = i % 2
                h2f = h2f2[s_]
                Rh2f = Rh2f2[s_]
                Rsm = RsmB
                for kc in range(8):
                    P.op("pe", I(nc.tensor.transpose, out=Tf[:, kc * 128:(kc + 1) * 128],
                                 in_=h2f[:, kc * 128:(kc + 1) * 128], identity=identf[:]),
                         reads=[Rh2f, Rid], writes=[RTf] if kc == 0 else (), pwrites=[RTf] if kc else ())
                P.op("dve", I(nc.vector.tensor_copy, out=h2T[:], in_=Tf), reads=[RTf], writes=[Rh2T])
                for kc in range(8):
                    P.op("pe", I(nc.tensor.matmul, lg, lhsT=h2T[:, kc * 128:(kc + 1) * 128], rhs=rw[:, kc, :],
                                 start=(kc == 0), stop=(kc == 7)), reads=[Rh2T, Rw],
                         writes=[Rlg] if kc == 0 else (), pwrites=[Rlg] if kc else ())
                P.op("dve", I(nc.vector.tensor_reduce, out=sm[:, 8:9], in_=lg, axis=AX.X, op=ALU.max), reads=[Rlg],
                     pwrites=[Rsm])
                P.op("dve", I(nc.vector.tensor_scalar, out=sm[:, 9:10], in0=sm[:, 8:9], scalar1=-1.0, scalar2=None,
                              op0=ALU.mult), reads=[Rsm], pwrites=[Rsm])
                P.op("act", I(nc.scalar.activation, out=sm[:, 16:32], in_=lg, func=AF.Exp, bias=sm[:, 9:10],
                              accum_out=sm[:, 10:11]), reads=[Rlg, Rsm], pwrites=[Rsm])
                P.op("dve", I(nc.vector.reciprocal, out=sm[:, 11:12], in_=sm[:, 10:11]), reads=[Rsm], pwrites=[Rsm])
                P.op("dve", I(nc.vector.tensor_scalar, out=aff[:, i, :], in0=sm[:, 16:32], scalar1=sm[:, 11:12],
                              scalar2=None, op0=ALU.mult), reads=[Rsm], pwrites=[Raff])
            Da(0)
            for i in range(NT):
                if i + 1 < NT:
                    Da(i + 1)
                Db(i)
            P.barrier()
            P.emit()
        if upto < "E":
            return
        with ExitStack() as es2:
            affT = es2.enter_context(sb("e_affT", [NE, T], F32))
            mk = es2.enter_context(sb("e_mk", [NE, T], F32))
            cs = es2.enter_context(sb("e_cs", [NE, T], F32))
            on = es2.enter_context(sb("e_on", [NE, T], F32))
            bs = es2.enter_context(sb("e_bs", [NE, 8], F32))
            ps = es2.enter_context(nc.psum_tensor("e_ps", [128, 4 * 512], F32))
            RaT, Rmk, Rcs, Ron, Rbs, Rps = [R(n) for n in "eaT emk ecs eon ebs eps".split()]
            P.op("pool", I(nc.gpsimd.memset, on[:], 1.0), writes=[Ron])
            P.op("pool", I(nc.gpsimd.memset, bs[:], 0.0), writes=[Rbs])
            for half in range(2):
                for j in range(16):
                    i = half * 16 + j
                    P.op("pe", I(nc.tensor.transpose, out=ps[0:NE, j * 128:(j + 1) * 128], in_=aff[:, i, :],
                                 identity=identf[:]), reads=[Raff, Rid], writes=[Rps] if j == 0 else (),
                         pwrites=[Rps] if j else ())
                P.op("act", I(nc.scalar.copy, out=affT[:, half * 2048:(half + 1) * 2048], in_=ps[0:NE, 0:2048]),
                     reads=[Rps], pwrites=[RaT])
            lo, mid, cntc, gw = bs[:, 0:1], bs[:, 1:2], bs[:, 2:3], bs[:, 3:4]
            for it in range(28):
                w = 2.0 ** (-(it + 1))
                P.op("dve", I(nc.vector.tensor_scalar, out=mid, in0=lo, scalar1=w, scalar2=None, op0=ALU.add),
                     reads=[Rbs], pwrites=[Rbs])
                P.op("dve", I(nc.vector.tensor_scalar, out=mk[:], in0=affT[:], scalar1=mid, scalar2=0.0, op0=ALU.is_gt,
                              op1=ALU.add, accum_out=cntc), reads=[RaT, Rbs], writes=[Rmk], pwrites=[Rbs])
                P.op("dve", I(nc.vector.tensor_scalar, out=gw, in0=cntc, scalar1=CAP - 0.5, scalar2=w, op0=ALU.is_gt,
                              op1=ALU.mult), reads=[Rbs], pwrites=[Rbs])
                P.op("dve", I(nc.vector.tensor_tensor, out=lo, in0=lo, in1=gw, op=ALU.add), reads=[Rbs],
                     pwrites=[Rbs])
            P.op("dve", I(nc.vector.tensor_scalar, out=mk[:], in0=affT[:], scalar1=lo, scalar2=None, op0=ALU.is_gt),
                 reads=[RaT, Rbs], writes=[Rmk])
            P.op("dve", I(nc.vector.tensor_tensor_scan, out=cs[:], data0=on[:], data1=mk[:], initial=0.0, op0=ALU.mult,
                          op1=ALU.add), reads=[Ron, Rmk], writes=[Rcs])
            P.op("dve", I(nc.vector.tensor_tensor, out=cs[:], in0=cs[:], in1=mk[:], op=ALU.mult), reads=[Rcs, Rmk],
                 writes=[Rcs])
            for i in range(NT):
                P.op("pe", I(nc.tensor.transpose, out=ps[:, i * NE:(i + 1) * NE], in_=cs[:, i * 128:(i + 1) * 128],
                             identity=identf[0:NE, 0:NE]), reads=[Rcs, Rid], writes=[Rps] if i == 0 else (),
                     pwrites=[Rps] if i else ())
            P.op("act", I(nc.scalar.copy, out=posm[:].rearrange("p a b -> p (a b)"), in_=ps[:, 0:NT * NE]),
                 reads=[Rps], writes=[Rposm])
            P.barrier()
            P.emit()
        with ExitStack() as es2:
            wsl.append(es2.enter_context(sb("e_w3", [128, 16384], BF16)))
            iota = es2.enter_context(sb("e_iota", [128, CAP], F32))
            tokpi = es2.enter_context(sb("e_tokpi", [128, NT, 4], BF16))
            sel = [es2.enter_context(sb("e_sel%d" % k, [128, CAP], BF16)) for k in range(3)]
            Rsel = [R("esel%d" % k) for k in range(3)]
            idxrow = es2.enter_context(sb("e_idxrow", [4, CAP], F32))
            ic = es2.enter_context(sb("e_ic", [128, 4, 4], F32))
            idf = es2.enter_context(sb("e_idf", [128, 4], F32))
            idx = [[es2.enter_context(sb("e_idx%d%d" % (k, c), [128, 1], I32)) for c in range(4)] for k in range(2)]
            gate = [es2.enter_context(sb("e_gate%d" % k, [128, 4], F32)) for k in range(2)]
            xe = [es2.enter_context(sb("e_xe%d" % k, [128, 1024], BF16)) for k in range(4)]
            Rxe = [R("exe%d" % k) for k in range(4)]
            xeT = es2.enter_context(sb("e_xeT", [128, 8, CAP], BF16))
            hT = es2.enter_context(sb("e_hT", [128, 16, CAP], BF16))
            sg = [es2.enter_context(sb("e_sg%d" % k, [128, CAP], F32)) for k in range(2)]
            Rsg = [R("esg0"), R("esg1")]
            ye = [es2.enter_context(sb("e_ye%d" % k, [128, 1024], F32)) for k in range(2)]
            Rye = [R("eye0"), R("eye1")]
            psF = es2.enter_context(nc.psum_tensor("e_psF", [128, 7 * 512], F32))
            psB = es2.enter_context(nc.psum_tensor("e_psB", [128, 1024], BF16))
            gub = [(psF[:, 0:512], psF[:, 512:1024]), (psF[:, 1024:1536], psF[:, 1536:2048])]
            Rgu = [R("egu0"), R("egu1")]
            dbk = [psF[:, 2048:2560], psF[:, 2560:3072]]
            Rdb = [R("edb0"), R("edb1")]
            ipb = psF[:, 3072:3584]
            Ripb, RTb = R("eipb"), R("eTb")
            Rtok, Ridr, Ric, Ridx, RxeT, RhT, Rsc, Rc = [R(n) for n in "etok eidr eic eidx exeT ehT esc ec".split()]
            Ridxs = [R("eidx0"), R("eidx1")]
            P.dma("sp", I(nc.sync.dma_start, out=iota[:], in_=iota_d.partition_broadcast(128)), Rc, pwrites=[Rc])
            P.dma("pool", I(nc.gpsimd.dma_start, out=tokpi[:, :, 0:2], in_=tokpi_d[:, :, :]), Rc, pwrites=[Rtok])

            def route_tok(e):
                P.op("dve", I(nc.vector.tensor_copy, out=tokpi[:, :, 2], in_=aff[:, :, e]), reads=[Raff],
                     writes=[Rtok])
                P.op("dve", I(nc.vector.tensor_tensor, out=tokpi[:, :, 3], in0=aff[:, :, e], in1=tokpi[:, :, 2],
                              op=ALU.subtract), reads=[Raff, Rtok], pwrites=[Rtok])

            def route_sel(e, i):
                r3 = (e * NT + i) % 3
                P.op("dve", I(nc.vector.tensor_scalar, out=sel[r3][:], in0=iota[:], scalar1=posm[:, i, e:e + 1],
                              scalar2=None, op0=ALU.is_equal), reads=[Rc, Rposm], writes=[Rsel[r3]])
                P.op("pe", I(nc.tensor.matmul, ipb[0:4, :], lhsT=tokpi[:, i, :], rhs=sel[r3][:], start=(i == 0),
                             stop=(i == NT - 1)), reads=[Rtok, Rsel[r3]], writes=[Ripb] if i == 0 else (),
                     pwrites=[Ripb] if i else ())

            def route_idx(e):
                es_ = e % 2
                P.op("act", I(nc.scalar.copy, out=idxrow[:], in_=ipb[0:4, :]), reads=[Ripb], writes=[Ridr])
                for cc in range(4):
                    P.op("pe", I(nc.tensor.transpose, out=ipb[:, cc * 4:(cc + 1) * 4],
                                 in_=idxrow[0:4, cc * 128:(cc + 1) * 128], identity=identf[0:4, 0:4]),
                         reads=[Ridr, Rid], writes=[Ripb] if cc == 0 else (), pwrites=[Ripb] if cc else ())
                P.op("dve", I(nc.vector.tensor_copy, out=ic[:].rearrange("p a b -> p (a b)"), in_=ipb[:, 0:16]),
                     reads=[Ripb], writes=[Ric])
                P.op("dve", I(nc.vector.scalar_tensor_tensor, out=idf[:], in0=ic[:, :, 1], scalar=128.0,
                              in1=ic[:, :, 0], op0=ALU.mult, op1=ALU.add), reads=[Ric], writes=[Ridx])
                P.op("dve", I(nc.vector.tensor_tensor, out=gate[es_][:], in0=ic[:, :, 2], in1=ic[:, :, 3], op=ALU.add),
                     reads=[Ric], writes=[Ridxs[es_]])
                for cc in range(4):
                    P.op("dve", I(nc.vector.tensor_copy, out=idx[es_][cc][:], in_=idf[:, cc:cc + 1]), reads=[Ridx],
                         pwrites=[Ridxs[es_]])
                for cc in range(4):
                    P.dma("pool", I(nc.gpsimd.indirect_dma_start, out=xe[cc][:], out_offset=None, in_=H2[:, :],
                                    in_offset=bass.IndirectOffsetOnAxis(ap=idx[es_][cc][:, :], axis=0)), Rxe[cc],
                          reads=[Ridxs[es_]], writes=[Rxe[cc]])

            def route_T(e):
                for kc in range(8):
                    for cc in range(4):
                        P.op("pe", I(nc.tensor.transpose, out=psB[:, cc * 128:(cc + 1) * 128],
                                     in_=xe[cc][:, kc * 128:(kc + 1) * 128], identity=identb[:]),
                             reads=[Rxe[cc], Rid], writes=[RTb] if cc == 0 else (), pwrites=[RTb] if cc else ())
                    if kc % 2 == 0:
                        P.op("act", I(nc.scalar.copy, out=xeT[:, kc, :], in_=psB[:, 0:512]), reads=[RTb],
                             writes=[RxeT] if kc == 0 else (), pwrites=[RxeT] if kc else ())
                    else:
                        P.op("dve", I(nc.vector.tensor_copy, out=xeT[:, kc, :], in_=psB[:, 0:512]), reads=[RTb],
                             pwrites=[RxeT])

            route_tok(0)
            for i in range(NT):
                route_sel(0, i)
            route_idx(0)
            route_T(0)
            gstep = 0
            dstep = 0
            for e in range(NE):
                es_ = e % 2
                nxt = e + 1 < NE
                wgt = wsl[(3 * e) % 4][:].rearrange("p (kc f) -> p kc f", kc=8)
                wut = wsl[(3 * e + 1) % 4][:].rearrange("p (kc f) -> p kc f", kc=8)
                wdt = wsl[(3 * e + 2) % 4][:].rearrange("p (j c) -> p j c", j=16)
                Rwg, Rwu, Rwd = Rws[(3 * e) % 4], Rws[(3 * e + 1) % 4], Rws[(3 * e + 2) % 4]
                if nxt:
                    wload(e + 1, (0,))
                    route_tok(e + 1)
                for j in range(16):
                    gs = gstep % 2
                    gstep += 1
                    gb_, ub_ = gub[gs]
                    for kc in range(8):
                        P.op("pe", I(nc.tensor.matmul, gb_, lhsT=wgt[:, kc, j * 128:(j + 1) * 128], rhs=xeT[:, kc, :],
                                     start=(kc == 0), stop=(kc == 7)), reads=[Rwg, RxeT],
                             writes=[Rgu[gs]] if kc == 0 else (), pwrites=[Rgu[gs]] if kc else ())
                    for kc in range(8):
                        P.op("pe", I(nc.tensor.matmul, ub_, lhsT=wut[:, kc, j * 128:(j + 1) * 128], rhs=xeT[:, kc, :],
                                     start=(kc == 0), stop=(kc == 7)), reads=[Rwu, RxeT], pwrites=[Rgu[gs]])
                    if nxt:
                        route_sel(e + 1, 2 * j)
                        route_sel(e + 1, 2 * j + 1)
                    P.op("act", I(nc.scalar.activation, out=sg[gs][:], in_=gb_, func=AF.Tanh, scale=0.5),
                         reads=[Rgu[gs]], writes=[Rsg[gs]])
                    P.op("dve", I(nc.vector.scalar_tensor_tensor, out=sg[gs][:], in0=sg[gs][:], scalar=1.0, in1=gb_,
                                  op0=ALU.add, op1=ALU.mult), reads=[Rsg[gs], Rgu[gs]], writes=[Rsg[gs]])
                    P.op("dve", I(nc.vector.scalar_tensor_tensor, out=hT[:, j, :], in0=sg[gs][:], scalar=0.5, in1=ub_,
                                  op0=ALU.mult, op1=ALU.mult), reads=[Rsg[gs], Rgu[gs]],
                         writes=[RhT] if j == 0 else (), pwrites=[RhT] if j else ())
                if nxt:
                    route_idx(e + 1)
                    wload(e + 1, (1,))
                for cc in range(4):
                    ys = cc % 2
                    for half in range(2):
                        ds = dstep % 2
                        dstep += 1
                        for j in range(16):
                            P.op("pe", I(nc.tensor.matmul, dbk[ds], lhsT=hT[:, j, cc * 128:(cc + 1) * 128],
                                         rhs=wdt[:, j, half * 512:(half + 1) * 512], start=(j == 0), stop=(j == 15)),
                                 reads=[RhT, Rwd], writes=[Rdb[ds]] if j == 0 else (), pwrites=[Rdb[ds]] if j else ())
                        if half == 0:
                            P.op("dve", I(nc.vector.tensor_scalar, out=ye[ys][:, 0:512], in0=dbk[ds],
                                          scalar1=gate[es_][:, cc:cc + 1], scalar2=None, op0=ALU.mult),
                                 reads=[Rdb[ds], Ridxs[es_]], writes=[Rye[ys]])
                        else:
                            P.op("dve", I(nc.vector.tensor_scalar, out=ye[ys][:, 512:1024], in0=dbk[ds],
                                          scalar1=gate[es_][:, cc:cc + 1], scalar2=None, op0=ALU.mult),
                                 reads=[Rdb[ds], Ridxs[es_]], pwrites=[Rye[ys]])
                    P.dma("pool", I(nc.gpsimd.indirect_dma_start, out=out_d[:, :],
                                    out_offset=bass.IndirectOffsetOnAxis(ap=idx[es_][cc][:, :], axis=0),
                                    in_=ye[ys][:], in_offset=None, compute_op=ALU.add), Rye[ys],
                          reads=[Rye[ys], Ridxs[es_], Rsc], pwrites=[Rsc])
                if nxt:
                    wload(e + 1, (2,))
                    route_T(e + 1)
            P.barrier()
            P.emit()


IN_COLS = (256, 256, 32, 1024, 1024, 1024, 1024, 1024)


def _win_perm():
    off = np.cumsum((0,) + IN_COLS)
    seg = [np.arange(off[k], off[k + 1]) for k in range(8)]
    return np.concatenate([seg[0], seg[1], seg[3], seg[4], seg[5], seg[6], seg[7], seg[2]])


def make_in_maps(inputs, cores):
    f = lambda a: np.ascontiguousarray(np.asarray(a))
    perm = _win_perm()
    bg = f(inputs["b_gate"])[0]
    shared = {
        "w_in": f(f(inputs["w_in"])[0][:, perm]),
        "b_gate": bg,
        "w_uq": f(inputs["mla_w_uq"])[0],
        "w_ukv": f(inputs["mla_w_ukv"])[0],
        "w_out": f(inputs["w_out"])[0],
        "router_w": f(inputs["router_w"])[0],
        "w_gate": f(inputs["expert_w_gate"])[0],
        "w_up": f(inputs["expert_w_up"])[0],
        "w_down": f(inputs["expert_w_down"])[0],
        "attn_norm_w": f(inputs["attn_norm_w"])[0],
        "q_norm_w": f(inputs["mla_q_norm_w"])[0],
        "kv_norm_w": f(inputs["mla_kv_norm_w"])[0],
        "q_hn": f(inputs["mla_q_hnorm_w"])[0],
        "k_hn": f(inputs["mla_k_hnorm_w"])[0],
        "dq_hn": f(inputs["diff_q_hnorm_w"])[0],
        "dk_hn": f(inputs["diff_k_hnorm_w"])[0],
        "lam": f(inputs["diff_lambda"])[0].reshape(-1),
        "subln": f(inputs["diff_subln_w"])[0],
        "ffn_norm_w": f(inputs["ffn_norm_w"])[0],
        "ident": np.eye(128, dtype=np.float32),
        "invf": (1.0 / (10000.0 ** (np.arange(0, 32, 2, dtype=np.float32) / 32.0))).astype(np.float32),
        "iota512": np.arange(1, 513, dtype=np.float32),
        "tokpi": np.ascontiguousarray(np.stack([np.broadcast_to(np.arange(128, dtype=np.float32)[:, None], (128, NT)),
                                                np.broadcast_to(np.arange(NT, dtype=np.float32)[None, :], (128, NT))],
                                               axis=-1)),
        "slopes": np.array([2.0 ** (-(i + 1)) for i in range(8)], dtype=np.float32),
    }
    x = f(inputs["x"])
    pos = f(inputs["positions"]).astype(np.int32)
    return [dict(shared, x=x[c], pos=pos[c], pos_t=np.ascontiguousarray(pos[c].reshape(NT, 128).T)) for c in cores]


_NC = {}


def classify(pos):
    pos = np.asarray(pos).astype(np.int64)
    n = pos.shape[0]
    q = pos.reshape(n, 16, 256)
    k = pos.reshape(n, NT, 128)
    qmin, qmax = q.min(-1)[:, :, None], q.max(-1)[:, :, None]
    kmin, kmax = k.min(-1)[:, None, :], k.max(-1)[:, None, :]
    below = (kmax <= qmin).all(0)
    above = (kmin >= qmax).all(0)
    cls = np.where(below, 1, np.where(above, 2, 0))
    dmin = np.maximum(np.maximum(qmin - kmax, kmin - qmax), 0).min(0).astype(np.float64)
    return cls, dmin


def kernel(**inputs):
    cls, dmin = classify(np.asarray(inputs["positions"]))
    gq = float(np.abs(np.asarray(inputs["diff_q_hnorm_w"])).max())
    gk = float(np.abs(np.asarray(inputs["diff_k_hnorm_w"])).max())
    skip_t = max(48.0, 2.0 * 8.0 * gq * gk + 25.0)
    key = (cls.tobytes(), dmin.tobytes(), skip_t)
    if key not in _NC:
        _NC[key] = build(CLS=cls, DMIN=dmin, skip_t=skip_t)
    nc = _NC[key]
    in_maps = make_in_maps(inputs, list(range(8)))
    res = run_bass_kernel_spmd(nc, in_maps, core_ids=list(range(8)))
    return np.stack([r["out"] for r in res.results], axis=0).astype(np.float32)
```

```python
import math
from functools import partial as I
from contextlib import ExitStack
import numpy as np
import concourse.bass as bass
import concourse.mybir as mybir
from concourse.bass_utils import run_bass_kernel_spmd

F32 = mybir.dt.float32
BF16 = mybir.dt.bfloat16
I32 = mybir.dt.int32
AF = mybir.ActivationFunctionType
ALU = mybir.AluOpType
AX = mybir.AxisListType

T = 4096
D = 1024
NT = T // 128
EPS = 1e-6
LAM_INIT = 0.8 - 0.6 * math.exp(-0.3 * 0)
NE = 16
CAP = 512
FF = 2048
TWO_PI = 2.0 * math.pi


class R:
    __slots__ = ("name", "w", "r", "pr", "dsem", "dcnt")

    def __init__(self, name):
        self.name = name
        self.w = {}
        self.r = {}
        self.pr = {}
        self.dsem = None
        self.dcnt = 0


class Prog:
    ENG = ("pe", "act", "dve", "pool", "sp")

    def __init__(self, nc):
        self.nc = nc
        self.streams = {e: [] for e in self.ENG}
        self.nops = {e: 0 for e in self.ENG}
        self.known = {e: {} for e in self.ENG}
        self.signal = {e: set() for e in self.ENG}
        self.sigcount = {e: 0 for e in self.ENG}
        self.cnt = {e: {} for e in self.ENG}
        self.sems = {}
        self._semctx = []
        self.dstreams = []
        self.tag = ""
        for e in self.ENG:
            self.sems[("E", e)] = self._new_sem("sem_" + e)

    def _new_sem(self, name):
        ctx = self.nc.semaphore(name)
        s = ctx.__enter__()
        self._semctx.append(ctx)
        return s

    def close(self):
        for ctx in reversed(self._semctx):
            ctx.__exit__(None, None, None)

    def _wait(self, eng, key, val):
        k = self.known[eng]
        if k.get(key, -1) >= val:
            return
        k[key] = val
        if key[0] == "E":
            self.signal[key[1]].add(val)
        self.streams[eng].append(("wait", key, val))

    def _deps(self, eng, reads, writes, pwrites):
        me = ("E", eng)
        for res in reads:
            for key, val in res.w.items():
                if key == me and eng == "pe":
                    continue
                self._wait(eng, key, val)
        for res in writes:
            for key, val in list(res.w.items()) + list(res.r.items()):
                if key == me and eng == "pe":
                    continue
                self._wait(eng, key, val)
        for res in pwrites:
            for key, val in list(res.r.items()) + list(res.pr.items()):
                if key == me and eng == "pe":
                    continue
                self._wait(eng, key, val)

    def _mark(self, key, val, reads, writes, pwrites):
        for res in reads:
            if res.r.get(key, -1) < val:
                res.r[key] = val
        for res in writes:
            pr = dict(res.w)
            for k_, v_ in res.r.items():
                if pr.get(k_, -1) < v_:
                    pr[k_] = v_
            res.pr = pr
            res.w = {key: val}
            res.r = {}
        for res in pwrites:
            if res.w.get(key, -1) < val:
                res.w[key] = val

    def op(self, eng, fn, reads=(), writes=(), pwrites=()):
        self._deps(eng, reads, writes, pwrites)
        idx = self.nops[eng]
        self.nops[eng] += 1
        self.streams[eng].append(("op", fn, idx, self.tag))
        self._mark(("E", eng), idx, reads, writes, pwrites)

    def dma(self, eng, fn, stream, reads=(), writes=(), pwrites=()):
        self._deps(eng, reads, writes, pwrites)
        if stream.dsem is None:
            stream.dsem = {}
            stream.dcnt = {}
        key = ("D", id(stream), eng)
        if eng not in stream.dsem:
            stream.dsem[eng] = self._new_sem("d_%s_%s" % (stream.name, eng))
            stream.dcnt[eng] = 0
            self.sems[key] = stream.dsem[eng]
            self.dstreams.append((stream, eng))
        stream.dcnt[eng] += 1
        val = 16 * stream.dcnt[eng]
        self.streams[eng].append(("dma", fn, stream.dsem[eng]))
        self._mark(key, val, reads, writes, pwrites)

    def barrier(self):
        for e in self.ENG:
            for f in self.ENG:
                if f != e and f != "sp" and self.nops[f] > 0:
                    self._wait(e, ("E", f), self.nops[f] - 1)
            for s, q in self.dstreams:
                self._wait(e, ("D", id(s), q), 16 * s.dcnt[q])

    def emit(self):
        nc = self.nc
        for e in self.ENG:
            for idx in sorted(self.signal[e]):
                if idx not in self.cnt[e]:
                    self.sigcount[e] += 1
                    self.cnt[e][idx] = self.sigcount[e]
            self.signal[e] = set()
        streams = self.streams
        self.streams = {e: [] for e in self.ENG}

        def make(e):
            stream = streams[e]

            def body(engine):
                for ent in stream:
                    if ent[0] == "wait":
                        _, key, val = ent
                        v = self.cnt[key[1]][val] if key[0] == "E" else val
                        engine.wait_ge(self.sems[key], v)
                    elif ent[0] == "op":
                        _, fn, idx, tag = ent
                        ins = fn()
                        if tag:
                            ins.annotate(tag)
                        if idx in self.cnt[e]:
                            ins.then_inc(self.sems[("E", e)], 1)
                    else:
                        _, fn, sem = ent
                        try:
                            ins = fn()
                        except Exception:
                            print("DMA build failed:", fn.func.__name__, {k: str(v)[:200] for k, v in fn.keywords.items()})
                            raise
                        ins.then_inc(sem, 16)
            return body

        with nc.Block() as block:
            block.tensor(make("pe"))
            block.scalar(make("act"))
            block.vector(make("dve"))
            block.gpsimd(make("pool"))
            block.sync(make("sp"))


SKIP_T = 48.0


def build(dbg=False, upto="E", CLS=None, DMIN=None, skip_t=None):
    global SKIP_T
    if skip_t is not None:
        SKIP_T = float(skip_t)
    if CLS is None:
        CLS = np.zeros((16, NT), np.int64)
        DMIN = np.zeros((16, NT), np.float64)
    nc = bass.Bass("TRN2", target_bir_lowering=False)

    def din(name, shape, dt=F32):
        return nc.dram_tensor(name, list(shape), dt, kind="ExternalInput").ap()

    def dscr(name, shape, dt=BF16):
        return nc.dram_tensor(name, list(shape), dt, kind="ExternalOutput" if dbg else "Internal").ap()

    x_d = din("x", [T, D])
    pos_d = din("pos", [T], I32)
    post_d = din("pos_t", [128, NT], I32)
    win_d = din("w_in", [D, 5664])
    bg_d = din("b_gate", [2048])
    wuq_d = din("w_uq", [256, 1536])
    wukv_d = din("w_ukv", [256, 2048])
    wout_d = din("w_out", [D, D])
    rw_d = din("router_w", [D, NE])
    wg_d = din("w_gate", [NE, D, FF])
    wu_d = din("w_up", [NE, D, FF])
    wd_d = din("w_down", [NE, FF, D])
    vec_d = {n: din(n, [k]) for n, k in [("attn_norm_w", 1024), ("q_norm_w", 256), ("kv_norm_w", 256),
                                          ("q_hn", 96), ("k_hn", 96), ("dq_hn", 64), ("dk_hn", 64),
                                          ("lam", 256), ("subln", 128), ("ffn_norm_w", 1024)]}
    ident_d = din("ident", [128, 128])
    invf_d = din("invf", [16])
    iota_d = din("iota512", [512])
    slopes_d = din("slopes", [8])
    tokpi_d = din("tokpi", [128, NT, 2])
    out_d = nc.dram_tensor("out", [T, D], F32, kind="ExternalOutput").ap()

    QmT = dscr("QmT", [16, 96, T])
    KmT = dscr("KmT", [16, 96, T])
    Vm = dscr("Vm", [T, 1024])
    QdT = dscr("QdT", [8, 128, T])
    KdT = dscr("KdT", [8, 128, T])
    Vd = dscr("Vd", [T, 1024])
    G = dscr("G", [T, 2048])
    H2 = dscr("H2", [T, D])
    DBG = dbg
    dbg_idx = nc.dram_tensor("dbg_idx", [4, 128, 1], I32, kind="ExternalOutput").ap() if dbg else None
    dbg_gate = nc.dram_tensor("dbg_gate", [128, 4], F32, kind="ExternalOutput").ap() if dbg else None
    dbg_xe = nc.dram_tensor("dbg_xe", [128, 1024], BF16, kind="ExternalOutput").ap() if dbg else None
    dbg_ye = nc.dram_tensor("dbg_ye", [128, 1024], F32, kind="ExternalOutput").ap() if dbg else None
    dbg_posm = nc.dram_tensor("dbg_posm", [128, NT * NE], F32, kind="ExternalOutput").ap() if dbg else None
    dbg_aff = nc.dram_tensor("dbg_aff", [128, NT * NE], F32, kind="ExternalOutput").ap() if dbg else None
    dbg_hT = nc.dram_tensor("dbg_hT", [128, 16 * CAP], BF16, kind="ExternalOutput").ap() if dbg else None
    PQHL = dscr("PQHL", [8, 2, T])
    MG = dscr("MG", [T, D])
    RQmT, RKmT, RVm, RQdT, RKdT, RVd, RG, RH2, RMG, Rout = [R(n) for n in
                                                          "QmT KmT Vm QdT KdT Vd G H2 MG out".split()]

    P = Prog(nc)
    sb = nc.sbuf_tensor
    with ExitStack() as es1:
        identb = es1.enter_context(sb("identb", [128, 128], BF16))
        identf = es1.enter_context(sb("identf", [128, 128], F32))
        mhalf = es1.enter_context(sb("mhalf", [128, 64], F32))
        cosT = es1.enter_context(sb("cosT", [128, NT, 16], F32))
        sinT = es1.enter_context(sb("sinT", [128, NT, 16], F32))
        posf = es1.enter_context(sb("posf", [128, NT], F32))
        nlam = es1.enter_context(sb("nlam", [128, 2], F32))
        vq_hn = es1.enter_context(sb("vq_hn", [128, 96], F32))
        vk_hn = es1.enter_context(sb("vk_hn", [128, 96], F32))
        vdq_hn = es1.enter_context(sb("vdq_hn", [128, 64], F32))
        vdk_hn = es1.enter_context(sb("vdk_hn", [128, 64], F32))
        vsubln = es1.enter_context(sb("vsubln", [128, 128], F32))
        vqn = es1.enter_context(sb("vqn", [128, 256], F32))
        vkvn = es1.enter_context(sb("vkvn", [128, 256], F32))
        Rid, Rmh, Rcs, Rposf, Rnlam, Rvec = [R(n) for n in "id mh cs posf nlam vec".split()]

        with ExitStack() as es2:
            posi = es2.enter_context(sb("p0_posi", [128, NT], I32))
            invf = es2.enter_context(sb("p0_invf", [128, 16], F32))
            ang = es2.enter_context(sb("p0_ang", [128, NT, 16], F32))
            kf = es2.enter_context(sb("p0_kf", [128, NT, 16], F32))
            ki = es2.enter_context(sb("p0_ki", [128, NT, 16], I32))
            mm_ = es2.enter_context(sb("p0_m", [128, NT, 16], F32))
            lamt = es2.enter_context(sb("p0_lam", [128, 256], F32))
            lp = es2.enter_context(sb("p0_lp", [128, 128], F32))
            ls = es2.enter_context(sb("p0_ls", [128, 4], F32))
            Rt = R("p0tmp")
            P.dma("sp", I(nc.sync.dma_start, out=identf[:], in_=ident_d[:, :]), Rid, pwrites=[Rid])
            P.dma("pool", I(nc.gpsimd.dma_start, out=identb[:], in_=ident_d[:, :]), Rid, pwrites=[Rid])
            P.op("pool", I(nc.gpsimd.memset, mhalf[:], -0.5), writes=[Rmh])
            for tl, nm in [(vq_hn, "q_hn"), (vk_hn, "k_hn"), (vdq_hn, "dq_hn"), (vdk_hn, "dk_hn"),
                           (vsubln, "subln"), (vqn, "q_norm_w"), (vkvn, "kv_norm_w"), (lamt, "lam")]:
                P.dma("sp", I(nc.sync.dma_start, out=tl[:], in_=vec_d[nm].partition_broadcast(128)), Rvec,
                      pwrites=[Rvec])
            P.dma("sp", I(nc.sync.dma_start, out=invf[:], in_=invf_d.partition_broadcast(128)), Rvec, pwrites=[Rvec])
            P.dma("sp", I(nc.sync.dma_start, out=posi[:], in_=post_d[:, :]), Rvec,
                  pwrites=[Rvec])
            P.op("dve", I(nc.vector.tensor_scalar, out=vq_hn[:], in0=vq_hn[:], scalar1=96.0 ** -0.5, scalar2=None,
                          op0=ALU.mult), reads=[Rvec], pwrites=[Rvec])
            P.op("dve", I(nc.vector.tensor_scalar, out=vdq_hn[:], in0=vdq_hn[:], scalar1=64.0 ** -0.5, scalar2=None,
                          op0=ALU.mult), reads=[Rvec], pwrites=[Rvec])
            P.op("dve", I(nc.vector.tensor_scalar, out=vsubln[:], in0=vsubln[:], scalar1=1.0 - LAM_INIT, scalar2=None,
                          op0=ALU.mult), reads=[Rvec], pwrites=[Rvec])
            P.op("dve", I(nc.vector.tensor_tensor, out=lp[:].rearrange("p (a b) -> p a b", a=2),
                          in0=lamt[:].rearrange("p (a two b) -> p a two b", a=2, two=2)[:, :, 0, :],
                          in1=lamt[:].rearrange("p (a two b) -> p a two b", a=2, two=2)[:, :, 1, :], op=ALU.mult),
                 reads=[Rvec], writes=[Rt])
            P.op("dve", I(nc.vector.tensor_reduce, out=ls[:, 0:2], in_=lp[:].rearrange("p (a b) -> p a b", a=2),
                          axis=AX.X, op=ALU.add), reads=[Rt], writes=[Rnlam])
            P.op("act", I(nc.scalar.activation, out=ls[:, 2:4], in_=ls[:, 0:2], func=AF.Exp), reads=[Rnlam],
                 writes=[Rt])
            P.op("dve", I(nc.vector.tensor_tensor, out=nlam[:, 0:1], in0=ls[:, 3:4], in1=ls[:, 2:3], op=ALU.subtract),
                 reads=[Rt], writes=[Rnlam])
            P.op("dve", I(nc.vector.tensor_scalar, out=nlam[:, 0:1], in0=nlam[:, 0:1], scalar1=-LAM_INIT, scalar2=None,
                          op0=ALU.add), reads=[Rnlam], writes=[Rnlam])
            P.op("dve", I(nc.vector.tensor_copy, out=posf[:], in_=posi[:]), reads=[Rvec], writes=[Rposf])
            P.op("dve", I(nc.vector.tensor_tensor, out=ang[:], in0=posf[:].unsqueeze(2).to_broadcast([128, NT, 16]),
                          in1=invf[:].unsqueeze(1).to_broadcast([128, NT, 16]), op=ALU.mult),
                 reads=[Rposf, Rvec], writes=[Rt])

            Rk = R("rk")

            def reduce_sin(dst, shift):
                P.op("dve", I(nc.vector.tensor_scalar, out=kf[:], in0=ang[:], scalar1=1.0 / TWO_PI,
                              scalar2=0.5 + shift / TWO_PI, op0=ALU.mult, op1=ALU.add), reads=[Rt], writes=[Rk])
                P.op("dve", I(nc.vector.tensor_copy, out=ki[:], in_=kf[:]), reads=[Rk], writes=[Rk])
                P.op("dve", I(nc.vector.tensor_copy, out=kf[:], in_=ki[:]), reads=[Rk], writes=[Rk])
                c1 = 6.28125
                c2 = TWO_PI - c1
                P.op("dve", I(nc.vector.scalar_tensor_tensor, out=mm_[:], in0=kf[:], scalar=-c1, in1=ang[:],
                              op0=ALU.mult, op1=ALU.add), reads=[Rk, Rt], writes=[Rk])
                P.op("dve", I(nc.vector.scalar_tensor_tensor, out=mm_[:], in0=kf[:], scalar=-c2, in1=mm_[:],
                              op0=ALU.mult, op1=ALU.add), reads=[Rk], writes=[Rk])
                if shift:
                    P.op("dve", I(nc.vector.tensor_scalar, out=mm_[:], in0=mm_[:], scalar1=shift, scalar2=None,
                                  op0=ALU.add), reads=[Rk], writes=[Rk])
                for thr, op_, adj in [(math.pi, ALU.is_gt, -TWO_PI), (-math.pi, ALU.is_lt, TWO_PI)]:
                    P.op("dve", I(nc.vector.tensor_scalar, out=kf[:], in0=mm_[:], scalar1=thr, scalar2=adj,
                                  op0=op_, op1=ALU.mult), reads=[Rk], writes=[Rk])
                    P.op("dve", I(nc.vector.tensor_tensor, out=mm_[:], in0=mm_[:], in1=kf[:], op=ALU.add),
                         reads=[Rk], writes=[Rk])
                P.op("dve", I(nc.vector.tensor_scalar, out=mm_[:], in0=mm_[:], scalar1=math.pi, scalar2=-math.pi,
                              op0=ALU.min, op1=ALU.max), reads=[Rk], writes=[Rk])
                P.op("act", I(nc.scalar.activation, out=dst[:], in_=mm_[:], func=AF.Sin), reads=[Rk], pwrites=[Rcs])

            reduce_sin(sinT, 0.0)
            reduce_sin(cosT, math.pi / 2)
            P.barrier()
            P.emit()

        env = locals()
        if upto >= "A":
            phaseA(nc, P, env)
        if upto >= "B":
            phaseBC(nc, P, env, upto)
        if upto >= "D":
            phaseDE(nc, P, env, upto)
        P.barrier()
        P.emit()
    P.close()
    return nc


def phaseA(nc, P, env):
    g = env
    sb = nc.sbuf_tensor
    x_d, win_d, bg_d, wuq_d, wukv_d, vec_d = g["x_d"], g["win_d"], g["bg_d"], g["wuq_d"], g["wukv_d"], g["vec_d"]
    identb, mhalf, cosT, sinT = g["identb"], g["mhalf"], g["cosT"], g["sinT"]
    vq_hn, vk_hn, vdq_hn, vdk_hn, vqn, vkvn = g["vq_hn"], g["vk_hn"], g["vdq_hn"], g["vdk_hn"], g["vqn"], g["vkvn"]
    Rid, Rmh, Rcs, Rvec = g["Rid"], g["Rmh"], g["Rcs"], g["Rvec"]
    QmT, KmT, Vm, QdT, KdT, Vd, G = g["QmT"], g["KmT"], g["Vm"], g["QdT"], g["KdT"], g["Vd"], g["G"]
    RQmT, RKmT, RVm, RQdT, RKdT, RVd, RG = g["RQmT"], g["RKmT"], g["RVm"], g["RQdT"], g["RKdT"], g["RVd"], g["RG"]
    with ExitStack() as es3:
        win = es3.enter_context(sb("a_win", [128, 8, 5664], BF16))
        wuq = es3.enter_context(sb("a_wuq", [128, 2, 1536], BF16))
        wukv = es3.enter_context(sb("a_wukv", [128, 2, 2048], BF16))
        wn = es3.enter_context(sb("a_wn", [128, 1024], F32))
        biasr = es3.enter_context(sb("a_bias", [1, 2048], BF16))
        onesr = es3.enter_context(sb("a_ones", [1, 128], BF16))
        xt0 = es3.enter_context(sb("a_xt0", [128, 1024], F32))
        xt1 = es3.enter_context(sb("a_xt1", [128, 1024], F32))
        hb = es3.enter_context(sb("a_hb", [128, 1024], BF16))
        hT = es3.enter_context(sb("a_hT", [128, 1024], BF16))
        sq = es3.enter_context(sb("a_sq", [128, 2048], F32))
        st = es3.enter_context(sb("a_st", [128, 64], F32))
        rstd = es3.enter_context(sb("a_rstd", [128, 64], F32))
        lat = es3.enter_context(sb("a_lat", [128, 512], BF16))
        latT = es3.enter_context(sb("a_latT", [128, 4, 128], BF16))
        qf = es3.enter_context(sb("a_qf", [128, 16, 96], F32))
        qb = es3.enter_context(sb("a_qb", [128, 16, 96], BF16))
        kf = es3.enter_context(sb("a_kf", [128, 16, 32], F32))
        kb = es3.enter_context(sb("a_kb", [128, 16, 96], BF16))
        rt = es3.enter_context(sb("a_rt", [128, 4, 16, 16], F32))
        vb = es3.enter_context(sb("a_vb", [128, 1024], BF16))
        qTs = es3.enter_context(sb("a_qTs", [128, 16, 128], BF16))
        kTs = es3.enter_context(sb("a_kTs", [128, 16, 128], BF16))
        df = es3.enter_context(sb("a_df", [128, 512], F32))
        dqb = es3.enter_context(sb("a_dqb", [128, 1024], BF16))
        dkb = es3.enter_context(sb("a_dkb", [128, 1024], BF16))
        dvb = es3.enter_context(sb("a_dvb", [128, 1024], BF16))
        dqTs = es3.enter_context(sb("a_dqTs", [128, 8, 128], BF16))
        dkTs = es3.enter_context(sb("a_dkTs", [128, 8, 128], BF16))
        gt = es3.enter_context(sb("a_gt", [128, 512], BF16))
        gb = es3.enter_context(sb("a_gb", [128, 2048], BF16))
        kr = es3.enter_context(sb("a_kr", [128, 32], F32))
        krw = es3.enter_context(sb("a_krw", [128, 32], F32))
        psF = es3.enter_context(nc.psum_tensor("a_psF", [128, 6 * 512], F32))
        psB = es3.enter_context(nc.psum_tensor("a_psB", [128, 2 * 1024], BF16))
        Rw = R("aw")
        P.dma("pool", I(nc.gpsimd.dma_start, out=win[:], in_=win_d.rearrange("(kc p) c -> p kc c", p=128)), Rw,
              pwrites=[Rw])
        P.dma("pool", I(nc.gpsimd.dma_start, out=wuq[:], in_=wuq_d.rearrange("(kc p) c -> p kc c", p=128)), Rw,
              pwrites=[Rw])
        P.dma("pool", I(nc.gpsimd.dma_start, out=wukv[:], in_=wukv_d.rearrange("(kc p) c -> p kc c", p=128)), Rw,
              pwrites=[Rw])
        P.dma("pool", I(nc.gpsimd.dma_start, out=biasr[:], in_=bg_d.rearrange("(o c) -> o c", o=1)), Rw, pwrites=[Rw])
        P.dma("sp", I(nc.sync.dma_start, out=wn[:], in_=vec_d["attn_norm_w"].partition_broadcast(128)), Rw,
              pwrites=[Rw])
        P.op("pool", I(nc.gpsimd.memset, onesr[:], 1.0), pwrites=[Rw])

        xts = [xt0, xt1]
        Rxt = [R("xt0"), R("xt1")]
        Rz = [R("z0"), R("z1")]
        zb = [psF[:, 0:512], psF[:, 512:1024]]
        ps4 = psF[:, 1024:3072]
        Rps4 = R("ps4")
        Tb = [psB[:, 0:1024], psB[:, 1024:2048]]
        RT = [R("T0"), R("T1")]
        names = "hb hT sq stx stl stq stk stkr std lat latT qf qb kf kb rt vb qTs kTs df dqb dkb dvb dqTs dkTs gt gb kr krw"
        RR = {n: R(n) for n in names.split()}

        def rs(src, dst, n, Rs, w):
            P.op("dve", I(nc.vector.tensor_scalar, out=dst, in0=src, scalar1=1.0 / n, scalar2=EPS, op0=ALU.mult,
                          op1=ALU.add), reads=[Rs], writes=[Rs])
            P.op("pool", I(nc.gpsimd.tensor_tensor, out=dst, in0=dst, in1=mhalf[:, 0:w], op=ALU.pow),
                 reads=[Rs, Rmh], writes=[Rs])

        def zblock(i, blk):
            P.tag = "z%d" % blk
            bank = zb[blk % 2]
            Rb = Rz[blk % 2]
            ncols = 512 if blk < 11 else 32
            c0 = blk * 512
            gate = 7 <= blk <= 10
            for kc in range(8):
                P.op("pe", I(nc.tensor.matmul, bank[:, 0:ncols], lhsT=hT[:, kc * 128:(kc + 1) * 128],
                             rhs=win[:, kc, c0:c0 + ncols], start=(kc == 0), stop=(kc == 7 and not gate)),
                     reads=[RR["hT"], Rw], writes=[Rb] if kc == 0 else (), pwrites=[Rb] if kc else ())
            if gate:
                gc = (blk - 7) * 512
                P.op("pe", I(nc.tensor.matmul, bank[:, 0:512], lhsT=onesr[0:1, :], rhs=biasr[0:1, gc:gc + 512],
                             start=False, stop=True), reads=[Rw], pwrites=[Rb])
            return bank, Rb

        def rope(i, src, dst, Rsrc, Rdst):
            c = cosT[:, i, :].unsqueeze(1).to_broadcast([128, 16, 16])
            s = sinT[:, i, :].unsqueeze(1).to_broadcast([128, 16, 16])
            x1 = src[:, :, 0:16]
            x2 = src[:, :, 16:32]
            Rrt = RR["rt"]
            P.op("dve", I(nc.vector.tensor_tensor, out=rt[:, 0], in0=x1, in1=c, op=ALU.mult), reads=[Rsrc, Rcs],
                 pwrites=[Rrt])
            P.op("dve", I(nc.vector.tensor_tensor, out=rt[:, 1], in0=x2, in1=s, op=ALU.mult), reads=[Rsrc, Rcs],
                 pwrites=[Rrt])
            P.op("dve", I(nc.vector.tensor_tensor, out=rt[:, 2], in0=x1, in1=s, op=ALU.mult), reads=[Rsrc, Rcs],
                 pwrites=[Rrt])
            P.op("dve", I(nc.vector.tensor_tensor, out=rt[:, 3], in0=x2, in1=c, op=ALU.mult), reads=[Rsrc, Rcs],
                 pwrites=[Rrt])
            P.op("dve", I(nc.vector.tensor_tensor, out=dst[:, :, 0:16], in0=rt[:, 0], in1=rt[:, 1], op=ALU.subtract),
                 reads=[Rrt], pwrites=[Rdst])
            P.op("dve", I(nc.vector.tensor_tensor, out=dst[:, :, 16:32], in0=rt[:, 2], in1=rt[:, 3], op=ALU.add),
                 reads=[Rrt], pwrites=[Rdst])

        latT2 = [latT, es3.enter_context(sb("a_latT1", [128, 4, 128], BF16))]
        kr2 = [kr, es3.enter_context(sb("a_kr1", [128, 32], F32))]
        krw2 = [krw, es3.enter_context(sb("a_krw1", [128, 32], F32))]
        qb2 = [qb, es3.enter_context(sb("a_qb1", [128, 16, 96], BF16))]
        kb2 = [kb, es3.enter_context(sb("a_kb1", [128, 16, 96], BF16))]
        sq2 = sq[:, 512:2048]
        RlatT = [R("latT0"), R("latT1")]
        Rkr = [R("kr0"), R("kr1")]
        Rkrw = [R("krw0"), R("krw1")]
        Rstkr = [R("stkr0"), R("stkr1")]
        Rqb = [R("qb0"), R("qb1")]
        Rkb = [R("kb0"), R("kb1")]
        Rsq2 = R("sq2")

        dfr = [df, es3.enter_context(sb("a_df1", [128, 512], F32)), es3.enter_context(sb("a_df2", [128, 512], F32))]
        Rdfr = [R("df0"), R("df1"), R("df2")]
        Rstd = [R("std0"), R("std1")]
        dfc = [0]

        def dqk(i, blks, dstb, hn, Rdst, dTs, RdTs, dram, Rdram):
            t0 = i * 128
            for j, blk in enumerate(blks):
                bank, Rb = zblock(i, blk)
                k3 = dfc[0] % 3
                dfc[0] += 1
                dfk, Rdfk = dfr[k3], Rdfr[k3]
                c0 = 40 + 8 * (dfc[0] % 2)
                P.op("act", I(nc.scalar.copy, out=dfk[:], in_=bank[:, 0:512]), reads=[Rb], writes=[Rdfk])
                P.op("act", I(nc.scalar.activation, out=sq[:, 0:512], in_=dfk[:], func=AF.Square),
                     reads=[Rdfk], writes=[RR["sq"]])
                P.op("dve", I(nc.vector.tensor_reduce, out=st[:, c0:c0 + 8],
                              in_=sq[:, 0:512].rearrange("p (a b) -> p a b", a=8), axis=AX.X, op=ALU.add),
                     reads=[RR["sq"]], writes=[Rstd[dfc[0] % 2]])
                rs(st[:, c0:c0 + 8], rstd[:, c0:c0 + 8], 64.0, Rstd[dfc[0] % 2], 8)
                P.op("dve", I(nc.vector.tensor_tensor, out=dfk[:].rearrange("p (a b) -> p a b", a=8),
                              in0=dfk[:].rearrange("p (a b) -> p a b", a=8),
                              in1=rstd[:, c0:c0 + 8].unsqueeze(2).to_broadcast([128, 8, 64]),
                              op=ALU.mult), reads=[Rdfk, Rstd[dfc[0] % 2]], writes=[Rdfk])
                P.op("dve", I(nc.vector.tensor_tensor,
                              out=dstb[:, j * 512:(j + 1) * 512].rearrange("p (a b) -> p a b", a=8),
                              in0=dfk[:].rearrange("p (a b) -> p a b", a=8),
                              in1=hn[:].unsqueeze(1).to_broadcast([128, 8, 64]), op=ALU.mult),
                     reads=[Rdfk, Rvec], writes=[Rdst] if j == 0 else (), pwrites=[Rdst] if j else ())

        def dqkT(i, dstb, Rdst, dTs, RdTs, dram, Rdram):
            P.tag = "dqkT"
            t0 = i * 128
            for h in range(8):
                P.op("pe", I(nc.tensor.transpose, out=Tb[0][:, h * 128:(h + 1) * 128],
                             in_=dstb[:, h * 128:(h + 1) * 128], identity=identb[:]),
                     reads=[Rdst, Rid], writes=[RT[0]] if h == 0 else (), pwrites=[RT[0]] if h else ())
            P.op("act", I(nc.scalar.copy, out=dTs[:].rearrange("p a b -> p (a b)"), in_=Tb[0][:, 0:1024]),
                 reads=[RT[0]], writes=[RdTs])
            P.dma("sp", I(nc.sync.dma_start, out=dram[:, :, t0:t0 + 128].rearrange("h d t -> d h t"), in_=dTs[:]),
                  RdTs, reads=[RdTs], pwrites=[Rdram])

        xts3 = [xt0, xt1, es3.enter_context(sb("a_xt2", [128, 1024], F32))]
        Rxt3 = [R("xt0"), R("xt1"), R("xt2")]
        hb2 = [hb, es3.enter_context(sb("a_hb1", [128, 1024], BF16))]
        Rhb = [R("hb0"), R("hb1")]
        Rstx = [R("stx0"), R("stx1")]

        def S1a(i):
            P.tag = "S1a"
            s_ = i % 2
            xt = xts3[i % 3]
            Rx = Rxt3[i % 3]
            t0 = i * 128
            c = 56 + 2 * s_
            P.dma("sp", I(nc.sync.dma_start, out=xt[:], in_=x_d[t0:t0 + 128, :]), Rx, writes=[Rx])
            P.op("act", I(nc.scalar.activation, out=sq[:, 0:1024], in_=xt[:], func=AF.Square,
                          accum_out=st[:, c:c + 1]), reads=[Rx], writes=[RR["sq"], Rstx[s_]])
            rs(st[:, c:c + 1], rstd[:, c:c + 1], 1024.0, Rstx[s_], 1)
            P.op("dve", I(nc.vector.scalar_tensor_tensor, out=hb2[s_][:], in0=xt[:], scalar=rstd[:, c:c + 1], in1=wn[:],
                          op0=ALU.mult, op1=ALU.mult), reads=[Rx, Rstx[s_], Rw], writes=[Rhb[s_]])

        def S1(i):
            P.tag = "S1head"
            s_ = i % 2
            t0 = i * 128
            for kc in range(8):
                P.op("pe", I(nc.tensor.transpose, out=Tb[0][:, kc * 128:(kc + 1) * 128],
                             in_=hb2[s_][:, kc * 128:(kc + 1) * 128], identity=identb[:]),
                     reads=[Rhb[s_], Rid], writes=[RT[0]] if kc == 0 else (), pwrites=[RT[0]] if kc else ())
            P.op("act", I(nc.scalar.copy, out=hT[:], in_=Tb[0][:, 0:1024]), reads=[RT[0]], writes=[RR["hT"]])
            bank, Rb = zblock(i, 0)
            k3 = dfc[0] % 3
            dfc[0] += 1
            latf, Rlatf = dfr[k3], Rdfr[k3]
            P.op("act", I(nc.scalar.copy, out=latf[:], in_=bank[:, 0:512]), reads=[Rb], writes=[Rlatf])
            P.op("act", I(nc.scalar.activation, out=sq[:, 0:512], in_=latf[:], func=AF.Square), reads=[Rlatf],
                 writes=[RR["sq"]])
            P.op("dve", I(nc.vector.tensor_reduce, out=st[:, 1:3], in_=sq[:, 0:512].rearrange("p (a b) -> p a b", a=2),
                          axis=AX.X, op=ALU.add), reads=[RR["sq"]], writes=[RR["stl"]])
            rs(st[:, 1:3], rstd[:, 1:3], 256.0, RR["stl"], 2)
            P.op("dve", I(nc.vector.scalar_tensor_tensor, out=lat[:, 0:256], in0=latf[:, 0:256], scalar=rstd[:, 1:2],
                          in1=vqn[:], op0=ALU.mult, op1=ALU.mult), reads=[Rlatf, RR["stl"], Rvec], writes=[RR["lat"]])
            P.op("dve", I(nc.vector.scalar_tensor_tensor, out=lat[:, 256:512], in0=latf[:, 256:512],
                          scalar=rstd[:, 2:3], in1=vkvn[:], op0=ALU.mult, op1=ALU.mult),
                 reads=[Rlatf, RR["stl"], Rvec], pwrites=[RR["lat"]])
            dqk(i, [1, 2], dqb, vdq_hn, RR["dqb"], dqTs, RR["dqTs"], QdT, RQdT)
            P.tag = "latT"
            for j in range(4):
                P.op("pe", I(nc.tensor.transpose, out=Tb[1][:, j * 128:(j + 1) * 128],
                             in_=lat[:, j * 128:(j + 1) * 128], identity=identb[:]),
                     reads=[RR["lat"], Rid], writes=[RT[1]] if j == 0 else (), pwrites=[RT[1]] if j else ())
            P.op("dve", I(nc.vector.tensor_copy, out=latT2[s_][:].rearrange("p a b -> p (a b)"), in_=Tb[1][:, 0:512]),
                 reads=[RT[1]], writes=[RlatT[s_]])
            dqk(i, [3, 4], dkb, vdk_hn, RR["dkb"], dkTs, RR["dkTs"], KdT, RKdT)
            for j, blk in enumerate([5, 6]):
                bank, Rb = zblock(i, blk)
                P.op("act", I(nc.scalar.copy, out=dvb[:, j * 512:(j + 1) * 512], in_=bank[:, 0:512]), reads=[Rb],
                     writes=[RR["dvb"]] if j == 0 else (), pwrites=[RR["dvb"]] if j else ())
            P.dma("sp", I(nc.sync.dma_start, out=Vd[t0:t0 + 128, :], in_=dvb[:]), RR["dvb"], reads=[RR["dvb"]],
                  pwrites=[RVd])
            dqkT(i, dqb, RR["dqb"], dqTs, RR["dqTs"], QdT, RQdT)
            for j, blk in enumerate([7, 8, 9, 10]):
                if j == 2:
                    dqkT(i, dkb, RR["dkb"], dkTs, RR["dkTs"], KdT, RKdT)
                bank, Rb = zblock(i, blk)
                P.op("act", I(nc.scalar.activation, out=gt[:], in_=bank[:, 0:512], func=AF.Tanh, scale=0.5),
                     reads=[Rb], writes=[RR["gt"]])
                P.op("dve", I(nc.vector.tensor_scalar, out=gb[:, j * 512:(j + 1) * 512], in0=gt[:], scalar1=0.5,
                              scalar2=0.5, op0=ALU.mult, op1=ALU.add), reads=[RR["gt"]],
                     writes=[RR["gb"]] if j == 0 else (), pwrites=[RR["gb"]] if j else ())
            P.dma("sp", I(nc.sync.dma_start, out=G[t0:t0 + 128, :], in_=gb[:]), RR["gb"], reads=[RR["gb"]],
                  pwrites=[RG])
            bank, Rb = zblock(i, 11)
            P.op("act", I(nc.scalar.copy, out=kr2[s_][:], in_=bank[:, 0:32]), reads=[Rb], writes=[Rkr[s_]])
            P.op("act", I(nc.scalar.activation, out=sq[:, 0:32], in_=kr2[s_][:], func=AF.Square,
                          accum_out=st[:, 36 + s_:37 + s_]), reads=[Rkr[s_]], writes=[RR["sq"], Rstkr[s_]])
            P.op("dve", I(nc.vector.tensor_tensor, out=krw2[s_][:], in0=kr2[s_][:], in1=vk_hn[:, 64:96], op=ALU.mult),
                 reads=[Rkr[s_], Rvec], writes=[Rkrw[s_]])

        def S2q(i):
            P.tag = "S2q"
            s_ = i % 2
            for b_ in range(4):
                for kc in range(2):
                    P.op("pe", I(nc.tensor.matmul, ps4[:, b_ * 512:b_ * 512 + 384], lhsT=latT2[s_][:, kc, :],
                                 rhs=wuq[:, kc, b_ * 384:(b_ + 1) * 384], start=(kc == 0), stop=(kc == 1)),
                         reads=[RlatT[s_], Rw], writes=[Rps4] if (b_ == 0 and kc == 0) else (),
                         pwrites=() if (b_ == 0 and kc == 0) else [Rps4])
            qv = ps4.rearrange("p (b x) -> p b x", b=4)[:, :, 0:384].rearrange("p b (h d) -> p b h d", h=4)
            sqv = sq2.rearrange("p (b h d) -> p b h d", b=4, h=4)
            qfv = qf[:].rearrange("p (b h) d -> p b h d", b=4)
            P.op("act", I(nc.scalar.copy, out=qfv, in_=qv), reads=[Rps4], writes=[RR["qf"]])
            P.op("act", I(nc.scalar.activation, out=sqv, in_=qfv, func=AF.Square), reads=[RR["qf"]], writes=[Rsq2])
            P.op("dve", I(nc.vector.tensor_reduce, out=st[:, 4:20].rearrange("p (b h) -> p b h", b=4), in_=sqv,
                          axis=AX.X, op=ALU.add), reads=[Rsq2], writes=[RR["stq"]])
            rs(st[:, 4:20], rstd[:, 4:20], 96.0, RR["stq"], 16)
            P.op("dve", I(nc.vector.tensor_tensor, out=qfv, in0=qfv,
                          in1=rstd[:, 4:20].rearrange("p (b h) -> p b h", b=4).unsqueeze(3).to_broadcast(
                              [128, 4, 4, 96]), op=ALU.mult), reads=[RR["qf"], RR["stq"]], writes=[RR["qf"]])
            P.op("dve", I(nc.vector.tensor_tensor, out=qf[:], in0=qf[:],
                          in1=vq_hn[:].unsqueeze(1).to_broadcast([128, 16, 96]), op=ALU.mult),
                 reads=[RR["qf"], Rvec], writes=[RR["qf"]])
            P.op("dve", I(nc.vector.tensor_copy, out=qb2[s_][:, :, 0:64], in_=qf[:, :, 0:64]), reads=[RR["qf"]],
                 writes=[Rqb[s_]])
            rope(i, qf[:, :, 64:96], qb2[s_][:, :, 64:96], RR["qf"], Rqb[s_])

        def S2k(i):
            P.tag = "S2k"
            s_ = i % 2
            t0 = i * 128
            for b_ in range(4):
                for kc in range(2):
                    P.op("pe", I(nc.tensor.matmul, ps4[:, b_ * 512:(b_ + 1) * 512], lhsT=latT2[s_][:, 2 + kc, :],
                                 rhs=wukv[:, kc, b_ * 512:(b_ + 1) * 512], start=(kc == 0), stop=(kc == 1)),
                         reads=[RlatT[s_], Rw], writes=[Rps4] if (b_ == 0 and kc == 0) else (),
                         pwrites=() if (b_ == 0 and kc == 0) else [Rps4])
            kvv = ps4.rearrange("p (h d) -> p h d", h=16)
            sqk = sq2[:, 0:1024].rearrange("p (h d) -> p h d", h=16)
            P.op("act", I(nc.scalar.activation, out=sqk, in_=kvv[:, :, 0:64], func=AF.Square), reads=[Rps4],
                 writes=[Rsq2])
            P.op("dve", I(nc.vector.tensor_reduce, out=st[:, 20:36], in_=sqk, axis=AX.X, op=ALU.add),
                 reads=[Rsq2], writes=[RR["stk"]])
            P.op("dve", I(nc.vector.tensor_scalar, out=st[:, 20:36], in0=st[:, 20:36], scalar1=st[:, 36 + s_:37 + s_],
                          scalar2=None, op0=ALU.add), reads=[RR["stk"], Rstkr[s_]], writes=[RR["stk"]])
            rs(st[:, 20:36], rstd[:, 20:36], 96.0, RR["stk"], 16)
            P.op("dve", I(nc.vector.tensor_tensor, out=qf[:, :, 0:64], in0=kvv[:, :, 0:64],
                          in1=rstd[:, 20:36].unsqueeze(2).to_broadcast([128, 16, 64]), op=ALU.mult),
                 reads=[Rps4, RR["stk"]], writes=[RR["qf"]])
            P.op("dve", I(nc.vector.tensor_tensor, out=kb2[s_][:, :, 0:64], in0=qf[:, :, 0:64],
                          in1=vk_hn[:, 0:64].unsqueeze(1).to_broadcast([128, 16, 64]), op=ALU.mult),
                 reads=[RR["qf"], Rvec], writes=[Rkb[s_]])
            P.op("dve", I(nc.vector.tensor_tensor, out=kf[:], in0=krw2[s_][:].unsqueeze(1).to_broadcast([128, 16, 32]),
                          in1=rstd[:, 20:36].unsqueeze(2).to_broadcast([128, 16, 32]), op=ALU.mult),
                 reads=[Rkrw[s_], RR["stk"]], writes=[RR["kf"]])
            rope(i, kf[:], kb2[s_][:, :, 64:96], RR["kf"], Rkb[s_])
            P.op("act", I(nc.scalar.copy, out=vb[:].rearrange("p (h d) -> p h d", h=16), in_=kvv[:, :, 64:128]),
                 reads=[Rps4], writes=[RR["vb"]])
            P.dma("sp", I(nc.sync.dma_start, out=Vm[t0:t0 + 128, :], in_=vb[:]), RR["vb"], reads=[RR["vb"]],
                  pwrites=[RVm])

        def S3(i):
            P.tag = "S3"
            s_ = i % 2
            t0 = i * 128
            for src, Rsrc, dst, Rdst, dram, Rdram in [(qb2[s_], Rqb[s_], qTs, RR["qTs"], QmT, RQmT),
                                                       (kb2[s_], Rkb[s_], kTs, RR["kTs"], KmT, RKmT)]:
                for half in range(2):
                    for hh in range(8):
                        h = half * 8 + hh
                        P.op("pe", I(nc.tensor.transpose, out=Tb[half][0:96, hh * 128:(hh + 1) * 128],
                                     in_=src[:, h, :], identity=identb[:]), reads=[Rsrc, Rid],
                             writes=[RT[half]] if hh == 0 else (), pwrites=[RT[half]] if hh else ())
                    eng = "act" if half == 0 else "dve"
                    fn = nc.scalar.copy if half == 0 else nc.vector.tensor_copy
                    P.op(eng, I(fn, out=dst[0:96, half * 8:(half + 1) * 8, :].rearrange("p a b -> p (a b)"),
                                in_=Tb[half][0:96, 0:1024]), reads=[RT[half]],
                         writes=[Rdst] if half == 0 else (), pwrites=[Rdst] if half else ())
                P.dma("sp", I(nc.sync.dma_start, out=dram[:, :, t0:t0 + 128].rearrange("h d t -> d h t"),
                              in_=dst[0:96, :, :]), Rdst, reads=[Rdst], pwrites=[Rdram])

        S1a(0)
        S1a(1)
        S1(0)
        for i in range(NT):
            if i + 1 < NT:
                S1(i + 1)
            if i >= 1:
                S3(i - 1)
            if i + 2 < NT:
                S1a(i + 2)
            S2q(i)
            S2k(i)
        S3(NT - 1)
        P.barrier()
        P.emit()


def skip_tile(h, q0, q1, k0, k1):
    return False


def phaseBC(nc, P, env, upto):
    g = env
    sb = nc.sbuf_tensor
    QmT, KmT, Vm, QdT, KdT, Vd, G, MG = [g[k] for k in "QmT KmT Vm QdT KdT Vd G MG".split()]
    RMG = g["RMG"]
    mhalf, nlam, vsubln, posf, pos_d = g["mhalf"], g["nlam"], g["vsubln"], g["posf"], g["pos_d"]
    Rmh, Rnlam, Rvec, Rposf = g["Rmh"], g["Rnlam"], g["Rvec"], g["Rposf"]
    with ExitStack() as es:
        omla = es.enter_context(sb("om", [128, NT, 1024], BF16))
        Rom = R("omla")
        with ExitStack() as es2:
            qT = [es2.enter_context(sb("b_qT%d" % k, [96, T], BF16)) for k in range(2)]
            kT = [es2.enter_context(sb("b_kT%d" % k, [96, T], BF16)) for k in range(2)]
            vv = [es2.enter_context(sb("b_v%d" % k, [128, NT, 65], BF16)) for k in range(2)]
            ga = [es2.enter_context(sb("b_ga%d" % k, [128, NT, 64], BF16)) for k in range(2)]
            pT = [es2.enter_context(sb("b_pT%d" % k, [128, 1536], BF16)) for k in range(3)]
            rc = es2.enter_context(sb("b_rc", [128, 8], F32))
            ps = es2.enter_context(nc.psum_tensor("b_ps", [128, 8 * 512], F32))
            Rq = [R("bq0"), R("bq1")]
            RpT = [R("pT0"), R("pT1"), R("pT2")]
            Rs = [R("bs0"), R("bs1")]
            Ro = [R("bo0"), R("bo1")]
            Rrc = R("brc")
            sbank = [ps[:, 1536 * k:1536 * (k + 1)] for k in range(2)]
            GRP = [(3 * g_, 3) for g_ in range(10)] + [(30, 2)]

            def obank(os_, qs):
                b0 = 3072 + 512 * os_ + 66 * qs
                return ps[:, b0:b0 + 65]
            for k in range(2):
                P.op("pool", I(nc.gpsimd.memset, vv[k][:, :, 64:65], 1.0), pwrites=[Rq[k]])
            step = 0
            for h in range(16):
                s_ = h % 2
                P.dma("sp", I(nc.sync.dma_start, out=qT[s_][:], in_=QmT[h, :, :]), Rq[s_], pwrites=[Rq[s_]])
                P.dma("sp", I(nc.sync.dma_start, out=kT[s_][:], in_=KmT[h, :, :]), Rq[s_], pwrites=[Rq[s_]])
                P.dma("sp", I(nc.sync.dma_start, out=vv[s_][:, :, 0:64],
                              in_=Vm[:, h * 64:(h + 1) * 64].rearrange("(i p) d -> p i d", p=128)), Rq[s_],
                      pwrites=[Rq[s_]])
                P.dma("sp", I(nc.sync.dma_start, out=ga[s_][:],
                              in_=G[:, h * 64:(h + 1) * 64].rearrange("(i p) d -> p i d", p=128)), Rq[s_],
                      pwrites=[Rq[s_]])
                steps = [(qb, kp) for qb in range(8) for kp in range(len(GRP))]

                def b_front(qb, kp, sb_, pb_, s_=s_):
                    P.tag = "Bf"
                    k0, nk = GRP[kp]
                    for kk in range(nk):
                        kt = k0 + kk
                        P.op("pe", I(nc.tensor.matmul, sbank[sb_][:, kk * 512:(kk + 1) * 512],
                                     lhsT=kT[s_][:, kt * 128:(kt + 1) * 128],
                                     rhs=qT[s_][:, qb * 512:(qb + 1) * 512], start=True, stop=True),
                             reads=[Rq[s_]], writes=[Rs[sb_]] if kk == 0 else (), pwrites=[Rs[sb_]] if kk else ())
                    P.op("act", I(nc.scalar.activation, out=pT[pb_][:, 0:512 * nk], in_=sbank[sb_][:, 0:512 * nk],
                                  func=AF.Exp), reads=[Rs[sb_]], writes=[RpT[pb_]])

                def b_back(qb, kp, pb_, s_=s_, h=h):
                    P.tag = "Bb"
                    os_ = (h * 8 + qb) % 2
                    k0, nk = GRP[kp]
                    for kk in range(nk):
                        kt = k0 + kk
                        for qs in range(4):
                            w_ = kt == 0 and qs == 0
                            P.op("pe", I(nc.tensor.matmul, obank(os_, qs),
                                         lhsT=pT[pb_][:, kk * 512 + qs * 128:kk * 512 + (qs + 1) * 128],
                                         rhs=vv[s_][:, kt, :], start=w_, stop=(kt == NT - 1), skip_group_check=True),
                                 reads=[RpT[pb_], Rq[s_]], writes=[Ro[os_]] if w_ else (),
                                 pwrites=() if w_ else [Ro[os_]])
                    if kp == len(GRP) - 1:
                        P.tag = "Bep"
                        ov = ps[:, 3072 + 512 * os_:3072 + 512 * os_ + 264].rearrange("p (q c) -> p q c", q=4)
                        P.op("dve", I(nc.vector.reciprocal, out=rc[:, 0:4], in_=ov[:, :, 64]), reads=[Ro[os_]],
                             writes=[Rrc])
                        for qs in range(4):
                            ti = qb * 4 + qs
                            P.op("dve", I(nc.vector.scalar_tensor_tensor, out=omla[:, ti, h * 64:(h + 1) * 64],
                                          in0=obank(os_, qs)[:, 0:64], scalar=rc[:, qs:qs + 1], in1=ga[s_][:, ti, :],
                                          op0=ALU.mult, op1=ALU.mult), reads=[Ro[os_], Rrc, Rq[s_]], pwrites=[Rom])

                ring = []
                for n in range(len(steps) + 1):
                    if n < len(steps):
                        sb_, pb_ = step % 2, step % 3
                        step += 1
                        b_front(steps[n][0], steps[n][1], sb_, pb_)
                        ring.append(pb_)
                    if n >= 1:
                        b_back(steps[n - 1][0], steps[n - 1][1], ring[n - 1])
            if upto < "C":
                for i in range(NT):
                    P.dma("sp", I(nc.sync.dma_start, out=MG[i * 128:(i + 1) * 128, :], in_=omla[:, i, :]), Rom,
                          reads=[Rom], pwrites=[RMG])
            P.barrier()
            P.emit()
        if upto < "C":
            return

        CLS, DMIN = g["CLS"], g["DMIN"]
        slp_d, PQHL = g["slopes_d"], g["PQHL"]
        RPQHL = R("PQHL")
        with ExitStack() as es2:
            pq = es2.enter_context(sb("c_pq", [128, T], F32))
            Rpq = R("cpq")
            with ExitStack() as es3:
                pqi = es3.enter_context(sb("c_pqi", [128, T], I32))
                ahi = es3.enter_context(sb("c_ahi", [8, T], BF16))
                alo = es3.enter_context(sb("c_alo", [8, T], BF16))
                shi = es3.enter_context(sb("c_shi", [8, T], BF16))
                slo = es3.enter_context(sb("c_slo", [8, T], BF16))
                msl = es3.enter_context(sb("c_msl", [8, 1], F32))
                Rt = R("ctmp")
                P.dma("sp", I(nc.sync.dma_start, out=pqi[:], in_=pos_d.partition_broadcast(128)), Rpq, writes=[Rpq])
                P.dma("sp", I(nc.sync.dma_start, out=msl[:], in_=slp_d.rearrange("(h o) -> h o", o=1)), Rt,
                      writes=[Rt])
                P.op("dve", I(nc.vector.tensor_copy, out=pq[:], in_=pqi[:]), reads=[Rpq], writes=[Rpq])
                P.op("dve", I(nc.vector.tensor_copy, out=ahi[:], in_=pq[0:8, :]), reads=[Rpq], pwrites=[Rt])
                P.op("dve", I(nc.vector.tensor_tensor, out=alo[:], in0=pq[0:8, :], in1=ahi[:], op=ALU.subtract),
                     reads=[Rpq, Rt], pwrites=[Rt])
                P.op("dve", I(nc.vector.tensor_scalar, out=shi[:], in0=ahi[:], scalar1=msl[:, 0:1], scalar2=None,
                              op0=ALU.mult), reads=[Rt], pwrites=[Rt])
                P.op("dve", I(nc.vector.tensor_scalar, out=slo[:], in0=alo[:], scalar1=msl[:, 0:1], scalar2=None,
                              op0=ALU.mult), reads=[Rt], pwrites=[Rt])
                P.dma("sp", I(nc.sync.dma_start, out=PQHL[:, 0, :], in_=shi[:]), Rt, reads=[Rt], pwrites=[RPQHL])
                P.dma("sp", I(nc.sync.dma_start, out=PQHL[:, 1, :], in_=slo[:]), Rt, reads=[Rt], pwrites=[RPQHL])
                P.barrier()
                P.emit()
            nposf = es2.enter_context(sb("c_nposf", [128, NT], F32))
            bia = es2.enter_context(sb("c_bia", [128, 2, 2, NT], F32))
            P.op("dve", I(nc.vector.tensor_scalar, out=nposf[:], in0=posf[:], scalar1=-1.0, scalar2=None,
                          op0=ALU.mult), reads=[Rposf], writes=[Rpq])
            qT = [[es2.enter_context(sb("c_qT%d%d" % (k, m), [68, T], BF16)) for m in range(2)] for k in range(2)]
            kT = [[es2.enter_context(sb("c_kT%d%d" % (k, m), [68, T], BF16)) for m in range(2)] for k in range(2)]
            vv = [es2.enter_context(sb("c_v%d" % k, [128, NT, 129], BF16)) for k in range(2)]
            gbt = [es2.enter_context(sb("c_gb%d" % k, [128, NT, 128], BF16)) for k in range(2)]
            pT = [es2.enter_context(sb("c_pT%d" % k, [128, 512], BF16)) for k in range(4)]
            sp_ = [es2.enter_context(sb("c_sp%d" % k, [128, 512], F32)) for k in range(2)]
            dt_ = [es2.enter_context(sb("c_dt%d" % k, [128, 256], F32)) for k in range(2)]
            rc = es2.enter_context(sb("c_rc", [128, 16], F32))
            of = es2.enter_context(sb("c_of", [128, 4, 128], F32))
            jk = es2.enter_context(sb("c_jk", [128, 2, 128], F32))
            ps = es2.enter_context(nc.psum_tensor("c_ps", [128, 8 * 512], F32))
            Rq = [R("cq0"), R("cq1")]
            Rgs = [R("cgs0"), R("cgs1")]
            Rbia = [R("cbia0"), R("cbia1")]
            RpT = [R("cpT%d" % k) for k in range(4)]
            Rsp = [R("csp%d" % k) for k in range(2)]
            Rdt = [R("cdt%d" % k) for k in range(2)]
            Rs = [R("cs%d" % k) for k in range(4)]
            Ro = [R("co0"), R("co1")]
            Rrc, Rof, Rjk = R("crc"), R("cof"), R("cjk")
            sbank = [ps[:, 512 * k:512 * (k + 1)] for k in range(4)]
            def oacc(s, mp, qs):
                b0 = 2048 + (2 * s + mp) * 512 + qs * 132
                return ps[:, b0:b0 + 129]

            def oset(s):
                return ps[:, 2048 + 2 * s * 512:2048 + (2 * s + 2) * 512]
            for k in range(2):
                P.op("pool", I(nc.gpsimd.memset, vv[k][:, :, 128:129], 1.0), pwrites=[Rq[k]])
                for m in range(2):
                    P.op("dve", I(nc.vector.memset, kT[k][m][64:68, :], 2.0), pwrites=[Rq[k]])
                    P.op("dve", I(nc.vector.memset, kT[k][m][64:66, :], -1.0), pwrites=[Rq[k]])
            step = 0
            dstep = 0
            oset_i = 0
            for h in range(8):
                s_ = h % 2
                slope = 2.0 ** (-(h + 1))
                for m in range(2):
                    P.dma("sp", I(nc.sync.dma_start, out=qT[s_][m][0:64, :], in_=QdT[h, m * 64:(m + 1) * 64, :]),
                          Rq[s_], pwrites=[Rq[s_]])
                    for a_ in range(2):
                        P.dma("sp", I(nc.sync.dma_start, out=qT[s_][m][64 + 2 * a_:66 + 2 * a_, :], in_=PQHL[h, :, :]),
                              Rq[s_], reads=[RPQHL], pwrites=[Rq[s_]])
                    P.dma("sp", I(nc.sync.dma_start, out=kT[s_][m][0:64, :], in_=KdT[h, m * 64:(m + 1) * 64, :]),
                          Rq[s_], pwrites=[Rq[s_]])
                P.dma("sp", I(nc.sync.dma_start, out=vv[s_][:, :, 0:128],
                              in_=Vd[:, h * 128:(h + 1) * 128].rearrange("(i p) d -> p i d", p=128)), Rq[s_],
                      pwrites=[Rq[s_]])
                P.dma("sp", I(nc.sync.dma_start, out=gbt[s_][:],
                              in_=G[:, 1024 + h * 128:1024 + (h + 1) * 128].rearrange("(i p) d -> p i d", p=128)),
                      Rq[s_], pwrites=[Rq[s_]])
                P.op("dve", I(nc.vector.tensor_tensor, out=gbt[s_][:], in0=gbt[s_][:],
                              in1=vsubln[:].unsqueeze(1).to_broadcast([128, NT, 128]), op=ALU.mult),
                     reads=[Rq[s_], Rvec], writes=[Rgs[s_]])
                P.op("dve", I(nc.vector.tensor_scalar, out=bia[:, s_, 0, :], in0=posf[:], scalar1=slope, scalar2=None,
                              op0=ALU.mult), reads=[Rposf], writes=[Rbia[s_]])
                P.op("dve", I(nc.vector.tensor_scalar, out=bia[:, s_, 1, :], in0=posf[:], scalar1=-slope, scalar2=None,
                              op0=ALU.mult), reads=[Rposf], pwrites=[Rbia[s_]])
                steps = []
                for qb in range(16):
                    kts = [kt for kt in range(NT) if slope * DMIN[qb][kt] < SKIP_T]
                    for n_, kt in enumerate(kts):
                        steps.append((qb, kt, int(CLS[qb][kt]), n_ == 0, n_ == len(kts) - 1))

                def st1(n, r4, d2, s_=s_):
                    P.tag = "C1"
                    qb, kt, cl, first, last = steps[n]
                    q0 = qb * 256
                    K_ = (64, 66, 68)[cl]
                    for mp in range(2):
                        P.op("pe", I(nc.tensor.matmul, sbank[r4][:, mp * 256:(mp + 1) * 256],
                                     lhsT=kT[s_][mp][0:K_, kt * 128:(kt + 1) * 128],
                                     rhs=qT[s_][mp][0:K_, q0:q0 + 256], start=True, stop=True),
                             reads=[Rq[s_]], writes=[Rs[r4]] if mp == 0 else (),
                             pwrites=[Rs[r4]] if mp else ())
                    if cl == 0:
                        P.op("act", I(nc.scalar.activation, out=dt_[d2][:], in_=pq[:, q0:q0 + 256], func=AF.Abs,
                                      bias=nposf[:, kt:kt + 1]), reads=[Rpq], writes=[Rdt[d2]])

                def st23(n, r4, d2, s_=s_, slope=slope):
                    P.tag = "C2"
                    qb, kt, cl, first, last = steps[n]
                    if cl == 0:
                        P.op("dve", I(nc.vector.scalar_tensor_tensor,
                                      out=sp_[d2][:].rearrange("p (m q) -> p m q", m=2),
                                      in0=dt_[d2][:].unsqueeze(1).to_broadcast([128, 2, 256]), scalar=-slope,
                                      in1=sbank[r4].rearrange("p (m q) -> p m q", m=2), op0=ALU.mult, op1=ALU.add),
                             reads=[Rdt[d2], Rs[r4]], writes=[Rsp[d2]])
                        P.op("act", I(nc.scalar.activation, out=pT[r4][:], in_=sp_[d2][:], func=AF.Exp),
                             reads=[Rsp[d2]], writes=[RpT[r4]])
                    else:
                        P.op("act", I(nc.scalar.activation, out=pT[r4][:], in_=sbank[r4], func=AF.Exp,
                                      bias=bia[:, s_, cl - 1, kt:kt + 1]), reads=[Rs[r4], Rbia[s_]],
                             writes=[RpT[r4]])

                def st4(n, r4, os_, s_=s_, h=h):
                    P.tag = "C4"
                    qb, kt, cl, first, last = steps[n]
                    for mp in range(2):
                        for qs in range(2):
                            w_ = first and mp == 0 and qs == 0
                            P.op("pe", I(nc.tensor.matmul, oacc(os_, mp, qs),
                                         lhsT=pT[r4][:, mp * 256 + qs * 128:mp * 256 + (qs + 1) * 128],
                                         rhs=vv[s_][:, kt, :], start=(first and qs == 0), stop=last,
                                         skip_group_check=True),
                                 reads=[RpT[r4], Rq[s_]], writes=[Ro[os_]] if w_ else (),
                                 pwrites=() if w_ else [Ro[os_]])
                    if last:
                        pend.append((qb, os_))

                def ep(qb, os_, s_=s_, h=h):
                    P.tag = "Cep"
                    ti = qb * 2
                    v = oset(os_).rearrange("p (m c) -> p m c", m=2)[:, :, 0:264].rearrange("p m (q c) -> p m q c", q=2)
                    o1, o2 = v[:, 0, :, 0:128], v[:, 1, :, 0:128]
                    bc = lambda ap: ap.unsqueeze(2).to_broadcast([128, 2, 128])
                    P.op("dve", I(nc.vector.reciprocal, out=rc[:, 0:4].rearrange("p (m q) -> p m q", m=2),
                                  in_=v[:, :, :, 128]), reads=[Ro[os_]], writes=[Rrc])
                    P.op("dve", I(nc.vector.tensor_scalar, out=rc[:, 4:6], in0=rc[:, 2:4], scalar1=nlam[:, 0:1],
                                  scalar2=None, op0=ALU.mult), reads=[Rrc, Rnlam], pwrites=[Rrc])
                    P.op("dve", I(nc.vector.tensor_tensor, out=of[:, 0:2, :], in0=o2, in1=bc(rc[:, 4:6]), op=ALU.mult),
                         reads=[Ro[os_], Rrc], writes=[Rof])
                    P.op("dve", I(nc.vector.tensor_tensor, out=of[:, 2:4, :], in0=o1, in1=bc(rc[:, 0:2]), op=ALU.mult),
                         reads=[Ro[os_], Rrc], pwrites=[Rof])
                    P.op("dve", I(nc.vector.tensor_tensor, out=of[:, 0:2, :], in0=of[:, 0:2, :], in1=of[:, 2:4, :],
                                  op=ALU.add), reads=[Rof], pwrites=[Rof])
                    P.op("act", I(nc.scalar.activation, out=jk[:], in_=of[:, 0:2, :], func=AF.Square), reads=[Rof],
                         writes=[Rjk])
                    P.op("dve", I(nc.vector.tensor_reduce, out=rc[:, 6:8], in_=jk[:], axis=AX.X, op=ALU.add),
                         reads=[Rjk], pwrites=[Rrc])
                    P.op("dve", I(nc.vector.tensor_scalar, out=rc[:, 8:10], in0=rc[:, 6:8], scalar1=1.0 / 128,
                                  scalar2=EPS, op0=ALU.mult, op1=ALU.add), reads=[Rrc], pwrites=[Rrc])
                    P.op("pool", I(nc.gpsimd.tensor_tensor, out=rc[:, 10:12], in0=rc[:, 8:10], in1=mhalf[:, 0:2],
                                   op=ALU.pow), reads=[Rrc, Rmh], pwrites=[Rrc])
                    P.op("dve", I(nc.vector.tensor_tensor, out=of[:, 2:4, :], in0=of[:, 0:2, :], in1=bc(rc[:, 10:12]),
                                  op=ALU.mult), reads=[Rof, Rrc], pwrites=[Rof])
                    P.op("dve", I(nc.vector.tensor_tensor, out=of[:, 0:2, :], in0=of[:, 2:4, :],
                                  in1=gbt[s_][:, ti:ti + 2, :], op=ALU.mult), reads=[Rof, Rgs[s_], Rq[s_]],
                         pwrites=[Rof])
                    P.op("dve", I(nc.vector.tensor_tensor, out=omla[:, ti:ti + 2, h * 128:(h + 1) * 128],
                                  in0=of[:, 0:2, :], in1=omla[:, ti:ti + 2, h * 128:(h + 1) * 128], op=ALU.add),
                         reads=[Rof, Rom], pwrites=[Rom])

                ep_after = {}
                qbs = sorted(set(st_[0] for st_ in steps))
                for a_, qb_ in enumerate(qbs[:-1]):
                    nq = qbs[a_ + 1]
                    idxs = [k for k, st_ in enumerate(steps) if st_[0] == nq]
                    dg = [k for k in idxs if steps[k][2] == 0]
                    at = dg[-1] if dg else min(idxs[0] + 1, idxs[-1])
                    at = min(at, idxs[-1] - 1) if len(idxs) > 1 else idxs[0]
                    ep_after.setdefault(at, []).append(qb_)
                pend = []
                due = set()
                r4s, d2s, oss = [], [], []
                for n in range(len(steps) + 2):
                    if n < len(steps):
                        r4 = step % 4
                        step += 1
                        d2 = dstep % 2
                        if steps[n][2] == 0:
                            dstep += 1
                        if steps[n][3]:
                            oset_i += 1
                        r4s.append(r4)
                        d2s.append(d2)
                        oss.append(oset_i % 2)
                        st1(n, r4, d2)
                    if 1 <= n <= len(steps):
                        st23(n - 1, r4s[n - 1], d2s[n - 1])
                        due.update(ep_after.get(n - 1, []))
                        for pq_ in [p_ for p_ in pend if p_[0] in due]:
                            pend.remove(pq_)
                            ep(*pq_)
                    if n >= 2:
                        st4(n - 2, r4s[n - 2], oss[n - 2])
                        for pq_ in [p_ for p_ in pend if p_[0] in due]:
                            pend.remove(pq_)
                            ep(*pq_)
                for pq_ in pend:
                    ep(*pq_)
                pend = []
            for i in range(NT):
                P.dma("sp", I(nc.sync.dma_start, out=MG[i * 128:(i + 1) * 128, :], in_=omla[:, i, :]), Rom,
                      reads=[Rom], pwrites=[RMG])
            P.barrier()
            P.emit()


def phaseDE(nc, P, env, upto):
    g = env
    sb = nc.sbuf_tensor
    x_d, out_d, MG, H2, wout_d, rw_d, vec_d = [g[k] for k in "x_d out_d MG H2 wout_d rw_d vec_d".split()]
    wg_d, wu_d, wd_d, iota_d, tokpi_d = [g[k] for k in "wg_d wu_d wd_d iota_d tokpi_d".split()]
    identb, identf, mhalf = g["identb"], g["identf"], g["mhalf"]
    Rid, Rmh = g["Rid"], g["Rmh"]
    with ExitStack() as es:
        aff = es.enter_context(sb("aff", [128, NT, NE], F32))
        posm = es.enter_context(sb("posm", [128, NT, NE], F32))
        Raff, Rposm = R("aff"), R("posm")
        wsl = [es.enter_context(sb("e_w%d" % k, [128, 16384], BF16)) for k in range(3)]
        Rws = [R("ew%d" % k) for k in range(4)]

        def wload(e, which):
            for k, src in enumerate([wg_d, wu_d]):
                if k not in which:
                    continue
                sl = (3 * e + k) % 4
                P.dma("pool", I(nc.gpsimd.dma_start, out=wsl[sl][:].rearrange("p (kc f) -> p kc f", kc=8),
                                in_=src[e].rearrange("(kc p) f -> p kc f", p=128)), Rws[sl], writes=[Rws[sl]])
            if 2 in which:
                sl = (3 * e + 2) % 4
                P.dma("pool", I(nc.gpsimd.dma_start, out=wsl[sl][:].rearrange("p (j c) -> p j c", j=16),
                                in_=wd_d[e].rearrange("(j p) c -> p j c", p=128)), Rws[sl], writes=[Rws[sl]])
        with ExitStack() as es2:
            wout = es2.enter_context(sb("d_wout", [128, 8, 1024], BF16))
            rw = es2.enter_context(sb("d_rw", [128, 8, NE], F32))
            wn2 = es2.enter_context(sb("d_wn2", [128, 1024], F32))
            mg = [es2.enter_context(sb("d_mg%d" % k, [128, 1024], BF16)) for k in range(2)]
            xt = [es2.enter_context(sb("d_xt%d" % k, [128, 1024], F32)) for k in range(2)]
            x1 = [es2.enter_context(sb("d_x1%d" % k, [128, 1024], F32)) for k in range(2)]
            mT = es2.enter_context(sb("d_mT", [128, 1024], BF16))
            sq = es2.enter_context(sb("d_sq", [128, 1024], F32))
            h2f = es2.enter_context(sb("d_h2f", [128, 1024], F32))
            h2b = [es2.enter_context(sb("d_h2b%d" % k, [128, 1024], BF16)) for k in range(2)]
            h2T = es2.enter_context(sb("d_h2T", [128, 1024], F32))
            sm = es2.enter_context(sb("d_sm", [128, 64], F32))
            psB = es2.enter_context(nc.psum_tensor("d_psB", [128, 1024], BF16))
            psF = es2.enter_context(nc.psum_tensor("d_psF", [128, 5 * 512], F32))
            Rw = R("dw")
            Rmg, Rxt, Rx1, Rh2b = [[R(n + str(k)) for k in range(2)] for n in ("dmg", "dxt", "dx1", "dh2b")]
            RmT, Rsq, Rh2f, Rh2T, Rsm, RTb, Racc, RTf, Rlg = [R(n) for n in
                                                              "dmT dsq dh2f dh2T dsm dTb dacc dTf dlg".split()]
            RH2, Rout = g["RH2"], g["Rout"]
            P.dma("pool", I(nc.gpsimd.dma_start, out=wout[:], in_=wout_d.rearrange("(kc p) c -> p kc c", p=128)), Rw,
                  pwrites=[Rw])
            P.dma("sp", I(nc.sync.dma_start, out=rw[:], in_=rw_d.rearrange("(kc p) c -> p kc c", p=128)), Rw,
                  pwrites=[Rw])
            P.dma("sp", I(nc.sync.dma_start, out=wn2[:], in_=vec_d["ffn_norm_w"].partition_broadcast(128)), Rw,
                  pwrites=[Rw])
            wload(0, (0, 1, 2))
            acc = psF[:, 0:1024]
            Tf = psF[:, 1024:2048]
            lg = psF[:, 2048:2048 + NE]
            h2f2 = [h2f, es2.enter_context(sb("d_h2f1", [128, 1024], F32))]
            Rh2f2 = [R("dh2f0"), R("dh2f1")]
            RsmA = [R("dsmA0"), R("dsmA1")]
            RsmB = R("dsmB")
            def Da(i):
                P.tag = "Da"
                s_ = i % 2
                t0 = i * 128
                ca = 48 + 4 * s_
                h2f = h2f2[s_]
                Rh2f = Rh2f2[s_]
                Rsm = RsmA[s_]
                P.dma("sp", I(nc.sync.dma_start, out=mg[s_][:], in_=MG[t0:t0 + 128, :]), Rmg[s_], writes=[Rmg[s_]])
                P.dma("sp", I(nc.sync.dma_start, out=xt[s_][:], in_=x_d[t0:t0 + 128, :]), Rxt[s_], writes=[Rxt[s_]])
                for kc in range(8):
                    P.op("pe", I(nc.tensor.transpose, out=psB[:, kc * 128:(kc + 1) * 128],
                                 in_=mg[s_][:, kc * 128:(kc + 1) * 128], identity=identb[:]),
                         reads=[Rmg[s_], Rid], writes=[RTb] if kc == 0 else (), pwrites=[RTb] if kc else ())
                P.op("act", I(nc.scalar.copy, out=mT[:], in_=psB[:, 0:1024]), reads=[RTb], writes=[RmT])
                for half in range(2):
                    for kc in range(8):
                        first = half == 0 and kc == 0
                        P.op("pe", I(nc.tensor.matmul, acc[:, half * 512:(half + 1) * 512],
                                     lhsT=mT[:, kc * 128:(kc + 1) * 128], rhs=wout[:, kc, half * 512:(half + 1) * 512],
                                     start=(kc == 0), stop=(kc == 7)), reads=[RmT, Rw],
                             writes=[Racc] if first else (), pwrites=() if first else [Racc])
                P.op("dve", I(nc.vector.tensor_tensor, out=x1[s_][:], in0=acc, in1=xt[s_][:], op=ALU.add),
                     reads=[Racc, Rxt[s_]], writes=[Rx1[s_]])
                P.dma("sp", I(nc.sync.dma_start, out=out_d[t0:t0 + 128, :], in_=x1[s_][:]), Rx1[s_], reads=[Rx1[s_]],
                      pwrites=[Rout])
                P.op("act", I(nc.scalar.activation, out=sq[:], in_=x1[s_][:], func=AF.Square, accum_out=sm[:, ca:ca + 1]),
                     reads=[Rx1[s_]], writes=[Rsq, Rsm])
                P.op("dve", I(nc.vector.tensor_scalar, out=sm[:, ca + 1:ca + 2], in0=sm[:, ca:ca + 1], scalar1=1.0 / D, scalar2=EPS,
                              op0=ALU.mult, op1=ALU.add), reads=[Rsm], pwrites=[Rsm])
                P.op("pool", I(nc.gpsimd.tensor_tensor, out=sm[:, ca + 2:ca + 3], in0=sm[:, ca + 1:ca + 2], in1=mhalf[:, 0:1], op=ALU.pow),
                     reads=[Rsm, Rmh], pwrites=[Rsm])
                P.op("dve", I(nc.vector.scalar_tensor_tensor, out=h2f[:], in0=x1[s_][:], scalar=sm[:, ca + 2:ca + 3], in1=wn2[:],
                              op0=ALU.mult, op1=ALU.mult), reads=[Rx1[s_], Rsm, Rw], writes=[Rh2f])
                P.op("act", I(nc.scalar.copy, out=h2b[s_][:], in_=h2f[:]), reads=[Rh2f], writes=[Rh2b[s_]])
                P.dma("sp", I(nc.sync.dma_start, out=H2[t0:t0 + 128, :], in_=h2b[s_][:]), Rh2b[s_], reads=[Rh2b[s_]],
                      pwrites=[RH2])
            def Db(i):
                P.tag = "Db"
                s_ = i % 2
                h2f = h2f2[s_]
                Rh2f = Rh2f2[s_]
                Rsm = RsmB
                for kc in range(8):
                    P.op("pe", I(nc.tensor.transpose, out=Tf[:, kc * 128:(kc + 1) * 128],
                                 in_=h2f[:, kc * 128:(kc + 1) * 128], identity=identf[:]),
                         reads=[Rh2f, Rid], writes=[RTf] if kc == 0 else (), pwrites=[RTf] if kc else ())
                P.op("dve", I(nc.vector.tensor_copy, out=h2T[:], in_=Tf), reads=[RTf], writes=[Rh2T])
                for kc in range(8):
                    P.op("pe", I(nc.tensor.matmul, lg, lhsT=h2T[:, kc * 128:(kc + 1) * 128], rhs=rw[:, kc, :],
                                 start=(kc == 0), stop=(kc == 7)), reads=[Rh2T, Rw],
                         writes=[Rlg] if kc == 0 else (), pwrites=[Rlg] if kc else ())
                P.op("dve", I(nc.vector.tensor_reduce, out=sm[:, 8:9], in_=lg, axis=AX.X, op=ALU.max), reads=[Rlg],
                     pwrites=[Rsm])
                P.op("dve", I(nc.vector.tensor_scalar, out=sm[:, 9:10], in0=sm[:, 8:9], scalar1=-1.0, scalar2=None,
                              op0=ALU.mult), reads=[Rsm], pwrites=[Rsm])
                P.op("act", I(nc.scalar.activation, out=sm[:, 16:32], in_=lg, func=AF.Exp, bias=sm[:, 9:10],
                              accum_out=sm[:, 10:11]), reads=[Rlg, Rsm], pwrites=[Rsm])
                P.op("dve", I(nc.vector.reciprocal, out=sm[:, 11:12], in_=sm[:, 10:11]), reads=[Rsm], pwrites=[Rsm])
                P.op("dve", I(nc.vector.tensor_scalar, out=aff[:, i, :], in0=sm[:, 16:32], scalar1=sm[:, 11:12],
                              scalar2=None, op0=ALU.mult), reads=[Rsm], pwrites=[Raff])
            Da(0)
            for i in range(NT):
                if i + 1 < NT:
                    Da(i + 1)
                Db(i)
            P.barrier()
            P.emit()
        if upto < "E":
            return
        with ExitStack() as es2:
            affT = es2.enter_context(sb("e_affT", [NE, T], F32))
            mk = es2.enter_context(sb("e_mk", [NE, T], F32))
            cs = es2.enter_context(sb("e_cs", [NE, T], F32))
            on = es2.enter_context(sb("e_on", [NE, T], F32))
            bs = es2.enter_context(sb("e_bs", [NE, 8], F32))
            ps = es2.enter_context(nc.psum_tensor("e_ps", [128, 4 * 512], F32))
            RaT, Rmk, Rcs, Ron, Rbs, Rps = [R(n) for n in "eaT emk ecs eon ebs eps".split()]
            P.op("pool", I(nc.gpsimd.memset, on[:], 1.0), writes=[Ron])
            P.op("pool", I(nc.gpsimd.memset, bs[:], 0.0), writes=[Rbs])
            for half in range(2):
                for j in range(16):
                    i = half * 16 + j
                    P.op("pe", I(nc.tensor.transpose, out=ps[0:NE, j * 128:(j + 1) * 128], in_=aff[:, i, :],
                                 identity=identf[:]), reads=[Raff, Rid], writes=[Rps] if j == 0 else (),
                         pwrites=[Rps] if j else ())
                P.op("act", I(nc.scalar.copy, out=affT[:, half * 2048:(half + 1) * 2048], in_=ps[0:NE, 0:2048]),
                     reads=[Rps], pwrites=[RaT])
            lo, mid, cntc, gw = bs[:, 0:1], bs[:, 1:2], bs[:, 2:3], bs[:, 3:4]
            for it in range(28):
                w = 2.0 ** (-(it + 1))
                P.op("dve", I(nc.vector.tensor_scalar, out=mid, in0=lo, scalar1=w, scalar2=None, op0=ALU.add),
                     reads=[Rbs], pwrites=[Rbs])
                P.op("dve", I(nc.vector.tensor_scalar, out=mk[:], in0=affT[:], scalar1=mid, scalar2=0.0, op0=ALU.is_gt,
                              op1=ALU.add, accum_out=cntc), reads=[RaT, Rbs], writes=[Rmk], pwrites=[Rbs])
                P.op("dve", I(nc.vector.tensor_scalar, out=gw, in0=cntc, scalar1=CAP - 0.5, scalar2=w, op0=ALU.is_gt,
                              op1=ALU.mult), reads=[Rbs], pwrites=[Rbs])
                P.op("dve", I(nc.vector.tensor_tensor, out=lo, in0=lo, in1=gw, op=ALU.add), reads=[Rbs],
                     pwrites=[Rbs])
            P.op("dve", I(nc.vector.tensor_scalar, out=mk[:], in0=affT[:], scalar1=lo, scalar2=None, op0=ALU.is_gt),
                 reads=[RaT, Rbs], writes=[Rmk])
            P.op("dve", I(nc.vector.tensor_tensor_scan, out=cs[:], data0=on[:], data1=mk[:], initial=0.0, op0=ALU.mult,
                          op1=ALU.add), reads=[Ron, Rmk], writes=[Rcs])
            P.op("dve", I(nc.vector.tensor_tensor, out=cs[:], in0=cs[:], in1=mk[:], op=ALU.mult), reads=[Rcs, Rmk],
                 writes=[Rcs])
            for i in range(NT):
                P.op("pe", I(nc.tensor.transpose, out=ps[:, i * NE:(i + 1) * NE], in_=cs[:, i * 128:(i + 1) * 128],
                             identity=identf[0:NE, 0:NE]), reads=[Rcs, Rid], writes=[Rps] if i == 0 else (),
                     pwrites=[Rps] if i else ())
            P.op("act", I(nc.scalar.copy, out=posm[:].rearrange("p a b -> p (a b)"), in_=ps[:, 0:NT * NE]),
                 reads=[Rps], writes=[Rposm])
            P.barrier()
            P.emit()
        with ExitStack() as es2:
            wsl.append(es2.enter_context(sb("e_w3", [128, 16384], BF16)))
            iota = es2.enter_context(sb("e_iota", [128, CAP], F32))
            tokpi = es2.enter_context(sb("e_tokpi", [128, NT, 4], BF16))
            sel = [es2.enter_context(sb("e_sel%d" % k, [128, CAP], BF16)) for k in range(3)]
            Rsel = [R("esel%d" % k) for k in range(3)]
            idxrow = es2.enter_context(sb("e_idxrow", [4, CAP], F32))
            ic = es2.enter_context(sb("e_ic", [128, 4, 4], F32))
            idf = es2.enter_context(sb("e_idf", [128, 4], F32))
            idx = [[es2.enter_context(sb("e_idx%d%d" % (k, c), [128, 1], I32)) for c in range(4)] for k in range(2)]
            gate = [es2.enter_context(sb("e_gate%d" % k, [128, 4], F32)) for k in range(2)]
            xe = [es2.enter_context(sb("e_xe%d" % k, [128, 1024], BF16)) for k in range(4)]
            Rxe = [R("exe%d" % k) for k in range(4)]
            xeT = es2.enter_context(sb("e_xeT", [128, 8, CAP], BF16))
            hT = es2.enter_context(sb("e_hT", [128, 16, CAP], BF16))
            sg = [es2.enter_context(sb("e_sg%d" % k, [128, CAP], F32)) for k in range(2)]
            Rsg = [R("esg0"), R("esg1")]
            ye = [es2.enter_context(sb("e_ye%d" % k, [128, 1024], F32)) for k in range(2)]
            Rye = [R("eye0"), R("eye1")]
            psF = es2.enter_context(nc.psum_tensor("e_psF", [128, 7 * 512], F32))
            psB = es2.enter_context(nc.psum_tensor("e_psB", [128, 1024], BF16))
            gub = [(psF[:, 0:512], psF[:, 512:1024]), (psF[:, 1024:1536], psF[:, 1536:2048])]
            Rgu = [R("egu0"), R("egu1")]
            dbk = [psF[:, 2048:2560], psF[:, 2560:3072]]
            Rdb = [R("edb0"), R("edb1")]
            ipb = psF[:, 3072:3584]
            Ripb, RTb = R("eipb"), R("eTb")
            Rtok, Ridr, Ric, Ridx, RxeT, RhT, Rsc, Rc = [R(n) for n in "etok eidr eic eidx exeT ehT esc ec".split()]
            Ridxs = [R("eidx0"), R("eidx1")]
            P.dma("sp", I(nc.sync.dma_start, out=iota[:], in_=iota_d.partition_broadcast(128)), Rc, pwrites=[Rc])
            P.dma("pool", I(nc.gpsimd.dma_start, out=tokpi[:, :, 0:2], in_=tokpi_d[:, :, :]), Rc, pwrites=[Rtok])

            def route_tok(e):
                P.op("dve", I(nc.vector.tensor_copy, out=tokpi[:, :, 2], in_=aff[:, :, e]), reads=[Raff],
                     writes=[Rtok])
                P.op("dve", I(nc.vector.tensor_tensor, out=tokpi[:, :, 3], in0=aff[:, :, e], in1=tokpi[:, :, 2],
                              op=ALU.subtract), reads=[Raff, Rtok], pwrites=[Rtok])

            def route_sel(e, i):
                r3 = (e * NT + i) % 3
                P.op("dve", I(nc.vector.tensor_scalar, out=sel[r3][:], in0=iota[:], scalar1=posm[:, i, e:e + 1],
                              scalar2=None, op0=ALU.is_equal), reads=[Rc, Rposm], writes=[Rsel[r3]])
                P.op("pe", I(nc.tensor.matmul, ipb[0:4, :], lhsT=tokpi[:, i, :], rhs=sel[r3][:], start=(i == 0),
                             stop=(i == NT - 1)), reads=[Rtok, Rsel[r3]], writes=[Ripb] if i == 0 else (),
                     pwrites=[Ripb] if i else ())

            def route_idx(e):
                es_ = e % 2
                P.op("act", I(nc.scalar.copy, out=idxrow[:], in_=ipb[0:4, :]), reads=[Ripb], writes=[Ridr])
                for cc in range(4):
                    P.op("pe", I(nc.tensor.transpose, out=ipb[:, cc * 4:(cc + 1) * 4],
                                 in_=idxrow[0:4, cc * 128:(cc + 1) * 128], identity=identf[0:4, 0:4]),
                         reads=[Ridr, Rid], writes=[Ripb] if cc == 0 else (), pwrites=[Ripb] if cc else ())
                P.op("dve", I(nc.vector.tensor_copy, out=ic[:].rearrange("p a b -> p (a b)"), in_=ipb[:, 0:16]),
                     reads=[Ripb], writes=[Ric])
                P.op("dve", I(nc.vector.scalar_tensor_tensor, out=idf[:], in0=ic[:, :, 1], scalar=128.0,
                              in1=ic[:, :, 0], op0=ALU.mult, op1=ALU.add), reads=[Ric], writes=[Ridx])
                P.op("dve", I(nc.vector.tensor_tensor, out=gate[es_][:], in0=ic[:, :, 2], in1=ic[:, :, 3], op=ALU.add),
                     reads=[Ric], writes=[Ridxs[es_]])
                for cc in range(4):
                    P.op("dve", I(nc.vector.tensor_copy, out=idx[es_][cc][:], in_=idf[:, cc:cc + 1]), reads=[Ridx],
                         pwrites=[Ridxs[es_]])
                for cc in range(4):
                    P.dma("pool", I(nc.gpsimd.indirect_dma_start, out=xe[cc][:], out_offset=None, in_=H2[:, :],
                                    in_offset=bass.IndirectOffsetOnAxis(ap=idx[es_][cc][:, :], axis=0)), Rxe[cc],
                          reads=[Ridxs[es_]], writes=[Rxe[cc]])

            def route_T(e):
                for kc in range(8):
                    for cc in range(4):
                        P.op("pe", I(nc.tensor.transpose, out=psB[:, cc * 128:(cc + 1) * 128],
                                     in_=xe[cc][:, kc * 128:(kc + 1) * 128], identity=identb[:]),
                             reads=[Rxe[cc], Rid], writes=[RTb] if cc == 0 else (), pwrites=[RTb] if cc else ())
                    if kc % 2 == 0:
                        P.op("act", I(nc.scalar.copy, out=xeT[:, kc, :], in_=psB[:, 0:512]), reads=[RTb],
                             writes=[RxeT] if kc == 0 else (), pwrites=[RxeT] if kc else ())
                    else:
                        P.op("dve", I(nc.vector.tensor_copy, out=xeT[:, kc, :], in_=psB[:, 0:512]), reads=[RTb],
                             pwrites=[RxeT])

            route_tok(0)
            for i in range(NT):
                route_sel(0, i)
            route_idx(0)
            route_T(0)
            gstep = 0
            dstep = 0
            for e in range(NE):
                es_ = e % 2
                nxt = e + 1 < NE
                wgt = wsl[(3 * e) % 4][:].rearrange("p (kc f) -> p kc f", kc=8)
                wut = wsl[(3 * e + 1) % 4][:].rearrange("p (kc f) -> p kc f", kc=8)
                wdt = wsl[(3 * e + 2) % 4][:].rearrange("p (j c) -> p j c", j=16)
                Rwg, Rwu, Rwd = Rws[(3 * e) % 4], Rws[(3 * e + 1) % 4], Rws[(3 * e + 2) % 4]
                if nxt:
                    wload(e + 1, (0,))
                    route_tok(e + 1)
                for j in range(16):
                    gs = gstep % 2
                    gstep += 1
                    gb_, ub_ = gub[gs]
                    for kc in range(8):
                        P.op("pe", I(nc.tensor.matmul, gb_, lhsT=wgt[:, kc, j * 128:(j + 1) * 128], rhs=xeT[:, kc, :],
                                     start=(kc == 0), stop=(kc == 7)), reads=[Rwg, RxeT],
                             writes=[Rgu[gs]] if kc == 0 else (), pwrites=[Rgu[gs]] if kc else ())
                    for kc in range(8):
                        P.op("pe", I(nc.tensor.matmul, ub_, lhsT=wut[:, kc, j * 128:(j + 1) * 128], rhs=xeT[:, kc, :],
                                     start=(kc == 0), stop=(kc == 7)), reads=[Rwu, RxeT], pwrites=[Rgu[gs]])
                    if nxt:
                        route_sel(e + 1, 2 * j)
                        route_sel(e + 1, 2 * j + 1)
                    P.op("act", I(nc.scalar.activation, out=sg[gs][:], in_=gb_, func=AF.Tanh, scale=0.5),
                         reads=[Rgu[gs]], writes=[Rsg[gs]])
                    P.op("dve", I(nc.vector.scalar_tensor_tensor, out=sg[gs][:], in0=sg[gs][:], scalar=1.0, in1=gb_,
                                  op0=ALU.add, op1=ALU.mult), reads=[Rsg[gs], Rgu[gs]], writes=[Rsg[gs]])
                    P.op("dve", I(nc.vector.scalar_tensor_tensor, out=hT[:, j, :], in0=sg[gs][:], scalar=0.5, in1=ub_,
                                  op0=ALU.mult, op1=ALU.mult), reads=[Rsg[gs], Rgu[gs]],
                         writes=[RhT] if j == 0 else (), pwrites=[RhT] if j else ())
                if nxt:
                    route_idx(e + 1)
                    wload(e + 1, (1,))
                for cc in range(4):
                    ys = cc % 2
                    for half in range(2):
                        ds = dstep % 2
                        dstep += 1
                        for j in range(16):
                            P.op("pe", I(nc.tensor.matmul, dbk[ds], lhsT=hT[:, j, cc * 128:(cc + 1) * 128],
                                         rhs=wdt[:, j, half * 512:(half + 1) * 512], start=(j == 0), stop=(j == 15)),
                                 reads=[RhT, Rwd], writes=[Rdb[ds]] if j == 0 else (), pwrites=[Rdb[ds]] if j else ())
                        if half == 0:
                            P.op("dve", I(nc.vector.tensor_scalar, out=ye[ys][:, 0:512], in0=dbk[ds],
                                          scalar1=gate[es_][:, cc:cc + 1], scalar2=None, op0=ALU.mult),
                                 reads=[Rdb[ds], Ridxs[es_]], writes=[Rye[ys]])
                        else:
                            P.op("dve", I(nc.vector.tensor_scalar, out=ye[ys][:, 512:1024], in0=dbk[ds],
                                          scalar1=gate[es_][:, cc:cc + 1], scalar2=None, op0=ALU.mult),
                                 reads=[Rdb[ds], Ridxs[es_]], pwrites=[Rye[ys]])
                    P.dma("pool", I(nc.gpsimd.indirect_dma_start, out=out_d[:, :],
                                    out_offset=bass.IndirectOffsetOnAxis(ap=idx[es_][cc][:, :], axis=0),
                                    in_=ye[ys][:], in_offset=None, compute_op=ALU.add), Rye[ys],
                          reads=[Rye[ys], Ridxs[es_], Rsc], pwrites=[Rsc])
                if nxt:
                    wload(e + 1, (2,))
                    route_T(e + 1)
            P.barrier()
            P.emit()


IN_COLS = (256, 256, 32, 1024, 1024, 1024, 1024, 1024)


def _win_perm():
    off = np.cumsum((0,) + IN_COLS)
    seg = [np.arange(off[k], off[k + 1]) for k in range(8)]
    return np.concatenate([seg[0], seg[1], seg[3], seg[4], seg[5], seg[6], seg[7], seg[2]])


def make_in_maps(inputs, cores):
    f = lambda a: np.ascontiguousarray(np.asarray(a))
    perm = _win_perm()
    bg = f(inputs["b_gate"])[0]
    shared = {
        "w_in": f(f(inputs["w_in"])[0][:, perm]),
        "b_gate": bg,
        "w_uq": f(inputs["mla_w_uq"])[0],
        "w_ukv": f(inputs["mla_w_ukv"])[0],
        "w_out": f(inputs["w_out"])[0],
        "router_w": f(inputs["router_w"])[0],
        "w_gate": f(inputs["expert_w_gate"])[0],
        "w_up": f(inputs["expert_w_up"])[0],
        "w_down": f(inputs["expert_w_down"])[0],
        "attn_norm_w": f(inputs["attn_norm_w"])[0],
        "q_norm_w": f(inputs["mla_q_norm_w"])[0],
        "kv_norm_w": f(inputs["mla_kv_norm_w"])[0],
        "q_hn": f(inputs["mla_q_hnorm_w"])[0],
        "k_hn": f(inputs["mla_k_hnorm_w"])[0],
        "dq_hn": f(inputs["diff_q_hnorm_w"])[0],
        "dk_hn": f(inputs["diff_k_hnorm_w"])[0],
        "lam": f(inputs["diff_lambda"])[0].reshape(-1),
        "subln": f(inputs["diff_subln_w"])[0],
        "ffn_norm_w": f(inputs["ffn_norm_w"])[0],
        "ident": np.eye(128, dtype=np.float32),
        "invf": (1.0 / (10000.0 ** (np.arange(0, 32, 2, dtype=np.float32) / 32.0))).astype(np.float32),
        "iota512": np.arange(1, 513, dtype=np.float32),
        "tokpi": np.ascontiguousarray(np.stack([np.broadcast_to(np.arange(128, dtype=np.float32)[:, None], (128, NT)),
                                                np.broadcast_to(np.arange(NT, dtype=np.float32)[None, :], (128, NT))],
                                               axis=-1)),
        "slopes": np.array([2.0 ** (-(i + 1)) for i in range(8)], dtype=np.float32),
    }
    x = f(inputs["x"])
    pos = f(inputs["positions"]).astype(np.int32)
    return [dict(shared, x=x[c], pos=pos[c], pos_t=np.ascontiguousarray(pos[c].reshape(NT, 128).T)) for c in cores]


_NC = {}


def classify(pos):
    pos = np.asarray(pos).astype(np.int64)
    n = pos.shape[0]
    q = pos.reshape(n, 16, 256)
    k = pos.reshape(n, NT, 128)
    qmin, qmax = q.min(-1)[:, :, None], q.max(-1)[:, :, None]
    kmin, kmax = k.min(-1)[:, None, :], k.max(-1)[:, None, :]
    below = (kmax <= qmin).all(0)
    above = (kmin >= qmax).all(0)
    cls = np.where(below, 1, np.where(above, 2, 0))
    dmin = np.maximum(np.maximum(qmin - kmax, kmin - qmax), 0).min(0).astype(np.float64)
    return cls, dmin


def kernel(**inputs):
    cls, dmin = classify(np.asarray(inputs["positions"]))
    gq = float(np.abs(np.asarray(inputs["diff_q_hnorm_w"])).max())
    gk = float(np.abs(np.asarray(inputs["diff_k_hnorm_w"])).max())
    skip_t = max(48.0, 2.0 * 8.0 * gq * gk + 25.0)
    key = (cls.tobytes(), dmin.tobytes(), skip_t)
    if key not in _NC:
        _NC[key] = build(CLS=cls, DMIN=dmin, skip_t=skip_t)
    nc = _NC[key]
    in_maps = make_in_maps(inputs, list(range(8)))
    res = run_bass_kernel_spmd(nc, in_maps, core_ids=list(range(8)))
    return np.stack([r["out"] for r in res.results], axis=0).astype(np.float32)
```

```python
import math
from functools import partial as I
from contextlib import ExitStack
import numpy as np
import concourse.bass as bass
import concourse.mybir as mybir
from concourse.bass_utils import run_bass_kernel_spmd

F32 = mybir.dt.float32
BF16 = mybir.dt.bfloat16
I32 = mybir.dt.int32
AF = mybir.ActivationFunctionType
ALU = mybir.AluOpType
AX = mybir.AxisListType

T = 4096
D = 1024
NT = T // 128
EPS = 1e-6
LAM_INIT = 0.8 - 0.6 * math.exp(-0.3 * 0)
NE = 16
CAP = 512
FF = 2048
TWO_PI = 2.0 * math.pi


class R:
    __slots__ = ("name", "w", "r", "pr", "dsem", "dcnt")

    def __init__(self, name):
        self.name = name
        self.w = {}
        self.r = {}
        self.pr = {}
        self.dsem = None
        self.dcnt = 0


class Prog:
    ENG = ("pe", "act", "dve", "pool", "sp")

    def __init__(self, nc):
        self.nc = nc
        self.streams = {e: [] for e in self.ENG}
        self.nops = {e: 0 for e in self.ENG}
        self.known = {e: {} for e in self.ENG}
        self.signal = {e: set() for e in self.ENG}
        self.sigcount = {e: 0 for e in self.ENG}
        self.cnt = {e: {} for e in self.ENG}
        self.sems = {}
        self._semctx = []
        self.dstreams = []
        self.tag = ""
        for e in self.ENG:
            self.sems[("E", e)] = self._new_sem("sem_" + e)

    def _new_sem(self, name):
        ctx = self.nc.semaphore(name)
        s = ctx.__enter__()
        self._semctx.append(ctx)
        return s

    def close(self):
        for ctx in reversed(self._semctx):
            ctx.__exit__(None, None, None)

    def _wait(self, eng, key, val):
        k = self.known[eng]
        if k.get(key, -1) >= val:
            return
        k[key] = val
        if key[0] == "E":
            self.signal[key[1]].add(val)
        self.streams[eng].append(("wait", key, val))

    def _deps(self, eng, reads, writes, pwrites):
        me = ("E", eng)
        for res in reads:
            for key, val in res.w.items():
                if key == me and eng == "pe":
                    continue
                self._wait(eng, key, val)
        for res in writes:
            for key, val in list(res.w.items()) + list(res.r.items()):
                if key == me and eng == "pe":
                    continue
                self._wait(eng, key, val)
        for res in pwrites:
            for key, val in list(res.r.items()) + list(res.pr.items()):
                if key == me and eng == "pe":
                    continue
                self._wait(eng, key, val)

    def _mark(self, key, val, reads, writes, pwrites):
        for res in reads:
            if res.r.get(key, -1) < val:
                res.r[key] = val
        for res in writes:
            pr = dict(res.w)
            for k_, v_ in res.r.items():
                if pr.get(k_, -1) < v_:
                    pr[k_] = v_
            res.pr = pr
            res.w = {key: val}
            res.r = {}
        for res in pwrites:
            if res.w.get(key, -1) < val:
                res.w[key] = val

    def op(self, eng, fn, reads=(), writes=(), pwrites=()):
        self._deps(eng, reads, writes, pwrites)
        idx = self.nops[eng]
        self.nops[eng] += 1
        self.streams[eng].append(("op", fn, idx, self.tag))
        self._mark(("E", eng), idx, reads, writes, pwrites)

    def dma(self, eng, fn, stream, reads=(), writes=(), pwrites=()):
        self._deps(eng, reads, writes, pwrites)
        if stream.dsem is None:
            stream.dsem = {}
            stream.dcnt = {}
        key = ("D", id(stream), eng)
        if eng not in stream.dsem:
            stream.dsem[eng] = self._new_sem("d_%s_%s" % (stream.name, eng))
            stream.dcnt[eng] = 0
            self.sems[key] = stream.dsem[eng]
            self.dstreams.append((stream, eng))
        stream.dcnt[eng] += 1
        val = 16 * stream.dcnt[eng]
        self.streams[eng].append(("dma", fn, stream.dsem[eng]))
        self._mark(key, val, reads, writes, pwrites)

    def barrier(self):
        for e in self.ENG:
            for f in self.ENG:
                if f != e and f != "sp" and self.nops[f] > 0:
                    self._wait(e, ("E", f), self.nops[f] - 1)
            for s, q in self.dstreams:
                self._wait(e, ("D", id(s), q), 16 * s.dcnt[q])

    def emit(self):
        nc = self.nc
        for e in self.ENG:
            for idx in sorted(self.signal[e]):
                if idx not in self.cnt[e]:
                    self.sigcount[e] += 1
                    self.cnt[e][idx] = self.sigcount[e]
            self.signal[e] = set()
        streams = self.streams
        self.streams = {e: [] for e in self.ENG}

        def make(e):
            stream = streams[e]

            def body(engine):
                for ent in stream:
                    if ent[0] == "wait":
                        _, key, val = ent
                        v = self.cnt[key[1]][val] if key[0] == "E" else val
                        engine.wait_ge(self.sems[key], v)
                    elif ent[0] == "op":
                        _, fn, idx, tag = ent
                        ins = fn()
                        if tag:
                            ins.annotate(tag)
                        if idx in self.cnt[e]:
                            ins.then_inc(self.sems[("E", e)], 1)
                    else:
                        _, fn, sem = ent
                        try:
                            ins = fn()
                        except Exception:
                            print("DMA build failed:", fn.func.__name__, {k: str(v)[:200] for k, v in fn.keywords.items()})
                            raise
                        ins.then_inc(sem, 16)
            return body

        with nc.Block() as block:
            block.tensor(make("pe"))
            block.scalar(make("act"))
            block.vector(make("dve"))
            block.gpsimd(make("pool"))
            block.sync(make("sp"))


SKIP_T = 48.0


def build(dbg=False, upto="E", CLS=None, DMIN=None, skip_t=None):
    global SKIP_T
    if skip_t is not None:
        SKIP_T = float(skip_t)
    if CLS is None:
        CLS = np.zeros((16, NT), np.int64)
        DMIN = np.zeros((16, NT), np.float64)
    nc = bass.Bass("TRN2", target_bir_lowering=False)

    def din(name, shape, dt=F32):
        return nc.dram_tensor(name, list(shape), dt, kind="ExternalInput").ap()

    def dscr(name, shape, dt=BF16):
        return nc.dram_tensor(name, list(shape), dt, kind="ExternalOutput" if dbg else "Internal").ap()

    x_d = din("x", [T, D])
    pos_d = din("pos", [T], I32)
    post_d = din("pos_t", [128, NT], I32)
    win_d = din("w_in", [D, 5664])
    bg_d = din("b_gate", [2048])
    wuq_d = din("w_uq", [256, 1536])
    wukv_d = din("w_ukv", [256, 2048])
    wout_d = din("w_out", [D, D])
    rw_d = din("router_w", [D, NE])
    wg_d = din("w_gate", [NE, D, FF])
    wu_d = din("w_up", [NE, D, FF])
    wd_d = din("w_down", [NE, FF, D])
    vec_d = {n: din(n, [k]) for n, k in [("attn_norm_w", 1024), ("q_norm_w", 256), ("kv_norm_w", 256),
                                          ("q_hn", 96), ("k_hn", 96), ("dq_hn", 64), ("dk_hn", 64),
                                          ("lam", 256), ("subln", 128), ("ffn_norm_w", 1024)]}
    ident_d = din("ident", [128, 128])
    invf_d = din("invf", [16])
    iota_d = din("iota512", [512])
    slopes_d = din("slopes", [8])
    tokpi_d = din("tokpi", [128, NT, 2])
    augk_d = din("augk", [4, T])
    augq_d = din("augq", [4, T])
    out_d = nc.dram_tensor("out", [T, D], F32, kind="ExternalOutput").ap()

    QmT = dscr("QmT", [16, 96, T])
    KmT = dscr("KmT", [16, 96, T])
    Vm = dscr("Vm", [T, 1024])
    QdT = dscr("QdT", [8, 128, T])
    KdT = dscr("KdT", [8, 128, T])
    Vd = dscr("Vd", [T, 1024])
    G = dscr("G", [T, 2048])
    H2 = dscr("H2", [T, D])
    DBG = dbg
    dbg_idx = nc.dram_tensor("dbg_idx", [4, 128, 1], I32, kind="ExternalOutput").ap() if dbg else None
    dbg_gate = nc.dram_tensor("dbg_gate", [128, 4], F32, kind="ExternalOutput").ap() if dbg else None
    dbg_xe = nc.dram_tensor("dbg_xe", [128, 1024], BF16, kind="ExternalOutput").ap() if dbg else None
    dbg_ye = nc.dram_tensor("dbg_ye", [128, 1024], F32, kind="ExternalOutput").ap() if dbg else None
    dbg_posm = nc.dram_tensor("dbg_posm", [128, NT * NE], F32, kind="ExternalOutput").ap() if dbg else None
    dbg_aff = nc.dram_tensor("dbg_aff", [128, NT * NE], F32, kind="ExternalOutput").ap() if dbg else None
    dbg_hT = nc.dram_tensor("dbg_hT", [128, 16 * CAP], BF16, kind="ExternalOutput").ap() if dbg else None
    PQHL = dscr("PQHL", [8, 2, T])
    MG = dscr("MG", [T, D])
    RQmT, RKmT, RVm, RQdT, RKdT, RVd, RG, RH2, RMG, Rout = [R(n) for n in
                                                          "QmT KmT Vm QdT KdT Vd G H2 MG out".split()]

    P = Prog(nc)
    sb = nc.sbuf_tensor
    with ExitStack() as es1:
        identb = es1.enter_context(sb("identb", [128, 128], BF16))
        identf = es1.enter_context(sb("identf", [128, 128], F32))
        mhalf = es1.enter_context(sb("mhalf", [128, 64], F32))
        cosT = es1.enter_context(sb("cosT", [128, NT, 16], F32))
        sinT = es1.enter_context(sb("sinT", [128, NT, 16], F32))
        posf = es1.enter_context(sb("posf", [128, NT], F32))
        nlam = es1.enter_context(sb("nlam", [128, 2], F32))
        vq_hn = es1.enter_context(sb("vq_hn", [128, 96], F32))
        vk_hn = es1.enter_context(sb("vk_hn", [128, 96], F32))
        vdq_hn = es1.enter_context(sb("vdq_hn", [128, 64], F32))
        vdk_hn = es1.enter_context(sb("vdk_hn", [128, 64], F32))
        vsubln = es1.enter_context(sb("vsubln", [128, 128], F32))
        vqn = es1.enter_context(sb("vqn", [128, 256], F32))
        vkvn = es1.enter_context(sb("vkvn", [128, 256], F32))
        Rid, Rmh, Rcs, Rposf, Rnlam, Rvec = [R(n) for n in "id mh cs posf nlam vec".split()]

        with ExitStack() as es2:
            posi = es2.enter_context(sb("p0_posi", [128, NT], I32))
            invf = es2.enter_context(sb("p0_invf", [128, 16], F32))
            ang = es2.enter_context(sb("p0_ang", [128, NT, 16], F32))
            kf = es2.enter_context(sb("p0_kf", [128, NT, 16], F32))
            ki = es2.enter_context(sb("p0_ki", [128, NT, 16], I32))
            mm_ = es2.enter_context(sb("p0_m", [128, NT, 16], F32))
            lamt = es2.enter_context(sb("p0_lam", [128, 256], F32))
            lp = es2.enter_context(sb("p0_lp", [128, 128], F32))
            ls = es2.enter_context(sb("p0_ls", [128, 4], F32))
            Rt = R("p0tmp")
            P.dma("sp", I(nc.sync.dma_start, out=identf[:], in_=ident_d[:, :]), Rid, pwrites=[Rid])
            P.dma("pool", I(nc.gpsimd.dma_start, out=identb[:], in_=ident_d[:, :]), Rid, pwrites=[Rid])
            P.op("pool", I(nc.gpsimd.memset, mhalf[:], -0.5), writes=[Rmh])
            for tl, nm in [(vq_hn, "q_hn"), (vk_hn, "k_hn"), (vdq_hn, "dq_hn"), (vdk_hn, "dk_hn"),
                           (vsubln, "subln"), (vqn, "q_norm_w"), (vkvn, "kv_norm_w"), (lamt, "lam")]:
                P.dma("sp", I(nc.sync.dma_start, out=tl[:], in_=vec_d[nm].partition_broadcast(128)), Rvec,
                      pwrites=[Rvec])
            P.dma("sp", I(nc.sync.dma_start, out=invf[:], in_=invf_d.partition_broadcast(128)), Rvec, pwrites=[Rvec])
            P.dma("sp", I(nc.sync.dma_start, out=posi[:], in_=post_d[:, :]), Rvec,
                  pwrites=[Rvec])
            P.op("dve", I(nc.vector.tensor_scalar, out=vq_hn[:], in0=vq_hn[:], scalar1=96.0 ** -0.5, scalar2=None,
                          op0=ALU.mult), reads=[Rvec], pwrites=[Rvec])
            P.op("dve", I(nc.vector.tensor_scalar, out=vdq_hn[:], in0=vdq_hn[:], scalar1=64.0 ** -0.5, scalar2=None,
                          op0=ALU.mult), reads=[Rvec], pwrites=[Rvec])
            P.op("dve", I(nc.vector.tensor_scalar, out=vsubln[:], in0=vsubln[:], scalar1=1.0 - LAM_INIT, scalar2=None,
                          op0=ALU.mult), reads=[Rvec], pwrites=[Rvec])
            P.op("dve", I(nc.vector.tensor_tensor, out=lp[:].rearrange("p (a b) -> p a b", a=2),
                          in0=lamt[:].rearrange("p (a two b) -> p a two b", a=2, two=2)[:, :, 0, :],
                          in1=lamt[:].rearrange("p (a two b) -> p a two b", a=2, two=2)[:, :, 1, :], op=ALU.mult),
                 reads=[Rvec], writes=[Rt])
            P.op("dve", I(nc.vector.tensor_reduce, out=ls[:, 0:2], in_=lp[:].rearrange("p (a b) -> p a b", a=2),
                          axis=AX.X, op=ALU.add), reads=[Rt], writes=[Rnlam])
            P.op("act", I(nc.scalar.activation, out=ls[:, 2:4], in_=ls[:, 0:2], func=AF.Exp), reads=[Rnlam],
                 writes=[Rt])
            P.op("dve", I(nc.vector.tensor_tensor, out=nlam[:, 0:1], in0=ls[:, 3:4], in1=ls[:, 2:3], op=ALU.subtract),
                 reads=[Rt], writes=[Rnlam])
            P.op("dve", I(nc.vector.tensor_scalar, out=nlam[:, 0:1], in0=nlam[:, 0:1], scalar1=-LAM_INIT, scalar2=None,
                          op0=ALU.add), reads=[Rnlam], writes=[Rnlam])
            P.op("dve", I(nc.vector.tensor_copy, out=posf[:], in_=posi[:]), reads=[Rvec], writes=[Rposf])
            P.op("dve", I(nc.vector.tensor_tensor, out=ang[:], in0=posf[:].unsqueeze(2).to_broadcast([128, NT, 16]),
                          in1=invf[:].unsqueeze(1).to_broadcast([128, NT, 16]), op=ALU.mult),
                 reads=[Rposf, Rvec], writes=[Rt])

            Rk = R("rk")

            def reduce_sin(dst, shift):
                P.op("dve", I(nc.vector.tensor_scalar, out=kf[:], in0=ang[:], scalar1=1.0 / TWO_PI,
                              scalar2=0.5 + shift / TWO_PI, op0=ALU.mult, op1=ALU.add), reads=[Rt], writes=[Rk])
                P.op("dve", I(nc.vector.tensor_copy, out=ki[:], in_=kf[:]), reads=[Rk], writes=[Rk])
                P.op("dve", I(nc.vector.tensor_copy, out=kf[:], in_=ki[:]), reads=[Rk], writes=[Rk])
                c1 = 6.28125
                c2 = TWO_PI - c1
                P.op("dve", I(nc.vector.scalar_tensor_tensor, out=mm_[:], in0=kf[:], scalar=-c1, in1=ang[:],
                              op0=ALU.mult, op1=ALU.add), reads=[Rk, Rt], writes=[Rk])
                P.op("dve", I(nc.vector.scalar_tensor_tensor, out=mm_[:], in0=kf[:], scalar=-c2, in1=mm_[:],
                              op0=ALU.mult, op1=ALU.add), reads=[Rk], writes=[Rk])
                if shift:
                    P.op("dve", I(nc.vector.tensor_scalar, out=mm_[:], in0=mm_[:], scalar1=shift, scalar2=None,
                                  op0=ALU.add), reads=[Rk], writes=[Rk])
                for thr, op_, adj in [(math.pi, ALU.is_gt, -TWO_PI), (-math.pi, ALU.is_lt, TWO_PI)]:
                    P.op("dve", I(nc.vector.tensor_scalar, out=kf[:], in0=mm_[:], scalar1=thr, scalar2=adj,
                                  op0=op_, op1=ALU.mult), reads=[Rk], writes=[Rk])
                    P.op("dve", I(nc.vector.tensor_tensor, out=mm_[:], in0=mm_[:], in1=kf[:], op=ALU.add),
                         reads=[Rk], writes=[Rk])
                P.op("dve", I(nc.vector.tensor_scalar, out=mm_[:], in0=mm_[:], scalar1=math.pi, scalar2=-math.pi,
                              op0=ALU.min, op1=ALU.max), reads=[Rk], writes=[Rk])
                P.op("act", I(nc.scalar.activation, out=dst[:], in_=mm_[:], func=AF.Sin), reads=[Rk], pwrites=[Rcs])

            reduce_sin(sinT, 0.0)
            reduce_sin(cosT, math.pi / 2)
            P.barrier()
            P.emit()

        env = locals()
        if upto >= "A":
            phaseA(nc, P, env)
        if upto >= "B":
            phaseBC(nc, P, env, upto)
        if upto >= "D":
            phaseDE(nc, P, env, upto)
        P.barrier()
        P.emit()
    P.close()
    return nc


def phaseA(nc, P, env):
    g = env
    sb = nc.sbuf_tensor
    x_d, win_d, bg_d, wuq_d, wukv_d, vec_d = g["x_d"], g["win_d"], g["bg_d"], g["wuq_d"], g["wukv_d"], g["vec_d"]
    identb, mhalf, cosT, sinT = g["identb"], g["mhalf"], g["cosT"], g["sinT"]
    vq_hn, vk_hn, vdq_hn, vdk_hn, vqn, vkvn = g["vq_hn"], g["vk_hn"], g["vdq_hn"], g["vdk_hn"], g["vqn"], g["vkvn"]
    Rid, Rmh, Rcs, Rvec = g["Rid"], g["Rmh"], g["Rcs"], g["Rvec"]
    QmT, KmT, Vm, QdT, KdT, Vd, G = g["QmT"], g["KmT"], g["Vm"], g["QdT"], g["KdT"], g["Vd"], g["G"]
    RQmT, RKmT, RVm, RQdT, RKdT, RVd, RG = g["RQmT"], g["RKmT"], g["RVm"], g["RQdT"], g["RKdT"], g["RVd"], g["RG"]
    with ExitStack() as es3:
        win = es3.enter_context(sb("a_win", [128, 8, 5664], BF16))
        wuq = es3.enter_context(sb("a_wuq", [128, 2, 1536], BF16))
        wukv = es3.enter_context(sb("a_wukv", [128, 2, 2048], BF16))
        wn = es3.enter_context(sb("a_wn", [128, 1024], F32))
        biasr = es3.enter_context(sb("a_bias", [1, 2048], BF16))
        onesr = es3.enter_context(sb("a_ones", [1, 128], BF16))
        xt0 = es3.enter_context(sb("a_xt0", [128, 1024], F32))
        xt1 = es3.enter_context(sb("a_xt1", [128, 1024], F32))
        hb = es3.enter_context(sb("a_hb", [128, 1024], BF16))
        hT = es3.enter_context(sb("a_hT", [128, 1024], BF16))
        sq = es3.enter_context(sb("a_sq", [128, 2048], F32))
        st = es3.enter_context(sb("a_st", [128, 64], F32))
        rstd = es3.enter_context(sb("a_rstd", [128, 64], F32))
        lat = es3.enter_context(sb("a_lat", [128, 512], BF16))
        latT = es3.enter_context(sb("a_latT", [128, 4, 128], BF16))
        qf = es3.enter_context(sb("a_qf", [128, 16, 96], F32))
        qb = es3.enter_context(sb("a_qb", [128, 16, 96], BF16))
        kf = es3.enter_context(sb("a_kf", [128, 16, 32], F32))
        kb = es3.enter_context(sb("a_kb", [128, 16, 96], BF16))
        rt = es3.enter_context(sb("a_rt", [128, 4, 16, 16], F32))
        vb = es3.enter_context(sb("a_vb", [128, 1024], BF16))
        qTs = es3.enter_context(sb("a_qTs", [128, 16, 128], BF16))
        kTs = es3.enter_context(sb("a_kTs", [128, 16, 128], BF16))
        df = es3.enter_context(sb("a_df", [128, 512], F32))
        dqb = es3.enter_context(sb("a_dqb", [128, 1024], BF16))
        dkb = es3.enter_context(sb("a_dkb", [128, 1024], BF16))
        dvb = es3.enter_context(sb("a_dvb", [128, 1024], BF16))
        dqTs = es3.enter_context(sb("a_dqTs", [128, 8, 128], BF16))
        dkTs = es3.enter_context(sb("a_dkTs", [128, 8, 128], BF16))
        gt = es3.enter_context(sb("a_gt", [128, 512], BF16))
        gb = es3.enter_context(sb("a_gb", [128, 2048], BF16))
        kr = es3.enter_context(sb("a_kr", [128, 32], F32))
        krw = es3.enter_context(sb("a_krw", [128, 32], F32))
        psF = es3.enter_context(nc.psum_tensor("a_psF", [128, 6 * 512], F32))
        psB = es3.enter_context(nc.psum_tensor("a_psB", [128, 2 * 1024], BF16))
        Rw = R("aw")
        P.dma("pool", I(nc.gpsimd.dma_start, out=win[:], in_=win_d.rearrange("(kc p) c -> p kc c", p=128)), Rw,
              pwrites=[Rw])
        P.dma("pool", I(nc.gpsimd.dma_start, out=wuq[:], in_=wuq_d.rearrange("(kc p) c -> p kc c", p=128)), Rw,
              pwrites=[Rw])
        P.dma("pool", I(nc.gpsimd.dma_start, out=wukv[:], in_=wukv_d.rearrange("(kc p) c -> p kc c", p=128)), Rw,
              pwrites=[Rw])
        P.dma("pool", I(nc.gpsimd.dma_start, out=biasr[:], in_=bg_d.rearrange("(o c) -> o c", o=1)), Rw, pwrites=[Rw])
        P.dma("sp", I(nc.sync.dma_start, out=wn[:], in_=vec_d["attn_norm_w"].partition_broadcast(128)), Rw,
              pwrites=[Rw])
        P.op("pool", I(nc.gpsimd.memset, onesr[:], 1.0), pwrites=[Rw])

        xts = [xt0, xt1]
        Rxt = [R("xt0"), R("xt1")]
        Rz = [R("z0"), R("z1")]
        zb = [psF[:, 0:512], psF[:, 512:1024]]
        ps4 = psF[:, 1024:3072]
        Rps4 = R("ps4")
        Tb = [psB[:, 0:1024], psB[:, 1024:2048]]
        RT = [R("T0"), R("T1")]
        names = "hb hT sq stx stl stq stk stkr std lat latT qf qb kf kb rt vb qTs kTs df dqb dkb dvb dqTs dkTs gt gb kr krw"
        RR = {n: R(n) for n in names.split()}

        def rs(src, dst, n, Rs, w):
            P.op("dve", I(nc.vector.tensor_scalar, out=dst, in0=src, scalar1=1.0 / n, scalar2=EPS, op0=ALU.mult,
                          op1=ALU.add), reads=[Rs], writes=[Rs])
            P.op("pool", I(nc.gpsimd.tensor_tensor, out=dst, in0=dst, in1=mhalf[:, 0:w], op=ALU.pow),
                 reads=[Rs, Rmh], writes=[Rs])

        def zblock(i, blk):
            P.tag = "z%d" % blk
            bank = zb[blk % 2]
            Rb = Rz[blk % 2]
            ncols = 512 if blk < 11 else 32
            c0 = blk * 512
            gate = 7 <= blk <= 10
            for kc in range(8):
                P.op("pe", I(nc.tensor.matmul, bank[:, 0:ncols], lhsT=hT[:, kc * 128:(kc + 1) * 128],
                             rhs=win[:, kc, c0:c0 + ncols], start=(kc == 0), stop=(kc == 7 and not gate)),
                     reads=[RR["hT"], Rw], writes=[Rb] if kc == 0 else (), pwrites=[Rb] if kc else ())
            if gate:
                gc = (blk - 7) * 512
                P.op("pe", I(nc.tensor.matmul, bank[:, 0:512], lhsT=onesr[0:1, :], rhs=biasr[0:1, gc:gc + 512],
                             start=False, stop=True), reads=[Rw], pwrites=[Rb])
            return bank, Rb

        def rope(i, src, dst, Rsrc, Rdst):
            c = cosT[:, i, :].unsqueeze(1).to_broadcast([128, 16, 16])
            s = sinT[:, i, :].unsqueeze(1).to_broadcast([128, 16, 16])
            x1 = src[:, :, 0:16]
            x2 = src[:, :, 16:32]
            Rrt = RR["rt"]
            P.op("dve", I(nc.vector.tensor_tensor, out=rt[:, 0], in0=x1, in1=c, op=ALU.mult), reads=[Rsrc, Rcs],
                 pwrites=[Rrt])
            P.op("dve", I(nc.vector.tensor_tensor, out=rt[:, 1], in0=x2, in1=s, op=ALU.mult), reads=[Rsrc, Rcs],
                 pwrites=[Rrt])
            P.op("dve", I(nc.vector.tensor_tensor, out=rt[:, 2], in0=x1, in1=s, op=ALU.mult), reads=[Rsrc, Rcs],
                 pwrites=[Rrt])
            P.op("dve", I(nc.vector.tensor_tensor, out=rt[:, 3], in0=x2, in1=c, op=ALU.mult), reads=[Rsrc, Rcs],
                 pwrites=[Rrt])
            P.op("dve", I(nc.vector.tensor_tensor, out=dst[:, :, 0:16], in0=rt[:, 0], in1=rt[:, 1], op=ALU.subtract),
                 reads=[Rrt], pwrites=[Rdst])
            P.op("dve", I(nc.vector.tensor_tensor, out=dst[:, :, 16:32], in0=rt[:, 2], in1=rt[:, 3], op=ALU.add),
                 reads=[Rrt], pwrites=[Rdst])

        latT2 = [latT, es3.enter_context(sb("a_latT1", [128, 4, 128], BF16))]
        kr2 = [kr, es3.enter_context(sb("a_kr1", [128, 32], F32))]
        krw2 = [krw, es3.enter_context(sb("a_krw1", [128, 32], F32))]
        qb2 = [qb, es3.enter_context(sb("a_qb1", [128, 16, 96], BF16))]
        kb2 = [kb, es3.enter_context(sb("a_kb1", [128, 16, 96], BF16))]
        sq2 = sq[:, 512:2048]
        RlatT = [R("latT0"), R("latT1")]
        Rkr = [R("kr0"), R("kr1")]
        Rkrw = [R("krw0"), R("krw1")]
        Rstkr = [R("stkr0"), R("stkr1")]
        Rqb = [R("qb0"), R("qb1")]
        Rkb = [R("kb0"), R("kb1")]
        Rsq2 = R("sq2")

        dfr = [df, es3.enter_context(sb("a_df1", [128, 512], F32)), es3.enter_context(sb("a_df2", [128, 512], F32))]
        Rdfr = [R("df0"), R("df1"), R("df2")]
        Rstd = [R("std0"), R("std1")]
        dfc = [0]

        def dqk(i, blks, dstb, hn, Rdst, dTs, RdTs, dram, Rdram):
            t0 = i * 128
            for j, blk in enumerate(blks):
                bank, Rb = zblock(i, blk)
                k3 = dfc[0] % 3
                dfc[0] += 1
                dfk, Rdfk = dfr[k3], Rdfr[k3]
                c0 = 40 + 8 * (dfc[0] % 2)
                P.op("act", I(nc.scalar.copy, out=dfk[:], in_=bank[:, 0:512]), reads=[Rb], writes=[Rdfk])
                P.op("act", I(nc.scalar.activation, out=sq[:, 0:512], in_=dfk[:], func=AF.Square),
                     reads=[Rdfk], writes=[RR["sq"]])
                P.op("dve", I(nc.vector.tensor_reduce, out=st[:, c0:c0 + 8],
                              in_=sq[:, 0:512].rearrange("p (a b) -> p a b", a=8), axis=AX.X, op=ALU.add),
                     reads=[RR["sq"]], writes=[Rstd[dfc[0] % 2]])
                rs(st[:, c0:c0 + 8], rstd[:, c0:c0 + 8], 64.0, Rstd[dfc[0] % 2], 8)
                P.op("dve", I(nc.vector.tensor_tensor, out=dfk[:].rearrange("p (a b) -> p a b", a=8),
                              in0=dfk[:].rearrange("p (a b) -> p a b", a=8),
                              in1=rstd[:, c0:c0 + 8].unsqueeze(2).to_broadcast([128, 8, 64]),
                              op=ALU.mult), reads=[Rdfk, Rstd[dfc[0] % 2]], writes=[Rdfk])
                P.op("dve", I(nc.vector.tensor_tensor,
                              out=dstb[:, j * 512:(j + 1) * 512].rearrange("p (a b) -> p a b", a=8),
                              in0=dfk[:].rearrange("p (a b) -> p a b", a=8),
                              in1=hn[:].unsqueeze(1).to_broadcast([128, 8, 64]), op=ALU.mult),
                     reads=[Rdfk, Rvec], writes=[Rdst] if j == 0 else (), pwrites=[Rdst] if j else ())

        def dqkT(i, dstb, Rdst, dTs, RdTs, dram, Rdram):
            P.tag = "dqkT"
            t0 = i * 128
            for h in range(8):
                P.op("pe", I(nc.tensor.transpose, out=Tb[0][:, h * 128:(h + 1) * 128],
                             in_=dstb[:, h * 128:(h + 1) * 128], identity=identb[:]),
                     reads=[Rdst, Rid], writes=[RT[0]] if h == 0 else (), pwrites=[RT[0]] if h else ())
            P.op("act", I(nc.scalar.copy, out=dTs[:].rearrange("p a b -> p (a b)"), in_=Tb[0][:, 0:1024]),
                 reads=[RT[0]], writes=[RdTs])
            P.dma("sp", I(nc.sync.dma_start, out=dram[:, :, t0:t0 + 128].rearrange("h d t -> d h t"), in_=dTs[:]),
                  RdTs, reads=[RdTs], pwrites=[Rdram])

        xts3 = [xt0, xt1, es3.enter_context(sb("a_xt2", [128, 1024], F32))]
        Rxt3 = [R("xt0"), R("xt1"), R("xt2")]
        hb2 = [hb, es3.enter_context(sb("a_hb1", [128, 1024], BF16))]
        Rhb = [R("hb0"), R("hb1")]
        Rstx = [R("stx0"), R("stx1")]

        def S1a(i):
            P.tag = "S1a"
            s_ = i % 2
            xt = xts3[i % 3]
            Rx = Rxt3[i % 3]
            t0 = i * 128
            c = 56 + 2 * s_
            P.dma("sp", I(nc.sync.dma_start, out=xt[:], in_=x_d[t0:t0 + 128, :]), Rx, writes=[Rx])
            P.op("act", I(nc.scalar.activation, out=sq[:, 0:1024], in_=xt[:], func=AF.Square,
                          accum_out=st[:, c:c + 1]), reads=[Rx], writes=[RR["sq"], Rstx[s_]])
            rs(st[:, c:c + 1], rstd[:, c:c + 1], 1024.0, Rstx[s_], 1)
            P.op("dve", I(nc.vector.scalar_tensor_tensor, out=hb2[s_][:], in0=xt[:], scalar=rstd[:, c:c + 1], in1=wn[:],
                          op0=ALU.mult, op1=ALU.mult), reads=[Rx, Rstx[s_], Rw], writes=[Rhb[s_]])

        def S1(i):
            P.tag = "S1head"
            s_ = i % 2
            t0 = i * 128
            for kc in range(8):
                P.op("pe", I(nc.tensor.transpose, out=Tb[0][:, kc * 128:(kc + 1) * 128],
                             in_=hb2[s_][:, kc * 128:(kc + 1) * 128], identity=identb[:]),
                     reads=[Rhb[s_], Rid], writes=[RT[0]] if kc == 0 else (), pwrites=[RT[0]] if kc else ())
            P.op("act", I(nc.scalar.copy, out=hT[:], in_=Tb[0][:, 0:1024]), reads=[RT[0]], writes=[RR["hT"]])
            bank, Rb = zblock(i, 0)
            k3 = dfc[0] % 3
            dfc[0] += 1
            latf, Rlatf = dfr[k3], Rdfr[k3]
            P.op("act", I(nc.scalar.copy, out=latf[:], in_=bank[:, 0:512]), reads=[Rb], writes=[Rlatf])
            P.op("act", I(nc.scalar.activation, out=sq[:, 0:512], in_=latf[:], func=AF.Square), reads=[Rlatf],
                 writes=[RR["sq"]])
            P.op("dve", I(nc.vector.tensor_reduce, out=st[:, 1:3], in_=sq[:, 0:512].rearrange("p (a b) -> p a b", a=2),
                          axis=AX.X, op=ALU.add), reads=[RR["sq"]], writes=[RR["stl"]])
            rs(st[:, 1:3], rstd[:, 1:3], 256.0, RR["stl"], 2)
            P.op("dve", I(nc.vector.scalar_tensor_tensor, out=lat[:, 0:256], in0=latf[:, 0:256], scalar=rstd[:, 1:2],
                          in1=vqn[:], op0=ALU.mult, op1=ALU.mult), reads=[Rlatf, RR["stl"], Rvec], writes=[RR["lat"]])
            P.op("dve", I(nc.vector.scalar_tensor_tensor, out=lat[:, 256:512], in0=latf[:, 256:512],
                          scalar=rstd[:, 2:3], in1=vkvn[:], op0=ALU.mult, op1=ALU.mult),
                 reads=[Rlatf, RR["stl"], Rvec], pwrites=[RR["lat"]])
            dqk(i, [1, 2], dqb, vdq_hn, RR["dqb"], dqTs, RR["dqTs"], QdT, RQdT)
            P.tag = "latT"
            for j in range(4):
                P.op("pe", I(nc.tensor.transpose, out=Tb[1][:, j * 128:(j + 1) * 128],
                             in_=lat[:, j * 128:(j + 1) * 128], identity=identb[:]),
                     reads=[RR["lat"], Rid], writes=[RT[1]] if j == 0 else (), pwrites=[RT[1]] if j else ())
            P.op("dve", I(nc.vector.tensor_copy, out=latT2[s_][:].rearrange("p a b -> p (a b)"), in_=Tb[1][:, 0:512]),
                 reads=[RT[1]], writes=[RlatT[s_]])
            dqk(i, [3, 4], dkb, vdk_hn, RR["dkb"], dkTs, RR["dkTs"], KdT, RKdT)
            for j, blk in enumerate([5, 6]):
                bank, Rb = zblock(i, blk)
                P.op("act", I(nc.scalar.copy, out=dvb[:, j * 512:(j + 1) * 512], in_=bank[:, 0:512]), reads=[Rb],
                     writes=[RR["dvb"]] if j == 0 else (), pwrites=[RR["dvb"]] if j else ())
            P.dma("sp", I(nc.sync.dma_start, out=Vd[t0:t0 + 128, :], in_=dvb[:]), RR["dvb"], reads=[RR["dvb"]],
                  pwrites=[RVd])
            dqkT(i, dqb, RR["dqb"], dqTs, RR["dqTs"], QdT, RQdT)
            for j, blk in enumerate([7, 8, 9, 10]):
                if j == 2:
                    dqkT(i, dkb, RR["dkb"], dkTs, RR["dkTs"], KdT, RKdT)
                bank, Rb = zblock(i, blk)
                P.op("act", I(nc.scalar.activation, out=gt[:], in_=bank[:, 0:512], func=AF.Tanh, scale=0.5),
                     reads=[Rb], writes=[RR["gt"]])
                P.op("dve", I(nc.vector.tensor_scalar, out=gb[:, j * 512:(j + 1) * 512], in0=gt[:], scalar1=0.5,
                              scalar2=0.5, op0=ALU.mult, op1=ALU.add), reads=[RR["gt"]],
                     writes=[RR["gb"]] if j == 0 else (), pwrites=[RR["gb"]] if j else ())
            P.dma("sp", I(nc.sync.dma_start, out=G[t0:t0 + 128, :], in_=gb[:]), RR["gb"], reads=[RR["gb"]],
                  pwrites=[RG])
            bank, Rb = zblock(i, 11)
            P.op("act", I(nc.scalar.copy, out=kr2[s_][:], in_=bank[:, 0:32]), reads=[Rb], writes=[Rkr[s_]])
            P.op("act", I(nc.scalar.activation, out=sq[:, 0:32], in_=kr2[s_][:], func=AF.Square,
                          accum_out=st[:, 36 + s_:37 + s_]), reads=[Rkr[s_]], writes=[RR["sq"], Rstkr[s_]])
            P.op("dve", I(nc.vector.tensor_tensor, out=krw2[s_][:], in0=kr2[s_][:], in1=vk_hn[:, 64:96], op=ALU.mult),
                 reads=[Rkr[s_], Rvec], writes=[Rkrw[s_]])

        def S2q(i):
            P.tag = "S2q"
            s_ = i % 2
            for b_ in range(4):
                for kc in range(2):
                    P.op("pe", I(nc.tensor.matmul, ps4[:, b_ * 512:b_ * 512 + 384], lhsT=latT2[s_][:, kc, :],
                                 rhs=wuq[:, kc, b_ * 384:(b_ + 1) * 384], start=(kc == 0), stop=(kc == 1)),
                         reads=[RlatT[s_], Rw], writes=[Rps4] if (b_ == 0 and kc == 0) else (),
                         pwrites=() if (b_ == 0 and kc == 0) else [Rps4])
            qv = ps4.rearrange("p (b x) -> p b x", b=4)[:, :, 0:384].rearrange("p b (h d) -> p b h d", h=4)
            sqv = sq2.rearrange("p (b h d) -> p b h d", b=4, h=4)
            qfv = qf[:].rearrange("p (b h) d -> p b h d", b=4)
            P.op("act", I(nc.scalar.copy, out=qfv, in_=qv), reads=[Rps4], writes=[RR["qf"]])
            P.op("act", I(nc.scalar.activation, out=sqv, in_=qfv, func=AF.Square), reads=[RR["qf"]], writes=[Rsq2])
            P.op("dve", I(nc.vector.tensor_reduce, out=st[:, 4:20].rearrange("p (b h) -> p b h", b=4), in_=sqv,
                          axis=AX.X, op=ALU.add), reads=[Rsq2], writes=[RR["stq"]])
            rs(st[:, 4:20], rstd[:, 4:20], 96.0, RR["stq"], 16)
            P.op("dve", I(nc.vector.tensor_tensor, out=qfv, in0=qfv,
                          in1=rstd[:, 4:20].rearrange("p (b h) -> p b h", b=4).unsqueeze(3).to_broadcast(
                              [128, 4, 4, 96]), op=ALU.mult), reads=[RR["qf"], RR["stq"]], writes=[RR["qf"]])
            P.op("dve", I(nc.vector.tensor_tensor, out=qf[:], in0=qf[:],
                          in1=vq_hn[:].unsqueeze(1).to_broadcast([128, 16, 96]), op=ALU.mult),
                 reads=[RR["qf"], Rvec], writes=[RR["qf"]])
            P.op("dve", I(nc.vector.tensor_copy, out=qb2[s_][:, :, 0:64], in_=qf[:, :, 0:64]), reads=[RR["qf"]],
                 writes=[Rqb[s_]])
            rope(i, qf[:, :, 64:96], qb2[s_][:, :, 64:96], RR["qf"], Rqb[s_])

        def S2k(i):
            P.tag = "S2k"
            s_ = i % 2
            t0 = i * 128
            for b_ in range(4):
                for kc in range(2):
                    P.op("pe", I(nc.tensor.matmul, ps4[:, b_ * 512:(b_ + 1) * 512], lhsT=latT2[s_][:, 2 + kc, :],
                                 rhs=wukv[:, kc, b_ * 512:(b_ + 1) * 512], start=(kc == 0), stop=(kc == 1)),
                         reads=[RlatT[s_], Rw], writes=[Rps4] if (b_ == 0 and kc == 0) else (),
                         pwrites=() if (b_ == 0 and kc == 0) else [Rps4])
            kvv = ps4.rearrange("p (h d) -> p h d", h=16)
            sqk = sq2[:, 0:1024].rearrange("p (h d) -> p h d", h=16)
            P.op("act", I(nc.scalar.activation, out=sqk, in_=kvv[:, :, 0:64], func=AF.Square), reads=[Rps4],
                 writes=[Rsq2])
            P.op("dve", I(nc.vector.tensor_reduce, out=st[:, 20:36], in_=sqk, axis=AX.X, op=ALU.add),
                 reads=[Rsq2], writes=[RR["stk"]])
            P.op("dve", I(nc.vector.tensor_scalar, out=st[:, 20:36], in0=st[:, 20:36], scalar1=st[:, 36 + s_:37 + s_],
                          scalar2=None, op0=ALU.add), reads=[RR["stk"], Rstkr[s_]], writes=[RR["stk"]])
            rs(st[:, 20:36], rstd[:, 20:36], 96.0, RR["stk"], 16)
            P.op("dve", I(nc.vector.tensor_tensor, out=qf[:, :, 0:64], in0=kvv[:, :, 0:64],
                          in1=rstd[:, 20:36].unsqueeze(2).to_broadcast([128, 16, 64]), op=ALU.mult),
                 reads=[Rps4, RR["stk"]], writes=[RR["qf"]])
            P.op("dve", I(nc.vector.tensor_tensor, out=kb2[s_][:, :, 0:64], in0=qf[:, :, 0:64],
                          in1=vk_hn[:, 0:64].unsqueeze(1).to_broadcast([128, 16, 64]), op=ALU.mult),
                 reads=[RR["qf"], Rvec], writes=[Rkb[s_]])
            P.op("dve", I(nc.vector.tensor_tensor, out=kf[:], in0=krw2[s_][:].unsqueeze(1).to_broadcast([128, 16, 32]),
                          in1=rstd[:, 20:36].unsqueeze(2).to_broadcast([128, 16, 32]), op=ALU.mult),
                 reads=[Rkrw[s_], RR["stk"]], writes=[RR["kf"]])
            rope(i, kf[:], kb2[s_][:, :, 64:96], RR["kf"], Rkb[s_])
            P.op("act", I(nc.scalar.copy, out=vb[:].rearrange("p (h d) -> p h d", h=16), in_=kvv[:, :, 64:128]),
                 reads=[Rps4], writes=[RR["vb"]])
            P.dma("sp", I(nc.sync.dma_start, out=Vm[t0:t0 + 128, :], in_=vb[:]), RR["vb"], reads=[RR["vb"]],
                  pwrites=[RVm])

        def S3(i):
            P.tag = "S3"
            s_ = i % 2
            t0 = i * 128
            for src, Rsrc, dst, Rdst, dram, Rdram in [(qb2[s_], Rqb[s_], qTs, RR["qTs"], QmT, RQmT),
                                                       (kb2[s_], Rkb[s_], kTs, RR["kTs"], KmT, RKmT)]:
                for half in range(2):
                    for hh in range(8):
                        h = half * 8 + hh
                        P.op("pe", I(nc.tensor.transpose, out=Tb[half][0:96, hh * 128:(hh + 1) * 128],
                                     in_=src[:, h, :], identity=identb[:]), reads=[Rsrc, Rid],
                             writes=[RT[half]] if hh == 0 else (), pwrites=[RT[half]] if hh else ())
                    eng = "act" if half == 0 else "dve"
                    fn = nc.scalar.copy if half == 0 else nc.vector.tensor_copy
                    P.op(eng, I(fn, out=dst[0:96, half * 8:(half + 1) * 8, :].rearrange("p a b -> p (a b)"),
                                in_=Tb[half][0:96, 0:1024]), reads=[RT[half]],
                         writes=[Rdst] if half == 0 else (), pwrites=[Rdst] if half else ())
                P.dma("sp", I(nc.sync.dma_start, out=dram[:, :, t0:t0 + 128].rearrange("h d t -> d h t"),
                              in_=dst[0:96, :, :]), Rdst, reads=[Rdst], pwrites=[Rdram])

        S1a(0)
        S1a(1)
        S1(0)
        for i in range(NT):
            if i + 1 < NT:
                S1(i + 1)
            if i >= 1:
                S3(i - 1)
            if i + 2 < NT:
                S1a(i + 2)
            S2q(i)
            S2k(i)
        S3(NT - 1)
        P.barrier()
        P.emit()


def skip_tile(h, q0, q1, k0, k1):
    return False


def phaseBC(nc, P, env, upto):
    g = env
    sb = nc.sbuf_tensor
    QmT, KmT, Vm, QdT, KdT, Vd, G, MG = [g[k] for k in "QmT KmT Vm QdT KdT Vd G MG".split()]
    RMG = g["RMG"]
    mhalf, nlam, vsubln, posf, pos_d = g["mhalf"], g["nlam"], g["vsubln"], g["posf"], g["pos_d"]
    Rmh, Rnlam, Rvec, Rposf = g["Rmh"], g["Rnlam"], g["Rvec"], g["Rposf"]
    with ExitStack() as es:
        omla = es.enter_context(sb("om", [128, NT, 1024], BF16))
        Rom = R("omla")
        with ExitStack() as es2:
            qT = [es2.enter_context(sb("b_qT%d" % k, [96, T], BF16)) for k in range(2)]
            kT = [es2.enter_context(sb("b_kT%d" % k, [96, T], BF16)) for k in range(2)]
            vv = [es2.enter_context(sb("b_v%d" % k, [128, NT, 65], BF16)) for k in range(2)]
            ga = [es2.enter_context(sb("b_ga%d" % k, [128, NT, 64], BF16)) for k in range(2)]
            pT = [es2.enter_context(sb("b_pT%d" % k, [128, 1536], BF16)) for k in range(3)]
            rc = es2.enter_context(sb("b_rc", [128, 8], F32))
            ps = es2.enter_context(nc.psum_tensor("b_ps", [128, 8 * 512], F32))
            Rq = [R("bq0"), R("bq1")]
            RpT = [R("pT0"), R("pT1"), R("pT2")]
            Rs = [R("bs0"), R("bs1")]
            Ro = [R("bo0"), R("bo1")]
            Rrc = R("brc")
            sbank = [ps[:, 1536 * k:1536 * (k + 1)] for k in range(2)]
            GRP = [(3 * g_, 3) for g_ in range(10)] + [(30, 2)]

            def obank(os_, qs):
                b0 = 3072 + 512 * os_ + 66 * qs
                return ps[:, b0:b0 + 65]
            for k in range(2):
                P.op("pool", I(nc.gpsimd.memset, vv[k][:, :, 64:65], 1.0), pwrites=[Rq[k]])
            step = 0
            for h in range(16):
                s_ = h % 2
                P.dma("sp", I(nc.sync.dma_start, out=qT[s_][:], in_=QmT[h, :, :]), Rq[s_], pwrites=[Rq[s_]])
                P.dma("sp", I(nc.sync.dma_start, out=kT[s_][:], in_=KmT[h, :, :]), Rq[s_], pwrites=[Rq[s_]])
                P.dma("sp", I(nc.sync.dma_start, out=vv[s_][:, :, 0:64],
                              in_=Vm[:, h * 64:(h + 1) * 64].rearrange("(i p) d -> p i d", p=128)), Rq[s_],
                      pwrites=[Rq[s_]])
                P.dma("sp", I(nc.sync.dma_start, out=ga[s_][:],
                              in_=G[:, h * 64:(h + 1) * 64].rearrange("(i p) d -> p i d", p=128)), Rq[s_],
                      pwrites=[Rq[s_]])
                steps = [(qb, kp) for qb in range(8) for kp in range(len(GRP))]

                def b_front(qb, kp, sb_, pb_, s_=s_):
                    P.tag = "Bf"
                    k0, nk = GRP[kp]
                    for kk in range(nk):
                        kt = k0 + kk
                        P.op("pe", I(nc.tensor.matmul, sbank[sb_][:, kk * 512:(kk + 1) * 512],
                                     lhsT=kT[s_][:, kt * 128:(kt + 1) * 128],
                                     rhs=qT[s_][:, qb * 512:(qb + 1) * 512], start=True, stop=True),
                             reads=[Rq[s_]], writes=[Rs[sb_]] if kk == 0 else (), pwrites=[Rs[sb_]] if kk else ())
                    P.op("act", I(nc.scalar.activation, out=pT[pb_][:, 0:512 * nk], in_=sbank[sb_][:, 0:512 * nk],
                                  func=AF.Exp), reads=[Rs[sb_]], writes=[RpT[pb_]])

                def b_back(qb, kp, pb_, s_=s_, h=h):
                    P.tag = "Bb"
                    os_ = (h * 8 + qb) % 2
                    k0, nk = GRP[kp]
                    for kk in range(nk):
                        kt = k0 + kk
                        for qs in range(4):
                            w_ = kt == 0 and qs == 0
                            P.op("pe", I(nc.tensor.matmul, obank(os_, qs),
                                         lhsT=pT[pb_][:, kk * 512 + qs * 128:kk * 512 + (qs + 1) * 128],
                                         rhs=vv[s_][:, kt, :], start=w_, stop=(kt == NT - 1), skip_group_check=True),
                                 reads=[RpT[pb_], Rq[s_]], writes=[Ro[os_]] if w_ else (),
                                 pwrites=() if w_ else [Ro[os_]])
                    if kp == len(GRP) - 1:
                        P.tag = "Bep"
                        ov = ps[:, 3072 + 512 * os_:3072 + 512 * os_ + 264].rearrange("p (q c) -> p q c", q=4)
                        P.op("dve", I(nc.vector.reciprocal, out=rc[:, 0:4], in_=ov[:, :, 64]), reads=[Ro[os_]],
                             writes=[Rrc])
                        for qs in range(4):
                            ti = qb * 4 + qs
                            P.op("dve", I(nc.vector.scalar_tensor_tensor, out=omla[:, ti, h * 64:(h + 1) * 64],
                                          in0=obank(os_, qs)[:, 0:64], scalar=rc[:, qs:qs + 1], in1=ga[s_][:, ti, :],
                                          op0=ALU.mult, op1=ALU.mult), reads=[Ro[os_], Rrc, Rq[s_]], pwrites=[Rom])

                ring = []
                for n in range(len(steps) + 1):
                    if n < len(steps):
                        sb_, pb_ = step % 2, step % 3
                        step += 1
                        b_front(steps[n][0], steps[n][1], sb_, pb_)
                        ring.append(pb_)
                    if n >= 1:
                        b_back(steps[n - 1][0], steps[n - 1][1], ring[n - 1])
            if upto < "C":
                for i in range(NT):
                    P.dma("sp", I(nc.sync.dma_start, out=MG[i * 128:(i + 1) * 128, :], in_=omla[:, i, :]), Rom,
                          reads=[Rom], pwrites=[RMG])
            P.barrier()
            P.emit()
        if upto < "C":
            return

        CLS, DMIN = g["CLS"], g["DMIN"]
        slp_d, PQHL = g["slopes_d"], g["PQHL"]
        augk_d, augq_d = g["augk_d"], g["augq_d"]
        RPQHL = R("PQHL")
        with ExitStack() as es2:
            pq = es2.enter_context(sb("c_pq", [128, T], F32))
            Rpq = R("cpq")
            with ExitStack() as es3:
                pqi = es3.enter_context(sb("c_pqi", [128, T], I32))
                ahi = es3.enter_context(sb("c_ahi", [8, T], BF16))
                alo = es3.enter_context(sb("c_alo", [8, T], BF16))
                shi = es3.enter_context(sb("c_shi", [8, T], BF16))
                slo = es3.enter_context(sb("c_slo", [8, T], BF16))
                msl = es3.enter_context(sb("c_msl", [8, 1], F32))
                Rt = R("ctmp")
                P.dma("sp", I(nc.sync.dma_start, out=pqi[:], in_=pos_d.partition_broadcast(128)), Rpq, writes=[Rpq])
                P.dma("sp", I(nc.sync.dma_start, out=msl[:], in_=slp_d.rearrange("(h o) -> h o", o=1)), Rt,
                      writes=[Rt])
                P.op("dve", I(nc.vector.tensor_copy, out=pq[:], in_=pqi[:]), reads=[Rpq], writes=[Rpq])
                P.op("dve", I(nc.vector.tensor_copy, out=ahi[:], in_=pq[0:8, :]), reads=[Rpq], pwrites=[Rt])
                P.op("dve", I(nc.vector.tensor_tensor, out=alo[:], in0=pq[0:8, :], in1=ahi[:], op=ALU.subtract),
                     reads=[Rpq, Rt], pwrites=[Rt])
                P.op("dve", I(nc.vector.tensor_scalar, out=shi[:], in0=ahi[:], scalar1=msl[:, 0:1], scalar2=None,
                              op0=ALU.mult), reads=[Rt], pwrites=[Rt])
                P.op("dve", I(nc.vector.tensor_scalar, out=slo[:], in0=alo[:], scalar1=msl[:, 0:1], scalar2=None,
                              op0=ALU.mult), reads=[Rt], pwrites=[Rt])
                P.dma("sp", I(nc.sync.dma_start, out=PQHL[:, 0, :], in_=shi[:]), Rt, reads=[Rt], pwrites=[RPQHL])
                P.dma("sp", I(nc.sync.dma_start, out=PQHL[:, 1, :], in_=slo[:]), Rt, reads=[Rt], pwrites=[RPQHL])
                P.barrier()
                P.emit()
            nposf = es2.enter_context(sb("c_nposf", [128, NT], F32))
            bia = es2.enter_context(sb("c_bia", [128, 2, 2, NT], F32))
            P.op("dve", I(nc.vector.tensor_scalar, out=nposf[:], in0=posf[:], scalar1=-1.0, scalar2=None,
                          op0=ALU.mult), reads=[Rposf], writes=[Rpq])
            qT = [[es2.enter_context(sb("c_qT%d%d" % (k, m), [72, T], BF16)) for m in range(2)] for k in range(2)]
            kT = [[es2.enter_context(sb("c_kT%d%d" % (k, m), [72, T], BF16)) for m in range(2)] for k in range(2)]
            vv = [es2.enter_context(sb("c_v%d" % k, [128, NT, 129], BF16)) for k in range(2)]
            gbt = [es2.enter_context(sb("c_gb%d" % k, [128, NT, 128], BF16)) for k in range(2)]
            pT2 = [es2.enter_context(sb("c_pT%d" % k, [128, 1024], BF16)) for k in range(2)]
            pT = [pT2[k // 2][:, (k % 2) * 512:(k % 2 + 1) * 512] for k in range(4)]
            sp_ = [es2.enter_context(sb("c_sp%d" % k, [128, 512], F32)) for k in range(2)]
            dt_ = [es2.enter_context(sb("c_dt%d" % k, [128, 256], F32)) for k in range(2)]
            rc = es2.enter_context(sb("c_rc", [128, 16], F32))
            of = es2.enter_context(sb("c_of", [128, 4, 128], F32))
            jk = es2.enter_context(sb("c_jk", [128, 2, 128], F32))
            ps = es2.enter_context(nc.psum_tensor("c_ps", [128, 8 * 512], F32))
            Rq = [R("cq0"), R("cq1")]
            Rgs = [R("cgs0"), R("cgs1")]
            Rbia = [R("cbia0"), R("cbia1")]
            RpT = [R("cpT%d" % k) for k in range(4)]
            Rsp = [R("csp%d" % k) for k in range(2)]
            Rdt = [R("cdt%d" % k) for k in range(2)]
            Rs = [R("cs%d" % k) for k in range(4)]
            Ro = [R("co0"), R("co1")]
            Rrc, Rof, Rjk = R("crc"), R("cof"), R("cjk")
            sbank = [ps[:, 512 * k:512 * (k + 1)] for k in range(4)]
            def oacc(s, mp, qs):
                b0 = 2048 + (2 * s + mp) * 512 + qs * 132
                return ps[:, b0:b0 + 129]

            def oset(s):
                return ps[:, 2048 + 2 * s * 512:2048 + (2 * s + 2) * 512]
            for k in range(2):
                P.op("pool", I(nc.gpsimd.memset, vv[k][:, :, 128:129], 1.0), pwrites=[Rq[k]])
                for m in range(2):
                    P.dma("pool", I(nc.gpsimd.dma_start, out=kT[k][m][64:66, :], in_=augk_d[0:2, :]), Rq[k], pwrites=[Rq[k]])
                    P.dma("pool", I(nc.gpsimd.dma_start, out=kT[k][m][68:70, :], in_=augk_d[2:4, :]), Rq[k], pwrites=[Rq[k]])
                    P.dma("pool", I(nc.gpsimd.dma_start, out=qT[k][m][66:68, :], in_=augq_d[0:2, :]), Rq[k], pwrites=[Rq[k]])
                    P.dma("pool", I(nc.gpsimd.dma_start, out=qT[k][m][70:72, :], in_=augq_d[2:4, :]), Rq[k], pwrites=[Rq[k]])
            step = 0
            dstep = 0
            oset_i = 0
            for h in range(8):
                s_ = h % 2
                slope = 2.0 ** (-(h + 1))
                for m in range(2):
                    P.dma("sp", I(nc.sync.dma_start, out=qT[s_][m][0:64, :], in_=QdT[h, m * 64:(m + 1) * 64, :]),
                          Rq[s_], pwrites=[Rq[s_]])
                    for a_ in range(2):
                        P.dma("sp", I(nc.sync.dma_start, out=qT[s_][m][64 + 4 * a_:66 + 4 * a_, :], in_=PQHL[h, :, :]),
                              Rq[s_], reads=[RPQHL], pwrites=[Rq[s_]])
                        P.dma("sp", I(nc.sync.dma_start, out=kT[s_][m][66 + 4 * a_:68 + 4 * a_, :], in_=PQHL[h, :, :]),
                              Rq[s_], reads=[RPQHL], pwrites=[Rq[s_]])
                    P.dma("sp", I(nc.sync.dma_start, out=kT[s_][m][0:64, :], in_=KdT[h, m * 64:(m + 1) * 64, :]),
                          Rq[s_], pwrites=[Rq[s_]])
                P.dma("sp", I(nc.sync.dma_start, out=vv[s_][:, :, 0:128],
                              in_=Vd[:, h * 128:(h + 1) * 128].rearrange("(i p) d -> p i d", p=128)), Rq[s_],
                      pwrites=[Rq[s_]])
                P.dma("sp", I(nc.sync.dma_start, out=gbt[s_][:],
                              in_=G[:, 1024 + h * 128:1024 + (h + 1) * 128].rearrange("(i p) d -> p i d", p=128)),
                      Rq[s_], pwrites=[Rq[s_]])
                P.op("dve", I(nc.vector.tensor_tensor, out=gbt[s_][:], in0=gbt[s_][:],
                              in1=vsubln[:].unsqueeze(1).to_broadcast([128, NT, 128]), op=ALU.mult),
                     reads=[Rq[s_], Rvec], writes=[Rgs[s_]])
                P.op("dve", I(nc.vector.tensor_scalar, out=bia[:, s_, 0, :], in0=posf[:], scalar1=slope, scalar2=None,
                              op0=ALU.mult), reads=[Rposf], writes=[Rbia[s_]])
                P.op("dve", I(nc.vector.tensor_scalar, out=bia[:, s_, 1, :], in0=posf[:], scalar1=-slope, scalar2=None,
                              op0=ALU.mult), reads=[Rposf], pwrites=[Rbia[s_]])
                steps = []
                for qb in range(16):
                    kts = [kt for kt in range(NT) if slope * DMIN[qb][kt] < SKIP_T]
                    for n_, kt in enumerate(kts):
                        steps.append((qb, kt, int(CLS[qb][kt]), n_ == 0, n_ == len(kts) - 1))

                def st1(n, r4, d2, s_=s_):
                    P.tag = "C1"
                    qb, kt, cl, first, last = steps[n]
                    q0 = qb * 256
                    K_ = (64, 68, 72)[cl]
                    for mp in range(2):
                        P.op("pe", I(nc.tensor.matmul, sbank[r4][:, mp * 256:(mp + 1) * 256],
                                     lhsT=kT[s_][mp][0:K_, kt * 128:(kt + 1) * 128],
                                     rhs=qT[s_][mp][0:K_, q0:q0 + 256], start=True, stop=True),
                             reads=[Rq[s_]], writes=[Rs[r4]] if mp == 0 else (),
                             pwrites=[Rs[r4]] if mp else ())
                    if cl == 0:
                        P.op("act", I(nc.scalar.activation, out=dt_[d2][:], in_=pq[:, q0:q0 + 256], func=AF.Abs,
                                      bias=nposf[:, kt:kt + 1]), reads=[Rpq], writes=[Rdt[d2]])

                def st23(n, r4, d2, s_=s_, slope=slope):
                    P.tag = "C2"
                    qb, kt, cl, first, last = steps[n]
                    if cl == 0:
                        P.op("dve", I(nc.vector.scalar_tensor_tensor,
                                      out=sp_[d2][:].rearrange("p (m q) -> p m q", m=2),
                                      in0=dt_[d2][:].unsqueeze(1).to_broadcast([128, 2, 256]), scalar=-slope,
                                      in1=sbank[r4].rearrange("p (m q) -> p m q", m=2), op0=ALU.mult, op1=ALU.add),
                             reads=[Rdt[d2], Rs[r4]], writes=[Rsp[d2]])
                        P.op("act", I(nc.scalar.activation, out=pT[r4], in_=sp_[d2][:], func=AF.Exp),
                             reads=[Rsp[d2]], writes=[RpT[r4]])
                    elif pair_first[n]:
                        pass
                    elif n >= 1 and pair_first[n - 1]:
                        ra = r4s[n - 1]
                        P.op("act", I(nc.scalar.activation, out=pT2[ra // 2][:], in_=ps[:, 512 * ra:512 * ra + 1024],
                                      func=AF.Exp), reads=[Rs[ra], Rs[r4]], writes=[RpT[ra], RpT[r4]])
                    else:
                        P.op("act", I(nc.scalar.activation, out=pT[r4], in_=sbank[r4], func=AF.Exp),
                             reads=[Rs[r4]], writes=[RpT[r4]])

                def st4(n, r4, os_, s_=s_, h=h):
                    P.tag = "C4"
                    qb, kt, cl, first, last = steps[n]
                    for mp in range(2):
                        for qs in range(2):
                            w_ = first and mp == 0 and qs == 0
                            P.op("pe", I(nc.tensor.matmul, oacc(os_, mp, qs),
                                         lhsT=pT[r4][:, mp * 256 + qs * 128:mp * 256 + (qs + 1) * 128],
                                         rhs=vv[s_][:, kt, :], start=(first and qs == 0), stop=last,
                                         skip_group_check=True),
                                 reads=[RpT[r4], Rq[s_]], writes=[Ro[os_]] if w_ else (),
                                 pwrites=() if w_ else [Ro[os_]])
                    if last:
                        pend.append((qb, os_))

                def ep(qb, os_, s_=s_, h=h):
                    P.tag = "Cep"
                    ti = qb * 2
                    v = oset(os_).rearrange("p (m c) -> p m c", m=2)[:, :, 0:264].rearrange("p m (q c) -> p m q c", q=2)
                    o1, o2 = v[:, 0, :, 0:128], v[:, 1, :, 0:128]
                    bc = lambda ap: ap.unsqueeze(2).to_broadcast([128, 2, 128])
                    P.op("dve", I(nc.vector.reciprocal, out=rc[:, 0:4].rearrange("p (m q) -> p m q", m=2),
                                  in_=v[:, :, :, 128]), reads=[Ro[os_]], writes=[Rrc])
                    P.op("dve", I(nc.vector.tensor_scalar, out=rc[:, 4:6], in0=rc[:, 2:4], scalar1=nlam[:, 0:1],
                                  scalar2=None, op0=ALU.mult), reads=[Rrc, Rnlam], pwrites=[Rrc])
                    P.op("dve", I(nc.vector.tensor_tensor, out=of[:, 0:2, :], in0=o2, in1=bc(rc[:, 4:6]), op=ALU.mult),
                         reads=[Ro[os_], Rrc], writes=[Rof])
                    P.op("dve", I(nc.vector.tensor_tensor, out=of[:, 2:4, :], in0=o1, in1=bc(rc[:, 0:2]), op=ALU.mult),
                         reads=[Ro[os_], Rrc], pwrites=[Rof])
                    P.op("dve", I(nc.vector.tensor_tensor, out=of[:, 0:2, :], in0=of[:, 0:2, :], in1=of[:, 2:4, :],
                                  op=ALU.add), reads=[Rof], pwrites=[Rof])
                    P.op("act", I(nc.scalar.activation, out=jk[:], in_=of[:, 0:2, :], func=AF.Square), reads=[Rof],
                         writes=[Rjk])
                    P.op("dve", I(nc.vector.tensor_reduce, out=rc[:, 6:8], in_=jk[:], axis=AX.X, op=ALU.add),
                         reads=[Rjk], pwrites=[Rrc])
                    P.op("dve", I(nc.vector.tensor_scalar, out=rc[:, 8:10], in0=rc[:, 6:8], scalar1=1.0 / 128,
                                  scalar2=EPS, op0=ALU.mult, op1=ALU.add), reads=[Rrc], pwrites=[Rrc])
                    P.op("pool", I(nc.gpsimd.tensor_tensor, out=rc[:, 10:12], in0=rc[:, 8:10], in1=mhalf[:, 0:2],
                                   op=ALU.pow), reads=[Rrc, Rmh], pwrites=[Rrc])
                    P.op("dve", I(nc.vector.tensor_tensor, out=of[:, 2:4, :], in0=of[:, 0:2, :], in1=bc(rc[:, 10:12]),
                                  op=ALU.mult), reads=[Rof, Rrc], pwrites=[Rof])
                    P.op("dve", I(nc.vector.tensor_tensor, out=of[:, 0:2, :], in0=of[:, 2:4, :],
                                  in1=gbt[s_][:, ti:ti + 2, :], op=ALU.mult), reads=[Rof, Rgs[s_], Rq[s_]],
                         pwrites=[Rof])
                    P.op("dve", I(nc.vector.tensor_tensor, out=omla[:, ti:ti + 2, h * 128:(h + 1) * 128],
                                  in0=of[:, 0:2, :], in1=omla[:, ti:ti + 2, h * 128:(h + 1) * 128], op=ALU.add),
                         reads=[Rof, Rom], pwrites=[Rom])

                ep_after = {}
                qbs = sorted(set(st_[0] for st_ in steps))
                for a_, qb_ in enumerate(qbs[:-1]):
                    nq = qbs[a_ + 1]
                    idxs = [k for k, st_ in enumerate(steps) if st_[0] == nq]
                    dg = [k for k in idxs if steps[k][2] == 0]
                    at = dg[-1] if dg else min(idxs[0] + 1, idxs[-1])
                    at = min(at, idxs[-1] - 1) if len(idxs) > 1 else idxs[0]
                    ep_after.setdefault(at, []).append(qb_)
                pend = []
                due = set()
                pair_first = [False] * len(steps)
                for n_ in range(len(steps) - 1):
                    if ((step + n_) % 2 == 0 and steps[n_][2] != 0 and steps[n_ + 1][2] != 0
                            and steps[n_][0] == steps[n_ + 1][0]):
                        pair_first[n_] = True
                r4s, d2s, oss = [], [], []
                for n in range(len(steps) + 3):
                    if n < len(steps):
                        r4 = step % 4
                        step += 1
                        d2 = dstep % 2
                        if steps[n][2] == 0:
                            dstep += 1
                        if steps[n][3]:
                            oset_i += 1
                        r4s.append(r4)
                        d2s.append(d2)
                        oss.append(oset_i % 2)
                        st1(n, r4, d2)
                    if 1 <= n <= len(steps):
                        st23(n - 1, r4s[n - 1], d2s[n - 1])
                        due.update(ep_after.get(n - 1, []))
                        for pq_ in [p_ for p_ in pend if p_[0] in due]:
                            pend.remove(pq_)
                            ep(*pq_)
                    if n >= 3:
                        st4(n - 3, r4s[n - 3], oss[n - 3])
                        for pq_ in [p_ for p_ in pend if p_[0] in due]:
                            pend.remove(pq_)
                            ep(*pq_)
                for pq_ in pend:
                    ep(*pq_)
                pend = []
            for i in range(NT):
                P.dma("sp", I(nc.sync.dma_start, out=MG[i * 128:(i + 1) * 128, :], in_=omla[:, i, :]), Rom,
                      reads=[Rom], pwrites=[RMG])
            P.barrier()
            P.emit()


def phaseDE(nc, P, env, upto):
    g = env
    sb = nc.sbuf_tensor
    x_d, out_d, MG, H2, wout_d, rw_d, vec_d = [g[k] for k in "x_d out_d MG H2 wout_d rw_d vec_d".split()]
    wg_d, wu_d, wd_d, iota_d, tokpi_d = [g[k] for k in "wg_d wu_d wd_d iota_d tokpi_d".split()]
    identb, identf, mhalf = g["identb"], g["identf"], g["mhalf"]
    Rid, Rmh = g["Rid"], g["Rmh"]
    with ExitStack() as es:
        aff = es.enter_context(sb("aff", [128, NT, NE], F32))
        posm = es.enter_context(sb("posm", [128, NT, NE], F32))
        Raff, Rposm = R("aff"), R("posm")
        wsl = [es.enter_context(sb("e_w%d" % k, [128, 16384], BF16)) for k in range(3)]
        Rws = [R("ew%d" % k) for k in range(4)]

        def wload(e, which):
            for k, src in enumerate([wg_d, wu_d]):
                if k not in which:
                    continue
                sl = (3 * e + k) % 4
                P.dma("pool", I(nc.gpsimd.dma_start, out=wsl[sl][:].rearrange("p (kc f) -> p kc f", kc=8),
                                in_=src[e].rearrange("(kc p) f -> p kc f", p=128)), Rws[sl], writes=[Rws[sl]])
            if 2 in which:
                sl = (3 * e + 2) % 4
                P.dma("pool", I(nc.gpsimd.dma_start, out=wsl[sl][:].rearrange("p (j c) -> p j c", j=16),
                                in_=wd_d[e].rearrange("(j p) c -> p j c", p=128)), Rws[sl], writes=[Rws[sl]])
        with ExitStack() as es2:
            wout = es2.enter_context(sb("d_wout", [128, 8, 1024], BF16))
            rw = es2.enter_context(sb("d_rw", [128, 8, NE], F32))
            wn2 = es2.enter_context(sb("d_wn2", [128, 1024], F32))
            mg = [es2.enter_context(sb("d_mg%d" % k, [128, 1024], BF16)) for k in range(2)]
            xt = [es2.enter_context(sb("d_xt%d" % k, [128, 1024], F32)) for k in range(2)]
            x1 = [es2.enter_context(sb("d_x1%d" % k, [128, 1024], F32)) for k in range(2)]
            mT = es2.enter_context(sb("d_mT", [128, 1024], BF16))
            sq = es2.enter_context(sb("d_sq", [128, 1024], F32))
            h2f = es2.enter_context(sb("d_h2f", [128, 1024], F32))
            h2b = [es2.enter_context(sb("d_h2b%d" % k, [128, 1024], BF16)) for k in range(2)]
            h2T = es2.enter_context(sb("d_h2T", [128, 1024], F32))
            sm = es2.enter_context(sb("d_sm", [128, 64], F32))
            psB = es2.enter_context(nc.psum_tensor("d_psB", [128, 1024], BF16))
            psF = es2.enter_context(nc.psum_tensor("d_psF", [128, 5 * 512], F32))
            Rw = R("dw")
            Rmg, Rxt, Rx1, Rh2b = [[R(n + str(k)) for k in range(2)] for n in ("dmg", "dxt", "dx1", "dh2b")]
            RmT, Rsq, Rh2f, Rh2T, Rsm, RTb, Racc, RTf, Rlg = [R(n) for n in
                                                              "dmT dsq dh2f dh2T dsm dTb dacc dTf dlg".split()]
            RH2, Rout = g["RH2"], g["Rout"]
            P.dma("pool", I(nc.gpsimd.dma_start, out=wout[:], in_=wout_d.rearrange("(kc p) c -> p kc c", p=128)), Rw,
                  pwrites=[Rw])
            P.dma("sp", I(nc.sync.dma_start, out=rw[:], in_=rw_d.rearrange("(kc p) c -> p kc c", p=128)), Rw,
                  pwrites=[Rw])
            P.dma("sp", I(nc.sync.dma_start, out=wn2[:], in_=vec_d["ffn_norm_w"].partition_broadcast(128)), Rw,
                  pwrites=[Rw])
            wload(0, (0, 1, 2))
            acc = psF[:, 0:1024]
            Tf = psF[:, 1024:2048]
            lg = psF[:, 2048:2048 + NE]
            h2f2 = [h2f, es2.enter_context(sb("d_h2f1", [128, 1024], F32))]
            Rh2f2 = [R("dh2f0"), R("dh2f1")]
            RsmA = [R("dsmA0"), R("dsmA1")]
            RsmB = R("dsmB")
            def Da(i):
                P.tag = "Da"
                s_ = i % 2
                t0 = i * 128
                ca = 48 + 4 * s_
                h2f = h2f2[s_]
                Rh2f = Rh2f2[s_]
                Rsm = RsmA[s_]
                P.dma("sp", I(nc.sync.dma_start, out=mg[s_][:], in_=MG[t0:t0 + 128, :]), Rmg[s_], writes=[Rmg[s_]])
                P.dma("sp", I(nc.sync.dma_start, out=xt[s_][:], in_=x_d[t0:t0 + 128, :]), Rxt[s_], writes=[Rxt[s_]])
                for kc in range(8):
                    P.op("pe", I(nc.tensor.transpose, out=psB[:, kc * 128:(kc + 1) * 128],
                                 in_=mg[s_][:, kc * 128:(kc + 1) * 128], identity=identb[:]),
                         reads=[Rmg[s_], Rid], writes=[RTb] if kc == 0 else (), pwrites=[RTb] if kc else ())
                P.op("act", I(nc.scalar.copy, out=mT[:], in_=psB[:, 0:1024]), reads=[RTb], writes=[RmT])
                for half in range(2):
                    for kc in range(8):
                        first = half == 0 and kc == 0
                        P.op("pe", I(nc.tensor.matmul, acc[:, half * 512:(half + 1) * 512],
                                     lhsT=mT[:, kc * 128:(kc + 1) * 128], rhs=wout[:, kc, half * 512:(half + 1) * 512],
                                     start=(kc == 0), stop=(kc == 7)), reads=[RmT, Rw],
                             writes=[Racc] if first else (), pwrites=() if first else [Racc])
                P.op("dve", I(nc.vector.tensor_tensor, out=x1[s_][:], in0=acc, in1=xt[s_][:], op=ALU.add),
                     reads=[Racc, Rxt[s_]], writes=[Rx1[s_]])
                P.dma("sp", I(nc.sync.dma_start, out=out_d[t0:t0 + 128, :], in_=x1[s_][:]), Rx1[s_], reads=[Rx1[s_]],
                      pwrites=[Rout])
                P.op("act", I(nc.scalar.activation, out=sq[:], in_=x1[s_][:], func=AF.Square, accum_out=sm[:, ca:ca + 1]),
                     reads=[Rx1[s_]], writes=[Rsq, Rsm])
                P.op("dve", I(nc.vector.tensor_scalar, out=sm[:, ca + 1:ca + 2], in0=sm[:, ca:ca + 1], scalar1=1.0 / D, scalar2=EPS,
                              op0=ALU.mult, op1=ALU.add), reads=[Rsm], pwrites=[Rsm])
                P.op("pool", I(nc.gpsimd.tensor_tensor, out=sm[:, ca + 2:ca + 3], in0=sm[:, ca + 1:ca + 2], in1=mhalf[:, 0:1], op=ALU.pow),
                     reads=[Rsm, Rmh], pwrites=[Rsm])
                P.op("dve", I(nc.vector.scalar_tensor_tensor, out=h2f[:], in0=x1[s_][:], scalar=sm[:, ca + 2:ca + 3], in1=wn2[:],
                              op0=ALU.mult, op1=ALU.mult), reads=[Rx1[s_], Rsm, Rw], writes=[Rh2f])
                P.op("act", I(nc.scalar.copy, out=h2b[s_][:], in_=h2f[:]), reads=[Rh2f], writes=[Rh2b[s_]])
                P.dma("sp", I(nc.sync.dma_start, out=H2[t0:t0 + 128, :], in_=h2b[s_][:]), Rh2b[s_], reads=[Rh2b[s_]],
                      pwrites=[RH2])
            def Db(i):
                P.tag = "Db"
                s_ = i % 2
                h2f = h2f2[s_]
                Rh2f = Rh2f2[s_]
                Rsm = RsmB
                for kc in range(8):
                    P.op("pe", I(nc.tensor.transpose, out=Tf[:, kc * 128:(kc + 1) * 128],
                                 in_=h2f[:, kc * 128:(kc + 1) * 128], identity=identf[:]),
                         reads=[Rh2f, Rid], writes=[RTf] if kc == 0 else (), pwrites=[RTf] if kc else ())
                P.op("dve", I(nc.vector.tensor_copy, out=h2T[:], in_=Tf), reads=[RTf], writes=[Rh2T])
                for kc in range(8):
                    P.op("pe", I(nc.tensor.matmul, lg, lhsT=h2T[:, kc * 128:(kc + 1) * 128], rhs=rw[:, kc, :],
                                 start=(kc == 0), stop=(kc == 7)), reads=[Rh2T, Rw],
                         writes=[Rlg] if kc == 0 else (), pwrites=[Rlg] if kc else ())
                P.op("dve", I(nc.vector.tensor_reduce, out=sm[:, 8:9], in_=lg, axis=AX.X, op=ALU.max), reads=[Rlg],
                     pwrites=[Rsm])
                P.op("dve", I(nc.vector.tensor_scalar, out=sm[:, 9:10], in0=sm[:, 8:9], scalar1=-1.0, scalar2=None,
                              op0=ALU.mult), reads=[Rsm], pwrites=[Rsm])
                P.op("act", I(nc.scalar.activation, out=sm[:, 16:32], in_=lg, func=AF.Exp, bias=sm[:, 9:10],
                              accum_out=sm[:, 10:11]), reads=[Rlg, Rsm], pwrites=[Rsm])
                P.op("dve", I(nc.vector.reciprocal, out=sm[:, 11:12], in_=sm[:, 10:11]), reads=[Rsm], pwrites=[Rsm])
                P.op("dve", I(nc.vector.tensor_scalar, out=aff[:, i, :], in0=sm[:, 16:32], scalar1=sm[:, 11:12],
                              scalar2=None, op0=ALU.mult), reads=[Rsm], pwrites=[Raff])
            Da(0)
            for i in range(NT):
                if i + 1 < NT:
                    Da(i + 1)
                Db(i)
            P.barrier()
            P.emit()
        if upto < "E":
            return
        with ExitStack() as es2:
            affT = es2.enter_context(sb("e_affT", [NE, T], F32))
            mk = es2.enter_context(sb("e_mk", [NE, T], F32))
            cs = es2.enter_context(sb("e_cs", [NE, T], F32))
            on = es2.enter_context(sb("e_on", [NE, T], F32))
            bs = es2.enter_context(sb("e_bs", [NE, 8], F32))
            ps = es2.enter_context(nc.psum_tensor("e_ps", [128, 4 * 512], F32))
            RaT, Rmk, Rcs, Ron, Rbs, Rps = [R(n) for n in "eaT emk ecs eon ebs eps".split()]
            P.op("pool", I(nc.gpsimd.memset, on[:], 1.0), writes=[Ron])
            P.op("pool", I(nc.gpsimd.memset, bs[:], 0.0), writes=[Rbs])
            for half in range(2):
                for j in range(16):
                    i = half * 16 + j
                    P.op("pe", I(nc.tensor.transpose, out=ps[0:NE, j * 128:(j + 1) * 128], in_=aff[:, i, :],
                                 identity=identf[:]), reads=[Raff, Rid], writes=[Rps] if j == 0 else (),
                         pwrites=[Rps] if j else ())
                P.op("act", I(nc.scalar.copy, out=affT[:, half * 2048:(half + 1) * 2048], in_=ps[0:NE, 0:2048]),
                     reads=[Rps], pwrites=[RaT])
            lo, mid, cntc, gw = bs[:, 0:1], bs[:, 1:2], bs[:, 2:3], bs[:, 3:4]
            for it in range(28):
                w = 2.0 ** (-(it + 1))
                P.op("dve", I(nc.vector.tensor_scalar, out=mid, in0=lo, scalar1=w, scalar2=None, op0=ALU.add),
                     reads=[Rbs], pwrites=[Rbs])
                P.op("dve", I(nc.vector.tensor_scalar, out=mk[:], in0=affT[:], scalar1=mid, scalar2=0.0, op0=ALU.is_gt,
                              op1=ALU.add, accum_out=cntc), reads=[RaT, Rbs], writes=[Rmk], pwrites=[Rbs])
                P.op("dve", I(nc.vector.tensor_scalar, out=gw, in0=cntc, scalar1=CAP - 0.5, scalar2=w, op0=ALU.is_gt,
                              op1=ALU.mult), reads=[Rbs], pwrites=[Rbs])
                P.op("dve", I(nc.vector.tensor_tensor, out=lo, in0=lo, in1=gw, op=ALU.add), reads=[Rbs],
                     pwrites=[Rbs])
            P.op("dve", I(nc.vector.tensor_scalar, out=mk[:], in0=affT[:], scalar1=lo, scalar2=None, op0=ALU.is_gt),
                 reads=[RaT, Rbs], writes=[Rmk])
            P.op("dve", I(nc.vector.tensor_tensor_scan, out=cs[:], data0=on[:], data1=mk[:], initial=0.0, op0=ALU.mult,
                          op1=ALU.add), reads=[Ron, Rmk], writes=[Rcs])
            P.op("dve", I(nc.vector.tensor_tensor, out=cs[:], in0=cs[:], in1=mk[:], op=ALU.mult), reads=[Rcs, Rmk],
                 writes=[Rcs])
            for i in range(NT):
                P.op("pe", I(nc.tensor.transpose, out=ps[:, i * NE:(i + 1) * NE], in_=cs[:, i * 128:(i + 1) * 128],
                             identity=identf[0:NE, 0:NE]), reads=[Rcs, Rid], writes=[Rps] if i == 0 else (),
                     pwrites=[Rps] if i else ())
            P.op("act", I(nc.scalar.copy, out=posm[:].rearrange("p a b -> p (a b)"), in_=ps[:, 0:NT * NE]),
                 reads=[Rps], writes=[Rposm])
            P.barrier()
            P.emit()
        with ExitStack() as es2:
            wsl.append(es2.enter_context(sb("e_w3", [128, 16384], BF16)))
            iota = es2.enter_context(sb("e_iota", [128, CAP], F32))
            tokpi = es2.enter_context(sb("e_tokpi", [128, NT, 4], BF16))
            sel = [es2.enter_context(sb("e_sel%d" % k, [128, CAP], BF16)) for k in range(3)]
            Rsel = [R("esel%d" % k) for k in range(3)]
            idxrow = es2.enter_context(sb("e_idxrow", [4, CAP], F32))
            ic = es2.enter_context(sb("e_ic", [128, 4, 4], F32))
            idf = es2.enter_context(sb("e_idf", [128, 4], F32))
            idx = [[es2.enter_context(sb("e_idx%d%d" % (k, c), [128, 1], I32)) for c in range(4)] for k in range(2)]
            gate = [es2.enter_context(sb("e_gate%d" % k, [128, 4], F32)) for k in range(2)]
            xe = [es2.enter_context(sb("e_xe%d" % k, [128, 1024], BF16)) for k in range(4)]
            Rxe = [R("exe%d" % k) for k in range(4)]
            xeT = es2.enter_context(sb("e_xeT", [128, 8, CAP], BF16))
            hT = es2.enter_context(sb("e_hT", [128, 16, CAP], BF16))
            sg = [es2.enter_context(sb("e_sg%d" % k, [128, CAP], F32)) for k in range(2)]
            Rsg = [R("esg0"), R("esg1")]
            ye = [es2.enter_context(sb("e_ye%d" % k, [128, 1024], F32)) for k in range(2)]
            Rye = [R("eye0"), R("eye1")]
            psF = es2.enter_context(nc.psum_tensor("e_psF", [128, 7 * 512], F32))
            psB = es2.enter_context(nc.psum_tensor("e_psB", [128, 1024], BF16))
            gub = [(psF[:, 0:512], psF[:, 512:1024]), (psF[:, 1024:1536], psF[:, 1536:2048])]
            Rgu = [R("egu0"), R("egu1")]
            dbk = [psF[:, 2048:2560], psF[:, 2560:3072]]
            Rdb = [R("edb0"), R("edb1")]
            ipb = psF[:, 3072:3584]
            Ripb, RTb = R("eipb"), R("eTb")
            Rtok, Ridr, Ric, Ridx, RxeT, RhT, Rsc, Rc = [R(n) for n in "etok eidr eic eidx exeT ehT esc ec".split()]
            Ridxs = [R("eidx0"), R("eidx1")]
            P.dma("sp", I(nc.sync.dma_start, out=iota[:], in_=iota_d.partition_broadcast(128)), Rc, pwrites=[Rc])
            P.dma("pool", I(nc.gpsimd.dma_start, out=tokpi[:, :, 0:2], in_=tokpi_d[:, :, :]), Rc, pwrites=[Rtok])

            def route_tok(e):
                P.op("dve", I(nc.vector.tensor_copy, out=tokpi[:, :, 2], in_=aff[:, :, e]), reads=[Raff],
                     writes=[Rtok])
                P.op("dve", I(nc.vector.tensor_tensor, out=tokpi[:, :, 3], in0=aff[:, :, e], in1=tokpi[:, :, 2],
                              op=ALU.subtract), reads=[Raff, Rtok], pwrites=[Rtok])

            def route_sel(e, i):
                r3 = (e * NT + i) % 3
                P.op("dve", I(nc.vector.tensor_scalar, out=sel[r3][:], in0=iota[:], scalar1=posm[:, i, e:e + 1],
                              scalar2=None, op0=ALU.is_equal), reads=[Rc, Rposm], writes=[Rsel[r3]])
                P.op("pe", I(nc.tensor.matmul, ipb[0:4, :], lhsT=tokpi[:, i, :], rhs=sel[r3][:], start=(i == 0),
                             stop=(i == NT - 1)), reads=[Rtok, Rsel[r3]], writes=[Ripb] if i == 0 else (),
                     pwrites=[Ripb] if i else ())

            def route_idx(e):
                es_ = e % 2
                P.op("act", I(nc.scalar.copy, out=idxrow[:], in_=ipb[0:4, :]), reads=[Ripb], writes=[Ridr])
                for cc in range(4):
                    P.op("pe", I(nc.tensor.transpose, out=ipb[:, cc * 4:(cc + 1) * 4],
                                 in_=idxrow[0:4, cc * 128:(cc + 1) * 128], identity=identf[0:4, 0:4]),
                         reads=[Ridr, Rid], writes=[Ripb] if cc == 0 else (), pwrites=[Ripb] if cc else ())
                P.op("dve", I(nc.vector.tensor_copy, out=ic[:].rearrange("p a b -> p (a b)"), in_=ipb[:, 0:16]),
                     reads=[Ripb], writes=[Ric])
                P.op("dve", I(nc.vector.scalar_tensor_tensor, out=idf[:], in0=ic[:, :, 1], scalar=128.0,
                              in1=ic[:, :, 0], op0=ALU.mult, op1=ALU.add), reads=[Ric], writes=[Ridx])
                P.op("dve", I(nc.vector.tensor_tensor, out=gate[es_][:], in0=ic[:, :, 2], in1=ic[:, :, 3], op=ALU.add),
                     reads=[Ric], writes=[Ridxs[es_]])
                for cc in range(4):
                    P.op("dve", I(nc.vector.tensor_copy, out=idx[es_][cc][:], in_=idf[:, cc:cc + 1]), reads=[Ridx],
                         pwrites=[Ridxs[es_]])
                for cc in range(4):
                    P.dma("pool", I(nc.gpsimd.indirect_dma_start, out=xe[cc][:], out_offset=None, in_=H2[:, :],
                                    in_offset=bass.IndirectOffsetOnAxis(ap=idx[es_][cc][:, :], axis=0)), Rxe[cc],
                          reads=[Ridxs[es_]], writes=[Rxe[cc]])

            def route_T(e):
                for kc in range(8):
                    for cc in range(4):
                        P.op("pe", I(nc.tensor.transpose, out=psB[:, cc * 128:(cc + 1) * 128],
                                     in_=xe[cc][:, kc * 128:(kc + 1) * 128], identity=identb[:]),
                             reads=[Rxe[cc], Rid], writes=[RTb] if cc == 0 else (), pwrites=[RTb] if cc else ())
                    if kc % 2 == 0:
                        P.op("act", I(nc.scalar.copy, out=xeT[:, kc, :], in_=psB[:, 0:512]), reads=[RTb],
                             writes=[RxeT] if kc == 0 else (), pwrites=[RxeT] if kc else ())
                    else:
                        P.op("dve", I(nc.vector.tensor_copy, out=xeT[:, kc, :], in_=psB[:, 0:512]), reads=[RTb],
                             pwrites=[RxeT])

            route_tok(0)
            for i in range(NT):
                route_sel(0, i)
            route_idx(0)
            route_T(0)
            gstep = 0
            dstep = 0
            for e in range(NE):
                es_ = e % 2
                nxt = e + 1 < NE
                wgt = wsl[(3 * e) % 4][:].rearrange("p (kc f) -> p kc f", kc=8)
                wut = wsl[(3 * e + 1) % 4][:].rearrange("p (kc f) -> p kc f", kc=8)
                wdt = wsl[(3 * e + 2) % 4][:].rearrange("p (j c) -> p j c", j=16)
                Rwg, Rwu, Rwd = Rws[(3 * e) % 4], Rws[(3 * e + 1) % 4], Rws[(3 * e + 2) % 4]
                if nxt:
                    wload(e + 1, (0,))
                    route_tok(e + 1)
                for j in range(16):
                    gs = gstep % 2
                    gstep += 1
                    gb_, ub_ = gub[gs]
                    for kc in range(8):
                        P.op("pe", I(nc.tensor.matmul, gb_, lhsT=wgt[:, kc, j * 128:(j + 1) * 128], rhs=xeT[:, kc, :],
                                     start=(kc == 0), stop=(kc == 7)), reads=[Rwg, RxeT],
                             writes=[Rgu[gs]] if kc == 0 else (), pwrites=[Rgu[gs]] if kc else ())
                    for kc in range(8):
                        P.op("pe", I(nc.tensor.matmul, ub_, lhsT=wut[:, kc, j * 128:(j + 1) * 128], rhs=xeT[:, kc, :],
                                     start=(kc == 0), stop=(kc == 7)), reads=[Rwu, RxeT], pwrites=[Rgu[gs]])
                    if nxt:
                        route_sel(e + 1, 2 * j)
                        route_sel(e + 1, 2 * j + 1)
                    P.op("act", I(nc.scalar.activation, out=sg[gs][:], in_=gb_, func=AF.Tanh, scale=0.5),
                         reads=[Rgu[gs]], writes=[Rsg[gs]])
                    P.op("dve", I(nc.vector.scalar_tensor_tensor, out=sg[gs][:], in0=sg[gs][:], scalar=1.0, in1=gb_,
                                  op0=ALU.add, op1=ALU.mult), reads=[Rsg[gs], Rgu[gs]], writes=[Rsg[gs]])
                    P.op("dve", I(nc.vector.scalar_tensor_tensor, out=hT[:, j, :], in0=sg[gs][:], scalar=0.5, in1=ub_,
                                  op0=ALU.mult, op1=ALU.mult), reads=[Rsg[gs], Rgu[gs]],
                         writes=[RhT] if j == 0 else (), pwrites=[RhT] if j else ())
                if nxt:
                    route_idx(e + 1)
                    wload(e + 1, (1,))
                for cc in range(4):
                    ys = cc % 2
                    for half in range(2):
                        ds = dstep % 2
                        dstep += 1
                        for j in range(16):
                            P.op("pe", I(nc.tensor.matmul, dbk[ds], lhsT=hT[:, j, cc * 128:(cc + 1) * 128],
                                         rhs=wdt[:, j, half * 512:(half + 1) * 512], start=(j == 0), stop=(j == 15)),
                                 reads=[RhT, Rwd], writes=[Rdb[ds]] if j == 0 else (), pwrites=[Rdb[ds]] if j else ())
                        if half == 0:
                            P.op("dve", I(nc.vector.tensor_scalar, out=ye[ys][:, 0:512], in0=dbk[ds],
                                          scalar1=gate[es_][:, cc:cc + 1], scalar2=None, op0=ALU.mult),
                                 reads=[Rdb[ds], Ridxs[es_]], writes=[Rye[ys]])
                        else:
                            P.op("dve", I(nc.vector.tensor_scalar, out=ye[ys][:, 512:1024], in0=dbk[ds],
                                          scalar1=gate[es_][:, cc:cc + 1], scalar2=None, op0=ALU.mult),
                                 reads=[Rdb[ds], Ridxs[es_]], pwrites=[Rye[ys]])
                    P.dma("pool", I(nc.gpsimd.indirect_dma_start, out=out_d[:, :],
                                    out_offset=bass.IndirectOffsetOnAxis(ap=idx[es_][cc][:, :], axis=0),
                                    in_=ye[ys][:], in_offset=None, compute_op=ALU.add), Rye[ys],
                          reads=[Rye[ys], Ridxs[es_], Rsc], pwrites=[Rsc])
                if nxt:
                    wload(e + 1, (2,))
                    route_T(e + 1)
            P.barrier()
            P.emit()


IN_COLS = (256, 256, 32, 1024, 1024, 1024, 1024, 1024)


def _win_perm():
    off = np.cumsum((0,) + IN_COLS)
    seg = [np.arange(off[k], off[k + 1]) for k in range(8)]
    return np.concatenate([seg[0], seg[1], seg[3], seg[4], seg[5], seg[6], seg[7], seg[2]])


def make_in_maps(inputs, cores):
    f = lambda a: np.ascontiguousarray(np.asarray(a))
    perm = _win_perm()
    bg = f(inputs["b_gate"])[0]
    shared = {
        "w_in": f(f(inputs["w_in"])[0][:, perm]),
        "b_gate": bg,
        "w_uq": f(inputs["mla_w_uq"])[0],
        "w_ukv": f(inputs["mla_w_ukv"])[0],
        "w_out": f(inputs["w_out"])[0],
        "router_w": f(inputs["router_w"])[0],
        "w_gate": f(inputs["expert_w_gate"])[0],
        "w_up": f(inputs["expert_w_up"])[0],
        "w_down": f(inputs["expert_w_down"])[0],
        "attn_norm_w": f(inputs["attn_norm_w"])[0],
        "q_norm_w": f(inputs["mla_q_norm_w"])[0],
        "kv_norm_w": f(inputs["mla_kv_norm_w"])[0],
        "q_hn": f(inputs["mla_q_hnorm_w"])[0],
        "k_hn": f(inputs["mla_k_hnorm_w"])[0],
        "dq_hn": f(inputs["diff_q_hnorm_w"])[0],
        "dk_hn": f(inputs["diff_k_hnorm_w"])[0],
        "lam": f(inputs["diff_lambda"])[0].reshape(-1),
        "subln": f(inputs["diff_subln_w"])[0],
        "ffn_norm_w": f(inputs["ffn_norm_w"])[0],
        "ident": np.eye(128, dtype=np.float32),
        "invf": (1.0 / (10000.0 ** (np.arange(0, 32, 2, dtype=np.float32) / 32.0))).astype(np.float32),
        "iota512": np.arange(1, 513, dtype=np.float32),
        "tokpi": np.ascontiguousarray(np.stack([np.broadcast_to(np.arange(128, dtype=np.float32)[:, None], (128, NT)),
                                                np.broadcast_to(np.arange(NT, dtype=np.float32)[None, :], (128, NT))],
                                               axis=-1)),
        "augk": np.ascontiguousarray(np.broadcast_to(np.array([-1, -1, 2, 2], np.float32)[:, None], (4, T))),
        "augq": np.ascontiguousarray(np.broadcast_to(np.array([1, 1, -2, -2], np.float32)[:, None], (4, T))),
        "slopes": np.array([2.0 ** (-(i + 1)) for i in range(8)], dtype=np.float32),
    }
    x = f(inputs["x"])
    pos = f(inputs["positions"]).astype(np.int32)
    return [dict(shared, x=x[c], pos=pos[c], pos_t=np.ascontiguousarray(pos[c].reshape(NT, 128).T)) for c in cores]


_NC = {}


def classify(pos):
    pos = np.asarray(pos).astype(np.int64)
    n = pos.shape[0]
    q = pos.reshape(n, 16, 256)
    k = pos.reshape(n, NT, 128)
    qmin, qmax = q.min(-1)[:, :, None], q.max(-1)[:, :, None]
    kmin, kmax = k.min(-1)[:, None, :], k.max(-1)[:, None, :]
    below = (kmax <= qmin).all(0)
    above = (kmin >= qmax).all(0)
    cls = np.where(below, 1, np.where(above, 2, 0))
    dmin = np.maximum(np.maximum(qmin - kmax, kmin - qmax), 0).min(0).astype(np.float64)
    return cls, dmin


def kernel(**inputs):
    cls, dmin = classify(np.asarray(inputs["positions"]))
    gq = float(np.abs(np.asarray(inputs["diff_q_hnorm_w"])).max())
    gk = float(np.abs(np.asarray(inputs["diff_k_hnorm_w"])).max())
    skip_t = max(48.0, 2.0 * 8.0 * gq * gk + 25.0)
    key = (cls.tobytes(), dmin.tobytes(), skip_t)
    if key not in _NC:
        _NC[key] = build(CLS=cls, DMIN=dmin, skip_t=skip_t)
    nc = _NC[key]
    in_maps = make_in_maps(inputs, list(range(8)))
    res = run_bass_kernel_spmd(nc, in_maps, core_ids=list(range(8)))
    return np.stack([r["out"] for r in res.results], axis=0).astype(np.float32)
```

```python
import math
from functools import partial as I
from contextlib import ExitStack
import numpy as np
import concourse.bass as bass
import concourse.mybir as mybir
from concourse.bass_utils import run_bass_kernel_spmd

F32 = mybir.dt.float32
BF16 = mybir.dt.bfloat16
I32 = mybir.dt.int32
AF = mybir.ActivationFunctionType
ALU = mybir.AluOpType
AX = mybir.AxisListType

T = 4096
D = 1024
NT = T // 128
EPS = 1e-6
LAM_INIT = 0.8 - 0.6 * math.exp(-0.3 * 0)
NE = 16
CAP = 512
FF = 2048
TWO_PI = 2.0 * math.pi


class R:
    __slots__ = ("name", "w", "r", "pr", "dsem", "dcnt")

    def __init__(self, name):
        self.name = name
        self.w = {}
        self.r = {}
        self.pr = {}
        self.dsem = None
        self.dcnt = 0


class Prog:
    ENG = ("pe", "act", "dve", "pool", "sp")

    def __init__(self, nc):
        self.nc = nc
        self.streams = {e: [] for e in self.ENG}
        self.nops = {e: 0 for e in self.ENG}
        self.known = {e: {} for e in self.ENG}
        self.signal = {e: set() for e in self.ENG}
        self.sigcount = {e: 0 for e in self.ENG}
        self.cnt = {e: {} for e in self.ENG}
        self.sems = {}
        self._semctx = []
        self.dstreams = []
        self.tag = ""
        for e in self.ENG:
            self.sems[("E", e)] = self._new_sem("sem_" + e)

    def _new_sem(self, name):
        ctx = self.nc.semaphore(name)
        s = ctx.__enter__()
        self._semctx.append(ctx)
        return s

    def close(self):
        for ctx in reversed(self._semctx):
            ctx.__exit__(None, None, None)

    def _wait(self, eng, key, val):
        k = self.known[eng]
        if k.get(key, -1) >= val:
            return
        k[key] = val
        if key[0] == "E":
            self.signal[key[1]].add(val)
        self.streams[eng].append(("wait", key, val))

    def _deps(self, eng, reads, writes, pwrites):
        me = ("E", eng)
        for res in reads:
            for key, val in res.w.items():
                if key == me and eng == "pe":
                    continue
                self._wait(eng, key, val)
        for res in writes:
            for key, val in list(res.w.items()) + list(res.r.items()):
                if key == me and eng == "pe":
                    continue
                self._wait(eng, key, val)
        for res in pwrites:
            for key, val in list(res.r.items()) + list(res.pr.items()):
                if key == me and eng == "pe":
                    continue
                self._wait(eng, key, val)

    def _mark(self, key, val, reads, writes, pwrites):
        for res in reads:
            if res.r.get(key, -1) < val:
                res.r[key] = val
        for res in writes:
            pr = dict(res.w)
            for k_, v_ in res.r.items():
                if pr.get(k_, -1) < v_:
                    pr[k_] = v_
            res.pr = pr
            res.w = {key: val}
            res.r = {}
        for res in pwrites:
            if res.w.get(key, -1) < val:
                res.w[key] = val

    def op(self, eng, fn, reads=(), writes=(), pwrites=()):
        self._deps(eng, reads, writes, pwrites)
        idx = self.nops[eng]
        self.nops[eng] += 1
        self.streams[eng].append(("op", fn, idx, self.tag))
        self._mark(("E", eng), idx, reads, writes, pwrites)

    def dma(self, eng, fn, stream, reads=(), writes=(), pwrites=()):
        self._deps(eng, reads, writes, pwrites)
        if stream.dsem is None:
            stream.dsem = {}
            stream.dcnt = {}
        key = ("D", id(stream), eng)
        if eng not in stream.dsem:
            stream.dsem[eng] = self._new_sem("d_%s_%s" % (stream.name, eng))
            stream.dcnt[eng] = 0
            self.sems[key] = stream.dsem[eng]
            self.dstreams.append((stream, eng))
        stream.dcnt[eng] += 1
        val = 16 * stream.dcnt[eng]
        self.streams[eng].append(("dma", fn, stream.dsem[eng]))
        self._mark(key, val, reads, writes, pwrites)

    def barrier(self):
        for e in self.ENG:
            for f in self.ENG:
                if f != e and f != "sp" and self.nops[f] > 0:
                    self._wait(e, ("E", f), self.nops[f] - 1)
            for s, q in self.dstreams:
                self._wait(e, ("D", id(s), q), 16 * s.dcnt[q])

    def emit(self):
        nc = self.nc
        for e in self.ENG:
            for idx in sorted(self.signal[e]):
                if idx not in self.cnt[e]:
                    self.sigcount[e] += 1
                    self.cnt[e][idx] = self.sigcount[e]
            self.signal[e] = set()
        streams = self.streams
        self.streams = {e: [] for e in self.ENG}

        def make(e):
            stream = streams[e]

            def body(engine):
                for ent in stream:
                    if ent[0] == "wait":
                        _, key, val = ent
                        v = self.cnt[key[1]][val] if key[0] == "E" else val
                        engine.wait_ge(self.sems[key], v)
                    elif ent[0] == "op":
                        _, fn, idx, tag = ent
                        ins = fn()
                        if tag:
                            ins.annotate(tag)
                        if idx in self.cnt[e]:
                            ins.then_inc(self.sems[("E", e)], 1)
                    else:
                        _, fn, sem = ent
                        try:
                            ins = fn()
                        except Exception:
                            print("DMA build failed:", fn.func.__name__, {k: str(v)[:200] for k, v in fn.keywords.items()})
                            raise
                        ins.then_inc(sem, 16)
            return body

        with nc.Block() as block:
            block.tensor(make("pe"))
            block.scalar(make("act"))
            block.vector(make("dve"))
            block.gpsimd(make("pool"))
            block.sync(make("sp"))


SKIP_T = 48.0


def build(dbg=False, upto="E", CLS=None, DMIN=None, skip_t=None):
    global SKIP_T
    if skip_t is not None:
        SKIP_T = float(skip_t)
    if CLS is None:
        CLS = np.zeros((16, NT), np.int64)
        DMIN = np.zeros((16, NT), np.float64)
    nc = bass.Bass("TRN2", target_bir_lowering=False)

    def din(name, shape, dt=F32):
        return nc.dram_tensor(name, list(shape), dt, kind="ExternalInput").ap()

    def dscr(name, shape, dt=BF16):
        return nc.dram_tensor(name, list(shape), dt, kind="ExternalOutput" if dbg else "Internal").ap()

    x_d = din("x", [T, D])
    pos_d = din("pos", [T], I32)
    post_d = din("pos_t", [128, NT], I32)
    win_d = din("w_in", [D, 5664])
    bg_d = din("b_gate", [2048])
    wuq_d = din("w_uq", [256, 1536])
    wukv_d = din("w_ukv", [256, 2048])
    wout_d = din("w_out", [D, D])
    rw_d = din("router_w", [D, NE])
    wg_d = din("w_gate", [NE, D, FF])
    wu_d = din("w_up", [NE, D, FF])
    wd_d = din("w_down", [NE, FF, D])
    vec_d = {n: din(n, [k]) for n, k in [("attn_norm_w", 1024), ("q_norm_w", 256), ("kv_norm_w", 256),
                                          ("q_hn", 96), ("k_hn", 96), ("dq_hn", 64), ("dk_hn", 64),
                                          ("lam", 256), ("subln", 128), ("ffn_norm_w", 1024)]}
    ident_d = din("ident", [128, 128])
    invf_d = din("invf", [16])
    iota_d = din("iota512", [512])
    slopes_d = din("slopes", [8])
    tokpi_d = din("tokpi", [128, NT, 2])
    out_d = nc.dram_tensor("out", [T, D], F32, kind="ExternalOutput").ap()

    QmT = dscr("QmT", [16, 96, T])
    KmT = dscr("KmT", [16, 96, T])
    Vm = dscr("Vm", [T, 1024])
    QdT = dscr("QdT", [8, 128, T])
    KdT = dscr("KdT", [8, 128, T])
    Vd = dscr("Vd", [T, 1024])
    G = dscr("G", [T, 2048])
    H2 = dscr("H2", [T, D])
    DBG = dbg
    dbg_idx = nc.dram_tensor("dbg_idx", [4, 128, 1], I32, kind="ExternalOutput").ap() if dbg else None
    dbg_gate = nc.dram_tensor("dbg_gate", [128, 4], F32, kind="ExternalOutput").ap() if dbg else None
    dbg_xe = nc.dram_tensor("dbg_xe", [128, 1024], BF16, kind="ExternalOutput").ap() if dbg else None
    dbg_ye = nc.dram_tensor("dbg_ye", [128, 1024], F32, kind="ExternalOutput").ap() if dbg else None
    dbg_posm = nc.dram_tensor("dbg_posm", [128, NT * NE], F32, kind="ExternalOutput").ap() if dbg else None
    dbg_aff = nc.dram_tensor("dbg_aff", [128, NT * NE], F32, kind="ExternalOutput").ap() if dbg else None
    dbg_hT = nc.dram_tensor("dbg_hT", [128, 16 * CAP], BF16, kind="ExternalOutput").ap() if dbg else None
    PQHL = dscr("PQHL", [8, 2, T])
    MG = dscr("MG", [T, D])
    RQmT, RKmT, RVm, RQdT, RKdT, RVd, RG, RH2, RMG, Rout = [R(n) for n in
                                                          "QmT KmT Vm QdT KdT Vd G H2 MG out".split()]

    P = Prog(nc)
    sb = nc.sbuf_tensor
    with ExitStack() as es1:
        identb = es1.enter_context(sb("identb", [128, 128], BF16))
        identf = es1.enter_context(sb("identf", [128, 128], F32))
        mhalf = es1.enter_context(sb("mhalf", [128, 64], F32))
        cosT = es1.enter_context(sb("cosT", [128, NT, 16], F32))
        sinT = es1.enter_context(sb("sinT", [128, NT, 16], F32))
        posf = es1.enter_context(sb("posf", [128, NT], F32))
        nlam = es1.enter_context(sb("nlam", [128, 2], F32))
        vq_hn = es1.enter_context(sb("vq_hn", [128, 96], F32))
        vk_hn = es1.enter_context(sb("vk_hn", [128, 96], F32))
        vdq_hn = es1.enter_context(sb("vdq_hn", [128, 64], F32))
        vdk_hn = es1.enter_context(sb("vdk_hn", [128, 64], F32))
        vsubln = es1.enter_context(sb("vsubln", [128, 128], F32))
        vqn = es1.enter_context(sb("vqn", [128, 256], F32))
        vkvn = es1.enter_context(sb("vkvn", [128, 256], F32))
        Rid, Rmh, Rcs, Rposf, Rnlam, Rvec = [R(n) for n in "id mh cs posf nlam vec".split()]

        with ExitStack() as es2:
            posi = es2.enter_context(sb("p0_posi", [128, NT], I32))
            invf = es2.enter_context(sb("p0_invf", [128, 16], F32))
            ang = es2.enter_context(sb("p0_ang", [128, NT, 16], F32))
            kf = es2.enter_context(sb("p0_kf", [128, NT, 16], F32))
            ki = es2.enter_context(sb("p0_ki", [128, NT, 16], I32))
            mm_ = es2.enter_context(sb("p0_m", [128, NT, 16], F32))
            lamt = es2.enter_context(sb("p0_lam", [128, 256], F32))
            lp = es2.enter_context(sb("p0_lp", [128, 128], F32))
            ls = es2.enter_context(sb("p0_ls", [128, 4], F32))
            Rt = R("p0tmp")
            P.dma("sp", I(nc.sync.dma_start, out=identf[:], in_=ident_d[:, :]), Rid, pwrites=[Rid])
            P.dma("pool", I(nc.gpsimd.dma_start, out=identb[:], in_=ident_d[:, :]), Rid, pwrites=[Rid])
            P.op("pool", I(nc.gpsimd.memset, mhalf[:], -0.5), writes=[Rmh])
            for tl, nm in [(vq_hn, "q_hn"), (vk_hn, "k_hn"), (vdq_hn, "dq_hn"), (vdk_hn, "dk_hn"),
                           (vsubln, "subln"), (vqn, "q_norm_w"), (vkvn, "kv_norm_w"), (lamt, "lam")]:
                P.dma("sp", I(nc.sync.dma_start, out=tl[:], in_=vec_d[nm].partition_broadcast(128)), Rvec,
                      pwrites=[Rvec])
            P.dma("sp", I(nc.sync.dma_start, out=invf[:], in_=invf_d.partition_broadcast(128)), Rvec, pwrites=[Rvec])
            P.dma("sp", I(nc.sync.dma_start, out=posi[:], in_=post_d[:, :]), Rvec,
                  pwrites=[Rvec])
            P.op("dve", I(nc.vector.tensor_scalar, out=vq_hn[:], in0=vq_hn[:], scalar1=96.0 ** -0.5, scalar2=None,
                          op0=ALU.mult), reads=[Rvec], pwrites=[Rvec])
            P.op("dve", I(nc.vector.tensor_scalar, out=vdq_hn[:], in0=vdq_hn[:], scalar1=64.0 ** -0.5, scalar2=None,
                          op0=ALU.mult), reads=[Rvec], pwrites=[Rvec])
            P.op("dve", I(nc.vector.tensor_scalar, out=vsubln[:], in0=vsubln[:], scalar1=1.0 - LAM_INIT, scalar2=None,
                          op0=ALU.mult), reads=[Rvec], pwrites=[Rvec])
            P.op("dve", I(nc.vector.tensor_tensor, out=lp[:].rearrange("p (a b) -> p a b", a=2),
                          in0=lamt[:].rearrange("p (a two b) -> p a two b", a=2, two=2)[:, :, 0, :],
                          in1=lamt[:].rearrange("p (a two b) -> p a two b", a=2, two=2)[:, :, 1, :], op=ALU.mult),
                 reads=[Rvec], writes=[Rt])
            P.op("dve", I(nc.vector.tensor_reduce, out=ls[:, 0:2], in_=lp[:].rearrange("p (a b) -> p a b", a=2),
                          axis=AX.X, op=ALU.add), reads=[Rt], writes=[Rnlam])
            P.op("act", I(nc.scalar.activation, out=ls[:, 2:4], in_=ls[:, 0:2], func=AF.Exp), reads=[Rnlam],
                 writes=[Rt])
            P.op("dve", I(nc.vector.tensor_tensor, out=nlam[:, 0:1], in0=ls[:, 3:4], in1=ls[:, 2:3], op=ALU.subtract),
                 reads=[Rt], writes=[Rnlam])
            P.op("dve", I(nc.vector.tensor_scalar, out=nlam[:, 0:1], in0=nlam[:, 0:1], scalar1=-LAM_INIT, scalar2=None,
                          op0=ALU.add), reads=[Rnlam], writes=[Rnlam])
            P.op("dve", I(nc.vector.tensor_copy, out=posf[:], in_=posi[:]), reads=[Rvec], writes=[Rposf])
            P.op("dve", I(nc.vector.tensor_tensor, out=ang[:], in0=posf[:].unsqueeze(2).to_broadcast([128, NT, 16]),
                          in1=invf[:].unsqueeze(1).to_broadcast([128, NT, 16]), op=ALU.mult),
                 reads=[Rposf, Rvec], writes=[Rt])

            Rk = R("rk")

            def reduce_sin(dst, shift):
                P.op("dve", I(nc.vector.tensor_scalar, out=kf[:], in0=ang[:], scalar1=1.0 / TWO_PI,
                              scalar2=0.5 + shift / TWO_PI, op0=ALU.mult, op1=ALU.add), reads=[Rt], writes=[Rk])
                P.op("dve", I(nc.vector.tensor_copy, out=ki[:], in_=kf[:]), reads=[Rk], writes=[Rk])
                P.op("dve", I(nc.vector.tensor_copy, out=kf[:], in_=ki[:]), reads=[Rk], writes=[Rk])
                c1 = 6.28125
                c2 = TWO_PI - c1
                P.op("dve", I(nc.vector.scalar_tensor_tensor, out=mm_[:], in0=kf[:], scalar=-c1, in1=ang[:],
                              op0=ALU.mult, op1=ALU.add), reads=[Rk, Rt], writes=[Rk])
                P.op("dve", I(nc.vector.scalar_tensor_tensor, out=mm_[:], in0=kf[:], scalar=-c2, in1=mm_[:],
                              op0=ALU.mult, op1=ALU.add), reads=[Rk], writes=[Rk])
                if shift:
                    P.op("dve", I(nc.vector.tensor_scalar, out=mm_[:], in0=mm_[:], scalar1=shift, scalar2=None,
                                  op0=ALU.add), reads=[Rk], writes=[Rk])
                for thr, op_, adj in [(math.pi, ALU.is_gt, -TWO_PI), (-math.pi, ALU.is_lt, TWO_PI)]:
                    P.op("dve", I(nc.vector.tensor_scalar, out=kf[:], in0=mm_[:], scalar1=thr, scalar2=adj,
                                  op0=op_, op1=ALU.mult), reads=[Rk], writes=[Rk])
                    P.op("dve", I(nc.vector.tensor_tensor, out=mm_[:], in0=mm_[:], in1=kf[:], op=ALU.add),
                         reads=[Rk], writes=[Rk])
                P.op("dve", I(nc.vector.tensor_scalar, out=mm_[:], in0=mm_[:], scalar1=math.pi, scalar2=-math.pi,
                              op0=ALU.min, op1=ALU.max), reads=[Rk], writes=[Rk])
                P.op("act", I(nc.scalar.activation, out=dst[:], in_=mm_[:], func=AF.Sin), reads=[Rk], pwrites=[Rcs])

            reduce_sin(sinT, 0.0)
            reduce_sin(cosT, math.pi / 2)
            P.barrier()
            P.emit()

        env = locals()
        if upto >= "A":
            phaseA(nc, P, env)
        if upto >= "B":
            phaseBC(nc, P, env, upto)
        if upto >= "D":
            phaseDE(nc, P, env, upto)
        P.barrier()
        P.emit()
    P.close()
    return nc


def phaseA(nc, P, env):
    g = env
    sb = nc.sbuf_tensor
    x_d, win_d, bg_d, wuq_d, wukv_d, vec_d = g["x_d"], g["win_d"], g["bg_d"], g["wuq_d"], g["wukv_d"], g["vec_d"]
    identb, mhalf, cosT, sinT = g["identb"], g["mhalf"], g["cosT"], g["sinT"]
    vq_hn, vk_hn, vdq_hn, vdk_hn, vqn, vkvn = g["vq_hn"], g["vk_hn"], g["vdq_hn"], g["vdk_hn"], g["vqn"], g["vkvn"]
    Rid, Rmh, Rcs, Rvec = g["Rid"], g["Rmh"], g["Rcs"], g["Rvec"]
    QmT, KmT, Vm, QdT, KdT, Vd, G = g["QmT"], g["KmT"], g["Vm"], g["QdT"], g["KdT"], g["Vd"], g["G"]
    RQmT, RKmT, RVm, RQdT, RKdT, RVd, RG = g["RQmT"], g["RKmT"], g["RVm"], g["RQdT"], g["RKdT"], g["RVd"], g["RG"]
    with ExitStack() as es3:
        win = es3.enter_context(sb("a_win", [128, 8, 5664], BF16))
        wuq = es3.enter_context(sb("a_wuq", [128, 2, 1536], BF16))
        wukv = es3.enter_context(sb("a_wukv", [128, 2, 2048], BF16))
        wn = es3.enter_context(sb("a_wn", [128, 1024], F32))
        biasr = es3.enter_context(sb("a_bias", [1, 2048], BF16))
        onesr = es3.enter_context(sb("a_ones", [1, 128], BF16))
        xt0 = es3.enter_context(sb("a_xt0", [128, 1024], F32))
        xt1 = es3.enter_context(sb("a_xt1", [128, 1024], F32))
        hb = es3.enter_context(sb("a_hb", [128, 1024], BF16))
        hT = es3.enter_context(sb("a_hT", [128, 1024], BF16))
        sq = es3.enter_context(sb("a_sq", [128, 2048], F32))
        st = es3.enter_context(sb("a_st", [128, 64], F32))
        rstd = es3.enter_context(sb("a_rstd", [128, 64], F32))
        lat = es3.enter_context(sb("a_lat", [128, 512], BF16))
        latT = es3.enter_context(sb("a_latT", [128, 4, 128], BF16))
        qf = es3.enter_context(sb("a_qf", [128, 16, 96], F32))
        qb = es3.enter_context(sb("a_qb", [128, 16, 96], BF16))
        kf = es3.enter_context(sb("a_kf", [128, 16, 32], F32))
        kb = es3.enter_context(sb("a_kb", [128, 16, 96], BF16))
        rt = es3.enter_context(sb("a_rt", [128, 4, 16, 16], F32))
        vb = es3.enter_context(sb("a_vb", [128, 1024], BF16))
        qTs = es3.enter_context(sb("a_qTs", [128, 16, 128], BF16))
        kTs = es3.enter_context(sb("a_kTs", [128, 16, 128], BF16))
        df = es3.enter_context(sb("a_df", [128, 512], F32))
        dqb = es3.enter_context(sb("a_dqb", [128, 1024], BF16))
        dkb = es3.enter_context(sb("a_dkb", [128, 1024], BF16))
        dvb = es3.enter_context(sb("a_dvb", [128, 1024], BF16))
        dqTs = es3.enter_context(sb("a_dqTs", [128, 8, 128], BF16))
        dkTs = es3.enter_context(sb("a_dkTs", [128, 8, 128], BF16))
        gt = es3.enter_context(sb("a_gt", [128, 512], BF16))
        gb = es3.enter_context(sb("a_gb", [128, 2048], BF16))
        kr = es3.enter_context(sb("a_kr", [128, 32], F32))
        krw = es3.enter_context(sb("a_krw", [128, 32], F32))
        psF = es3.enter_context(nc.psum_tensor("a_psF", [128, 6 * 512], F32))
        psB = es3.enter_context(nc.psum_tensor("a_psB", [128, 2 * 1024], BF16))
        Rw = R("aw")
        P.dma("pool", I(nc.gpsimd.dma_start, out=win[:], in_=win_d.rearrange("(kc p) c -> p kc c", p=128)), Rw,
              pwrites=[Rw])
        P.dma("pool", I(nc.gpsimd.dma_start, out=wuq[:], in_=wuq_d.rearrange("(kc p) c -> p kc c", p=128)), Rw,
              pwrites=[Rw])
        P.dma("pool", I(nc.gpsimd.dma_start, out=wukv[:], in_=wukv_d.rearrange("(kc p) c -> p kc c", p=128)), Rw,
              pwrites=[Rw])
        P.dma("pool", I(nc.gpsimd.dma_start, out=biasr[:], in_=bg_d.rearrange("(o c) -> o c", o=1)), Rw, pwrites=[Rw])
        P.dma("sp", I(nc.sync.dma_start, out=wn[:], in_=vec_d["attn_norm_w"].partition_broadcast(128)), Rw,
              pwrites=[Rw])
        P.op("pool", I(nc.gpsimd.memset, onesr[:], 1.0), pwrites=[Rw])

        xts = [xt0, xt1]
        Rxt = [R("xt0"), R("xt1")]
        Rz = [R("z0"), R("z1")]
        zb = [psF[:, 0:512], psF[:, 512:1024]]
        ps4 = psF[:, 1024:3072]
        Rps4 = R("ps4")
        Tb = [psB[:, 0:1024], psB[:, 1024:2048]]
        RT = [R("T0"), R("T1")]
        names = "hb hT sq stx stl stq stk stkr std lat latT qf qb kf kb rt vb qTs kTs df dqb dkb dvb dqTs dkTs gt gb kr krw"
        RR = {n: R(n) for n in names.split()}

        def rs(src, dst, n, Rs, w):
            P.op("dve", I(nc.vector.tensor_scalar, out=dst, in0=src, scalar1=1.0 / n, scalar2=EPS, op0=ALU.mult,
                          op1=ALU.add), reads=[Rs], writes=[Rs])
            P.op("pool", I(nc.gpsimd.tensor_tensor, out=dst, in0=dst, in1=mhalf[:, 0:w], op=ALU.pow),
                 reads=[Rs, Rmh], writes=[Rs])

        def zblock(i, blk):
            P.tag = "z%d" % blk
            bank = zb[blk % 2]
            Rb = Rz[blk % 2]
            ncols = 512 if blk < 11 else 32
            c0 = blk * 512
            gate = 7 <= blk <= 10
            for kc in range(8):
                P.op("pe", I(nc.tensor.matmul, bank[:, 0:ncols], lhsT=hT[:, kc * 128:(kc + 1) * 128],
                             rhs=win[:, kc, c0:c0 + ncols], start=(kc == 0), stop=(kc == 7 and not gate)),
                     reads=[RR["hT"], Rw], writes=[Rb] if kc == 0 else (), pwrites=[Rb] if kc else ())
            if gate:
                gc = (blk - 7) * 512
                P.op("pe", I(nc.tensor.matmul, bank[:, 0:512], lhsT=onesr[0:1, :], rhs=biasr[0:1, gc:gc + 512],
                             start=False, stop=True), reads=[Rw], pwrites=[Rb])
            return bank, Rb

        def rope(i, src, dst, Rsrc, Rdst):
            c = cosT[:, i, :].unsqueeze(1).to_broadcast([128, 16, 16])
            s = sinT[:, i, :].unsqueeze(1).to_broadcast([128, 16, 16])
            x1 = src[:, :, 0:16]
            x2 = src[:, :, 16:32]
            Rrt = RR["rt"]
            P.op("dve", I(nc.vector.tensor_tensor, out=rt[:, 0], in0=x1, in1=c, op=ALU.mult), reads=[Rsrc, Rcs],
                 pwrites=[Rrt])
            P.op("dve", I(nc.vector.tensor_tensor, out=rt[:, 1], in0=x2, in1=s, op=ALU.mult), reads=[Rsrc, Rcs],
                 pwrites=[Rrt])
            P.op("dve", I(nc.vector.tensor_tensor, out=rt[:, 2], in0=x1, in1=s, op=ALU.mult), reads=[Rsrc, Rcs],
                 pwrites=[Rrt])
            P.op("dve", I(nc.vector.tensor_tensor, out=rt[:, 3], in0=x2, in1=c, op=ALU.mult), reads=[Rsrc, Rcs],
                 pwrites=[Rrt])
            P.op("dve", I(nc.vector.tensor_tensor, out=dst[:, :, 0:16], in0=rt[:, 0], in1=rt[:, 1], op=ALU.subtract),
                 reads=[Rrt], pwrites=[Rdst])
            P.op("dve", I(nc.vector.tensor_tensor, out=dst[:, :, 16:32], in0=rt[:, 2], in1=rt[:, 3], op=ALU.add),
                 reads=[Rrt], pwrites=[Rdst])

        latT2 = [latT, es3.enter_context(sb("a_latT1", [128, 4, 128], BF16))]
        kr2 = [kr, es3.enter_context(sb("a_kr1", [128, 32], F32))]
        krw2 = [krw, es3.enter_context(sb("a_krw1", [128, 32], F32))]
        qb2 = [qb, es3.enter_context(sb("a_qb1", [128, 16, 96], BF16))]
        kb2 = [kb, es3.enter_context(sb("a_kb1", [128, 16, 96], BF16))]
        sq2 = sq[:, 512:2048]
        RlatT = [R("latT0"), R("latT1")]
        Rkr = [R("kr0"), R("kr1")]
        Rkrw = [R("krw0"), R("krw1")]
        Rstkr = [R("stkr0"), R("stkr1")]
        Rqb = [R("qb0"), R("qb1")]
        Rkb = [R("kb0"), R("kb1")]
        Rsq2 = R("sq2")

        dfr = [df, es3.enter_context(sb("a_df1", [128, 512], F32)), es3.enter_context(sb("a_df2", [128, 512], F32))]
        Rdfr = [R("df0"), R("df1"), R("df2")]
        Rstd = [R("std0"), R("std1")]
        dfc = [0]

        def dqk(i, blks, dstb, hn, Rdst, dTs, RdTs, dram, Rdram):
            t0 = i * 128
            for j, blk in enumerate(blks):
                bank, Rb = zblock(i, blk)
                k3 = dfc[0] % 3
                dfc[0] += 1
                dfk, Rdfk = dfr[k3], Rdfr[k3]
                c0 = 40 + 8 * (dfc[0] % 2)
                P.op("act", I(nc.scalar.copy, out=dfk[:], in_=bank[:, 0:512]), reads=[Rb], writes=[Rdfk])
                P.op("act", I(nc.scalar.activation, out=sq[:, 0:512], in_=dfk[:], func=AF.Square),
                     reads=[Rdfk], writes=[RR["sq"]])
                P.op("dve", I(nc.vector.tensor_reduce, out=st[:, c0:c0 + 8],
                              in_=sq[:, 0:512].rearrange("p (a b) -> p a b", a=8), axis=AX.X, op=ALU.add),
                     reads=[RR["sq"]], writes=[Rstd[dfc[0] % 2]])
                rs(st[:, c0:c0 + 8], rstd[:, c0:c0 + 8], 64.0, Rstd[dfc[0] % 2], 8)
                P.op("dve", I(nc.vector.tensor_tensor, out=dfk[:].rearrange("p (a b) -> p a b", a=8),
                              in0=dfk[:].rearrange("p (a b) -> p a b", a=8),
                              in1=rstd[:, c0:c0 + 8].unsqueeze(2).to_broadcast([128, 8, 64]),
                              op=ALU.mult), reads=[Rdfk, Rstd[dfc[0] % 2]], writes=[Rdfk])
                P.op("dve", I(nc.vector.tensor_tensor,
                              out=dstb[:, j * 512:(j + 1) * 512].rearrange("p (a b) -> p a b", a=8),
                              in0=dfk[:].rearrange("p (a b) -> p a b", a=8),
                              in1=hn[:].unsqueeze(1).to_broadcast([128, 8, 64]), op=ALU.mult),
                     reads=[Rdfk, Rvec], writes=[Rdst] if j == 0 else (), pwrites=[Rdst] if j else ())

        def dqkT(i, dstb, Rdst, dTs, RdTs, dram, Rdram):
            P.tag = "dqkT"
            t0 = i * 128
            for h in range(8):
                P.op("pe", I(nc.tensor.transpose, out=Tb[0][:, h * 128:(h + 1) * 128],
                             in_=dstb[:, h * 128:(h + 1) * 128], identity=identb[:]),
                     reads=[Rdst, Rid], writes=[RT[0]] if h == 0 else (), pwrites=[RT[0]] if h else ())
            P.op("act", I(nc.scalar.copy, out=dTs[:].rearrange("p a b -> p (a b)"), in_=Tb[0][:, 0:1024]),
                 reads=[RT[0]], writes=[RdTs])
            P.dma("sp", I(nc.sync.dma_start, out=dram[:, :, t0:t0 + 128].rearrange("h d t -> d h t"), in_=dTs[:]),
                  RdTs, reads=[RdTs], pwrites=[Rdram])

        xts3 = [xt0, xt1, es3.enter_context(sb("a_xt2", [128, 1024], F32))]
        Rxt3 = [R("xt0"), R("xt1"), R("xt2")]
        hb2 = [hb, es3.enter_context(sb("a_hb1", [128, 1024], BF16))]
        Rhb = [R("hb0"), R("hb1")]
        Rstx = [R("stx0"), R("stx1")]

        def S1a(i):
            P.tag = "S1a"
            s_ = i % 2
            xt = xts3[i % 3]
            Rx = Rxt3[i % 3]
            t0 = i * 128
            c = 56 + 2 * s_
            P.dma("sp", I(nc.sync.dma_start, out=xt[:], in_=x_d[t0:t0 + 128, :]), Rx, writes=[Rx])
            P.op("act", I(nc.scalar.activation, out=sq[:, 0:1024], in_=xt[:], func=AF.Square,
                          accum_out=st[:, c:c + 1]), reads=[Rx], writes=[RR["sq"], Rstx[s_]])
            rs(st[:, c:c + 1], rstd[:, c:c + 1], 1024.0, Rstx[s_], 1)
            P.op("dve", I(nc.vector.scalar_tensor_tensor, out=hb2[s_][:], in0=xt[:], scalar=rstd[:, c:c + 1], in1=wn[:],
                          op0=ALU.mult, op1=ALU.mult), reads=[Rx, Rstx[s_], Rw], writes=[Rhb[s_]])

        def S1(i):
            P.tag = "S1head"
            s_ = i % 2
            t0 = i * 128
            for kc in range(8):
                P.op("pe", I(nc.tensor.transpose, out=Tb[0][:, kc * 128:(kc + 1) * 128],
                             in_=hb2[s_][:, kc * 128:(kc + 1) * 128], identity=identb[:]),
                     reads=[Rhb[s_], Rid], writes=[RT[0]] if kc == 0 else (), pwrites=[RT[0]] if kc else ())
            P.op("act", I(nc.scalar.copy, out=hT[:], in_=Tb[0][:, 0:1024]), reads=[RT[0]], writes=[RR["hT"]])
            bank, Rb = zblock(i, 0)
            k3 = dfc[0] % 3
            dfc[0] += 1
            latf, Rlatf = dfr[k3], Rdfr[k3]
            P.op("act", I(nc.scalar.copy, out=latf[:], in_=bank[:, 0:512]), reads=[Rb], writes=[Rlatf])
            P.op("act", I(nc.scalar.activation, out=sq[:, 0:512], in_=latf[:], func=AF.Square), reads=[Rlatf],
                 writes=[RR["sq"]])
            P.op("dve", I(nc.vector.tensor_reduce, out=st[:, 1:3], in_=sq[:, 0:512].rearrange("p (a b) -> p a b", a=2),
                          axis=AX.X, op=ALU.add), reads=[RR["sq"]], writes=[RR["stl"]])
            rs(st[:, 1:3], rstd[:, 1:3], 256.0, RR["stl"], 2)
            P.op("dve", I(nc.vector.scalar_tensor_tensor, out=lat[:, 0:256], in0=latf[:, 0:256], scalar=rstd[:, 1:2],
                          in1=vqn[:], op0=ALU.mult, op1=ALU.mult), reads=[Rlatf, RR["stl"], Rvec], writes=[RR["lat"]])
            P.op("dve", I(nc.vector.scalar_tensor_tensor, out=lat[:, 256:512], in0=latf[:, 256:512],
                          scalar=rstd[:, 2:3], in1=vkvn[:], op0=ALU.mult, op1=ALU.mult),
                 reads=[Rlatf, RR["stl"], Rvec], pwrites=[RR["lat"]])
            dqk(i, [1, 2], dqb, vdq_hn, RR["dqb"], dqTs, RR["dqTs"], QdT, RQdT)
            P.tag = "latT"
            for j in range(4):
                P.op("pe", I(nc.tensor.transpose, out=Tb[1][:, j * 128:(j + 1) * 128],
                             in_=lat[:, j * 128:(j + 1) * 128], identity=identb[:]),
                     reads=[RR["lat"], Rid], writes=[RT[1]] if j == 0 else (), pwrites=[RT[1]] if j else ())
            P.op("dve", I(nc.vector.tensor_copy, out=latT2[s_][:].rearrange("p a b -> p (a b)"), in_=Tb[1][:, 0:512]),
                 reads=[RT[1]], writes=[RlatT[s_]])
            dqk(i, [3, 4], dkb, vdk_hn, RR["dkb"], dkTs, RR["dkTs"], KdT, RKdT)
            for j, blk in enumerate([5, 6]):
                bank, Rb = zblock(i, blk)
                P.op("act", I(nc.scalar.copy, out=dvb[:, j * 512:(j + 1) * 512], in_=bank[:, 0:512]), reads=[Rb],
                     writes=[RR["dvb"]] if j == 0 else (), pwrites=[RR["dvb"]] if j else ())
            P.dma("sp", I(nc.sync.dma_start, out=Vd[t0:t0 + 128, :], in_=dvb[:]), RR["dvb"], reads=[RR["dvb"]],
                  pwrites=[RVd])
            dqkT(i, dqb, RR["dqb"], dqTs, RR["dqTs"], QdT, RQdT)
            for j, blk in enumerate([7, 8, 9, 10]):
                if j == 2:
                    dqkT(i, dkb, RR["dkb"], dkTs, RR["dkTs"], KdT, RKdT)
                bank, Rb = zblock(i, blk)
                P.op("act", I(nc.scalar.activation, out=gt[:], in_=bank[:, 0:512], func=AF.Tanh, scale=0.5),
                     reads=[Rb], writes=[RR["gt"]])
                P.op("dve", I(nc.vector.tensor_scalar, out=gb[:, j * 512:(j + 1) * 512], in0=gt[:], scalar1=0.5,
                              scalar2=0.5, op0=ALU.mult, op1=ALU.add), reads=[RR["gt"]],
                     writes=[RR["gb"]] if j == 0 else (), pwrites=[RR["gb"]] if j else ())
            P.dma("sp", I(nc.sync.dma_start, out=G[t0:t0 + 128, :], in_=gb[:]), RR["gb"], reads=[RR["gb"]],
                  pwrites=[RG])
            bank, Rb = zblock(i, 11)
            P.op("act", I(nc.scalar.copy, out=kr2[s_][:], in_=bank[:, 0:32]), reads=[Rb], writes=[Rkr[s_]])
            P.op("act", I(nc.scalar.activation, out=sq[:, 0:32], in_=kr2[s_][:], func=AF.Square,
                          accum_out=st[:, 36 + s_:37 + s_]), reads=[Rkr[s_]], writes=[RR["sq"], Rstkr[s_]])
            P.op("dve", I(nc.vector.tensor_tensor, out=krw2[s_][:], in0=kr2[s_][:], in1=vk_hn[:, 64:96], op=ALU.mult),
                 reads=[Rkr[s_], Rvec], writes=[Rkrw[s_]])

        def S2q(i):
            P.tag = "S2q"
            s_ = i % 2
            for b_ in range(4):
                for kc in range(2):
                    P.op("pe", I(nc.tensor.matmul, ps4[:, b_ * 512:b_ * 512 + 384], lhsT=latT2[s_][:, kc, :],
                                 rhs=wuq[:, kc, b_ * 384:(b_ + 1) * 384], start=(kc == 0), stop=(kc == 1)),
                         reads=[RlatT[s_], Rw], writes=[Rps4] if (b_ == 0 and kc == 0) else (),
                         pwrites=() if (b_ == 0 and kc == 0) else [Rps4])
            qv = ps4.rearrange("p (b x) -> p b x", b=4)[:, :, 0:384].rearrange("p b (h d) -> p b h d", h=4)
            sqv = sq2.rearrange("p (b h d) -> p b h d", b=4, h=4)
            qfv = qf[:].rearrange("p (b h) d -> p b h d", b=4)
            P.op("act", I(nc.scalar.copy, out=qfv, in_=qv), reads=[Rps4], writes=[RR["qf"]])
            P.op("act", I(nc.scalar.activation, out=sqv, in_=qfv, func=AF.Square), reads=[RR["qf"]], writes=[Rsq2])
            P.op("dve", I(nc.vector.tensor_reduce, out=st[:, 4:20].rearrange("p (b h) -> p b h", b=4), in_=sqv,
                          axis=AX.X, op=ALU.add), reads=[Rsq2], writes=[RR["stq"]])
            rs(st[:, 4:20], rstd[:, 4:20], 96.0, RR["stq"], 16)
            P.op("dve", I(nc.vector.tensor_tensor, out=qfv, in0=qfv,
                          in1=rstd[:, 4:20].rearrange("p (b h) -> p b h", b=4).unsqueeze(3).to_broadcast(
                              [128, 4, 4, 96]), op=ALU.mult), reads=[RR["qf"], RR["stq"]], writes=[RR["qf"]])
            P.op("dve", I(nc.vector.tensor_tensor, out=qf[:], in0=qf[:],
                          in1=vq_hn[:].unsqueeze(1).to_broadcast([128, 16, 96]), op=ALU.mult),
                 reads=[RR["qf"], Rvec], writes=[RR["qf"]])
            P.op("dve", I(nc.vector.tensor_copy, out=qb2[s_][:, :, 0:64], in_=qf[:, :, 0:64]), reads=[RR["qf"]],
                 writes=[Rqb[s_]])
            rope(i, qf[:, :, 64:96], qb2[s_][:, :, 64:96], RR["qf"], Rqb[s_])

        def S2k(i):
            P.tag = "S2k"
            s_ = i % 2
            t0 = i * 128
            for b_ in range(4):
                for kc in range(2):
                    P.op("pe", I(nc.tensor.matmul, ps4[:, b_ * 512:(b_ + 1) * 512], lhsT=latT2[s_][:, 2 + kc, :],
                                 rhs=wukv[:, kc, b_ * 512:(b_ + 1) * 512], start=(kc == 0), stop=(kc == 1)),
                         reads=[RlatT[s_], Rw], writes=[Rps4] if (b_ == 0 and kc == 0) else (),
                         pwrites=() if (b_ == 0 and kc == 0) else [Rps4])
            kvv = ps4.rearrange("p (h d) -> p h d", h=16)
            sqk = sq2[:, 0:1024].rearrange("p (h d) -> p h d", h=16)
            P.op("act", I(nc.scalar.activation, out=sqk, in_=kvv[:, :, 0:64], func=AF.Square), reads=[Rps4],
                 writes=[Rsq2])
            P.op("dve", I(nc.vector.tensor_reduce, out=st[:, 20:36], in_=sqk, axis=AX.X, op=ALU.add),
                 reads=[Rsq2], writes=[RR["stk"]])
            P.op("dve", I(nc.vector.tensor_scalar, out=st[:, 20:36], in0=st[:, 20:36], scalar1=st[:, 36 + s_:37 + s_],
                          scalar2=None, op0=ALU.add), reads=[RR["stk"], Rstkr[s_]], writes=[RR["stk"]])
            rs(st[:, 20:36], rstd[:, 20:36], 96.0, RR["stk"], 16)
            P.op("dve", I(nc.vector.tensor_tensor, out=qf[:, :, 0:64], in0=kvv[:, :, 0:64],
                          in1=rstd[:, 20:36].unsqueeze(2).to_broadcast([128, 16, 64]), op=ALU.mult),
                 reads=[Rps4, RR["stk"]], writes=[RR["qf"]])
            P.op("dve", I(nc.vector.tensor_tensor, out=kb2[s_][:, :, 0:64], in0=qf[:, :, 0:64],
                          in1=vk_hn[:, 0:64].unsqueeze(1).to_broadcast([128, 16, 64]), op=ALU.mult),
                 reads=[RR["qf"], Rvec], writes=[Rkb[s_]])
            P.op("dve", I(nc.vector.tensor_tensor, out=kf[:], in0=krw2[s_][:].unsqueeze(1).to_broadcast([128, 16, 32]),
                          in1=rstd[:, 20:36].unsqueeze(2).to_broadcast([128, 16, 32]), op=ALU.mult),
                 reads=[Rkrw[s_], RR["stk"]], writes=[RR["kf"]])
            rope(i, kf[:], kb2[s_][:, :, 64:96], RR["kf"], Rkb[s_])
            P.op("act", I(nc.scalar.copy, out=vb[:].rearrange("p (h d) -> p h d", h=16), in_=kvv[:, :, 64:128]),
                 reads=[Rps4], writes=[RR["vb"]])
            P.dma("sp", I(nc.sync.dma_start, out=Vm[t0:t0 + 128, :], in_=vb[:]), RR["vb"], reads=[RR["vb"]],
                  pwrites=[RVm])

        def S3(i):
            P.tag = "S3"
            s_ = i % 2
            t0 = i * 128
            for src, Rsrc, dst, Rdst, dram, Rdram in [(qb2[s_], Rqb[s_], qTs, RR["qTs"], QmT, RQmT),
                                                       (kb2[s_], Rkb[s_], kTs, RR["kTs"], KmT, RKmT)]:
                for half in range(2):
                    for hh in range(8):
                        h = half * 8 + hh
                        P.op("pe", I(nc.tensor.transpose, out=Tb[half][0:96, hh * 128:(hh + 1) * 128],
                                     in_=src[:, h, :], identity=identb[:]), reads=[Rsrc, Rid],
                             writes=[RT[half]] if hh == 0 else (), pwrites=[RT[half]] if hh else ())
                    eng = "act" if half == 0 else "dve"
                    fn = nc.scalar.copy if half == 0 else nc.vector.tensor_copy
                    P.op(eng, I(fn, out=dst[0:96, half * 8:(half + 1) * 8, :].rearrange("p a b -> p (a b)"),
                                in_=Tb[half][0:96, 0:1024]), reads=[RT[half]],
                         writes=[Rdst] if half == 0 else (), pwrites=[Rdst] if half else ())
                P.dma("sp", I(nc.sync.dma_start, out=dram[:, :, t0:t0 + 128].rearrange("h d t -> d h t"),
                              in_=dst[0:96, :, :]), Rdst, reads=[Rdst], pwrites=[Rdram])

        S1a(0)
        S1a(1)
        S1(0)
        for i in range(NT):
            if i + 1 < NT:
                S1(i + 1)
            if i >= 1:
                S3(i - 1)
            if i + 2 < NT:
                S1a(i + 2)
            S2q(i)
            S2k(i)
        S3(NT - 1)
        P.barrier()
        P.emit()


def skip_tile(h, q0, q1, k0, k1):
    return False


def phaseBC(nc, P, env, upto):
    g = env
    sb = nc.sbuf_tensor
    QmT, KmT, Vm, QdT, KdT, Vd, G, MG = [g[k] for k in "QmT KmT Vm QdT KdT Vd G MG".split()]
    RMG = g["RMG"]
    mhalf, nlam, vsubln, posf, pos_d = g["mhalf"], g["nlam"], g["vsubln"], g["posf"], g["pos_d"]
    Rmh, Rnlam, Rvec, Rposf = g["Rmh"], g["Rnlam"], g["Rvec"], g["Rposf"]
    with ExitStack() as es:
        omla = es.enter_context(sb("om", [128, NT, 1024], BF16))
        Rom = R("omla")
        with ExitStack() as es2:
            qT = [es2.enter_context(sb("b_qT%d" % k, [96, T], BF16)) for k in range(2)]
            kT = [es2.enter_context(sb("b_kT%d" % k, [96, T], BF16)) for k in range(2)]
            vv = [es2.enter_context(sb("b_v%d" % k, [128, NT, 65], BF16)) for k in range(2)]
            ga = [es2.enter_context(sb("b_ga%d" % k, [128, NT, 64], BF16)) for k in range(2)]
            pT = [es2.enter_context(sb("b_pT%d" % k, [128, 1536], BF16)) for k in range(3)]
            rc = es2.enter_context(sb("b_rc", [128, 8], F32))
            ps = es2.enter_context(nc.psum_tensor("b_ps", [128, 8 * 512], F32))
            Rq = [R("bq0"), R("bq1")]
            RpT = [R("pT0"), R("pT1"), R("pT2")]
            Rs = [R("bs0"), R("bs1")]
            Ro = [R("bo0"), R("bo1")]
            Rrc = R("brc")
            sbank = [ps[:, 1536 * k:1536 * (k + 1)] for k in range(2)]
            GRP = [(3 * g_, 3) for g_ in range(10)] + [(30, 2)]

            def obank(os_, qs):
                b0 = 3072 + 512 * os_ + 66 * qs
                return ps[:, b0:b0 + 65]
            for k in range(2):
                P.op("pool", I(nc.gpsimd.memset, vv[k][:, :, 64:65], 1.0), pwrites=[Rq[k]])
            step = 0
            for h in range(16):
                s_ = h % 2
                P.dma("sp", I(nc.sync.dma_start, out=qT[s_][:], in_=QmT[h, :, :]), Rq[s_], pwrites=[Rq[s_]])
                P.dma("sp", I(nc.sync.dma_start, out=kT[s_][:], in_=KmT[h, :, :]), Rq[s_], pwrites=[Rq[s_]])
                P.dma("sp", I(nc.sync.dma_start, out=vv[s_][:, :, 0:64],
                              in_=Vm[:, h * 64:(h + 1) * 64].rearrange("(i p) d -> p i d", p=128)), Rq[s_],
                      pwrites=[Rq[s_]])
                P.dma("sp", I(nc.sync.dma_start, out=ga[s_][:],
                              in_=G[:, h * 64:(h + 1) * 64].rearrange("(i p) d -> p i d", p=128)), Rq[s_],
                      pwrites=[Rq[s_]])
                steps = [(qb, kp) for qb in range(8) for kp in range(len(GRP))]

                def b_front(qb, kp, sb_, pb_, s_=s_):
                    P.tag = "Bf"
                    k0, nk = GRP[kp]
                    for kk in range(nk):
                        kt = k0 + kk
                        P.op("pe", I(nc.tensor.matmul, sbank[sb_][:, kk * 512:(kk + 1) * 512],
                                     lhsT=kT[s_][:, kt * 128:(kt + 1) * 128],
                                     rhs=qT[s_][:, qb * 512:(qb + 1) * 512], start=True, stop=True),
                             reads=[Rq[s_]], writes=[Rs[sb_]] if kk == 0 else (), pwrites=[Rs[sb_]] if kk else ())
                    P.op("act", I(nc.scalar.activation, out=pT[pb_][:, 0:512 * nk], in_=sbank[sb_][:, 0:512 * nk],
                                  func=AF.Exp), reads=[Rs[sb_]], writes=[RpT[pb_]])

                def b_back(qb, kp, pb_, s_=s_, h=h):
                    P.tag = "Bb"
                    os_ = (h * 8 + qb) % 2
                    k0, nk = GRP[kp]
                    for kk in range(nk):
                        kt = k0 + kk
                        for qs in range(4):
                            w_ = kt == 0 and qs == 0
                            P.op("pe", I(nc.tensor.matmul, obank(os_, qs),
                                         lhsT=pT[pb_][:, kk * 512 + qs * 128:kk * 512 + (qs + 1) * 128],
                                         rhs=vv[s_][:, kt, :], start=w_, stop=(kt == NT - 1), skip_group_check=True),
                                 reads=[RpT[pb_], Rq[s_]], writes=[Ro[os_]] if w_ else (),
                                 pwrites=() if w_ else [Ro[os_]])
                    if kp == len(GRP) - 1:
                        P.tag = "Bep"
                        ov = ps[:, 3072 + 512 * os_:3072 + 512 * os_ + 264].rearrange("p (q c) -> p q c", q=4)
                        P.op("dve", I(nc.vector.reciprocal, out=rc[:, 0:4], in_=ov[:, :, 64]), reads=[Ro[os_]],
                             writes=[Rrc])
                        for qs in range(4):
                            ti = qb * 4 + qs
                            P.op("dve", I(nc.vector.scalar_tensor_tensor, out=omla[:, ti, h * 64:(h + 1) * 64],
                                          in0=obank(os_, qs)[:, 0:64], scalar=rc[:, qs:qs + 1], in1=ga[s_][:, ti, :],
                                          op0=ALU.mult, op1=ALU.mult), reads=[Ro[os_], Rrc, Rq[s_]], pwrites=[Rom])

                ring = []
                for n in range(len(steps) + 1):
                    if n < len(steps):
                        sb_, pb_ = step % 2, step % 3
                        step += 1
                        b_front(steps[n][0], steps[n][1], sb_, pb_)
                        ring.append(pb_)
                    if n >= 1:
                        b_back(steps[n - 1][0], steps[n - 1][1], ring[n - 1])
            if upto < "C":
                for i in range(NT):
                    P.dma("sp", I(nc.sync.dma_start, out=MG[i * 128:(i + 1) * 128, :], in_=omla[:, i, :]), Rom,
                          reads=[Rom], pwrites=[RMG])
            P.barrier()
            P.emit()
        if upto < "C":
            return

        CLS, DMIN = g["CLS"], g["DMIN"]
        slp_d, PQHL = g["slopes_d"], g["PQHL"]
        RPQHL = R("PQHL")
        with ExitStack() as es2:
            pq = es2.enter_context(sb("c_pq", [128, T], F32))
            Rpq = R("cpq")
            with ExitStack() as es3:
                pqi = es3.enter_context(sb("c_pqi", [128, T], I32))
                ahi = es3.enter_context(sb("c_ahi", [8, T], BF16))
                alo = es3.enter_context(sb("c_alo", [8, T], BF16))
                shi = es3.enter_context(sb("c_shi", [8, T], BF16))
                slo = es3.enter_context(sb("c_slo", [8, T], BF16))
                msl = es3.enter_context(sb("c_msl", [8, 1], F32))
                Rt = R("ctmp")
                P.dma("sp", I(nc.sync.dma_start, out=pqi[:], in_=pos_d.partition_broadcast(128)), Rpq, writes=[Rpq])
                P.dma("sp", I(nc.sync.dma_start, out=msl[:], in_=slp_d.rearrange("(h o) -> h o", o=1)), Rt,
                      writes=[Rt])
                P.op("dve", I(nc.vector.tensor_copy, out=pq[:], in_=pqi[:]), reads=[Rpq], writes=[Rpq])
                P.op("dve", I(nc.vector.tensor_copy, out=ahi[:], in_=pq[0:8, :]), reads=[Rpq], pwrites=[Rt])
                P.op("dve", I(nc.vector.tensor_tensor, out=alo[:], in0=pq[0:8, :], in1=ahi[:], op=ALU.subtract),
                     reads=[Rpq, Rt], pwrites=[Rt])
                P.op("dve", I(nc.vector.tensor_scalar, out=shi[:], in0=ahi[:], scalar1=msl[:, 0:1], scalar2=None,
                              op0=ALU.mult), reads=[Rt], pwrites=[Rt])
                P.op("dve", I(nc.vector.tensor_scalar, out=slo[:], in0=alo[:], scalar1=msl[:, 0:1], scalar2=None,
                              op0=ALU.mult), reads=[Rt], pwrites=[Rt])
                P.dma("sp", I(nc.sync.dma_start, out=PQHL[:, 0, :], in_=shi[:]), Rt, reads=[Rt], pwrites=[RPQHL])
                P.dma("sp", I(nc.sync.dma_start, out=PQHL[:, 1, :], in_=slo[:]), Rt, reads=[Rt], pwrites=[RPQHL])
                P.barrier()
                P.emit()
            nposf = es2.enter_context(sb("c_nposf", [128, NT], F32))
            bia = es2.enter_context(sb("c_bia", [128, 2, 2, NT], F32))
            P.op("dve", I(nc.vector.tensor_scalar, out=nposf[:], in0=posf[:], scalar1=-1.0, scalar2=None,
                          op0=ALU.mult), reads=[Rposf], writes=[Rpq])
            qT = [[es2.enter_context(sb("c_qT%d%d" % (k, m), [68, T], BF16)) for m in range(2)] for k in range(2)]
            kT = [[es2.enter_context(sb("c_kT%d%d" % (k, m), [68, T], BF16)) for m in range(2)] for k in range(2)]
            vv = [es2.enter_context(sb("c_v%d" % k, [128, NT, 129], BF16)) for k in range(2)]
            gbt = [es2.enter_context(sb("c_gb%d" % k, [128, NT, 128], BF16)) for k in range(2)]
            pT = [es2.enter_context(sb("c_pT%d" % k, [128, 512], BF16)) for k in range(4)]
            sp_ = [es2.enter_context(sb("c_sp%d" % k, [128, 512], F32)) for k in range(2)]
            dt_ = [es2.enter_context(sb("c_dt%d" % k, [128, 256], F32)) for k in range(2)]
            rc = es2.enter_context(sb("c_rc", [128, 16], F32))
            of = es2.enter_context(sb("c_of", [128, 4, 128], F32))
            jk = es2.enter_context(sb("c_jk", [128, 2, 128], F32))
            ps = es2.enter_context(nc.psum_tensor("c_ps", [128, 8 * 512], F32))
            Rq = [R("cq0"), R("cq1")]
            Rgs = [R("cgs0"), R("cgs1")]
            Rbia = [R("cbia0"), R("cbia1")]
            RpT = [R("cpT%d" % k) for k in range(4)]
            Rsp = [R("csp%d" % k) for k in range(2)]
            Rdt = [R("cdt%d" % k) for k in range(2)]
            Rs = [R("cs%d" % k) for k in range(4)]
            Ro = [R("co0"), R("co1")]
            Rrc, Rof, Rjk = R("crc"), R("cof"), R("cjk")
            sbank = [ps[:, 512 * k:512 * (k + 1)] for k in range(4)]
            def oacc(s, mp, qs):
                b0 = 2048 + (2 * s + mp) * 512 + qs * 132
                return ps[:, b0:b0 + 129]

            def oset(s):
                return ps[:, 2048 + 2 * s * 512:2048 + (2 * s + 2) * 512]
            Rkaug = [[R("ckaug%d%d" % (k, m)) for m in range(2)] for k in range(2)]
            for k in range(2):
                P.op("pool", I(nc.gpsimd.memset, vv[k][:, :, 128:129], 1.0), pwrites=[Rq[k]])
                for m in range(2):
                    P.op("dve", I(nc.vector.memset, kT[k][m][64:68, :], 2.0), writes=[Rkaug[k][m]])
                    P.op("dve", I(nc.vector.memset, kT[k][m][64:66, :], -1.0), writes=[Rkaug[k][m]])
            step = 0
            dstep = 0
            oset_i = 0
            for h in range(8):
                s_ = h % 2
                slope = 2.0 ** (-(h + 1))
                for m in range(2):
                    P.dma("sp", I(nc.sync.dma_start, out=qT[s_][m][0:64, :], in_=QdT[h, m * 64:(m + 1) * 64, :]),
                          Rq[s_], pwrites=[Rq[s_]])
                    for a_ in range(2):
                        P.dma("sp", I(nc.sync.dma_start, out=qT[s_][m][64 + 2 * a_:66 + 2 * a_, :], in_=PQHL[h, :, :]),
                              Rq[s_], reads=[RPQHL], pwrites=[Rq[s_]])
                    P.dma("sp", I(nc.sync.dma_start, out=kT[s_][m][0:64, :], in_=KdT[h, m * 64:(m + 1) * 64, :]),
                          Rq[s_], pwrites=[Rq[s_]])
                P.dma("sp", I(nc.sync.dma_start, out=vv[s_][:, :, 0:128],
                              in_=Vd[:, h * 128:(h + 1) * 128].rearrange("(i p) d -> p i d", p=128)), Rq[s_],
                      pwrites=[Rq[s_]])
                P.dma("sp", I(nc.sync.dma_start, out=gbt[s_][:],
                              in_=G[:, 1024 + h * 128:1024 + (h + 1) * 128].rearrange("(i p) d -> p i d", p=128)),
                      Rq[s_], pwrites=[Rq[s_]])
                P.op("dve", I(nc.vector.tensor_tensor, out=gbt[s_][:], in0=gbt[s_][:],
                              in1=vsubln[:].unsqueeze(1).to_broadcast([128, NT, 128]), op=ALU.mult),
                     reads=[Rq[s_], Rvec], writes=[Rgs[s_]])
                P.op("dve", I(nc.vector.tensor_scalar, out=bia[:, s_, 0, :], in0=posf[:], scalar1=slope, scalar2=None,
                              op0=ALU.mult), reads=[Rposf], writes=[Rbia[s_]])
                P.op("dve", I(nc.vector.tensor_scalar, out=bia[:, s_, 1, :], in0=posf[:], scalar1=-slope, scalar2=None,
                              op0=ALU.mult), reads=[Rposf], pwrites=[Rbia[s_]])
                steps = []
                for qb in range(16):
                    kts = [kt for kt in range(NT) if slope * DMIN[qb][kt] < SKIP_T]
                    for n_, kt in enumerate(kts):
                        steps.append((qb, kt, int(CLS[qb][kt]), n_ == 0, n_ == len(kts) - 1))

                def st1(n, r4, d2, s_=s_):
                    P.tag = "C1"
                    qb, kt, cl, first, last = steps[n]
                    q0 = qb * 256
                    K_ = (64, 66, 68)[cl]
                    for mp in range(2):
                        P.op("pe", I(nc.tensor.matmul, sbank[r4][:, mp * 256:(mp + 1) * 256],
                                     lhsT=kT[s_][mp][0:K_, kt * 128:(kt + 1) * 128],
                                     rhs=qT[s_][mp][0:K_, q0:q0 + 256], start=True, stop=True),
                             reads=[Rq[s_], Rkaug[s_][mp]], writes=[Rs[r4]] if mp == 0 else (),
                             pwrites=[Rs[r4]] if mp else ())
                    if cl == 0:
                        P.op("act", I(nc.scalar.activation, out=dt_[d2][:], in_=pq[:, q0:q0 + 256], func=AF.Abs,
                                      bias=nposf[:, kt:kt + 1]), reads=[Rpq], writes=[Rdt[d2]])

                def st23(n, r4, d2, s_=s_, slope=slope):
                    P.tag = "C2"
                    qb, kt, cl, first, last = steps[n]
                    if cl == 0:
                        P.op("dve", I(nc.vector.scalar_tensor_tensor,
                                      out=sp_[d2][:].rearrange("p (m q) -> p m q", m=2),
                                      in0=dt_[d2][:].unsqueeze(1).to_broadcast([128, 2, 256]), scalar=-slope,
                                      in1=sbank[r4].rearrange("p (m q) -> p m q", m=2), op0=ALU.mult, op1=ALU.add),
                             reads=[Rdt[d2], Rs[r4]], writes=[Rsp[d2]])
                        P.op("act", I(nc.scalar.activation, out=pT[r4][:], in_=sp_[d2][:], func=AF.Exp),
                             reads=[Rsp[d2]], writes=[RpT[r4]])
                    else:
                        P.op("act", I(nc.scalar.activation, out=pT[r4][:], in_=sbank[r4], func=AF.Exp,
                                      bias=bia[:, s_, cl - 1, kt:kt + 1]), reads=[Rs[r4], Rbia[s_]],
                             writes=[RpT[r4]])

                def st4(n, r4, os_, s_=s_, h=h):
                    P.tag = "C4"
                    qb, kt, cl, first, last = steps[n]
                    for mp in range(2):
                        for qs in range(2):
                            w_ = first and mp == 0 and qs == 0
                            P.op("pe", I(nc.tensor.matmul, oacc(os_, mp, qs),
                                         lhsT=pT[r4][:, mp * 256 + qs * 128:mp * 256 + (qs + 1) * 128],
                                         rhs=vv[s_][:, kt, :], start=(first and qs == 0), stop=last,
                                         skip_group_check=True),
                                 reads=[RpT[r4], Rq[s_]], writes=[Ro[os_]] if w_ else (),
                                 pwrites=() if w_ else [Ro[os_]])
                    if last:
                        pend.append((qb, os_))

                def ep(qb, os_, s_=s_, h=h):
                    P.tag = "Cep"
                    ti = qb * 2
                    v = oset(os_).rearrange("p (m c) -> p m c", m=2)[:, :, 0:264].rearrange("p m (q c) -> p m q c", q=2)
                    o1, o2 = v[:, 0, :, 0:128], v[:, 1, :, 0:128]
                    bc = lambda ap: ap.unsqueeze(2).to_broadcast([128, 2, 128])
                    P.op("dve", I(nc.vector.reciprocal, out=rc[:, 0:4].rearrange("p (m q) -> p m q", m=2),
                                  in_=v[:, :, :, 128]), reads=[Ro[os_]], writes=[Rrc])
                    P.op("dve", I(nc.vector.tensor_scalar, out=rc[:, 4:6], in0=rc[:, 2:4], scalar1=nlam[:, 0:1],
                                  scalar2=None, op0=ALU.mult), reads=[Rrc, Rnlam], pwrites=[Rrc])
                    P.op("dve", I(nc.vector.tensor_tensor, out=of[:, 0:2, :], in0=o2, in1=bc(rc[:, 4:6]), op=ALU.mult),
                         reads=[Ro[os_], Rrc], writes=[Rof])
                    P.op("dve", I(nc.vector.tensor_tensor, out=of[:, 2:4, :], in0=o1, in1=bc(rc[:, 0:2]), op=ALU.mult),
                         reads=[Ro[os_], Rrc], pwrites=[Rof])
                    P.op("dve", I(nc.vector.tensor_tensor, out=of[:, 0:2, :], in0=of[:, 0:2, :], in1=of[:, 2:4, :],
                                  op=ALU.add), reads=[Rof], pwrites=[Rof])
                    P.op("act", I(nc.scalar.activation, out=jk[:], in_=of[:, 0:2, :], func=AF.Square), reads=[Rof],
                         writes=[Rjk])
                    P.op("dve", I(nc.vector.tensor_reduce, out=rc[:, 6:8], in_=jk[:], axis=AX.X, op=ALU.add),
                         reads=[Rjk], pwrites=[Rrc])
                    P.op("dve", I(nc.vector.tensor_scalar, out=rc[:, 8:10], in0=rc[:, 6:8], scalar1=1.0 / 128,
                                  scalar2=EPS, op0=ALU.mult, op1=ALU.add), reads=[Rrc], pwrites=[Rrc])
                    P.op("pool", I(nc.gpsimd.tensor_tensor, out=rc[:, 10:12], in0=rc[:, 8:10], in1=mhalf[:, 0:2],
                                   op=ALU.pow), reads=[Rrc, Rmh], pwrites=[Rrc])
                    P.op("dve", I(nc.vector.tensor_tensor, out=of[:, 2:4, :], in0=of[:, 0:2, :], in1=bc(rc[:, 10:12]),
                                  op=ALU.mult), reads=[Rof, Rrc], pwrites=[Rof])
                    P.op("dve", I(nc.vector.tensor_tensor, out=of[:, 0:2, :], in0=of[:, 2:4, :],
                                  in1=gbt[s_][:, ti:ti + 2, :], op=ALU.mult), reads=[Rof, Rgs[s_], Rq[s_]],
                         pwrites=[Rof])
                    P.op("dve", I(nc.vector.tensor_tensor, out=omla[:, ti:ti + 2, h * 128:(h + 1) * 128],
                                  in0=of[:, 0:2, :], in1=omla[:, ti:ti + 2, h * 128:(h + 1) * 128], op=ALU.add),
                         reads=[Rof, Rom], pwrites=[Rom])

                ep_after = {}
                qbs = sorted(set(st_[0] for st_ in steps))
                for a_, qb_ in enumerate(qbs[:-1]):
                    nq = qbs[a_ + 1]
                    idxs = [k for k, st_ in enumerate(steps) if st_[0] == nq]
                    dg = [k for k in idxs if steps[k][2] == 0]
                    at = dg[-1] if dg else min(idxs[0] + 1, idxs[-1])
                    at = min(at, idxs[-1] - 1) if len(idxs) > 1 else idxs[0]
                    ep_after.setdefault(at, []).append(qb_)
                pend = []
                due = set()
                r4s, d2s, oss = [], [], []
                for n in range(len(steps) + 2):
                    if n < len(steps):
                        r4 = step % 4
                        step += 1
                        d2 = dstep % 2
                        if steps[n][2] == 0:
                            dstep += 1
                        if steps[n][3]:
                            oset_i += 1
                        r4s.append(r4)
                        d2s.append(d2)
                        oss.append(oset_i % 2)
                        st1(n, r4, d2)
                    if 1 <= n <= len(steps):
                        st23(n - 1, r4s[n - 1], d2s[n - 1])
                        due.update(ep_after.get(n - 1, []))
                        for pq_ in [p_ for p_ in pend if p_[0] in due]:
                            pend.remove(pq_)
                            ep(*pq_)
                    if n >= 2:
                        st4(n - 2, r4s[n - 2], oss[n - 2])
                        for pq_ in [p_ for p_ in pend if p_[0] in due]:
                            pend.remove(pq_)
                            ep(*pq_)
                for pq_ in pend:
                    ep(*pq_)
                pend = []
            for i in range(NT):
                P.dma("sp", I(nc.sync.dma_start, out=MG[i * 128:(i + 1) * 128, :], in_=omla[:, i, :]), Rom,
                      reads=[Rom], pwrites=[RMG])
            P.barrier()
            P.emit()


def phaseDE(nc, P, env, upto):
    g = env
    sb = nc.sbuf_tensor
    x_d, out_d, MG, H2, wout_d, rw_d, vec_d = [g[k] for k in "x_d out_d MG H2 wout_d rw_d vec_d".split()]
    wg_d, wu_d, wd_d, iota_d, tokpi_d = [g[k] for k in "wg_d wu_d wd_d iota_d tokpi_d".split()]
    identb, identf, mhalf = g["identb"], g["identf"], g["mhalf"]
    Rid, Rmh = g["Rid"], g["Rmh"]
    with ExitStack() as es:
        aff = es.enter_context(sb("aff", [128, NT, NE], F32))
        posm = es.enter_context(sb("posm", [128, NT, NE], F32))
        Raff, Rposm = R("aff"), R("posm")
        wsl = [es.enter_context(sb("e_w%d" % k, [128, 16384], BF16)) for k in range(3)]
        Rws = [R("ew%d" % k) for k in range(4)]

        def wload(e, which):
            for k, src in enumerate([wg_d, wu_d]):
                if k not in which:
                    continue
                sl = (3 * e + k) % 4
                P.dma("pool", I(nc.gpsimd.dma_start, out=wsl[sl][:].rearrange("p (kc f) -> p kc f", kc=8),
                                in_=src[e].rearrange("(kc p) f -> p kc f", p=128)), Rws[sl], writes=[Rws[sl]])
            if 2 in which:
                sl = (3 * e + 2) % 4
                P.dma("pool", I(nc.gpsimd.dma_start, out=wsl[sl][:].rearrange("p (j c) -> p j c", j=16),
                                in_=wd_d[e].rearrange("(j p) c -> p j c", p=128)), Rws[sl], writes=[Rws[sl]])
        with ExitStack() as es2:
            wout = es2.enter_context(sb("d_wout", [128, 8, 1024], BF16))
            rw = es2.enter_context(sb("d_rw", [128, 8, NE], F32))
            wn2 = es2.enter_context(sb("d_wn2", [128, 1024], F32))
            mg = [es2.enter_context(sb("d_mg%d" % k, [128, 1024], BF16)) for k in range(2)]
            xt = [es2.enter_context(sb("d_xt%d" % k, [128, 1024], F32)) for k in range(2)]
            x1 = [es2.enter_context(sb("d_x1%d" % k, [128, 1024], F32)) for k in range(2)]
            mT = es2.enter_context(sb("d_mT", [128, 1024], BF16))
            sq = es2.enter_context(sb("d_sq", [128, 1024], F32))
            h2f = es2.enter_context(sb("d_h2f", [128, 1024], F32))
            h2b = [es2.enter_context(sb("d_h2b%d" % k, [128, 1024], BF16)) for k in range(2)]
            h2T = es2.enter_context(sb("d_h2T", [128, 1024], F32))
            sm = es2.enter_context(sb("d_sm", [128, 64], F32))
            psB = es2.enter_context(nc.psum_tensor("d_psB", [128, 1024], BF16))
            psF = es2.enter_context(nc.psum_tensor("d_psF", [128, 5 * 512], F32))
            Rw = R("dw")
            Rmg, Rxt, Rx1, Rh2b = [[R(n + str(k)) for k in range(2)] for n in ("dmg", "dxt", "dx1", "dh2b")]
            RmT, Rsq, Rh2f, Rh2T, Rsm, RTb, Racc, RTf, Rlg = [R(n) for n in
                                                              "dmT dsq dh2f dh2T dsm dTb dacc dTf dlg".split()]
            RH2, Rout = g["RH2"], g["Rout"]
            P.dma("pool", I(nc.gpsimd.dma_start, out=wout[:], in_=wout_d.rearrange("(kc p) c -> p kc c", p=128)), Rw,
                  pwrites=[Rw])
            P.dma("sp", I(nc.sync.dma_start, out=rw[:], in_=rw_d.rearrange("(kc p) c -> p kc c", p=128)), Rw,
                  pwrites=[Rw])
            P.dma("sp", I(nc.sync.dma_start, out=wn2[:], in_=vec_d["ffn_norm_w"].partition_broadcast(128)), Rw,
                  pwrites=[Rw])
            wload(0, (0, 1, 2))
            acc = psF[:, 0:1024]
            Tf = psF[:, 1024:2048]
            lg = psF[:, 2048:2048 + NE]
            h2f2 = [h2f, es2.enter_context(sb("d_h2f1", [128, 1024], F32))]
            Rh2f2 = [R("dh2f0"), R("dh2f1")]
            RsmA = [R("dsmA0"), R("dsmA1")]
            RsmB = R("dsmB")
            def Da(i):
                P.tag = "Da"
                s_ = i % 2
                t0 = i * 128
                ca = 48 + 4 * s_
                h2f = h2f2[s_]
                Rh2f = Rh2f2[s_]
                Rsm = RsmA[s_]
                P.dma("sp", I(nc.sync.dma_start, out=mg[s_][:], in_=MG[t0:t0 + 128, :]), Rmg[s_], writes=[Rmg[s_]])
                P.dma("sp", I(nc.sync.dma_start, out=xt[s_][:], in_=x_d[t0:t0 + 128, :]), Rxt[s_], writes=[Rxt[s_]])
                for kc in range(8):
                    P.op("pe", I(nc.tensor.transpose, out=psB[:, kc * 128:(kc + 1) * 128],
                                 in_=mg[s_][:, kc * 128:(kc + 1) * 128], identity=identb[:]),
                         reads=[Rmg[s_], Rid], writes=[RTb] if kc == 0 else (), pwrites=[RTb] if kc else ())
                P.op("act", I(nc.scalar.copy, out=mT[:], in_=psB[:, 0:1024]), reads=[RTb], writes=[RmT])
                for half in range(2):
                    for kc in range(8):
                        first = half == 0 and kc == 0
                        P.op("pe", I(nc.tensor.matmul, acc[:, half * 512:(half + 1) * 512],
                                     lhsT=mT[:, kc * 128:(kc + 1) * 128], rhs=wout[:, kc, half * 512:(half + 1) * 512],
                                     start=(kc == 0), stop=(kc == 7)), reads=[RmT, Rw],
                             writes=[Racc] if first else (), pwrites=() if first else [Racc])
                P.op("dve", I(nc.vector.tensor_tensor, out=x1[s_][:], in0=acc, in1=xt[s_][:], op=ALU.add),
                     reads=[Racc, Rxt[s_]], writes=[Rx1[s_]])
                P.dma("sp", I(nc.sync.dma_start, out=out_d[t0:t0 + 128, :], in_=x1[s_][:]), Rx1[s_], reads=[Rx1[s_]],
                      pwrites=[Rout])
                P.op("act", I(nc.scalar.activation, out=sq[:], in_=x1[s_][:], func=AF.Square, accum_out=sm[:, ca:ca + 1]),
                     reads=[Rx1[s_]], writes=[Rsq, Rsm])
                P.op("dve", I(nc.vector.tensor_scalar, out=sm[:, ca + 1:ca + 2], in0=sm[:, ca:ca + 1], scalar1=1.0 / D, scalar2=EPS,
                              op0=ALU.mult, op1=ALU.add), reads=[Rsm], pwrites=[Rsm])
                P.op("pool", I(nc.gpsimd.tensor_tensor, out=sm[:, ca + 2:ca + 3], in0=sm[:, ca + 1:ca + 2], in1=mhalf[:, 0:1], op=ALU.pow),
                     reads=[Rsm, Rmh], pwrites=[Rsm])
                P.op("dve", I(nc.vector.scalar_tensor_tensor, out=h2f[:], in0=x1[s_][:], scalar=sm[:, ca + 2:ca + 3], in1=wn2[:],
                              op0=ALU.mult, op1=ALU.mult), reads=[Rx1[s_], Rsm, Rw], writes=[Rh2f])
                P.op("act", I(nc.scalar.copy, out=h2b[s_][:], in_=h2f[:]), reads=[Rh2f], writes=[Rh2b[s_]])
                P.dma("sp", I(nc.sync.dma_start, out=H2[t0:t0 + 128, :], in_=h2b[s_][:]), Rh2b[s_], reads=[Rh2b[s_]],
                      pwrites=[RH2])
            def Db(i):
                P.tag = "Db"
                s_ = i % 2
                h2f = h2f2[s_]
                Rh2f = Rh2f2[s_]
                Rsm = RsmB
                for kc in range(8):
                    P.op("pe", I(nc.tensor.transpose, out=Tf[:, kc * 128:(kc + 1) * 128],
                                 in_=h2f[:, kc * 128:(kc + 1) * 128], identity=identf[:]),
                         reads=[Rh2f, Rid], writes=[RTf] if kc == 0 else (), pwrites=[RTf] if kc else ())
                P.op("dve", I(nc.vector.tensor_copy, out=h2T[:], in_=Tf), reads=[RTf], writes=[Rh2T])
                for kc in range(8):
                    P.op("pe", I(nc.tensor.matmul, lg, lhsT=h2T[:, kc * 128:(kc + 1) * 128], rhs=rw[:, kc, :],
                                 start=(kc == 0), stop=(kc == 7)), reads=[Rh2T, Rw],
                         writes=[Rlg] if kc == 0 else (), pwrites=[Rlg] if kc else ())
                P.op("dve", I(nc.vector.tensor_reduce, out=sm[:, 8:9], in_=lg, axis=AX.X, op=ALU.max), reads=[Rlg],
                     pwrites=[Rsm])
                P.op("dve", I(nc.vector.tensor_scalar, out=sm[:, 9:10], in0=sm[:, 8:9], scalar1=-1.0, scalar2=None,
                              op0=ALU.mult), reads=[Rsm], pwrites=[Rsm])
                P.op("act", I(nc.scalar.activation, out=sm[:, 16:32], in_=lg, func=AF.Exp, bias=sm[:, 9:10],
                              accum_out=sm[:, 10:11]), reads=[Rlg, Rsm], pwrites=[Rsm])
                P.op("dve", I(nc.vector.reciprocal, out=sm[:, 11:12], in_=sm[:, 10:11]), reads=[Rsm], pwrites=[Rsm])
                P.op("dve", I(nc.vector.tensor_scalar, out=aff[:, i, :], in0=sm[:, 16:32], scalar1=sm[:, 11:12],
                              scalar2=None, op0=ALU.mult), reads=[Rsm], pwrites=[Raff])
            Da(0)
            for i in range(NT):
                if i + 1 < NT:
                    Da(i + 1)
                Db(i)
            P.barrier()
            P.emit()
        if upto < "E":
            return
        with ExitStack() as es2:
            affT = es2.enter_context(sb("e_affT", [NE, T], F32))
            mk = es2.enter_context(sb("e_mk", [NE, T], F32))
            cs = es2.enter_context(sb("e_cs", [NE, T], F32))
            on = es2.enter_context(sb("e_on", [NE, T], F32))
            bs = es2.enter_context(sb("e_bs", [NE, 8], F32))
            ps = es2.enter_context(nc.psum_tensor("e_ps", [128, 4 * 512], F32))
            RaT, Rmk, Rcs, Ron, Rbs, Rps = [R(n) for n in "eaT emk ecs eon ebs eps".split()]
            P.op("pool", I(nc.gpsimd.memset, on[:], 1.0), writes=[Ron])
            P.op("pool", I(nc.gpsimd.memset, bs[:], 0.0), writes=[Rbs])
            for half in range(2):
                for j in range(16):
                    i = half * 16 + j
                    P.op("pe", I(nc.tensor.transpose, out=ps[0:NE, j * 128:(j + 1) * 128], in_=aff[:, i, :],
                                 identity=identf[:]), reads=[Raff, Rid], writes=[Rps] if j == 0 else (),
                         pwrites=[Rps] if j else ())
                P.op("act", I(nc.scalar.copy, out=affT[:, half * 2048:(half + 1) * 2048], in_=ps[0:NE, 0:2048]),
                     reads=[Rps], pwrites=[RaT])
            lo, mid, cntc, gw = bs[:, 0:1], bs[:, 1:2], bs[:, 2:3], bs[:, 3:4]
            for it in range(28):
                w = 2.0 ** (-(it + 1))
                P.op("dve", I(nc.vector.tensor_scalar, out=mid, in0=lo, scalar1=w, scalar2=None, op0=ALU.add),
                     reads=[Rbs], pwrites=[Rbs])
                P.op("dve", I(nc.vector.tensor_scalar, out=mk[:], in0=affT[:], scalar1=mid, scalar2=0.0, op0=ALU.is_gt,
                              op1=ALU.add, accum_out=cntc), reads=[RaT, Rbs], writes=[Rmk], pwrites=[Rbs])
                P.op("dve", I(nc.vector.tensor_scalar, out=gw, in0=cntc, scalar1=CAP - 0.5, scalar2=w, op0=ALU.is_gt,
                              op1=ALU.mult), reads=[Rbs], pwrites=[Rbs])
                P.op("dve", I(nc.vector.tensor_tensor, out=lo, in0=lo, in1=gw, op=ALU.add), reads=[Rbs],
                     pwrites=[Rbs])
            P.op("dve", I(nc.vector.tensor_scalar, out=mk[:], in0=affT[:], scalar1=lo, scalar2=None, op0=ALU.is_gt),
                 reads=[RaT, Rbs], writes=[Rmk])
            P.op("dve", I(nc.vector.tensor_tensor_scan, out=cs[:], data0=on[:], data1=mk[:], initial=0.0, op0=ALU.mult,
                          op1=ALU.add), reads=[Ron, Rmk], writes=[Rcs])
            P.op("dve", I(nc.vector.tensor_tensor, out=cs[:], in0=cs[:], in1=mk[:], op=ALU.mult), reads=[Rcs, Rmk],
                 writes=[Rcs])
            for i in range(NT):
                P.op("pe", I(nc.tensor.transpose, out=ps[:, i * NE:(i + 1) * NE], in_=cs[:, i * 128:(i + 1) * 128],
                             identity=identf[0:NE, 0:NE]), reads=[Rcs, Rid], writes=[Rps] if i == 0 else (),
                     pwrites=[Rps] if i else ())
            P.op("act", I(nc.scalar.copy, out=posm[:].rearrange("p a b -> p (a b)"), in_=ps[:, 0:NT * NE]),
                 reads=[Rps], writes=[Rposm])
            P.barrier()
            P.emit()
        with ExitStack() as es2:
            wsl.append(es2.enter_context(sb("e_w3", [128, 16384], BF16)))
            iota = es2.enter_context(sb("e_iota", [128, CAP], F32))
            tokpi = es2.enter_context(sb("e_tokpi", [128, NT, 4], BF16))
            sel = [es2.enter_context(sb("e_sel%d" % k, [128, CAP], BF16)) for k in range(3)]
            Rsel = [R("esel%d" % k) for k in range(3)]
            idxrow = es2.enter_context(sb("e_idxrow", [4, CAP], F32))
            ic = es2.enter_context(sb("e_ic", [128, 4, 4], F32))
            idf = es2.enter_context(sb("e_idf", [128, 4], F32))
            idx = [[es2.enter_context(sb("e_idx%d%d" % (k, c), [128, 1], I32)) for c in range(4)] for k in range(2)]
            gate = [es2.enter_context(sb("e_gate%d" % k, [128, 4], F32)) for k in range(2)]
            xe = [es2.enter_context(sb("e_xe%d" % k, [128, 1024], BF16)) for k in range(4)]
            Rxe = [R("exe%d" % k) for k in range(4)]
            xeT = es2.enter_context(sb("e_xeT", [128, 8, CAP], BF16))
            hT = es2.enter_context(sb("e_hT", [128, 16, CAP], BF16))
            sg = [es2.enter_context(sb("e_sg%d" % k, [128, CAP], F32)) for k in range(2)]
            Rsg = [R("esg0"), R("esg1")]
            ye = [es2.enter_context(sb("e_ye%d" % k, [128, 1024], F32)) for k in range(2)]
            Rye = [R("eye0"), R("eye1")]
            psF = es2.enter_context(nc.psum_tensor("e_psF", [128, 7 * 512], F32))
            psB = es2.enter_context(nc.psum_tensor("e_psB", [128, 1024], BF16))
            gub = [(psF[:, 0:512], psF[:, 512:1024]), (psF[:, 1024:1536], psF[:, 1536:2048])]
            Rgu = [R("egu0"), R("egu1")]
            dbk = [psF[:, 2048:2560], psF[:, 2560:3072]]
            Rdb = [R("edb0"), R("edb1")]
            ipb = psF[:, 3072:3584]
            Ripb, RTb = R("eipb"), R("eTb")
            Rtok, Ridr, Ric, Ridx, RxeT, RhT, Rsc, Rc = [R(n) for n in "etok eidr eic eidx exeT ehT esc ec".split()]
            Ridxs = [R("eidx0"), R("eidx1")]
            P.dma("sp", I(nc.sync.dma_start, out=iota[:], in_=iota_d.partition_broadcast(128)), Rc, pwrites=[Rc])
            P.dma("pool", I(nc.gpsimd.dma_start, out=tokpi[:, :, 0:2], in_=tokpi_d[:, :, :]), Rc, pwrites=[Rtok])

            def route_tok(e):
                P.op("dve", I(nc.vector.tensor_copy, out=tokpi[:, :, 2], in_=aff[:, :, e]), reads=[Raff],
                     writes=[Rtok])
                P.op("dve", I(nc.vector.tensor_tensor, out=tokpi[:, :, 3], in0=aff[:, :, e], in1=tokpi[:, :, 2],
                              op=ALU.subtract), reads=[Raff, Rtok], pwrites=[Rtok])

            def route_sel(e, i):
                r3 = (e * NT + i) % 3
                P.op("dve", I(nc.vector.tensor_scalar, out=sel[r3][:], in0=iota[:], scalar1=posm[:, i, e:e + 1],
                              scalar2=None, op0=ALU.is_equal), reads=[Rc, Rposm], writes=[Rsel[r3]])
                P.op("pe", I(nc.tensor.matmul, ipb[0:4, :], lhsT=tokpi[:, i, :], rhs=sel[r3][:], start=(i == 0),
                             stop=(i == NT - 1)), reads=[Rtok, Rsel[r3]], writes=[Ripb] if i == 0 else (),
                     pwrites=[Ripb] if i else ())

            def route_idx(e):
                es_ = e % 2
                P.op("act", I(nc.scalar.copy, out=idxrow[:], in_=ipb[0:4, :]), reads=[Ripb], writes=[Ridr])
                for cc in range(4):
                    P.op("pe", I(nc.tensor.transpose, out=ipb[:, cc * 4:(cc + 1) * 4],
                                 in_=idxrow[0:4, cc * 128:(cc + 1) * 128], identity=identf[0:4, 0:4]),
                         reads=[Ridr, Rid], writes=[Ripb] if cc == 0 else (), pwrites=[Ripb] if cc else ())
                P.op("dve", I(nc.vector.tensor_copy, out=ic[:].rearrange("p a b -> p (a b)"), in_=ipb[:, 0:16]),
                     reads=[Ripb], writes=[Ric])
                P.op("dve", I(nc.vector.scalar_tensor_tensor, out=idf[:], in0=ic[:, :, 1], scalar=128.0,
                              in1=ic[:, :, 0], op0=ALU.mult, op1=ALU.add), reads=[Ric], writes=[Ridx])
                P.op("dve", I(nc.vector.tensor_tensor, out=gate[es_][:], in0=ic[:, :, 2], in1=ic[:, :, 3], op=ALU.add),
                     reads=[Ric], writes=[Ridxs[es_]])
                for cc in range(4):
                    P.op("dve", I(nc.vector.tensor_copy, out=idx[es_][cc][:], in_=idf[:, cc:cc + 1]), reads=[Ridx],
                         pwrites=[Ridxs[es_]])
                for cc in range(4):
                    P.dma("pool", I(nc.gpsimd.indirect_dma_start, out=xe[cc][:], out_offset=None, in_=H2[:, :],
                                    in_offset=bass.IndirectOffsetOnAxis(ap=idx[es_][cc][:, :], axis=0)), Rxe[cc],
                          reads=[Ridxs[es_]], writes=[Rxe[cc]])

            def route_T(e):
                for kc in range(8):
                    for cc in range(4):
                        P.op("pe", I(nc.tensor.transpose, out=psB[:, cc * 128:(cc + 1) * 128],
                                     in_=xe[cc][:, kc * 128:(kc + 1) * 128], identity=identb[:]),
                             reads=[Rxe[cc], Rid], writes=[RTb] if cc == 0 else (), pwrites=[RTb] if cc else ())
                    if kc % 2 == 0:
                        P.op("act", I(nc.scalar.copy, out=xeT[:, kc, :], in_=psB[:, 0:512]), reads=[RTb],
                             writes=[RxeT] if kc == 0 else (), pwrites=[RxeT] if kc else ())
                    else:
                        P.op("dve", I(nc.vector.tensor_copy, out=xeT[:, kc, :], in_=psB[:, 0:512]), reads=[RTb],
                             pwrites=[RxeT])

            route_tok(0)
            for i in range(NT):
                route_sel(0, i)
            route_idx(0)
            route_T(0)
            gstep = 0
            dstep = 0
            for e in range(NE):
                es_ = e % 2
                nxt = e + 1 < NE
                wgt = wsl[(3 * e) % 4][:].rearrange("p (kc f) -> p kc f", kc=8)
                wut = wsl[(3 * e + 1) % 4][:].rearrange("p (kc f) -> p kc f", kc=8)
                wdt = wsl[(3 * e + 2) % 4][:].rearrange("p (j c) -> p j c", j=16)
                Rwg, Rwu, Rwd = Rws[(3 * e) % 4], Rws[(3 * e + 1) % 4], Rws[(3 * e + 2) % 4]
                if nxt:
                    wload(e + 1, (0,))
                    route_tok(e + 1)
                for j in range(16):
                    gs = gstep % 2
                    gstep += 1
                    gb_, ub_ = gub[gs]
                    for kc in range(8):
                        P.op("pe", I(nc.tensor.matmul, gb_, lhsT=wgt[:, kc, j * 128:(j + 1) * 128], rhs=xeT[:, kc, :],
                                     start=(kc == 0), stop=(kc == 7)), reads=[Rwg, RxeT],
                             writes=[Rgu[gs]] if kc == 0 else (), pwrites=[Rgu[gs]] if kc else ())
                    for kc in range(8):
                        P.op("pe", I(nc.tensor.matmul, ub_, lhsT=wut[:, kc, j * 128:(j + 1) * 128], rhs=xeT[:, kc, :],
                                     start=(kc == 0), stop=(kc == 7)), reads=[Rwu, RxeT], pwrites=[Rgu[gs]])
                    if nxt:
                        route_sel(e + 1, 2 * j)
                        route_sel(e + 1, 2 * j + 1)
                    P.op("act", I(nc.scalar.activation, out=sg[gs][:], in_=gb_, func=AF.Tanh, scale=0.5),
                         reads=[Rgu[gs]], writes=[Rsg[gs]])
                    P.op("dve", I(nc.vector.scalar_tensor_tensor, out=sg[gs][:], in0=sg[gs][:], scalar=1.0, in1=gb_,
                                  op0=ALU.add, op1=ALU.mult), reads=[Rsg[gs], Rgu[gs]], writes=[Rsg[gs]])
                    P.op("dve", I(nc.vector.scalar_tensor_tensor, out=hT[:, j, :], in0=sg[gs][:], scalar=0.5, in1=ub_,
                                  op0=ALU.mult, op1=ALU.mult), reads=[Rsg[gs], Rgu[gs]],
                         writes=[RhT] if j == 0 else (), pwrites=[RhT] if j else ())
                if nxt:
                    route_idx(e + 1)
                    wload(e + 1, (1,))
                for cc in range(4):
                    ys = cc % 2
                    for half in range(2):
                        ds = dstep % 2
                        dstep += 1
                        for j in range(16):
                            P.op("pe", I(nc.tensor.matmul, dbk[ds], lhsT=hT[:, j, cc * 128:(cc + 1) * 128],
                                         rhs=wdt[:, j, half * 512:(half + 1) * 512], start=(j == 0), stop=(j == 15)),
                                 reads=[RhT, Rwd], writes=[Rdb[ds]] if j == 0 else (), pwrites=[Rdb[ds]] if j else ())
                        if half == 0:
                            P.op("dve", I(nc.vector.tensor_scalar, out=ye[ys][:, 0:512], in0=dbk[ds],
                                          scalar1=gate[es_][:, cc:cc + 1], scalar2=None, op0=ALU.mult),
                                 reads=[Rdb[ds], Ridxs[es_]], writes=[Rye[ys]])
                        else:
                            P.op("dve", I(nc.vector.tensor_scalar, out=ye[ys][:, 512:1024], in0=dbk[ds],
                                          scalar1=gate[es_][:, cc:cc + 1], scalar2=None, op0=ALU.mult),
                                 reads=[Rdb[ds], Ridxs[es_]], pwrites=[Rye[ys]])
                    P.dma("pool", I(nc.gpsimd.indirect_dma_start, out=out_d[:, :],
                                    out_offset=bass.IndirectOffsetOnAxis(ap=idx[es_][cc][:, :], axis=0),
                                    in_=ye[ys][:], in_offset=None, compute_op=ALU.add), Rye[ys],
                          reads=[Rye[ys], Ridxs[es_], Rsc], pwrites=[Rsc])
                if nxt:
                    wload(e + 1, (2,))
                    route_T(e + 1)
            P.barrier()
            P.emit()


IN_COLS = (256, 256, 32, 1024, 1024, 1024, 1024, 1024)


def _win_perm():
    off = np.cumsum((0,) + IN_COLS)
    seg = [np.arange(off[k], off[k + 1]) for k in range(8)]
    return np.concatenate([seg[0], seg[1], seg[3], seg[4], seg[5], seg[6], seg[7], seg[2]])


def make_in_maps(inputs, cores):
    f = lambda a: np.ascontiguousarray(np.asarray(a))
    perm = _win_perm()
    bg = f(inputs["b_gate"])[0]
    shared = {
        "w_in": f(f(inputs["w_in"])[0][:, perm]),
        "b_gate": bg,
        "w_uq": f(inputs["mla_w_uq"])[0],
        "w_ukv": f(inputs["mla_w_ukv"])[0],
        "w_out": f(inputs["w_out"])[0],
        "router_w": f(inputs["router_w"])[0],
        "w_gate": f(inputs["expert_w_gate"])[0],
        "w_up": f(inputs["expert_w_up"])[0],
        "w_down": f(inputs["expert_w_down"])[0],
        "attn_norm_w": f(inputs["attn_norm_w"])[0],
        "q_norm_w": f(inputs["mla_q_norm_w"])[0],
        "kv_norm_w": f(inputs["mla_kv_norm_w"])[0],
        "q_hn": f(inputs["mla_q_hnorm_w"])[0],
        "k_hn": f(inputs["mla_k_hnorm_w"])[0],
        "dq_hn": f(inputs["diff_q_hnorm_w"])[0],
        "dk_hn": f(inputs["diff_k_hnorm_w"])[0],
        "lam": f(inputs["diff_lambda"])[0].reshape(-1),
        "subln": f(inputs["diff_subln_w"])[0],
        "ffn_norm_w": f(inputs["ffn_norm_w"])[0],
        "ident": np.eye(128, dtype=np.float32),
        "invf": (1.0 / (10000.0 ** (np.arange(0, 32, 2, dtype=np.float32) / 32.0))).astype(np.float32),
        "iota512": np.arange(1, 513, dtype=np.float32),
        "tokpi": np.ascontiguousarray(np.stack([np.broadcast_to(np.arange(128, dtype=np.float32)[:, None], (128, NT)),
                                                np.broadcast_to(np.arange(NT, dtype=np.float32)[None, :], (128, NT))],
                                               axis=-1)),
        "slopes": np.array([2.0 ** (-(i + 1)) for i in range(8)], dtype=np.float32),
    }
    x = f(inputs["x"])
    pos = f(inputs["positions"]).astype(np.int32)
    return [dict(shared, x=x[c], pos=pos[c], pos_t=np.ascontiguousarray(pos[c].reshape(NT, 128).T)) for c in cores]


_NC = {}


def classify(pos):
    pos = np.asarray(pos).astype(np.int64)
    n = pos.shape[0]
    q = pos.reshape(n, 16, 256)
    k = pos.reshape(n, NT, 128)
    qmin, qmax = q.min(-1)[:, :, None], q.max(-1)[:, :, None]
    kmin, kmax = k.min(-1)[:, None, :], k.max(-1)[:, None, :]
    below = (kmax <= qmin).all(0)
    above = (kmin >= qmax).all(0)
    cls = np.where(below, 1, np.where(above, 2, 0))
    dmin = np.maximum(np.maximum(qmin - kmax, kmin - qmax), 0).min(0).astype(np.float64)
    return cls, dmin


def kernel(**inputs):
    cls, dmin = classify(np.asarray(inputs["positions"]))
    gq = float(np.abs(np.asarray(inputs["diff_q_hnorm_w"])).max())
    gk = float(np.abs(np.asarray(inputs["diff_k_hnorm_w"])).max())
    skip_t = max(48.0, 2.0 * 8.0 * gq * gk + 25.0)
    key = (cls.tobytes(), dmin.tobytes(), skip_t)
    if key not in _NC:
        _NC[key] = build(CLS=cls, DMIN=dmin, skip_t=skip_t)
    nc = _NC[key]
    in_maps = make_in_maps(inputs, list(range(8)))
    res = run_bass_kernel_spmd(nc, in_maps, core_ids=list(range(8)))
    return np.stack([r["out"] for r in res.results], axis=0).astype(np.float32)
```
